# Optimizing a Trainium2 kernel written in Bass

```python
import math, functools
import jax, jax.numpy as jnp
from jax import lax
import numpy as np

D_MODEL = 1024
BATCH = 8
SEQ = 4096
DEPTH = 1

GRID_W = 64
CTX_LEN = 256
D_MIX = D_MODEL
D_SSM = D_MIX // 2
SSM_HEADDIM = 64
SSM_HEADS = D_SSM // SSM_HEADDIM
SSM_STATE = 128
SSM_GROUPS = 2
SSM_CONV = 5
SSD_CHUNK = 128
XBC_DIM = D_SSM + 2 * SSM_GROUPS * SSM_STATE
D_CONV = D_MIX - D_SSM
CONV_WIDTH = 31
IN_COLS = D_SSM + XBC_DIM + 2 * SSM_HEADS + 2 * D_CONV
PEER_HEADS = 8
PEER_KEY_DIM = 256
N_KEYS = 128
N_EXPERTS = N_KEYS * N_KEYS
PEER_TOPK = 16
PEER_BLOCK = 128
EPS = 1e-6

kernel_name = "hymba_ssd_conformer_peer_dit_layer"


def rms_norm(h, w):
    hf = h.astype(jnp.float32)
    hf = hf * lax.rsqrt(jnp.mean(hf * hf, axis=-1, keepdims=True) + EPS)
    return (hf * w.astype(jnp.float32)).astype(h.dtype)


def layer_norm(h, w, b):
    hf = h.astype(jnp.float32)
    mu = jnp.mean(hf, axis=-1, keepdims=True)
    var = jnp.mean(jnp.square(hf - mu), axis=-1, keepdims=True)
    out = (hf - mu) * lax.rsqrt(var + EPS) * w.astype(jnp.float32) + b.astype(jnp.float32)
    return out.astype(h.dtype)


def gated_rms_norm(y, z, w):
    bsz, length, ch = y.shape
    g = (y * jax.nn.silu(z)).astype(jnp.float32).reshape(bsz, length, SSM_GROUPS, ch // SSM_GROUPS)
    g = g * lax.rsqrt(jnp.mean(g * g, axis=-1, keepdims=True) + EPS)
    return (g.reshape(bsz, length, ch) * w.astype(jnp.float32)).astype(y.dtype)


def modulate(h, shift, scale):
    return h * (1 + scale[:, None, :]) + shift[:, None, :]


def dwconv_seq(h, w, b):
    k, ch = w.shape
    pad = (k - 1) // 2
    out = lax.conv_general_dilated(h, w[:, None, :], window_strides=(1,), padding=[(pad, pad)],
                                   dimension_numbers=("NWC", "WIO", "NWC"), feature_group_count=ch)
    return out + b


def dwconv_grid_axial(h, w, b, rows):
    bsz, length, ch = h.shape
    k = w.shape[0]
    pad = (k - 1) // 2
    half = ch // 2
    g = h.reshape(bsz, rows, GRID_W, ch)
    dn = ("NHWC", "HWIO", "NHWC")
    horiz = lax.conv_general_dilated(g[..., :half], w[:, :half].reshape(1, k, 1, half), (1, 1),
                                     [(0, 0), (pad, pad)], dimension_numbers=dn,
                                     feature_group_count=half)
    vert = lax.conv_general_dilated(g[..., half:], w[:, half:].reshape(k, 1, 1, ch - half), (1, 1),
                                    [(pad, pad), (0, 0)], dimension_numbers=dn,
                                    feature_group_count=ch - half)
    return jnp.concatenate([horiz, vert], axis=-1).reshape(bsz, length, ch) + b


def segsum_exp(a):
    t = a.shape[-1]
    acum = jnp.cumsum(a, axis=-1)
    diff = acum[..., :, None] - acum[..., None, :]
    mask = jnp.tril(jnp.ones((t, t), dtype=bool))
    return jnp.where(mask, jnp.exp(jnp.where(mask, diff, 0.0)), 0.0)


def ssd(xs, dt, a, bm, cm, h0):
    bsz, length, nh, hp = xs.shape
    nc = length // SSD_CHUNK
    rep = nh // bm.shape[2]
    f32 = jnp.float32
    xdt = (xs.astype(f32) * dt[..., None]).reshape(bsz, nc, SSD_CHUNK, nh, hp)
    bh = jnp.repeat(bm.astype(f32), rep, axis=2).reshape(bsz, nc, SSD_CHUNK, nh, SSM_STATE)
    ch = jnp.repeat(cm.astype(f32), rep, axis=2).reshape(bsz, nc, SSD_CHUNK, nh, SSM_STATE)
    ac = (dt * a).reshape(bsz, nc, SSD_CHUNK, nh).transpose(0, 3, 1, 2)
    acum = jnp.cumsum(ac, axis=-1)
    scores = jnp.einsum("bclhn,bcshn->bhcls", ch, bh) * segsum_exp(ac)
    y_diag = jnp.einsum("bhcls,bcshp->bclhp", scores, xdt)
    decay_to_end = jnp.exp(acum[..., -1:] - acum)
    states = jnp.einsum("bclhn,bhcl,bclhp->bchpn", bh, decay_to_end, xdt)
    states = jnp.concatenate([h0[:, None], states], axis=1)
    chunk_decay = segsum_exp(jnp.pad(acum[..., -1], ((0, 0), (0, 0), (1, 0))))
    states = jnp.einsum("bhzc,bchpn->bzhpn", chunk_decay, states)
    y_off = jnp.einsum("bclhn,bchpn,bhcl->bclhp", ch, states[:, :-1], jnp.exp(acum))
    y = (y_diag + y_off).reshape(bsz, length, nh, hp).astype(xs.dtype)
    return y, states[:, -1]


def ssd_final_state(xs, dt, a, bm):
    rep = xs.shape[2] // bm.shape[2]
    acum = jnp.cumsum(dt * a, axis=1)
    w = jnp.exp(acum[:, -1:] - acum) * dt
    bh = jnp.repeat(bm.astype(jnp.float32), rep, axis=2)
    return jnp.einsum("blhn,blh,blhp->bhpn", bh, w, xs.astype(jnp.float32))


def split_in(p):
    return jnp.split(p, [D_SSM, D_SSM + XBC_DIM, D_SSM + XBC_DIM + 2 * SSM_HEADS], axis=-1)


def ssm_prep(xbc, dt_raw, conv_w, conv_b, dt_bias):
    bsz, length, _ = xbc.shape
    xbc = jax.nn.silu(dwconv_seq(xbc, conv_w, conv_b))
    xs, bm, cm = jnp.split(xbc, [D_SSM, D_SSM + SSM_GROUPS * SSM_STATE], axis=-1)
    dt = jax.nn.softplus((dt_raw.reshape(bsz, length, 2, SSM_HEADS) + dt_bias).astype(jnp.float32))
    return (xs.reshape(bsz, length, SSM_HEADS, SSM_HEADDIM),
            bm.reshape(bsz, length, SSM_GROUPS, SSM_STATE),
            cm.reshape(bsz, length, SSM_GROUPS, SSM_STATE),
            dt)


def conformer_branch(glu, conv_fn, conv_w, conv_b, ln_w, ln_b):
    u = glu[..., :D_CONV] * jax.nn.sigmoid(glu[..., D_CONV:])
    return jax.nn.silu(layer_norm(conv_fn(u, conv_w, conv_b), ln_w, ln_b))


def peer(h, wq, sub_keys, u_tab, v_tab):
    bsz, length, d = h.shape
    t = bsz * length
    hf = h.reshape(t, d)
    q = (hf @ wq).reshape(t, PEER_HEADS, 2, PEER_KEY_DIM // 2)
    s = jnp.einsum("thpd,hpkd->thpk", q, sub_keys).astype(jnp.float32)
    sv, si = lax.top_k(s, PEER_TOPK)
    cand = sv[:, :, 0, :, None] + sv[:, :, 1, None, :]
    best, flat = lax.top_k(cand.reshape(t, PEER_HEADS, PEER_TOPK * PEER_TOPK), PEER_TOPK)
    i1 = jnp.take_along_axis(si[:, :, 0], flat // PEER_TOPK, axis=-1)
    i2 = jnp.take_along_axis(si[:, :, 1], flat % PEER_TOPK, axis=-1)
    experts = (i1 * N_KEYS + i2).reshape(t, PEER_HEADS * PEER_TOPK)
    gates = jax.nn.softmax(best, axis=-1).reshape(t, PEER_HEADS * PEER_TOPK).astype(h.dtype)
    nb = t // PEER_BLOCK

    def block(args):
        xb, eb, gb = args
        act = jax.nn.gelu(jnp.einsum("pd,ped->pe", xb, u_tab[eb]), approximate=False)
        return jnp.einsum("pe,ped->pd", gb * act, v_tab[eb])

    out = lax.map(block, (hf.reshape(nb, PEER_BLOCK, d),
                          experts.reshape(nb, PEER_BLOCK, PEER_HEADS * PEER_TOPK),
                          gates.reshape(nb, PEER_BLOCK, PEER_HEADS * PEER_TOPK)))
    return out.reshape(bsz, length, d)


def mixer_sublayer(x, ctx, mod, mod_ctx, rows, norm_w, w_in, ssm_conv_w, ssm_conv_b, ssm_dt_bias,
                   ssm_a_log, ssm_d, ssm_norm_w, cfm_conv_w, cfm_conv_b, cfm_ln_w, cfm_ln_b, w_out,
                   update_ctx):
    a = -jnp.exp(ssm_a_log.astype(jnp.float32))
    flip = lambda t: t[:, ::-1]
    z_l, xbc_l, dt_l, glu_l = split_in(modulate(rms_norm(x, norm_w), mod[:, 0], mod[:, 1]) @ w_in)
    h_c = modulate(rms_norm(ctx, norm_w), mod_ctx[:, 0], mod_ctx[:, 1])
    if update_ctx:
        z_c, xbc_c, dt_c, glu_c = split_in(h_c @ w_in)
    else:
        xbc_c, dt_c = jnp.split(h_c @ w_in[:, D_SSM:D_SSM + XBC_DIM + 2 * SSM_HEADS], [XBC_DIM], axis=-1)
    xs_l, b_l, c_l, dts_l = ssm_prep(xbc_l, dt_l, ssm_conv_w, ssm_conv_b, ssm_dt_bias)
    xs_c, b_c, c_c, dts_c = ssm_prep(xbc_c, dt_c, ssm_conv_w, ssm_conv_b, ssm_dt_bias)

    if update_ctx:
        h0 = jnp.zeros((ctx.shape[0], SSM_HEADS, SSM_HEADDIM, SSM_STATE), jnp.float32)
        yc_f, hc_f = ssd(xs_c, dts_c[:, :, 0], a[0], b_c, c_c, h0)
        yc_b, hc_b = ssd(flip(xs_c), flip(dts_c[:, :, 1]), a[1], flip(b_c), flip(c_c), h0)
    else:
        hc_f = ssd_final_state(xs_c, dts_c[:, :, 0], a[0], b_c)
        hc_b = ssd_final_state(flip(xs_c), flip(dts_c[:, :, 1]), a[1], flip(b_c))
    y_f, _ = ssd(xs_l, dts_l[:, :, 0], a[0], b_l, c_l, hc_f)
    y_b, _ = ssd(flip(xs_l), flip(dts_l[:, :, 1]), a[1], flip(b_l), flip(c_l), hc_b)
    y_l = (y_f + flip(y_b) + ssm_d[:, None] * xs_l).reshape(x.shape[0], x.shape[1], D_SSM)
    ssm_l = gated_rms_norm(y_l, z_l, ssm_norm_w)
    cfm_l = conformer_branch(glu_l, functools.partial(dwconv_grid_axial, rows=rows),
                             cfm_conv_w, cfm_conv_b, cfm_ln_w, cfm_ln_b)
    x_out = x + mod[:, 2, None, :] * (jnp.concatenate([ssm_l, cfm_l], axis=-1) @ w_out)
    if not update_ctx:
        return x_out, ctx
    y_c = (yc_f + flip(yc_b) + ssm_d[:, None] * xs_c).reshape(ctx.shape[0], ctx.shape[1], D_SSM)
    ssm_c = gated_rms_norm(y_c, z_c, ssm_norm_w)
    cfm_c = conformer_branch(glu_c, dwconv_seq, cfm_conv_w, cfm_conv_b, cfm_ln_w, cfm_ln_b)
    ctx_out = ctx + mod_ctx[:, 2, None, :] * (jnp.concatenate([ssm_c, cfm_c], axis=-1) @ w_out)
    return x_out, ctx_out


def setup_inputs(seed: int = 0) -> dict:
    key = jax.random.key(seed)
    ks = jax.random.split(key, 26)
    D = D_MODEL

    def nrm(k, shape, scale):
        return jax.random.normal(k, shape, jnp.float32) * scale

    dt0 = jnp.exp(jax.random.uniform(ks[9], (DEPTH, 2, SSM_HEADS), jnp.float32,
                                     minval=math.log(1e-3), maxval=math.log(1e-1)))
    return {
        "x": nrm(ks[0], (BATCH, SEQ, D), 1.0),
        "c": nrm(ks[1], (BATCH, D), 1.0),
        "ctx": nrm(ks[2], (BATCH, CTX_LEN, D), 1.0),
        "c_ctx": nrm(ks[3], (D,), 1.0),
        "ada_w": nrm(ks[4], (DEPTH, D, 6 * D), 0.5 * D ** -0.5),
        "ada_b": nrm(ks[5], (DEPTH, 6 * D), 0.02),
        "norm1_w": 1.0 + nrm(ks[6], (DEPTH, D), 0.02),
        "norm2_w": 1.0 + nrm(ks[7], (DEPTH, D), 0.02),
        "w_in": nrm(ks[8], (DEPTH, D, IN_COLS), D ** -0.5),
        "ssm_conv_w": nrm(ks[10], (DEPTH, SSM_CONV, XBC_DIM), SSM_CONV ** -0.5),
        "ssm_conv_b": nrm(ks[11], (DEPTH, XBC_DIM), 0.02),
        "ssm_dt_bias": dt0 + jnp.log(-jnp.expm1(-dt0)),
        "ssm_a_log": jnp.log(jax.random.uniform(ks[12], (DEPTH, 2, SSM_HEADS), jnp.float32,
                                                 minval=1.0, maxval=16.0)),
        "ssm_d": 1.0 + nrm(ks[13], (DEPTH, SSM_HEADS), 0.1),
        "ssm_norm_w": 1.0 + nrm(ks[14], (DEPTH, D_SSM), 0.02),
        "cfm_conv_w": nrm(ks[15], (DEPTH, CONV_WIDTH, D_CONV), CONV_WIDTH ** -0.5),
        "cfm_conv_b": nrm(ks[16], (DEPTH, D_CONV), 0.02),
        "cfm_ln_w": 1.0 + nrm(ks[17], (DEPTH, D_CONV), 0.02),
        "cfm_ln_b": nrm(ks[18], (DEPTH, D_CONV), 0.02),
        "w_out": nrm(ks[19], (DEPTH, D_MIX, D), D_MIX ** -0.5),
        "peer_wq": nrm(ks[20], (DEPTH, D, PEER_HEADS * PEER_KEY_DIM), D ** -0.5),
        "peer_subkeys": nrm(ks[21], (DEPTH, PEER_HEADS, 2, N_KEYS, PEER_KEY_DIM // 2),
                            (PEER_KEY_DIM // 2) ** -0.5),
        "peer_u": nrm(ks[22], (DEPTH, N_EXPERTS, D), D ** -0.5),
        "peer_v": nrm(ks[23], (DEPTH, N_EXPERTS, D), PEER_HEADS ** -0.5),
        "final_norm_w": 1.0 + nrm(ks[24], (D,), 0.02),
    }


def reference(x, c, ctx, c_ctx, ada_w, ada_b, norm1_w, norm2_w, w_in, ssm_conv_w, ssm_conv_b,
              ssm_dt_bias, ssm_a_log, ssm_d, ssm_norm_w, cfm_conv_w, cfm_conv_b, cfm_ln_w, cfm_ln_b,
              w_out, peer_wq, peer_subkeys, peer_u, peer_v, final_norm_w):
    rows = x.shape[1] // GRID_W
    sc = jax.nn.silu(c)
    sc_ctx = jax.nn.silu(c_ctx)[None]
    for i in range(DEPTH):
        update_ctx = i < DEPTH - 1
        mod = (sc @ ada_w[i] + ada_b[i]).reshape(-1, 6, D_MODEL)
        mod_ctx = (sc_ctx @ ada_w[i] + ada_b[i]).reshape(1, 6, D_MODEL)
        x, ctx = mixer_sublayer(x, ctx, mod, mod_ctx, rows, norm1_w[i], w_in[i], ssm_conv_w[i],
                                ssm_conv_b[i], ssm_dt_bias[i], ssm_a_log[i], ssm_d[i], ssm_norm_w[i],
                                cfm_conv_w[i], cfm_conv_b[i], cfm_ln_w[i], cfm_ln_b[i], w_out[i],
                                update_ctx)
        x = x + mod[:, 5, None, :] * peer(modulate(rms_norm(x, norm2_w[i]), mod[:, 3], mod[:, 4]),
                                          peer_wq[i], peer_subkeys[i], peer_u[i], peer_v[i])
        if update_ctx:
            ctx = ctx + mod_ctx[:, 5, None, :] * peer(
                modulate(rms_norm(ctx, norm2_w[i]), mod_ctx[:, 3], mod_ctx[:, 4]),
                peer_wq[i], peer_subkeys[i], peer_u[i], peer_v[i])
    return rms_norm(x, final_norm_w)
```

```python
import re
import numpy as np
import concourse.bass as bass
import concourse.mybir as mybir
from concourse.bass_utils import run_bass_kernel_spmd

F32 = mybir.dt.float32
BF16 = mybir.dt.bfloat16
I32 = mybir.dt.int32
U32 = mybir.dt.uint32
AF = mybir.ActivationFunctionType
ALU = mybir.AluOpType
AX = mybir.AxisListType
ENGS = ("sync", "scalar", "vector", "gpsimd", "tensor")
CH = 4096
EPS = 1e-6
DSZ = {F32: 4, BF16: 2, I32: 4, U32: 4}


class Prog:
    def __init__(self, nc):
        self.nc = nc
        self.q = {e: [] for e in ENGS}
        self.cnt = {e: 0 for e in ENGS}
        self.known = {e: {} for e in ENGS}
        self.lastw = {}
        self.readers = {}
        self.sems = {}
        self.dmacum = {}
        self.pend = {e: {} for e in ENGS}
        self.lasttok = {}

    def sem(self, key):
        s = self.sems.get(key)
        if s is None:
            s = self.nc.alloc_semaphore(name="s%d" % len(self.sems))
            self.sems[key] = s
        return s

    def _waits(self, e, reads, writes):
        toks = dict(self.pend[e])
        self.pend[e] = {}

        def add(t):
            if t is None:
                return
            k, v = t
            if k[0] == "eng" and k[1] == e and e == "tensor":
                return
            if toks.get(k, 0) < v:
                toks[k] = v
        for r in reads:
            add(self.lastw.get(r))
        for w in writes:
            add(self.lastw.get(w))
            for t in self.readers.get(w, ()):
                add(t)
        out = []
        for k, v in toks.items():
            if self.known[e].get(k, 0) >= v:
                continue
            self.known[e][k] = v
            out.append((self.sem(k), v))
        return out

    def _commit(self, tok, reads, writes):
        self.lasttok[tok[0]] = tok[1]
        for w in writes:
            self.lastw[w] = tok
            self.readers[w] = []
        for r in reads:
            self.readers.setdefault(r, []).append(tok)

    def op(self, e, name, r="", w="", **kw):
        reads, writes = r.split(), w.split()
        ex = [k for k in reads if re.fullmatch(r"(ps|b)\d", k)]
        reads = [k for k in reads if k not in ex]
        writes = writes + [k for k in ex if k not in writes]
        waits = self._waits(e, reads, writes)
        idx = self.cnt[e]
        self.cnt[e] += 1
        k = ("eng", e, idx // CH)
        self.q[e].append((waits, name, kw, self.sem(k), 1))
        self._commit((k, idx % CH + 1), reads, writes)

    def v(self, name, r="", w="", **kw):
        self.op("vector", name, r, w, **kw)

    def s(self, name, r="", w="", **kw):
        self.op("scalar", name, r, w, **kw)

    def g(self, name, r="", w="", **kw):
        self.op("gpsimd", name, r, w, **kw)

    def t(self, name, r="", w="", **kw):
        self.op("tensor", name, r, w, **kw)

    def dma(self, out, in_, r="", w="", e="sync", grp=None, parts=None, **kw):
        reads, writes = r.split(), w.split()
        sk = ("dma", grp if grp is not None else writes[0])
        waits = self._waits(e, reads, writes)
        pieces = parts if parts is not None else [(out, in_)]
        for i, (o, a) in enumerate(pieces):
            self.dmacum[sk] = self.dmacum.get(sk, 0) + 16
            kk = dict(kw)
            kk.update(out=o, in_=a)
            self.q[e].append((waits if i == 0 else [], "dma_start", kk, self.sem(sk), 16))
        self._commit((sk, self.dmacum[sk]), reads, writes)

    def barrier(self):
        for e in ENGS:
            for k, v in self.lasttok.items():
                if self.pend[e].get(k, 0) < v:
                    self.pend[e][k] = v

    def finish(self):
        self.barrier()
        for e in ("sync", "gpsimd"):
            waits = self._waits(e, [], [])
            self.q[e].append((waits, None, None, None, 0))

    def emit(self):
        nc = self.nc
        with nc.Block() as block:
            def mk(e):
                def body(eng):
                    for waits, name, kw, s, inc in self.q[e]:
                        for ws, wv in waits:
                            eng.wait_ge(ws, wv)
                        if name is not None:
                            getattr(eng, name)(**kw).then_inc(s, inc)
                return body
            block.sync(mk("sync"))
            block.scalar(mk("scalar"))
            block.vector(mk("vector"))
            block.gpsimd(mk("gpsimd"))
            block.tensor(mk("tensor"))


class Arena:
    def __init__(self, nc, nbytes):
        self.t = nc.alloc_sbuf_tensor("arena", [128, nbytes // 4], F32)
        self.cap = nbytes
        self.off = 0
        self.peak = 0

    def a(self, free, dt=F32, parts=128):
        if isinstance(free, int):
            free = (free,)
        n = int(np.prod(free)) * DSZ[dt]
        n = (n + 63) // 64 * 64
        assert self.off + n <= self.cap, ("SBUF arena overflow", self.off, n, self.cap)
        ap = self.t[0:parts, self.off // 4:(self.off + n) // 4]
        self.off += n
        self.peak = max(self.peak, self.off)
        if dt != F32:
            ap = ap.bitcast(dt)
        ap = ap[:, 0:int(np.prod(free))]
        if len(free) == 2:
            ap = ap.rearrange("p (a b) -> p a b", a=free[0])
        elif len(free) == 3:
            ap = ap.rearrange("p (a b c) -> p a b c", a=free[0], b=free[1])
        return ap

    def mark(self):
        return self.off

    def release(self, m):
        self.off = m


L = 4096
LC = 256
NT = L // 128


def build(stop=None, dbg=False, npeer=128):
    nc = bass.Bass("TRN2", target_bir_lowering=False)
    D = nc.dram_tensor

    def din(name, shape, dt=F32):
        return D(name, list(shape), dt, kind="ExternalInput").ap()
    x_d = din("x", [L, 1024])
    ctx_d = din("ctx", [LC, 1024])
    cc_d = din("cc", [128, 8, 2])
    adaw_d = din("ada_w", [128, 8, 6144])
    adab_d = din("ada_b", [1, 6144])
    nrm_d = din("nrm", [3, 128, 1024])
    win_d = din("w_in", [128, 8, 2576])
    scw_d = din("scw", [128, 8, 5])
    scb_d = din("scb", [128, 8])
    dtb_d = din("dtb", [128, 16])
    alog_d = din("alog", [128, 16])
    drow_d = din("drow", [128, 512])
    snw_d = din("snw", [128, 512])
    ccw_d = din("ccw", [128, 4, 31])
    cvec_d = din("cvec", [128, 3, 4])
    wout_d = din("w_out", [128, 8, 1024])
    wq_d = din("wq", [128, 8, 2048])
    kt_d = din("kt", [128, 16, 128])
    ut_d = din("ut", [npeer, 128, 8, 128])
    vt_d = din("vt", [npeer, 128, 1024])
    out_d = D("out", [L, 1024], F32, kind="ExternalOutput").ap()
    dbg_d = {}

    def dout(name, shape):
        dbg_d[name] = D("dbg_" + name, list(shape), F32, kind="ExternalOutput").ap()
        return dbg_d[name]
    modb_s = D("modb_s", [8, 128, 1024], F32).ap()
    zs_s = D("zs_s", [L, 512], BF16).ap()
    x1_s = D("x1_s", [L, 1024], F32).ap()
    utb_s = D("utb_s", [128, 128, 1024], BF16).ap()
    vtb_s = D("vtb_s", [128, 128, 1024], BF16).ap()
    uts_s = D("uts_s", [128, 4, L], BF16).ap()
    wqb_s = D("wqb_s", [16, 128, 1024], BF16).ap()

    p = Prog(nc)
    ar = Arena(nc, 212000)
    banks = [nc.alloc_psum_tensor("pb%d" % i, [128, 512], F32) for i in range(8)]

    def PB(i):
        return banks[i][:]

    def PBb(i):
        return banks[i][:].bitcast(BF16)

    ident_f = ar.a(128)
    ident = ar.a(128, BF16)
    iota_f = ar.a(128)
    pidx = ar.a(128)
    ones_f = ar.a(128)
    ones_b = ar.a(128, BF16)
    triU = ar.a(128)
    triS = ar.a(128)
    maskF = ar.a(128, BF16)
    maskB = ar.a(128, BF16)
    p.g("iota", w="iota", out=iota_f, pattern=[[1, 128]], base=0, channel_multiplier=0, allow_small_or_imprecise_dtypes=True)
    p.g("iota", w="pidx", out=pidx, pattern=[[0, 128]], base=0, channel_multiplier=1, allow_small_or_imprecise_dtypes=True)
    p.v("tensor_tensor", r="iota pidx", w="identf", out=ident_f, in0=iota_f, in1=pidx, op=ALU.is_equal)
    p.v("tensor_copy", r="identf", w="ident", out=ident, in_=ident_f)
    p.v("memset", w="ones_f", ap=ones_f, constant=1.0)
    p.v("memset", w="ones_b", ap=ones_b, constant=1.0)
    p.v("tensor_tensor", r="iota pidx", w="triU", out=triU, in0=iota_f, in1=pidx, op=ALU.is_ge)
    p.v("tensor_tensor", r="iota pidx", w="triS", out=triS, in0=iota_f, in1=pidx, op=ALU.is_gt)
    p.v("tensor_scalar", r="triU", w="maskF", out=maskF, in0=triU, scalar1=-1.0, scalar2=30000.0, op0=ALU.add, op1=ALU.mult)
    tmpc = ar.a(128)
    p.v("tensor_tensor", r="iota pidx", w="tmpc", out=tmpc, in0=iota_f, in1=pidx, op=ALU.is_le)
    p.v("tensor_scalar", r="tmpc", w="maskB", out=maskB, in0=tmpc, scalar1=-1.0, scalar2=30000.0, op0=ALU.add, op1=ALU.mult)
    sel = ar.a((16, 128), BF16)
    p.v("memset", w="sel", ap=sel, constant=0.0)
    p.v("tensor_copy", r="identf sel", w="sel", out=sel[0:16], in_=ident_f[0:16, 0:16].unsqueeze(2).to_broadcast([16, 16, 128]))
    scw = ar.a((8, 5)); scb = ar.a(8); dtbias = ar.a(16); alog = ar.a(16); ccw = ar.a((4, 31)); cvec = ar.a((3, 4))
    Aneg = ar.a(16)
    p.dma(scw, scw_d, w="scw", grp="small")
    p.dma(scb, scb_d, w="scb", grp="small")
    p.dma(dtbias, dtb_d, w="dtbias", grp="small")
    p.dma(alog, alog_d, w="alog", grp="small")
    p.dma(ccw, ccw_d, w="ccw", grp="small")
    p.dma(cvec, cvec_d, w="cvec", grp="small")
    p.s("activation", r="alog", w="Aneg", out=Aneg, in_=alog, func=AF.Exp)
    p.v("tensor_scalar", r="Aneg", w="Aneg", out=Aneg, in0=Aneg, scalar1=-1.0, scalar2=None, op0=ALU.mult)
    dt_all = ar.a((NT + 2, 16))
    m_persist = ar.mark()

    m0 = ar.mark()
    cc = ar.a((8, 2)); scT = ar.a((8, 2))
    adab = ar.a(6144, parts=1)
    modrow = ar.a(6144, parts=1)
    modrow_c = ar.a(2048, parts=1)
    awb = [ar.a((8, 512)) for _ in range(2)]
    nrm = [ar.a(1024) for _ in range(2)]
    bct = [ar.a(1024) for _ in range(2)]
    p.dma(cc, cc_d, w="cc")
    p.dma(adab, adab_d, w="adab")
    p.dma(nrm[0], nrm_d[0], w="nrm0")
    p.dma(nrm[1], nrm_d[1], w="nrm1")
    p.s("activation", r="cc", w="scT", out=scT, in_=cc, func=AF.Silu)
    for blk in range(12):
        sl = blk % 2
        p.dma(awb[sl], adaw_d[:, :, blk * 512:(blk + 1) * 512], w="awb%d" % sl)
        for kc in range(8):
            p.t("matmul", r="scT awb%d" % sl, w="ps0", out=PB(0)[0:1, :], lhsT=scT[:, kc, 0:1], rhs=awb[sl][:, kc, :],
                start=(kc == 0), stop=(kc == 7))
        if blk < 4:
            for kc in range(8):
                p.t("matmul", r="scT awb%d" % sl, w="ps1", out=PB(1)[0:1, :], lhsT=scT[:, kc, 1:2], rhs=awb[sl][:, kc, :],
                    start=(kc == 0), stop=(kc == 7))
            p.v("tensor_tensor", r="ps1 adab", w="modrow_c", out=modrow_c[:, blk * 512:(blk + 1) * 512], in0=PB(1)[0:1, :],
                in1=adab[:, blk * 512:(blk + 1) * 512], op=ALU.add)
        p.v("tensor_tensor", r="ps0 adab", w="modrow", out=modrow[:, blk * 512:(blk + 1) * 512], in0=PB(0)[0:1, :],
            in1=adab[:, blk * 512:(blk + 1) * 512], op=ALU.add)
    plan = [(0, modrow, 1, 0), (1, modrow, 0, None), (2, modrow_c, 1, 0), (3, modrow_c, 0, None), (4, modrow, 2, None),
            (5, modrow, 4, 1), (6, modrow, 3, None), (7, modrow, 5, None)]
    for i, (ti, src, mi, ni) in enumerate(plan):
        bt = bct[i % 2]
        bk = "bct%d" % (i % 2)
        for hf in range(2):
            p.t("matmul", r="ones_f modrow modrow_c", w="ps%d" % (2 + hf), out=PB(2 + hf), lhsT=ones_f[0:1, :],
                rhs=src[:, mi * 1024 + hf * 512: mi * 1024 + (hf + 1) * 512], start=True, stop=True)
            if ni is None:
                p.v("tensor_copy", r="ps%d" % (2 + hf), w=bk, out=bt[:, hf * 512:(hf + 1) * 512], in_=PB(2 + hf))
            else:
                p.v("scalar_tensor_tensor", r="ps%d nrm%d" % (2 + hf, ni), w=bk, out=bt[:, hf * 512:(hf + 1) * 512], in0=PB(2 + hf),
                    scalar=1.0, in1=nrm[ni][:, hf * 512:(hf + 1) * 512], op0=ALU.add, op1=ALU.mult)
        p.dma(modb_s[ti], bt, r=bk, w="modb_s", grp="modb_w")
    if dbg:
        p.dma(dout("modrow", [1, 6144]), modrow, r="modrow", w="dbg_modrow")
    ar.release(m0)
    p.barrier()
    if stop == "0":
        return fin(nc, p, ar, dbg_d)

    m_big = ar.mark()
    xbcT = ar.a((8, L), BF16)
    xbcTc = ar.a((8, LC), BF16)
    m_uT = ar.mark()
    uT = ar.a((4, L), BF16)

    mA = ar.mark()
    Wb = ar.a((8, 2576), BF16)
    wst = [ar.a((8, 128)) for _ in range(2)]
    W1 = ar.a(1024); S1 = ar.a(1024)

    def load_mod(i0):
        p.dma(W1, modb_s[i0], r="modb_s", w="modA0", grp="modA")
        p.dma(S1, modb_s[i0 + 1], r="modb_s", w="modA1", grp="modA")
    ncol = 2576
    pieces = [(c0, min(128, ncol - c0)) for c0 in range(0, ncol, 128)]
    for i, (c0, cw) in enumerate(pieces):
        sl = i % 2
        p.dma(wst[sl][:, :, 0:cw], win_d[:, :, c0:c0 + cw], w="wst%d" % sl)
        if i % 2 == 0:
            p.v("tensor_copy", r="wst%d" % sl, w="Wb", out=Wb[:, :, c0:c0 + cw], in_=wst[sl][:, :, 0:cw])
        else:
            p.s("copy", r="wst%d" % sl, w="Wb", out=Wb[:, :, c0:c0 + cw], in_=wst[sl][:, :, 0:cw])
    xt = [ar.a(1024) for _ in range(2)]
    junk = ar.a(1024, BF16)
    hb = [ar.a(1024, BF16) for _ in range(2)]
    hT = ar.a((8, 512), BF16)
    ss = ar.a(2); rs = ar.a(2)
    sig = ar.a(512)
    zsb = [ar.a(512, BF16) for _ in range(2)]
    dtt = ar.a(16)

    def inproj_block(src_d, t0, ntile, Wm, Sm, is_ctx, tile0):
        ntok = ntile * 128
        for ts in range(ntile):
            sl = ts % 2
            p.dma(xt[sl], src_d[t0 + ts * 128:t0 + (ts + 1) * 128, :], w="xt%d" % sl)
            p.s("activation", r="xt%d" % sl, w="junk ss", out=junk, in_=xt[sl], func=AF.Square, accum_out=ss[:, 0:1])
            p.v("tensor_scalar", r="ss", w="rs", out=rs[:, 0:1], in0=ss[:, 0:1], scalar1=1.0 / 1024, scalar2=EPS, op0=ALU.mult, op1=ALU.add)
            p.s("activation", r="rs", w="rs", out=rs[:, 0:1], in_=rs[:, 0:1], func=AF.Ln)
            p.s("activation", r="rs", w="rs", out=rs[:, 0:1], in_=rs[:, 0:1], func=AF.Exp, scale=-0.5)
            p.v("scalar_tensor_tensor", r="xt%d rs modA0" % sl, w="xt%d" % sl, out=xt[sl], in0=xt[sl], scalar=rs[:, 0:1], in1=Wm,
                op0=ALU.mult, op1=ALU.mult)
            p.g("tensor_tensor", r="xt%d modA1" % sl, w="hb%d" % sl, out=hb[sl], in0=xt[sl], in1=Sm, op=ALU.add)
            for kc in range(8):
                p.t("transpose", r="hb%d ident" % sl, w="ps%d" % (4 + sl), out=PBb(4 + sl)[:, kc * 128:(kc + 1) * 128],
                    in_=hb[sl][:, kc * 128:(kc + 1) * 128], identity=ident)
            p.s("copy", r="ps%d" % (4 + sl), w="hT", out=hT[:, :, ts * 128:(ts + 1) * 128],
                in_=PBb(4 + sl).rearrange("p (a b) -> p a b", a=8))
        for j in range(8):
            pk = j % 2
            c0 = 512 + j * 128
            for kc in range(8):
                p.t("matmul", r="Wb hT", w="ps%d" % pk, out=PB(pk)[:, 0:ntok], lhsT=Wb[:, kc, c0:c0 + 128], rhs=hT[:, kc, 0:ntok],
                    start=(kc == 0), stop=(kc == 7))
            dst = xbcTc[:, j, 0:ntok] if is_ctx else xbcT[:, j, t0:t0 + ntok]
            if j % 2 == 0:
                p.s("copy", r="ps%d" % pk, w="xbcT", out=dst, in_=PB(pk)[:, 0:ntok])
            else:
                p.v("tensor_copy", r="ps%d" % pk, w="xbcT", out=dst, in_=PB(pk)[:, 0:ntok])
        if not is_ctx:
            for j in range(4):
                ca = 1552 + j * 128
                cb = 1552 + 512 + j * 128
                for kc in range(8):
                    p.t("matmul", r="Wb hT", w="ps2", out=PB(2)[:, 0:ntok], lhsT=Wb[:, kc, ca:ca + 128], rhs=hT[:, kc, 0:ntok],
                        start=(kc == 0), stop=(kc == 7))
                for kc in range(8):
                    p.t("matmul", r="Wb hT", w="ps3", out=PB(3)[:, 0:ntok], lhsT=Wb[:, kc, cb:cb + 128], rhs=hT[:, kc, 0:ntok],
                        start=(kc == 0), stop=(kc == 7))
                p.s("activation", r="ps3", w="sig", out=sig[:, 0:ntok], in_=PB(3)[:, 0:ntok], func=AF.Sigmoid)
                p.v("tensor_tensor", r="ps2 sig", w="uT", out=uT[:, j, t0:t0 + ntok], in0=PB(2)[:, 0:ntok], in1=sig[:, 0:ntok], op=ALU.mult)
        for ts in range(ntile):
            sl = ts % 2
            if not is_ctx:
                for kc in range(8):
                    p.t("matmul", r="Wb hT", w="ps%d" % (6 + sl), out=PB(6 + sl), lhsT=hT[:, kc, ts * 128:(ts + 1) * 128], rhs=Wb[:, kc, 0:512],
                        start=(kc == 0), stop=(kc == 7))
                p.s("activation", r="ps%d" % (6 + sl), w="zsb%d" % sl, out=zsb[sl], in_=PB(6 + sl), func=AF.Silu)
                p.dma(zs_s[t0 + ts * 128:t0 + (ts + 1) * 128, :], zsb[sl], r="zsb%d" % sl, w="zs_s", e="sync", grp="zsw%d" % sl)
            for kc in range(8):
                p.t("matmul", r="Wb hT", w="ps0", out=PB(0)[:, 0:16], lhsT=hT[:, kc, ts * 128:(ts + 1) * 128], rhs=Wb[:, kc, 1536:1552],
                    start=(kc == 0), stop=(kc == 7))
            p.v("tensor_tensor", r="ps0 dtbias", w="dtt", out=dtt, in0=PB(0)[:, 0:16], in1=dtbias, op=ALU.add)
            p.s("activation", r="dtt", w="dtt", out=dtt, in_=dtt, func=AF.Exp)
            p.s("activation", r="dtt", w="dt_all", out=dt_all[:, tile0 + ts, :], in_=dtt, func=AF.Ln, bias=1.0)

    load_mod(2)
    inproj_block(ctx_d, 0, 2, W1, S1, True, NT)
    if stop == "A1":
        p.dma(dout("dt_all", [128, NT + 2, 16]), dt_all, r="dt_all", w="dbg_dt")
        return fin(nc, p, ar, dbg_d)
    load_mod(0)
    for b in range(L // 512):
        inproj_block(x_d, b * 512, 4, W1, S1, False, b * 4)
    if dbg:
        p.dma(dout("dt_all", [128, NT + 2, 16]), dt_all, r="dt_all", w="dbg_dt")
        dxb = dout("xbc_pre", [128, 8, 128])
        tdb = ar.a((8, 128))
        p.v("tensor_copy", r="xbcT", w="tdb", out=tdb, in_=xbcT[:, :, 512:640])
        p.dma(dxb, tdb, r="tdb", w="dbg_xb")
        dub = dout("u_pre", [128, 4, 128])
        tdb2 = ar.a((4, 128))
        p.v("tensor_copy", r="uT", w="tdb2", out=tdb2, in_=uT[:, :, 512:640])
        p.dma(dub, tdb2, r="tdb2", w="dbg_ub")
    ar.release(mA)
    p.barrier()
    if stop == "A":
        return fin(nc, p, ar, dbg_d)

    mB = ar.mark()
    acc = [ar.a(L) for _ in range(2)]
    EN = [p.v, p.v]

    def ssm_conv(src, n, ei, j, key):
        E = EN[ei]
        ak = "acc%d" % ei
        a_ = acc[ei][:, 0:n]
        x_ = src[:, j, 0:n]
        E("tensor_scalar", r=key + " scw", w=ak, out=a_, in0=x_, scalar1=scw[:, j, 2:3], scalar2=None, op0=ALU.mult)
        for k in (0, 1, 3, 4):
            o = k - 2
            lo, hi = max(0, -o), min(n, n - o)
            E("scalar_tensor_tensor", r=key + " scw " + ak, w=ak, out=a_[:, lo:hi], in0=x_[:, lo + o:hi + o], scalar=scw[:, j, k:k + 1],
              in1=a_[:, lo:hi], op0=ALU.mult, op1=ALU.add)
        p.s("activation", r=ak + " scb", w=key, out=x_, in_=a_, func=AF.Silu, bias=scb[:, j:j + 1])

    tmpc2 = [ar.a(L) for _ in range(2)]

    def cfm_conv(j, ei):
        ak = "acc%d" % ei
        key = "uT%d" % j
        a_ = acc[ei]
        u_ = uT[:, j, :]
        a3 = a_.rearrange("p (r c) -> p r c", c=64)
        u3 = u_.rearrange("p (r c) -> p r c", c=64)
        if ei == 0:
            p.v("tensor_scalar", r=key + " ccw", w=ak, out=a_, in0=u_, scalar1=ccw[:, j, 15:16], scalar2=None, op0=ALU.mult)
        else:
            p.s("activation", r=key + " ccw", w=ak, out=a_, in_=u_, func=AF.Copy, scale=ccw[:, j, 15:16])
        n = 0
        for k in range(31):
            o = k - 15
            if o == 0:
                continue
            lo, hi = max(0, -o), min(64, 64 - o)
            if j < 2:
                oo, i0 = a3[:, :, lo:hi], u3[:, :, lo + o:hi + o]
            else:
                oo, i0 = a_[:, lo * 64:hi * 64], u_[:, (lo + o) * 64:(hi + o) * 64]
            if ei == 0:
                p.v("scalar_tensor_tensor", r=key + " ccw " + ak, w=ak, out=oo, in0=i0, scalar=ccw[:, j, k:k + 1], in1=oo, op0=ALU.mult, op1=ALU.add)
            else:
                tk = "tmpc2%d" % (n % 2)
                tt = tmpc2[n % 2]
                if j < 2:
                    t_ = tt.rearrange("p (r c) -> p r c", c=64)[:, :, lo:hi]
                else:
                    t_ = tt[:, lo * 64:hi * 64]
                p.s("activation", r=key + " ccw", w=tk, out=t_, in_=i0, func=AF.Copy, scale=ccw[:, j, k:k + 1])
                p.g("tensor_tensor", r=tk + " " + ak, w=ak, out=oo, in0=oo, in1=t_, op=ALU.add)
                n += 1
        p.s("activation", r=ak + " cvec", w=key, out=u_, in_=a_, func=AF.Identity, bias=cvec[:, 0, j:j + 1])

    for j in range(8):
        ssm_conv(xbcTc, LC, 0, j, "xbcTc%d" % j)
    cfm_conv(2, 1)
    for j in range(8):
        ssm_conv(xbcT, L, 0, j, "xbcT%d" % j)
    cfm_conv(3, 1)
    cfm_conv(0, 0)
    cfm_conv(1, 0)
    if dbg:
        tdb = ar.a((8, 128))
        p.v("tensor_copy", r=" ".join("xbcT%d" % j for j in range(8)), w="tdb", out=tdb, in_=xbcT[:, :, 512:640])
        p.dma(dout("xbc_post", [128, 8, 128]), tdb, r="tdb", w="dbg_xb2")
        tdb2 = ar.a((4, 128))
        p.v("tensor_copy", r=" ".join("uT%d" % j for j in range(4)), w="tdb2", out=tdb2, in_=uT[:, :, 512:640])
        p.dma(dout("u_post", [128, 4, 128]), tdb2, r="tdb2", w="dbg_ub2")
    for j in range(4):
        p.dma(uts_s[:, j, :], uT[:, j, :], r="uT%d" % j, w="uts_s", grp="utsw")
    ar.release(m_uT)
    p.barrier()
    if stop == "B":
        return fin(nc, p, ar, dbg_d)

    mE = ar.mark()
    SB_all = ar.a((NT, 512), BF16)
    Wo = ar.a((8, 1024), BF16)
    G1 = ar.a(1024); NW = ar.a(512); Drow = ar.a(512)
    p.dma(G1, modb_s[4], r="modb_s", w="G1")
    p.dma(NW, snw_d, w="NW")
    p.dma(Drow, drow_d, w="Drow")
    a16 = ar.a(16); cs = ar.a(32); dec = ar.a(16); tmp16 = ar.a(16); w16 = ar.a(16)
    rowsrc = ar.a(16); bias16 = ar.a(16); e16 = ar.a(16); RT = ar.a(128); RT3 = ar.a((3, 128), BF16); RTr = ar.a(128)
    xs_tok = ar.a(512, BF16); Btok = ar.a(256, BF16)
    xw = [ar.a(512, BF16) for _ in range(2)]
    xdt = [ar.a(512, BF16) for _ in range(2)]
    ST_sb = ar.a((2, 128))
    LT = [ar.a(128) for _ in range(2)]
    SmT = ar.a((16, 128), BF16)
    Sst = [ar.a(512) for _ in range(2)]
    Sbf = [ar.a(512, BF16) for _ in range(2)]
    yc = ar.a(512); yc2 = ar.a(512); zst = ar.a(512, BF16); gg = ar.a(512)
    ss2 = ar.a(2); rs2 = ar.a(2)
    ssm_tok = ar.a(512, BF16); catT = ar.a((8, 128), BF16)
    vsq = ar.a((4, 128)); vsqh = ar.a((4, 128), BF16); vsql = ar.a((4, 128), BF16); mean = ar.a(128); msq = ar.a(128); var = ar.a(128); ntmp = ar.a((4, 128))
    xe = [ar.a(1024) for _ in range(2)]
    for i in range(8):
        sl = i % 2
        wv_ = xe[sl].rearrange("p (a b) -> p a b", a=8)
        p.dma(wv_, wout_d[:, :, i * 128:(i + 1) * 128], w="xe%d" % sl)
        if i % 2 == 0:
            p.v("tensor_copy", r="xe%d" % sl, w="Wo", out=Wo[:, :, i * 128:(i + 1) * 128], in_=wv_)
        else:
            p.s("copy", r="xe%d" % sl, w="Wo", out=Wo[:, :, i * 128:(i + 1) * 128], in_=wv_)
    junk2 = ar.a(256, BF16)
    uTt = [ar.a((4, 128), BF16) for _ in range(2)]
    PT = PBb(0)
    PC = PB(1)
    PK = PB(2)
    PY = PB(3)
    PLS = PB(6)
    PM = PBb(7)
    PM2 = PB(7)

    def bc8(ap8):
        return ap8.unsqueeze(2).to_broadcast([128, 8, 64])

    def v3(ap512):
        return ap512.rearrange("p (h q) -> p h q", h=8)

    def prep(is_ctx, c, dirs):
        src = xbcTc if is_ctx else xbcT
        kx = "xbcTc%d" if is_ctx else "xbcT%d"
        tl = (NT + c) if is_ctx else c
        tok = slice(c * 128, (c + 1) * 128)
        p.v("tensor_tensor", r="dt_all Aneg", w="a16", out=a16, in0=dt_all[:, tl, :], in1=Aneg, op=ALU.mult)
        p.t("matmul", r="triU a16", w="b1", out=PC[:, 0:8], lhsT=triU, rhs=a16[:, 0:8], start=True, stop=True)
        p.t("matmul", r="triS a16", w="b1", out=PC[:, 8:16], lhsT=triS, rhs=a16[:, 8:16], start=True, stop=True)
        p.t("matmul", r="ones_f a16", w="b1", out=PC[:, 16:32], lhsT=ones_f, rhs=a16, start=True, stop=True)
        p.s("copy", r="b1", w="cs", out=cs, in_=PC[:, 0:32])
        p.s("activation", r="cs", w="dec", out=dec, in_=cs[:, 16:32], func=AF.Exp)
        p.v("tensor_tensor", r="cs", w="tmp16", out=tmp16[:, 0:8], in0=cs[:, 16:24], in1=cs[:, 0:8], op=ALU.subtract)
        p.v("tensor_copy", r="cs tmp16", w="tmp16", out=tmp16[:, 8:16], in_=cs[:, 8:16])
        p.s("activation", r="tmp16", w="tmp16", out=tmp16, in_=tmp16, func=AF.Exp)
        p.v("tensor_tensor", r="tmp16 dt_all", w="w16", out=w16, in0=tmp16, in1=dt_all[:, tl, :], op=ALU.mult)
        for j in range(4):
            p.t("transpose", r=(kx % j) + " ident", w="b0", out=PT[:, j * 128:(j + 1) * 128], in_=src[:, j, tok], identity=ident)
        for g in range(2):
            p.t("transpose", r=(kx % (4 + g)) + " ident", w="b0", out=PT[:, 512 + g * 128:512 + (g + 1) * 128], in_=src[:, 4 + g, tok], identity=ident)
        p.v("tensor_copy", r="b0", w="Btok", out=Btok, in_=PT[:, 512:768])
        for d in dirs:
            p.v("tensor_tensor", r="b0 w16", w="xw%d" % d, out=v3(xw[d]), in0=v3(PT[:, 0:512]), in1=bc8(w16[:, d * 8:(d + 1) * 8]), op=ALU.mult)

    def local_states(d):
        for h in range(8):
            g = h // 4
            p.t("matmul", r="Btok xw%d" % d, w="b6", out=PLS[:, h * 64:(h + 1) * 64], lhsT=Btok[:, g * 128:(g + 1) * 128],
                rhs=xw[d][:, h * 64:(h + 1) * 64], start=True, stop=True)

    def upd(d):
        p.v("tensor_tensor", r="S%d dec" % d, w="S%d" % d, out=v3(Sst[d]), in0=v3(Sst[d]), in1=bc8(dec[:, d * 8:(d + 1) * 8]), op=ALU.mult)
        p.v("tensor_tensor", r="S%d b6" % d, w="S%d" % d, out=Sst[d], in0=Sst[d], in1=PLS, op=ALU.add)
        p.s("copy", r="S%d" % d, w="Sbf%d" % d, out=Sbf[d], in_=Sst[d])

    p.v("memset", w="RT3", ap=RT3, constant=0.0)
    for d in range(2):
        p.v("memset", w="S%d" % d, ap=Sst[d], constant=0.0)
        p.v("memset", w="Sbf%d" % d, ap=Sbf[d], constant=0.0)
    prep(True, 0, (0,)); local_states(0); upd(0)
    prep(True, 1, (0, 1)); local_states(0); upd(0); local_states(1); upd(1)
    prep(True, 0, (1,)); local_states(1); upd(1)
    if stop == "C":
        p.dma(dout("S_f", [128, 512]), Sst[0], r="S0", w="dbg_sf")
        p.dma(dout("S_b", [128, 512]), Sst[1], r="S1", w="dbg_sb")
        return fin(nc, p, ar, dbg_d)
    for c in range(NT - 1, -1, -1):
        p.s("copy", r="Sbf1", w="SB%d" % c, out=SB_all[:, c, :], in_=Sbf[1])
        prep(False, c, (1,)); local_states(1); upd(1)
        if npeer == 128:
            if c >= 16:
                hp_ = c - 16
                p.dma(wqb_s[hp_].rearrange("p (k c) -> p k c", k=8), wq_d[:, :, hp_ * 128:(hp_ + 1) * 128], w="wqb_s", e="gpsimd", grp="castq")
            for i2 in range(c * 4, c * 4 + 4):
                p.dma(utb_s[i2], ut_d[i2].rearrange("p k i -> p (k i)"), w="utb_s", e="gpsimd", grp="castu")
                p.dma(vtb_s[i2], vt_d[i2], w="vtb_s", e="gpsimd", grp="castv")
    if dbg:
        p.dma(dout("S_b0", [128, 512]), Sst[1], r="S1", w="dbg_sb0")
    if stop == "D":
        return fin(nc, p, ar, dbg_d)
    for c in range(NT):
        tok = slice(c * 128, (c + 1) * 128)
        pe = c % 2
        p.dma(xe[pe], x_d[tok, :], w="xe%d" % pe)
        p.dma(zst, zs_s[tok, :], r="zs_s", w="zst")
        p.dma(uTt[pe], uts_s[:, :, tok], r="uts_s", w="uTt%d" % pe)
        uTc = uTt[pe]
        if stop == "E0":
            return fin(nc, p, ar, dbg_d)
        prep(False, c, (0,))
        if stop == "E1":
            return fin(nc, p, ar, dbg_d)
        p.v("tensor_copy", r="b0", w="xs_tok", out=xs_tok, in_=PT[:, 0:512])
        for d in range(2):
            p.v("tensor_tensor", r="b0 dt_all", w="xdt%d" % d, out=v3(xdt[d]), in0=v3(PT[:, 0:512]),
                in1=bc8(dt_all[:, c, d * 8:(d + 1) * 8]), op=ALU.mult)
        if stop == "E1a":
            return fin(nc, p, ar, dbg_d)
        for g in range(2):
            p.t("matmul", r="xbcT%d xbcT%d" % (4 + g, 6 + g), w="b1", out=PC[:, 256 + g * 128:256 + (g + 1) * 128],
                lhsT=xbcT[:, 4 + g, tok], rhs=xbcT[:, 6 + g, tok], start=True, stop=True)
        if stop == "E1b":
            return fin(nc, p, ar, dbg_d)
        p.s("copy", r="b1", w="ST_sb", out=ST_sb, in_=PC[:, 256:512].rearrange("p (g l) -> p g l", g=2))
        if stop == "E2":
            return fin(nc, p, ar, dbg_d)

        p.v("tensor_copy", r="cs", w="rowsrc", out=rowsrc[:, 0:8], in_=cs[:, 0:8])
        p.v("tensor_scalar", r="cs rowsrc", w="rowsrc", out=rowsrc[:, 8:16], in0=cs[:, 8:16], scalar1=-1.0, scalar2=None, op0=ALU.mult)
        p.v("tensor_scalar", r="rowsrc", w="bias16", out=bias16, in0=rowsrc, scalar1=-1.0, scalar2=None, op0=ALU.mult)
        p.t("transpose", r="rowsrc identf", w="b1", out=PC[0:16, 32:160], in_=rowsrc, identity=ident_f)
        p.s("copy", r="b1", w="RT", out=RT[0:16, :], in_=PC[0:16, 32:160])
        p.v("tensor_copy", r="RT", w="RT3", out=RT3[0:16, 0, :], in_=RT[0:16, :])
        p.v("tensor_tensor", r="RT RT3", w="RTr", out=RTr[0:16, :], in0=RT[0:16, :], in1=RT3[0:16, 0, :], op=ALU.subtract)
        p.v("tensor_copy", r="RTr RT3", w="RT3", out=RT3[0:16, 1, :], in_=RTr[0:16, :])
        p.v("tensor_tensor", r="RTr RT3", w="RTr", out=RTr[0:16, :], in0=RTr[0:16, :], in1=RT3[0:16, 1, :], op=ALU.subtract)
        p.v("tensor_copy", r="RTr RT3", w="RT3", out=RT3[0:16, 2, :], in_=RTr[0:16, :])
        if stop == "E3":
            return fin(nc, p, ar, dbg_d)

        p.v("tensor_copy", r="cs", w="e16", out=e16[:, 0:8], in_=cs[:, 0:8])
        p.v("tensor_tensor", r="cs e16", w="e16", out=e16[:, 8:16], in0=cs[:, 24:32], in1=cs[:, 8:16], op=ALU.subtract)
        p.s("activation", r="e16", w="e16", out=e16, in_=e16, func=AF.Exp)
        if stop == "E4":
            return fin(nc, p, ar, dbg_d)

        for idx in range(16):
            d, h = idx // 8, idx % 8
            g = h // 4
            pk = "b2" if idx % 2 == 0 else "b6"
            PKs = (PB(2) if idx % 2 == 0 else PB(6))[:, 0:128]
            for q in range(3):
                p.t("matmul", r="sel RT3", w=pk, out=PKs, lhsT=sel[:, idx, :], rhs=RT3[:, q, :], start=(q == 0), stop=False)
            p.t("matmul", r="ident maskF maskB", w=pk, out=PKs, lhsT=ident, rhs=(maskF if d == 0 else maskB), start=False, stop=True)
            lk = "LT%d" % (idx % 2)
            p.s("activation", r=pk + " bias16", w=lk, out=LT[idx % 2], in_=PKs, func=AF.Exp, bias=bias16[:, idx:idx + 1])
            p.v("tensor_tensor", r=lk + " ST_sb", w="SmT%d" % idx, out=SmT[:, idx, :], in0=LT[idx % 2], in1=ST_sb[:, g, :], op=ALU.mult)
        if stop == "E5a":
            return fin(nc, p, ar, dbg_d)
        for h in range(8):
            hs = slice(h * 64, (h + 1) * 64)
            p.t("matmul", r="SmT%d xdt0" % h, w="b3", out=PY[:, hs], lhsT=SmT[:, h, :], rhs=xdt[0][:, hs], start=True, stop=False)
            p.t("matmul", r="SmT%d xdt1" % (8 + h), w="b3", out=PY[:, hs], lhsT=SmT[:, 8 + h, :], rhs=xdt[1][:, hs], start=False, stop=True)
        for h in range(8):
            g = h // 4
            hs = slice(h * 64, (h + 1) * 64)
            p.t("matmul", r="xbcT%d Sbf0" % (6 + g), w="b4", out=PB(4)[:, hs], lhsT=xbcT[:, 6 + g, tok], rhs=Sbf[0][:, hs], start=True, stop=True)
            p.t("matmul", r="xbcT%d SB%d" % (6 + g, c), w="b5", out=PB(5)[:, hs], lhsT=xbcT[:, 6 + g, tok], rhs=SB_all[:, c, hs], start=True, stop=True)
        if stop == "E5b":
            return fin(nc, p, ar, dbg_d)
        p.v("tensor_tensor", r="b4 e16", w="yc", out=v3(yc), in0=v3(PB(4)), in1=bc8(e16[:, 0:8]), op=ALU.mult)
        p.v("tensor_tensor", r="b5 e16", w="yc2", out=v3(yc2), in0=v3(PB(5)), in1=bc8(e16[:, 8:16]), op=ALU.mult)
        p.g("tensor_tensor", r="yc yc2", w="yc", out=yc, in0=yc, in1=yc2, op=ALU.add)
        if stop == "E6":
            return fin(nc, p, ar, dbg_d)

        p.v("tensor_tensor", r="yc b3", w="yc", out=yc, in0=yc, in1=PY, op=ALU.add)
        p.g("tensor_tensor", r="xs_tok Drow", w="yc2", out=yc2, in0=xs_tok, in1=Drow, op=ALU.mult)
        p.v("tensor_tensor", r="yc yc2", w="yc", out=yc, in0=yc, in1=yc2, op=ALU.add)
        p.v("tensor_tensor", r="yc zst", w="gg", out=gg, in0=yc, in1=zst, op=ALU.mult)
        for g in range(2):
            p.s("activation", r="gg", w="junk2 ss2", out=junk2, in_=gg[:, g * 256:(g + 1) * 256], func=AF.Square, accum_out=ss2[:, g:g + 1])
        p.v("tensor_scalar", r="ss2", w="rs2", out=rs2, in0=ss2, scalar1=1.0 / 256, scalar2=EPS, op0=ALU.mult, op1=ALU.add)
        p.s("activation", r="rs2", w="rs2", out=rs2, in_=rs2, func=AF.Ln)
        p.s("activation", r="rs2", w="rs2", out=rs2, in_=rs2, func=AF.Exp, scale=-0.5)
        for g in range(2):
            gs = slice(g * 256, (g + 1) * 256)
            p.v("scalar_tensor_tensor", r="gg rs2 NW", w="ssm_tok", out=ssm_tok[:, gs], in0=gg[:, gs], scalar=rs2[:, g:g + 1], in1=NW[:, gs],
                op0=ALU.mult, op1=ALU.mult)
        for j in range(4):
            p.t("transpose", r="ssm_tok ident", w="b7", out=PM[:, j * 128:(j + 1) * 128], in_=ssm_tok[:, j * 128:(j + 1) * 128], identity=ident)
        p.s("copy", r="b7", w="catT", out=catT[:, 0:4, :], in_=PM[:, 0:512].rearrange("p (a b) -> p a b", a=4))
        if stop == "E7":
            return fin(nc, p, ar, dbg_d)

        ukeys = "uTt%d" % pe
        p.s("activation", r=ukeys, w="vsq", out=vsq, in_=uTc, func=AF.Square)
        for j in range(4):
            p.t("matmul", r="ones_b " + ukeys, w="b7", out=PM2[:, 256:384], lhsT=ones_b, rhs=uTc[:, j, :], start=(j == 0), stop=(j == 3))
        p.v("tensor_copy", r="vsq", w="vsqh", out=vsqh, in_=vsq)
        p.v("tensor_tensor", r="vsq vsqh", w="vsql", out=vsql, in0=vsq, in1=vsqh, op=ALU.subtract)
        for j in range(4):
            p.t("matmul", r="ones_b vsqh", w="b7", out=PM2[:, 384:512], lhsT=ones_b, rhs=vsqh[:, j, :], start=(j == 0), stop=False)
            p.t("matmul", r="ones_b vsql", w="b7", out=PM2[:, 384:512], lhsT=ones_b, rhs=vsql[:, j, :], start=False, stop=(j == 3))
        p.s("activation", r="b7", w="mean", out=mean, in_=PM2[:, 256:384], func=AF.Copy, scale=1.0 / 512)
        p.s("activation", r="b7", w="var", out=var, in_=PM2[:, 384:512], func=AF.Copy, scale=1.0 / 512)
        p.g("tensor_tensor", r="mean", w="msq", out=msq, in0=mean, in1=mean, op=ALU.mult)
        p.v("tensor_tensor", r="var msq", w="var", out=var, in0=var, in1=msq, op=ALU.subtract)
        p.v("tensor_scalar", r="var", w="var", out=var, in0=var, scalar1=EPS, scalar2=None, op0=ALU.add)
        p.s("activation", r="var", w="var", out=var, in_=var, func=AF.Ln)
        p.s("activation", r="var", w="var", out=var, in_=var, func=AF.Exp, scale=-0.5)
        p.v("tensor_tensor", r=ukeys + " mean", w="ntmp", out=ntmp, in0=uTc, in1=mean.unsqueeze(1).to_broadcast([128, 4, 128]), op=ALU.subtract)
        p.v("tensor_tensor", r="ntmp var", w="ntmp", out=ntmp, in0=ntmp, in1=var.unsqueeze(1).to_broadcast([128, 4, 128]), op=ALU.mult)
        for j in range(4):
            p.s("activation", r="ntmp cvec", w="catT", out=catT[:, 4 + j, :], in_=ntmp[:, j, :], func=AF.Silu, scale=cvec[:, 1, j:j + 1],
                bias=cvec[:, 2, j:j + 1])
        for nb in range(2):
            for kc in range(8):
                p.t("matmul", r="catT Wo", w="b%d" % (4 + nb), out=PB(4 + nb), lhsT=catT[:, kc, :], rhs=Wo[:, kc, nb * 512:(nb + 1) * 512],
                    start=(kc == 0), stop=(kc == 7))
            ns = slice(nb * 512, (nb + 1) * 512)
            tq, tk_ = (yc2, "yc2") if nb == 0 else (gg, "gg")
            p.v("tensor_tensor", r="b%d G1" % (4 + nb), w=tk_, out=tq, in0=PB(4 + nb), in1=G1[:, ns], op=ALU.mult)
            p.g("tensor_tensor", r=tk_ + " xe%d" % pe, w="xe%d" % pe, out=xe[pe][:, ns], in0=xe[pe][:, ns], in1=tq, op=ALU.add)
        p.dma(x1_s[tok, :], xe[pe], r="xe%d" % pe, w="x1_s", grp="x1w%d" % pe)
        local_states(0)
        upd(0)
    if dbg:
        dx1 = dout("x1", [L, 1024])
        p.dma(None, None, r="x1_s", w="dbg_x1", parts=[(dx1[i * 128:(i + 1) * 128, :], x1_s[i * 128:(i + 1) * 128, :]) for i in range(NT)])
    ar.release(mE)
    p.barrier()
    if stop == "E":
        return fin(nc, p, ar, dbg_d)

    ar.release(m_big)
    TB = 256
    KT = ar.a((16, 128), BF16)
    W2 = ar.a(1024); S2 = ar.a(1024); G2 = ar.a(1024); FW = ar.a(1024)
    p.dma(W2, modb_s[5], r="modb_s", w="W2")
    p.dma(S2, modb_s[6], r="modb_s", w="S2")
    p.dma(G2, modb_s[7], r="modb_s", w="G2")
    p.dma(FW, nrm_d[2], w="FW")
    xs1 = [ar.a(1024) for _ in range(2)]
    for i in range(4):
        sl = i % 2
        wv_ = xs1[sl][:, 0:512].rearrange("p (a b) -> p a b", a=4)
        p.dma(wv_, kt_d[:, i * 4:(i + 1) * 4, :], w="xs1%d" % sl)
        p.v("tensor_copy", r="xs1%d" % sl, w="KT", out=KT[:, i * 4:(i + 1) * 4, :], in_=wv_)
    iota_b = ar.a(128, BF16)
    p.v("tensor_copy", r="iota", w="iota_b", out=iota_b, in_=iota_f)
    Wqs = [ar.a((8, 128), BF16) for _ in range(3)]
    tmpF = ar.a(512)
    junkF = tmpF.bitcast(BF16)
    ssF = ar.a(2); rsF = ar.a(2)
    h2 = ar.a(1024, BF16)
    h2T = [ar.a((8, TB), BF16) for _ in range(3)]
    qT = ar.a((16, TB), BF16)
    sc = ar.a((16, 128))
    sv = ar.a((16, 16)); si = ar.a((16, 16), U32); sif = ar.a((16, 16)); wk = ar.a(128); wk2 = ar.a(256)
    best = ar.a((8, 16)); fl = ar.a((8, 16), U32); ai = ar.a((8, 16), U32); bi = ar.a((8, 16), U32)
    af = ar.a((8, 16)); bf_ = ar.a((8, 16)); gt = ar.a((8, 16)); gs = ar.a(8)
    IG = ar.a((3, 128))
    IGTb = [ar.a((2, 2, 128), BF16) for _ in range(3)]
    IGTg = [ar.a((2, 128)) for _ in range(3)]
    NG = 16
    ohA = ar.a((NG, 128), BF16)
    ohB = ar.a((NG, 64), BF16)
    oh = ar.a((8, 16, 16))
    GmH = [ar.a((TB, 64), BF16) for _ in range(2)]
    wu = [ar.a((2, 1024), BF16) for _ in range(3)]
    wv = [ar.a((2, 1024), BF16) for _ in range(3)]
    ga = [ar.a(TB) for _ in range(2)]
    gab = [ar.a(TB, BF16) for _ in range(2)]
    cand = sc.rearrange("p a b -> p (a b)").rearrange("p (h a b) -> p h a b", h=8, a=16)
    svv = sv.rearrange("p (h t) a -> p h t a", t=2)
    sifv = sif.rearrange("p (h t) a -> p h t a", t=2)
    io16 = iota_f[:, 0:16].unsqueeze(1).unsqueeze(1).to_broadcast([128, 8, 16, 16])
    NBLK = L // TB
    wqi = [0]

    def part1(blk):
        par = blk % 3
        hT = h2T[par]
        hk = "h2T%d" % par
        for ts in range(2):
            t0 = blk * TB + ts * 128
            xk = "xs1%d" % ts
            p.dma(xs1[ts], x1_s[t0:t0 + 128, :], r="x1_s", w=xk)
            p.s("activation", r=xk, w="tmpF ssF", out=junkF, in_=xs1[ts], func=AF.Square, accum_out=ssF[:, 0:1])
            p.v("tensor_scalar", r="ssF", w="rsF", out=rsF[:, 0:1], in0=ssF[:, 0:1], scalar1=1.0 / 1024, scalar2=EPS, op0=ALU.mult, op1=ALU.add)
            p.s("activation", r="rsF", w="rsF", out=rsF[:, 0:1], in_=rsF[:, 0:1], func=AF.Ln)
            p.s("activation", r="rsF", w="rsF", out=rsF[:, 0:1], in_=rsF[:, 0:1], func=AF.Exp, scale=-0.5)
            yield
            p.v("scalar_tensor_tensor", r=xk + " rsF W2", w=xk, out=xs1[ts], in0=xs1[ts], scalar=rsF[:, 0:1], in1=W2, op0=ALU.mult, op1=ALU.mult)
            p.g("tensor_tensor", r=xk + " S2", w="h2", out=h2, in0=xs1[ts], in1=S2, op=ALU.add)
            yield
            for kc in range(8):
                p.t("transpose", r="h2 ident", w="b2", out=PBb(2)[:, kc * 128:(kc + 1) * 128], in_=h2[:, kc * 128:(kc + 1) * 128], identity=ident)
            p.s("copy", r="b2", w=hk, out=hT[:, :, ts * 128:(ts + 1) * 128], in_=PBb(2).rearrange("p (a b) -> p a b", a=8))
            yield
        for hp in range(16):
            bk = 2 + hp % 2
            ws = wqi[0] % 3
            wqi[0] += 1
            p.dma(Wqs[ws], wqb_s[hp].rearrange("p (k c) -> p k c", k=8), r="wqb_s", w="Wqs%d" % ws)
            for kc in range(8):
                p.t("matmul", r=("Wqs%d " % ws) + hk, w="b%d" % bk, out=PB(bk)[:, 0:TB], lhsT=Wqs[ws][:, kc, :], rhs=hT[:, kc, :],
                    start=(kc == 0), stop=(kc == 7))
            if bk == 2:
                p.s("copy", r="b2", w="qT", out=qT[:, hp, :], in_=PB(2)[:, 0:TB])
            else:
                p.v("tensor_copy", r="b3", w="qT", out=qT[:, hp, :], in_=PB(3)[:, 0:TB])
            yield
        for ts in range(2):
            tsl = slice(ts * 128, (ts + 1) * 128)
            for g4 in range(4):
                bk = 2 + g4 % 2
                for q4 in range(4):
                    hp = g4 * 4 + q4
                    p.t("matmul", r="qT KT", w="b%d" % bk, out=PB(bk)[:, q4 * 128:(q4 + 1) * 128], lhsT=qT[:, hp, tsl], rhs=KT[:, hp, :],
                        start=True, stop=True)
                p.s("copy", r="b%d" % bk, w="sc", out=sc[:, g4 * 4:(g4 + 1) * 4, :], in_=PB(bk).rearrange("p (a b) -> p a b", a=4))
                yield
            for hp in range(16):
                p.v("max", r="sc", w="sv", out=sv[:, hp, 0:8], in_=sc[:, hp, :])
                p.v("max_index", r="sc sv", w="si", out=si[:, hp, 0:8], in_max=sv[:, hp, 0:8], in_values=sc[:, hp, :])
                p.v("match_replace", r="sc sv", w="wk", out=wk, in_to_replace=sv[:, hp, 0:8], in_values=sc[:, hp, :], imm_value=-1e30)
                yield
                p.v("max", r="wk", w="sv", out=sv[:, hp, 8:16], in_=wk)
                p.v("max_index", r="wk sv", w="si", out=si[:, hp, 8:16], in_max=sv[:, hp, 8:16], in_values=wk)
                yield
            p.v("tensor_copy", r="si", w="sif", out=sif, in_=si)
            p.v("tensor_tensor", r="sv sc", w="sc", out=cand, in0=svv[:, :, 0, :].unsqueeze(3).to_broadcast([128, 8, 16, 16]),
                in1=svv[:, :, 1, :].unsqueeze(2).to_broadcast([128, 8, 16, 16]), op=ALU.add)
            yield
            for h in range(8):
                ch = cand[:, h].rearrange("p a b -> p (a b)")
                p.v("max", r="sc", w="best", out=best[:, h, 0:8], in_=ch)
                p.v("max_index", r="sc best", w="fl", out=fl[:, h, 0:8], in_max=best[:, h, 0:8], in_values=ch)
                p.v("match_replace", r="sc best", w="wk2", out=wk2, in_to_replace=best[:, h, 0:8], in_values=ch, imm_value=-1e30)
                yield
                p.v("max", r="wk2", w="best", out=best[:, h, 8:16], in_=wk2)
                p.v("max_index", r="wk2 best", w="fl", out=fl[:, h, 8:16], in_max=best[:, h, 8:16], in_values=wk2)
                yield
            p.v("tensor_tensor", r="best", w="gt", out=gt, in0=best, in1=best[:, :, 0:1].to_broadcast([128, 8, 16]), op=ALU.subtract)
            p.s("activation", r="gt", w="gt", out=gt, in_=gt, func=AF.Exp)
            p.v("tensor_reduce", r="gt", w="gs", out=gs, in_=gt, axis=AX.X, op=ALU.add)
            p.v("reciprocal", r="gs", w="gs", out=gs, in_=gs)
            yield
            p.v("tensor_tensor", r="gt gs IG", w="IG", out=IG[:, 2, :].rearrange("p (h j) -> p h j", h=8), in0=gt,
                in1=gs.unsqueeze(2).to_broadcast([128, 8, 16]), op=ALU.mult)
            p.v("tensor_single_scalar", r="fl", w="ai", out=ai, in_=fl, scalar=4, op=ALU.logical_shift_right)
            p.v("tensor_single_scalar", r="fl", w="bi", out=bi, in_=fl, scalar=15, op=ALU.bitwise_and)
            p.v("tensor_copy", r="ai", w="af", out=af, in_=ai)
            p.v("tensor_copy", r="bi", w="bf", out=bf_, in_=bi)
            yield
            for t_, (xf, key) in enumerate(((af, "af"), (bf_, "bf"))):
                p.v("tensor_tensor", r=key + " iota", w="oh", out=oh, in0=xf.unsqueeze(3).to_broadcast([128, 8, 16, 16]), in1=io16, op=ALU.is_equal)
                yield
                p.v("tensor_tensor", r="oh sif", w="oh", out=oh, in0=oh, in1=sifv[:, :, t_, :].unsqueeze(2).to_broadcast([128, 8, 16, 16]), op=ALU.mult)
                yield
                p.v("tensor_reduce", r="oh IG", w="IG", out=IG[:, t_, :].rearrange("p (h j) -> p h j", h=8), in_=oh, axis=AX.X, op=ALU.add)
                yield
            for q in range(3):
                p.t("transpose", r="IG identf", w="b3", out=PB(3)[:, q * 128:(q + 1) * 128], in_=IG[:, q, :], identity=ident_f)
            p.s("copy", r="b3", w="IGTb%d" % par, out=IGTb[par][:, ts, :, :], in_=PB(3)[:, 0:256].rearrange("p (a b) -> p a b", a=2))
            p.s("copy", r="b3", w="IGTg%d" % par, out=IGTg[par][:, ts, :], in_=PB(3)[:, 256:384])
            yield

    def part2(blk, half):
        par = blk % 3
        kb, kg = "IGTb%d" % par, "IGTg%d" % par
        Gh = GmH[half]
        gk = "Gm%d" % half
        for ts in range(2):
            for tg in range(128 // NG):
                tq = slice(tg * NG, (tg + 1) * NG)
                p.v("tensor_tensor", r=kb + " iota_b", w="ohA", out=ohA, in0=iota_b.unsqueeze(1).to_broadcast([128, NG, 128]),
                    in1=IGTb[par][:, ts, 0, tq].unsqueeze(2).to_broadcast([128, NG, 128]), op=ALU.is_equal)
                p.v("tensor_tensor", r=kb + " iota_b", w="ohB", out=ohB, in0=iota_b[:, half * 64:(half + 1) * 64].unsqueeze(1).to_broadcast([128, NG, 64]),
                    in1=IGTb[par][:, ts, 1, tq].unsqueeze(2).to_broadcast([128, NG, 64]), op=ALU.is_equal)
                p.g("tensor_tensor", r=kg + " ohB", w="ohB", out=ohB, in0=ohB, in1=IGTg[par][:, ts, tq].unsqueeze(2).to_broadcast([128, NG, 64]), op=ALU.mult)
                for t8 in range(NG // 8):
                    bk = 2 + t8 % 2
                    for q8 in range(8):
                        tt_ = t8 * 8 + q8
                        p.t("matmul", r="ohA ohB", w="b%d" % bk, out=PB(bk)[:, q8 * 64:(q8 + 1) * 64], lhsT=ohA[:, tt_, :], rhs=ohB[:, tt_, :],
                            start=True, stop=True)
                    g0 = ts * 128 + tg * NG + t8 * 8
                    if bk == 2:
                        p.s("copy", r="b2", w=gk, out=Gh[:, g0:g0 + 8, :], in_=PB(2).rearrange("p (a b) -> p a b", a=8))
                    else:
                        p.v("tensor_copy", r="b3", w=gk, out=Gh[:, g0:g0 + 8, :], in_=PB(3).rearrange("p (a b) -> p a b", a=8))
                yield

    widx = [0]

    def main_pair(blk, pr):
        par = blk % 3
        hT = h2T[par]
        sl = widx[0] % 3
        widx[0] += 1
        p.dma(wu[sl], utb_s[2 * pr:2 * pr + 2].rearrange("a p c -> p a c"), r="utb_s", w="wu%d" % sl)
        p.dma(wv[sl], vtb_s[2 * pr:2 * pr + 2].rearrange("a p c -> p a c"), r="vtb_s", w="wv%d" % sl)
        for e2 in range(2):
            i2 = 2 * pr + e2
            half = i2 // 64
            ab = i2 % 2
            for kc in range(8):
                p.t("matmul", r="wu%d h2T%d" % (sl, par), w="b%d" % ab, out=PB(ab)[:, 0:TB], lhsT=wu[sl][:, e2, kc * 128:(kc + 1) * 128], rhs=hT[:, kc, :],
                    start=(kc == 0), stop=(kc == 7))
            p.s("activation", r="b%d" % ab, w="ga%d" % ab, out=ga[ab], in_=PB(ab)[:, 0:TB], func=AF.Gelu)
            p.v("tensor_tensor", r="ga%d Gm%d" % (ab, half), w="gab%d" % ab, out=gab[ab], in0=ga[ab], in1=GmH[half][:, :, i2 % 64], op=ALU.mult)
            for ts in range(2):
                for nb in range(2):
                    bk = 4 + 2 * ts + nb
                    p.t("matmul", r="gab%d wv%d" % (ab, sl), w="b%d" % bk, out=PB(bk), lhsT=gab[ab][:, ts * 128:(ts + 1) * 128],
                        rhs=wv[sl][:, e2, nb * 512:(nb + 1) * 512], start=(i2 == 0), stop=(i2 == 127))

    def finalize(blk):
        for ts in range(2):
            t0 = blk * TB + ts * 128
            xk = "xs1%d" % ts
            p.dma(xs1[ts], x1_s[t0:t0 + 128, :], r="x1_s", w=xk)
            for nb in range(2):
                bk = 4 + 2 * ts + nb
                ns = slice(nb * 512, (nb + 1) * 512)
                p.v("tensor_tensor", r="b%d G2" % bk, w="tmpF", out=tmpF, in0=PB(bk), in1=G2[:, ns], op=ALU.mult)
                p.g("tensor_tensor", r="tmpF " + xk, w=xk, out=xs1[ts][:, ns], in0=xs1[ts][:, ns], in1=tmpF, op=ALU.add)
            p.s("activation", r=xk, w="tmpF ssF", out=junkF, in_=xs1[ts], func=AF.Square, accum_out=ssF[:, 1:2])
            p.v("tensor_scalar", r="ssF", w="rsF", out=rsF[:, 1:2], in0=ssF[:, 1:2], scalar1=1.0 / 1024, scalar2=EPS, op0=ALU.mult, op1=ALU.add)
            p.s("activation", r="rsF", w="rsF", out=rsF[:, 1:2], in_=rsF[:, 1:2], func=AF.Ln)
            p.s("activation", r="rsF", w="rsF", out=rsF[:, 1:2], in_=rsF[:, 1:2], func=AF.Exp, scale=-0.5)
            p.v("scalar_tensor_tensor", r=xk + " rsF FW", w=xk, out=xs1[ts], in0=xs1[ts], scalar=rsF[:, 1:2], in1=FW, op0=ALU.mult, op1=ALU.mult)
            p.dma(out_d[t0:t0 + 128, :], xs1[ts], r=xk, w="out", grp="outw%d" % ts)

    nblk = 1 if stop == "F1" else (3 if stop == "F2" else NBLK)

    def drain(g):
        if g is not None:
            for _ in g:
                pass

    def drip(g, n):
        if g is None:
            return None
        try:
            for _ in range(n):
                next(g)
        except StopIteration:
            return None
        return g

    drain(part1(0))
    if nblk > 1:
        drain(part1(1))
    drain(part2(0, 0))
    for blk in range(nblk):
        g1 = part1(blk + 2) if blk + 2 < nblk else None
        gB = part2(blk, 1)
        gA = None
        for pr in range(64):
            main_pair(blk, pr)
            g1 = drip(g1, 3)
            if pr < 32:
                gB = drip(gB, 1)
                if pr == 31:
                    drain(gB)
                    gB = None
                    gA = part2(blk + 1, 0) if blk + 1 < nblk else None
            else:
                gA = drip(gA, 1)
        drain(g1)
        drain(gA)
        finalize(blk)
    return fin(nc, p, ar, dbg_d)


def fin(nc, p, ar, dbg_d):
    p.finish()
    p.emit()
    return nc, dbg_d


def host_layout(inp, b):
    f = np.float32
    A = np.ascontiguousarray
    d = {}
    d["x"] = A(inp["x"][b])
    d["ctx"] = A(inp["ctx"][b])
    cc = np.stack([inp["c"][b].reshape(8, 128).T, inp["c_ctx"].reshape(8, 128).T], axis=-1)
    d["cc"] = A(cc.astype(f))
    return d


def shared_layout(inp):
    A = np.ascontiguousarray
    d = {}
    d["ada_w"] = A(inp["ada_w"][0].reshape(8, 128, 6144).transpose(1, 0, 2))
    d["ada_b"] = A(inp["ada_b"][0].reshape(1, 6144))
    d["nrm"] = A(np.stack([np.broadcast_to(inp[k].reshape(1, 1024), (128, 1024)) for k in ("norm1_w", "norm2_w", "final_norm_w")]))
    d["w_in"] = A(inp["w_in"][0].reshape(8, 128, 2576).transpose(1, 0, 2))
    d["scw"] = A(inp["ssm_conv_w"][0].reshape(5, 8, 128).transpose(2, 1, 0))
    d["scb"] = A(inp["ssm_conv_b"][0].reshape(8, 128).T)
    d["dtb"] = A(np.broadcast_to(inp["ssm_dt_bias"][0].reshape(1, 16), (128, 16)))
    d["alog"] = A(np.broadcast_to(inp["ssm_a_log"][0].reshape(1, 16), (128, 16)))
    d["drow"] = A(np.broadcast_to(np.repeat(inp["ssm_d"][0], 64).reshape(1, 512), (128, 512)))
    d["snw"] = A(np.broadcast_to(inp["ssm_norm_w"][0].reshape(1, 512), (128, 512)))
    d["ccw"] = A(inp["cfm_conv_w"][0].reshape(31, 4, 128).transpose(2, 1, 0))
    d["cvec"] = A(np.stack([inp[k][0].reshape(4, 128).T for k in ("cfm_conv_b", "cfm_ln_w", "cfm_ln_b")], axis=1))
    d["w_out"] = A(inp["w_out"][0].reshape(8, 128, 1024).transpose(1, 0, 2))
    d["wq"] = A(inp["peer_wq"][0].reshape(8, 128, 2048).transpose(1, 0, 2))
    d["kt"] = A(inp["peer_subkeys"][0].reshape(16, 128, 128).transpose(2, 0, 1))
    d["ut"] = A(inp["peer_u"][0].reshape(128, 128, 8, 128).transpose(1, 3, 2, 0))
    d["vt"] = A(inp["peer_v"][0].reshape(128, 128, 1024).transpose(1, 0, 2))
    return d


_CACHE = {}


def kernel(**inputs):
    inp = {k: np.asarray(v) for k, v in inputs.items()}
    if "nc" not in _CACHE:
        _CACHE["nc"] = build()[0]
    nc = _CACHE["nc"]
    sh = shared_layout(inp)
    in_maps = []
    for b in range(8):
        d = dict(sh)
        d.update(host_layout(inp, b))
        in_maps.append(d)
    res = run_bass_kernel_spmd(nc, in_maps, core_ids=list(range(8)))
    return np.stack([np.asarray(r["out"], dtype=np.float32) for r in res.results], axis=0)
```

```python
import re
import numpy as np
import concourse.bass as bass
import concourse.mybir as mybir
from concourse.bass_utils import run_bass_kernel_spmd

F32 = mybir.dt.float32
BF16 = mybir.dt.bfloat16
I32 = mybir.dt.int32
U32 = mybir.dt.uint32
AF = mybir.ActivationFunctionType
ALU = mybir.AluOpType
AX = mybir.AxisListType
ENGS = ("sync", "scalar", "vector", "gpsimd", "tensor")
CH = 4096
EPS = 1e-6
DSZ = {F32: 4, BF16: 2, I32: 4, U32: 4}


class Prog:
    def __init__(self, nc):
        self.nc = nc
        self.q = {e: [] for e in ENGS}
        self.cnt = {e: 0 for e in ENGS}
        self.known = {e: {} for e in ENGS}
        self.lastw = {}
        self.readers = {}
        self.sems = {}
        self.dmacum = {}
        self.pend = {e: {} for e in ENGS}
        self.lasttok = {}

    def sem(self, key):
        s = self.sems.get(key)
        if s is None:
            s = self.nc.alloc_semaphore(name="s%d" % len(self.sems))
            self.sems[key] = s
        return s

    def _waits(self, e, reads, writes):
        toks = dict(self.pend[e])
        self.pend[e] = {}

        def add(t):
            if t is None:
                return
            k, v = t
            if k[0] == "eng" and k[1] == e and e == "tensor":
                return
            if toks.get(k, 0) < v:
                toks[k] = v
        for r in reads:
            add(self.lastw.get(r))
        for w in writes:
            add(self.lastw.get(w))
            for t in self.readers.get(w, ()):
                add(t)
        out = []
        for k, v in toks.items():
            if self.known[e].get(k, 0) >= v:
                continue
            self.known[e][k] = v
            out.append((self.sem(k), v))
        return out

    def _commit(self, tok, reads, writes):
        self.lasttok[tok[0]] = tok[1]
        for w in writes:
            self.lastw[w] = tok
            self.readers[w] = []
        for r in reads:
            self.readers.setdefault(r, []).append(tok)

    def op(self, e, name, r="", w="", **kw):
        reads, writes = r.split(), w.split()
        ex = [k for k in reads if re.fullmatch(r"(ps|b)\d", k)]
        reads = [k for k in reads if k not in ex]
        writes = writes + [k for k in ex if k not in writes]
        waits = self._waits(e, reads, writes)
        idx = self.cnt[e]
        self.cnt[e] += 1
        k = ("eng", e, idx // CH)
        self.q[e].append((waits, name, kw, self.sem(k), 1))
        self._commit((k, idx % CH + 1), reads, writes)

    def v(self, name, r="", w="", **kw):
        self.op("vector", name, r, w, **kw)

    def s(self, name, r="", w="", **kw):
        self.op("scalar", name, r, w, **kw)

    def g(self, name, r="", w="", **kw):
        self.op("gpsimd", name, r, w, **kw)

    def t(self, name, r="", w="", **kw):
        self.op("tensor", name, r, w, **kw)

    def dma(self, out, in_, r="", w="", e="sync", grp=None, parts=None, **kw):
        reads, writes = r.split(), w.split()
        sk = ("dma", grp if grp is not None else writes[0])
        waits = self._waits(e, reads, writes)
        pieces = parts if parts is not None else [(out, in_)]
        for i, (o, a) in enumerate(pieces):
            self.dmacum[sk] = self.dmacum.get(sk, 0) + 16
            kk = dict(kw)
            kk.update(out=o, in_=a)
            self.q[e].append((waits if i == 0 else [], "dma_start", kk, self.sem(sk), 16))
        self._commit((sk, self.dmacum[sk]), reads, writes)

    def barrier(self):
        for e in ENGS:
            for k, v in self.lasttok.items():
                if self.pend[e].get(k, 0) < v:
                    self.pend[e][k] = v

    def finish(self):
        self.barrier()
        for e in ("sync", "gpsimd"):
            waits = self._waits(e, [], [])
            self.q[e].append((waits, None, None, None, 0))

    def emit(self):
        nc = self.nc
        with nc.Block() as block:
            def mk(e):
                def body(eng):
                    for waits, name, kw, s, inc in self.q[e]:
                        for ws, wv in waits:
                            eng.wait_ge(ws, wv)
                        if name is not None:
                            getattr(eng, name)(**kw).then_inc(s, inc)
                return body
            block.sync(mk("sync"))
            block.scalar(mk("scalar"))
            block.vector(mk("vector"))
            block.gpsimd(mk("gpsimd"))
            block.tensor(mk("tensor"))


class Arena:
    def __init__(self, nc, nbytes):
        self.t = nc.alloc_sbuf_tensor("arena", [128, nbytes // 4], F32)
        self.cap = nbytes
        self.off = 0
        self.peak = 0

    def a(self, free, dt=F32, parts=128):
        if isinstance(free, int):
            free = (free,)
        n = int(np.prod(free)) * DSZ[dt]
        n = (n + 63) // 64 * 64
        assert self.off + n <= self.cap, ("SBUF arena overflow", self.off, n, self.cap)
        ap = self.t[0:parts, self.off // 4:(self.off + n) // 4]
        self.off += n
        self.peak = max(self.peak, self.off)
        if dt != F32:
            ap = ap.bitcast(dt)
        ap = ap[:, 0:int(np.prod(free))]
        if len(free) == 2:
            ap = ap.rearrange("p (a b) -> p a b", a=free[0])
        elif len(free) == 3:
            ap = ap.rearrange("p (a b c) -> p a b c", a=free[0], b=free[1])
        return ap

    def mark(self):
        return self.off

    def release(self, m):
        self.off = m


L = 4096
LC = 256
NT = L // 128


def build(stop=None, dbg=False, npeer=128):
    nc = bass.Bass("TRN2", target_bir_lowering=False)
    D = nc.dram_tensor

    def din(name, shape, dt=F32):
        return D(name, list(shape), dt, kind="ExternalInput").ap()
    x_d = din("x", [L, 1024])
    ctx_d = din("ctx", [LC, 1024])
    cc_d = din("cc", [128, 8, 2])
    adaw_d = din("ada_w", [128, 8, 6144])
    adab_d = din("ada_b", [1, 6144])
    nrm_d = din("nrm", [3, 128, 1024])
    win_d = din("w_in", [128, 8, 2576])
    scw_d = din("scw", [128, 8, 5])
    scb_d = din("scb", [128, 8])
    dtb_d = din("dtb", [128, 16])
    alog_d = din("alog", [128, 16])
    drow_d = din("drow", [128, 512])
    snw_d = din("snw", [128, 512])
    ccw_d = din("ccw", [128, 4, 31])
    cvec_d = din("cvec", [128, 3, 4])
    wout_d = din("w_out", [128, 8, 1024])
    wq_d = din("wq", [128, 8, 2048])
    kt_d = din("kt", [128, 16, 128])
    ut_d = din("ut", [npeer, 128, 8, 128])
    vt_d = din("vt", [npeer, 128, 1024])
    out_d = D("out", [L, 1024], F32, kind="ExternalOutput").ap()
    dbg_d = {}

    def dout(name, shape):
        dbg_d[name] = D("dbg_" + name, list(shape), F32, kind="ExternalOutput").ap()
        return dbg_d[name]
    modb_s = D("modb_s", [8, 128, 1024], F32).ap()
    zs_s = D("zs_s", [L, 512], BF16).ap()
    x1_s = D("x1_s", [L, 1024], F32).ap()
    utb_s = D("utb_s", [128, 128, 1024], BF16).ap()
    vtb_s = D("vtb_s", [128, 128, 1024], BF16).ap()
    uts_s = D("uts_s", [128, 4, L], BF16).ap()
    wqb_s = D("wqb_s", [16, 128, 1024], BF16).ap()

    p = Prog(nc)
    ar = Arena(nc, 212000)
    banks = [nc.alloc_psum_tensor("pb%d" % i, [128, 512], F32) for i in range(8)]

    def PB(i):
        return banks[i][:]

    def PBb(i):
        return banks[i][:].bitcast(BF16)

    ident_f = ar.a(128)
    ident = ar.a(128, BF16)
    iota_f = ar.a(128)
    pidx = ar.a(128)
    ones_f = ar.a(128)
    ones_b = ar.a(128, BF16)
    triU = ar.a(128)
    triS = ar.a(128)
    maskF = ar.a(128, BF16)
    maskB = ar.a(128, BF16)
    p.g("iota", w="iota", out=iota_f, pattern=[[1, 128]], base=0, channel_multiplier=0, allow_small_or_imprecise_dtypes=True)
    p.g("iota", w="pidx", out=pidx, pattern=[[0, 128]], base=0, channel_multiplier=1, allow_small_or_imprecise_dtypes=True)
    p.v("tensor_tensor", r="iota pidx", w="identf", out=ident_f, in0=iota_f, in1=pidx, op=ALU.is_equal)
    p.v("tensor_copy", r="identf", w="ident", out=ident, in_=ident_f)
    p.v("memset", w="ones_f", ap=ones_f, constant=1.0)
    p.v("memset", w="ones_b", ap=ones_b, constant=1.0)
    p.v("tensor_tensor", r="iota pidx", w="triU", out=triU, in0=iota_f, in1=pidx, op=ALU.is_ge)
    p.v("tensor_tensor", r="iota pidx", w="triS", out=triS, in0=iota_f, in1=pidx, op=ALU.is_gt)
    p.v("tensor_scalar", r="triU", w="maskF", out=maskF, in0=triU, scalar1=-1.0, scalar2=30000.0, op0=ALU.add, op1=ALU.mult)
    tmpc = ar.a(128)
    p.v("tensor_tensor", r="iota pidx", w="tmpc", out=tmpc, in0=iota_f, in1=pidx, op=ALU.is_le)
    p.v("tensor_scalar", r="tmpc", w="maskB", out=maskB, in0=tmpc, scalar1=-1.0, scalar2=30000.0, op0=ALU.add, op1=ALU.mult)
    sel = ar.a((16, 128), BF16)
    p.v("memset", w="sel", ap=sel, constant=0.0)
    p.v("tensor_copy", r="identf sel", w="sel", out=sel[0:16], in_=ident_f[0:16, 0:16].unsqueeze(2).to_broadcast([16, 16, 128]))
    scw = ar.a((8, 5)); scb = ar.a(8); dtbias = ar.a(16); alog = ar.a(16); ccw = ar.a((4, 31)); cvec = ar.a((3, 4))
    Aneg = ar.a(16)
    p.dma(scw, scw_d, w="scw", grp="small")
    p.dma(scb, scb_d, w="scb", grp="small")
    p.dma(dtbias, dtb_d, w="dtbias", grp="small")
    p.dma(alog, alog_d, w="alog", grp="small")
    p.dma(ccw, ccw_d, w="ccw", grp="small")
    p.dma(cvec, cvec_d, w="cvec", grp="small")
    p.s("activation", r="alog", w="Aneg", out=Aneg, in_=alog, func=AF.Exp)
    p.v("tensor_scalar", r="Aneg", w="Aneg", out=Aneg, in0=Aneg, scalar1=-1.0, scalar2=None, op0=ALU.mult)
    dt_all = ar.a((NT + 2, 16))
    m_persist = ar.mark()

    m0 = ar.mark()
    cc = ar.a((8, 2)); scT = ar.a((8, 2))
    adab = ar.a(6144, parts=1)
    modrow = ar.a(6144, parts=1)
    modrow_c = ar.a(2048, parts=1)
    awb = [ar.a((8, 512)) for _ in range(2)]
    nrm = [ar.a(1024) for _ in range(2)]
    bct = [ar.a(1024) for _ in range(2)]
    p.dma(cc, cc_d, w="cc")
    p.dma(adab, adab_d, w="adab")
    p.dma(nrm[0], nrm_d[0], w="nrm0")
    p.dma(nrm[1], nrm_d[1], w="nrm1")
    p.s("activation", r="cc", w="scT", out=scT, in_=cc, func=AF.Silu)
    for blk in range(12):
        sl = blk % 2
        p.dma(awb[sl], adaw_d[:, :, blk * 512:(blk + 1) * 512], w="awb%d" % sl)
        for kc in range(8):
            p.t("matmul", r="scT awb%d" % sl, w="ps0", out=PB(0)[0:1, :], lhsT=scT[:, kc, 0:1], rhs=awb[sl][:, kc, :],
                start=(kc == 0), stop=(kc == 7))
        if blk < 4:
            for kc in range(8):
                p.t("matmul", r="scT awb%d" % sl, w="ps1", out=PB(1)[0:1, :], lhsT=scT[:, kc, 1:2], rhs=awb[sl][:, kc, :],
                    start=(kc == 0), stop=(kc == 7))
            p.v("tensor_tensor", r="ps1 adab", w="modrow_c", out=modrow_c[:, blk * 512:(blk + 1) * 512], in0=PB(1)[0:1, :],
                in1=adab[:, blk * 512:(blk + 1) * 512], op=ALU.add)
        p.v("tensor_tensor", r="ps0 adab", w="modrow", out=modrow[:, blk * 512:(blk + 1) * 512], in0=PB(0)[0:1, :],
            in1=adab[:, blk * 512:(blk + 1) * 512], op=ALU.add)
    plan = [(0, modrow, 1, 0), (1, modrow, 0, None), (2, modrow_c, 1, 0), (3, modrow_c, 0, None), (4, modrow, 2, None),
            (5, modrow, 4, 1), (6, modrow, 3, None), (7, modrow, 5, None)]
    for i, (ti, src, mi, ni) in enumerate(plan):
        bt = bct[i % 2]
        bk = "bct%d" % (i % 2)
        for hf in range(2):
            p.t("matmul", r="ones_f modrow modrow_c", w="ps%d" % (2 + hf), out=PB(2 + hf), lhsT=ones_f[0:1, :],
                rhs=src[:, mi * 1024 + hf * 512: mi * 1024 + (hf + 1) * 512], start=True, stop=True)
            if ni is None:
                p.v("tensor_copy", r="ps%d" % (2 + hf), w=bk, out=bt[:, hf * 512:(hf + 1) * 512], in_=PB(2 + hf))
            else:
                p.v("scalar_tensor_tensor", r="ps%d nrm%d" % (2 + hf, ni), w=bk, out=bt[:, hf * 512:(hf + 1) * 512], in0=PB(2 + hf),
                    scalar=1.0, in1=nrm[ni][:, hf * 512:(hf + 1) * 512], op0=ALU.add, op1=ALU.mult)
        p.dma(modb_s[ti], bt, r=bk, w="modb_s", grp="modb_w")
    if dbg:
        p.dma(dout("modrow", [1, 6144]), modrow, r="modrow", w="dbg_modrow")
    ar.release(m0)
    p.barrier()
    if stop == "0":
        return fin(nc, p, ar, dbg_d)

    m_big = ar.mark()
    xbcT = ar.a((8, L), BF16)
    xbcTc = ar.a((8, LC), BF16)
    m_uT = ar.mark()
    uT = ar.a((4, L), BF16)

    mA = ar.mark()
    Wb = ar.a((8, 2576), BF16)
    wst = [ar.a((8, 128)) for _ in range(2)]
    W1 = ar.a(1024); S1 = ar.a(1024)

    def load_mod(i0):
        p.dma(W1, modb_s[i0], r="modb_s", w="modA0", grp="modA")
        p.dma(S1, modb_s[i0 + 1], r="modb_s", w="modA1", grp="modA")
    ncol = 2576
    pieces = [(c0, min(128, ncol - c0)) for c0 in range(0, ncol, 128)]
    for i, (c0, cw) in enumerate(pieces):
        sl = i % 2
        p.dma(wst[sl][:, :, 0:cw], win_d[:, :, c0:c0 + cw], w="wst%d" % sl)
        if i % 2 == 0:
            p.v("tensor_copy", r="wst%d" % sl, w="Wb", out=Wb[:, :, c0:c0 + cw], in_=wst[sl][:, :, 0:cw])
        else:
            p.s("copy", r="wst%d" % sl, w="Wb", out=Wb[:, :, c0:c0 + cw], in_=wst[sl][:, :, 0:cw])
    xt = [ar.a(1024) for _ in range(2)]
    junk = ar.a(1024, BF16)
    hb = [ar.a(1024, BF16) for _ in range(2)]
    hT = ar.a((8, 512), BF16)
    ss = ar.a(2); rs = ar.a(2)
    sig = ar.a(512)
    zsb = [ar.a(512, BF16) for _ in range(2)]
    dtt = ar.a(16)

    def inproj_block(src_d, t0, ntile, Wm, Sm, is_ctx, tile0):
        ntok = ntile * 128
        for ts in range(ntile):
            sl = ts % 2
            p.dma(xt[sl], src_d[t0 + ts * 128:t0 + (ts + 1) * 128, :], w="xt%d" % sl)
            p.s("activation", r="xt%d" % sl, w="junk ss", out=junk, in_=xt[sl], func=AF.Square, accum_out=ss[:, 0:1])
            p.v("tensor_scalar", r="ss", w="rs", out=rs[:, 0:1], in0=ss[:, 0:1], scalar1=1.0 / 1024, scalar2=EPS, op0=ALU.mult, op1=ALU.add)
            p.s("activation", r="rs", w="rs", out=rs[:, 0:1], in_=rs[:, 0:1], func=AF.Ln)
            p.s("activation", r="rs", w="rs", out=rs[:, 0:1], in_=rs[:, 0:1], func=AF.Exp, scale=-0.5)
            p.v("scalar_tensor_tensor", r="xt%d rs modA0" % sl, w="xt%d" % sl, out=xt[sl], in0=xt[sl], scalar=rs[:, 0:1], in1=Wm,
                op0=ALU.mult, op1=ALU.mult)
            p.g("tensor_tensor", r="xt%d modA1" % sl, w="hb%d" % sl, out=hb[sl], in0=xt[sl], in1=Sm, op=ALU.add)
            for kc in range(8):
                p.t("transpose", r="hb%d ident" % sl, w="ps%d" % (4 + sl), out=PBb(4 + sl)[:, kc * 128:(kc + 1) * 128],
                    in_=hb[sl][:, kc * 128:(kc + 1) * 128], identity=ident)
            p.s("copy", r="ps%d" % (4 + sl), w="hT", out=hT[:, :, ts * 128:(ts + 1) * 128],
                in_=PBb(4 + sl).rearrange("p (a b) -> p a b", a=8))
        for j in range(8):
            pk = j % 2
            c0 = 512 + j * 128
            for kc in range(8):
                p.t("matmul", r="Wb hT", w="ps%d" % pk, out=PB(pk)[:, 0:ntok], lhsT=Wb[:, kc, c0:c0 + 128], rhs=hT[:, kc, 0:ntok],
                    start=(kc == 0), stop=(kc == 7))
            dst = xbcTc[:, j, 0:ntok] if is_ctx else xbcT[:, j, t0:t0 + ntok]
            if j % 2 == 0:
                p.s("copy", r="ps%d" % pk, w="xbcT", out=dst, in_=PB(pk)[:, 0:ntok])
            else:
                p.v("tensor_copy", r="ps%d" % pk, w="xbcT", out=dst, in_=PB(pk)[:, 0:ntok])
        if not is_ctx:
            for j in range(4):
                ca = 1552 + j * 128
                cb = 1552 + 512 + j * 128
                for kc in range(8):
                    p.t("matmul", r="Wb hT", w="ps2", out=PB(2)[:, 0:ntok], lhsT=Wb[:, kc, ca:ca + 128], rhs=hT[:, kc, 0:ntok],
                        start=(kc == 0), stop=(kc == 7))
                for kc in range(8):
                    p.t("matmul", r="Wb hT", w="ps3", out=PB(3)[:, 0:ntok], lhsT=Wb[:, kc, cb:cb + 128], rhs=hT[:, kc, 0:ntok],
                        start=(kc == 0), stop=(kc == 7))
                p.s("activation", r="ps3", w="sig", out=sig[:, 0:ntok], in_=PB(3)[:, 0:ntok], func=AF.Sigmoid)
                p.v("tensor_tensor", r="ps2 sig", w="uT", out=uT[:, j, t0:t0 + ntok], in0=PB(2)[:, 0:ntok], in1=sig[:, 0:ntok], op=ALU.mult)
        for ts in range(ntile):
            sl = ts % 2
            if not is_ctx:
                for kc in range(8):
                    p.t("matmul", r="Wb hT", w="ps%d" % (6 + sl), out=PB(6 + sl), lhsT=hT[:, kc, ts * 128:(ts + 1) * 128], rhs=Wb[:, kc, 0:512],
                        start=(kc == 0), stop=(kc == 7))
                p.s("activation", r="ps%d" % (6 + sl), w="zsb%d" % sl, out=zsb[sl], in_=PB(6 + sl), func=AF.Silu)
                p.dma(zs_s[t0 + ts * 128:t0 + (ts + 1) * 128, :], zsb[sl], r="zsb%d" % sl, w="zs_s", e="sync", grp="zsw%d" % sl)
            for kc in range(8):
                p.t("matmul", r="Wb hT", w="ps0", out=PB(0)[:, 0:16], lhsT=hT[:, kc, ts * 128:(ts + 1) * 128], rhs=Wb[:, kc, 1536:1552],
                    start=(kc == 0), stop=(kc == 7))
            p.v("tensor_tensor", r="ps0 dtbias", w="dtt", out=dtt, in0=PB(0)[:, 0:16], in1=dtbias, op=ALU.add)
            p.s("activation", r="dtt", w="dtt", out=dtt, in_=dtt, func=AF.Exp)
            p.s("activation", r="dtt", w="dt_all", out=dt_all[:, tile0 + ts, :], in_=dtt, func=AF.Ln, bias=1.0)

    load_mod(2)
    inproj_block(ctx_d, 0, 2, W1, S1, True, NT)
    if stop == "A1":
        p.dma(dout("dt_all", [128, NT + 2, 16]), dt_all, r="dt_all", w="dbg_dt")
        return fin(nc, p, ar, dbg_d)
    load_mod(0)
    for b in range(L // 512):
        inproj_block(x_d, b * 512, 4, W1, S1, False, b * 4)
    if dbg:
        p.dma(dout("dt_all", [128, NT + 2, 16]), dt_all, r="dt_all", w="dbg_dt")
        dxb = dout("xbc_pre", [128, 8, 128])
        tdb = ar.a((8, 128))
        p.v("tensor_copy", r="xbcT", w="tdb", out=tdb, in_=xbcT[:, :, 512:640])
        p.dma(dxb, tdb, r="tdb", w="dbg_xb")
        dub = dout("u_pre", [128, 4, 128])
        tdb2 = ar.a((4, 128))
        p.v("tensor_copy", r="uT", w="tdb2", out=tdb2, in_=uT[:, :, 512:640])
        p.dma(dub, tdb2, r="tdb2", w="dbg_ub")
    ar.release(mA)
    p.barrier()
    if stop == "A":
        return fin(nc, p, ar, dbg_d)

    mB = ar.mark()
    acc = [ar.a(L) for _ in range(2)]
    EN = [p.v, p.v]

    def ssm_conv(src, n, ei, j, key):
        E = EN[ei]
        ak = "acc%d" % ei
        a_ = acc[ei][:, 0:n]
        x_ = src[:, j, 0:n]
        E("tensor_scalar", r=key + " scw", w=ak, out=a_, in0=x_, scalar1=scw[:, j, 2:3], scalar2=None, op0=ALU.mult)
        for k in (0, 1, 3, 4):
            o = k - 2
            lo, hi = max(0, -o), min(n, n - o)
            E("scalar_tensor_tensor", r=key + " scw " + ak, w=ak, out=a_[:, lo:hi], in0=x_[:, lo + o:hi + o], scalar=scw[:, j, k:k + 1],
              in1=a_[:, lo:hi], op0=ALU.mult, op1=ALU.add)
        p.s("activation", r=ak + " scb", w=key, out=x_, in_=a_, func=AF.Silu, bias=scb[:, j:j + 1])

    tmpc2 = [ar.a(L) for _ in range(2)]

    def cfm_conv(j, ei):
        ak = "acc%d" % ei
        key = "uT%d" % j
        a_ = acc[ei]
        u_ = uT[:, j, :]
        a3 = a_.rearrange("p (r c) -> p r c", c=64)
        u3 = u_.rearrange("p (r c) -> p r c", c=64)
        if ei == 0:
            p.v("tensor_scalar", r=key + " ccw", w=ak, out=a_, in0=u_, scalar1=ccw[:, j, 15:16], scalar2=None, op0=ALU.mult)
        else:
            p.s("activation", r=key + " ccw", w=ak, out=a_, in_=u_, func=AF.Copy, scale=ccw[:, j, 15:16])
        n = 0
        for k in range(31):
            o = k - 15
            if o == 0:
                continue
            lo, hi = max(0, -o), min(64, 64 - o)
            if j < 2:
                oo, i0 = a3[:, :, lo:hi], u3[:, :, lo + o:hi + o]
            else:
                oo, i0 = a_[:, lo * 64:hi * 64], u_[:, (lo + o) * 64:(hi + o) * 64]
            if ei == 0:
                p.v("scalar_tensor_tensor", r=key + " ccw " + ak, w=ak, out=oo, in0=i0, scalar=ccw[:, j, k:k + 1], in1=oo, op0=ALU.mult, op1=ALU.add)
            else:
                tk = "tmpc2%d" % (n % 2)
                tt = tmpc2[n % 2]
                if j < 2:
                    t_ = tt.rearrange("p (r c) -> p r c", c=64)[:, :, lo:hi]
                else:
                    t_ = tt[:, lo * 64:hi * 64]
                p.s("activation", r=key + " ccw", w=tk, out=t_, in_=i0, func=AF.Copy, scale=ccw[:, j, k:k + 1])
                p.g("tensor_tensor", r=tk + " " + ak, w=ak, out=oo, in0=oo, in1=t_, op=ALU.add)
                n += 1
        p.s("activation", r=ak + " cvec", w=key, out=u_, in_=a_, func=AF.Identity, bias=cvec[:, 0, j:j + 1])

    for j in range(8):
        ssm_conv(xbcTc, LC, 0, j, "xbcTc%d" % j)
    cfm_conv(2, 1)
    for j in range(8):
        ssm_conv(xbcT, L, 0, j, "xbcT%d" % j)
    cfm_conv(3, 1)
    cfm_conv(0, 0)
    cfm_conv(1, 0)
    if dbg:
        tdb = ar.a((8, 128))
        p.v("tensor_copy", r=" ".join("xbcT%d" % j for j in range(8)), w="tdb", out=tdb, in_=xbcT[:, :, 512:640])
        p.dma(dout("xbc_post", [128, 8, 128]), tdb, r="tdb", w="dbg_xb2")
        tdb2 = ar.a((4, 128))
        p.v("tensor_copy", r=" ".join("uT%d" % j for j in range(4)), w="tdb2", out=tdb2, in_=uT[:, :, 512:640])
        p.dma(dout("u_post", [128, 4, 128]), tdb2, r="tdb2", w="dbg_ub2")
    for j in range(4):
        p.dma(uts_s[:, j, :], uT[:, j, :], r="uT%d" % j, w="uts_s", grp="utsw")
    ar.release(m_uT)
    p.barrier()
    if stop == "B":
        return fin(nc, p, ar, dbg_d)

    mE = ar.mark()
    SB_all = ar.a((NT, 512), BF16)
    Wo = ar.a((8, 1024), BF16)
    G1 = ar.a(1024); NW = ar.a(512); Drow = ar.a(512)
    p.dma(G1, modb_s[4], r="modb_s", w="G1")
    p.dma(NW, snw_d, w="NW")
    p.dma(Drow, drow_d, w="Drow")
    a16 = ar.a(16); cs = ar.a(32); dec = ar.a(16); tmp16 = ar.a(16); w16 = ar.a(16)
    rowsrc = ar.a(16); bias16 = ar.a(16); e16 = ar.a(16); RT = ar.a(128); RT3 = ar.a((3, 128), BF16); RTr = ar.a(128)
    xs_tok = ar.a(512, BF16); Btok = ar.a(256, BF16)
    xw = [ar.a(512, BF16) for _ in range(2)]
    xdt = [ar.a(512, BF16) for _ in range(2)]
    ST_sb = ar.a((2, 128))
    LT = [ar.a(128) for _ in range(2)]
    SmT = ar.a((16, 128), BF16)
    Sst = [ar.a(512) for _ in range(2)]
    Sbf = [ar.a(512, BF16) for _ in range(2)]
    yc = ar.a(512); yc2 = ar.a(512); zst = ar.a(512, BF16); gg = ar.a(512)
    ss2 = ar.a(2); rs2 = ar.a(2)
    ssm_tok = ar.a(512, BF16); catT = ar.a((8, 128), BF16)
    vsq = ar.a((4, 128)); vsqh = ar.a((4, 128), BF16); vsql = ar.a((4, 128), BF16); mean = ar.a(128); msq = ar.a(128); var = ar.a(128); ntmp = ar.a((4, 128))
    xe = [ar.a(1024) for _ in range(2)]
    for i in range(8):
        sl = i % 2
        wv_ = xe[sl].rearrange("p (a b) -> p a b", a=8)
        p.dma(wv_, wout_d[:, :, i * 128:(i + 1) * 128], w="xe%d" % sl)
        if i % 2 == 0:
            p.v("tensor_copy", r="xe%d" % sl, w="Wo", out=Wo[:, :, i * 128:(i + 1) * 128], in_=wv_)
        else:
            p.s("copy", r="xe%d" % sl, w="Wo", out=Wo[:, :, i * 128:(i + 1) * 128], in_=wv_)
    junk2 = ar.a(256, BF16)
    uTt = [ar.a((4, 128), BF16) for _ in range(2)]
    PT = PBb(0)
    PC = PB(1)
    PK = PB(2)
    PY = PB(3)
    PLS = PB(6)
    PM = PBb(7)
    PM2 = PB(7)

    def bc8(ap8):
        return ap8.unsqueeze(2).to_broadcast([128, 8, 64])

    def v3(ap512):
        return ap512.rearrange("p (h q) -> p h q", h=8)

    def prep(is_ctx, c, dirs):
        src = xbcTc if is_ctx else xbcT
        kx = "xbcTc%d" if is_ctx else "xbcT%d"
        tl = (NT + c) if is_ctx else c
        tok = slice(c * 128, (c + 1) * 128)
        p.v("tensor_tensor", r="dt_all Aneg", w="a16", out=a16, in0=dt_all[:, tl, :], in1=Aneg, op=ALU.mult)
        p.t("matmul", r="triU a16", w="b1", out=PC[:, 0:8], lhsT=triU, rhs=a16[:, 0:8], start=True, stop=True)
        p.t("matmul", r="triS a16", w="b1", out=PC[:, 8:16], lhsT=triS, rhs=a16[:, 8:16], start=True, stop=True)
        p.t("matmul", r="ones_f a16", w="b1", out=PC[:, 16:32], lhsT=ones_f, rhs=a16, start=True, stop=True)
        p.s("copy", r="b1", w="cs", out=cs, in_=PC[:, 0:32])
        p.s("activation", r="cs", w="dec", out=dec, in_=cs[:, 16:32], func=AF.Exp)
        p.v("tensor_tensor", r="cs", w="tmp16", out=tmp16[:, 0:8], in0=cs[:, 16:24], in1=cs[:, 0:8], op=ALU.subtract)
        p.v("tensor_copy", r="cs tmp16", w="tmp16", out=tmp16[:, 8:16], in_=cs[:, 8:16])
        p.s("activation", r="tmp16", w="tmp16", out=tmp16, in_=tmp16, func=AF.Exp)
        p.v("tensor_tensor", r="tmp16 dt_all", w="w16", out=w16, in0=tmp16, in1=dt_all[:, tl, :], op=ALU.mult)
        for j in range(4):
            p.t("transpose", r=(kx % j) + " ident", w="b0", out=PT[:, j * 128:(j + 1) * 128], in_=src[:, j, tok], identity=ident)
        for g in range(2):
            p.t("transpose", r=(kx % (4 + g)) + " ident", w="b0", out=PT[:, 512 + g * 128:512 + (g + 1) * 128], in_=src[:, 4 + g, tok], identity=ident)
        p.v("tensor_copy", r="b0", w="Btok", out=Btok, in_=PT[:, 512:768])
        for d in dirs:
            p.v("tensor_tensor", r="b0 w16", w="xw%d" % d, out=v3(xw[d]), in0=v3(PT[:, 0:512]), in1=bc8(w16[:, d * 8:(d + 1) * 8]), op=ALU.mult)

    def local_states(d):
        for h in range(8):
            g = h // 4
            p.t("matmul", r="Btok xw%d" % d, w="b6", out=PLS[:, h * 64:(h + 1) * 64], lhsT=Btok[:, g * 128:(g + 1) * 128],
                rhs=xw[d][:, h * 64:(h + 1) * 64], start=True, stop=True)

    def upd(d):
        p.v("tensor_tensor", r="S%d dec" % d, w="S%d" % d, out=v3(Sst[d]), in0=v3(Sst[d]), in1=bc8(dec[:, d * 8:(d + 1) * 8]), op=ALU.mult)
        p.v("tensor_tensor", r="S%d b6" % d, w="S%d" % d, out=Sst[d], in0=Sst[d], in1=PLS, op=ALU.add)
        p.s("copy", r="S%d" % d, w="Sbf%d" % d, out=Sbf[d], in_=Sst[d])

    p.v("memset", w="RT3", ap=RT3, constant=0.0)
    for d in range(2):
        p.v("memset", w="S%d" % d, ap=Sst[d], constant=0.0)
        p.v("memset", w="Sbf%d" % d, ap=Sbf[d], constant=0.0)
    prep(True, 0, (0,)); local_states(0); upd(0)
    prep(True, 1, (0, 1)); local_states(0); upd(0); local_states(1); upd(1)
    prep(True, 0, (1,)); local_states(1); upd(1)
    if stop == "C":
        p.dma(dout("S_f", [128, 512]), Sst[0], r="S0", w="dbg_sf")
        p.dma(dout("S_b", [128, 512]), Sst[1], r="S1", w="dbg_sb")
        return fin(nc, p, ar, dbg_d)
    for c in range(NT - 1, -1, -1):
        p.s("copy", r="Sbf1", w="SB%d" % c, out=SB_all[:, c, :], in_=Sbf[1])
        prep(False, c, (1,)); local_states(1); upd(1)
        if npeer == 128:
            if c >= 16:
                hp_ = c - 16
                p.dma(wqb_s[hp_].rearrange("p (k c) -> p k c", k=8), wq_d[:, :, hp_ * 128:(hp_ + 1) * 128], w="wqb_s", e="gpsimd", grp="castq")
            for i2 in range(c * 4, c * 4 + 4):
                p.dma(utb_s[i2], ut_d[i2].rearrange("p k i -> p (k i)"), w="utb_s", e="gpsimd", grp="castu")
                p.dma(vtb_s[i2], vt_d[i2], w="vtb_s", e="gpsimd", grp="castv")
    if dbg:
        p.dma(dout("S_b0", [128, 512]), Sst[1], r="S1", w="dbg_sb0")
    if stop == "D":
        return fin(nc, p, ar, dbg_d)
    for c in range(NT):
        tok = slice(c * 128, (c + 1) * 128)
        pe = c % 2
        p.dma(xe[pe], x_d[tok, :], w="xe%d" % pe)
        p.dma(zst, zs_s[tok, :], r="zs_s", w="zst")
        p.dma(uTt[pe], uts_s[:, :, tok], r="uts_s", w="uTt%d" % pe)
        uTc = uTt[pe]
        if stop == "E0":
            return fin(nc, p, ar, dbg_d)
        prep(False, c, (0,))
        if stop == "E1":
            return fin(nc, p, ar, dbg_d)
        p.v("tensor_copy", r="b0", w="xs_tok", out=xs_tok, in_=PT[:, 0:512])
        for d in range(2):
            p.v("tensor_tensor", r="b0 dt_all", w="xdt%d" % d, out=v3(xdt[d]), in0=v3(PT[:, 0:512]),
                in1=bc8(dt_all[:, c, d * 8:(d + 1) * 8]), op=ALU.mult)
        if stop == "E1a":
            return fin(nc, p, ar, dbg_d)
        for g in range(2):
            p.t("matmul", r="xbcT%d xbcT%d" % (4 + g, 6 + g), w="b1", out=PC[:, 256 + g * 128:256 + (g + 1) * 128],
                lhsT=xbcT[:, 4 + g, tok], rhs=xbcT[:, 6 + g, tok], start=True, stop=True)
        if stop == "E1b":
            return fin(nc, p, ar, dbg_d)
        p.s("copy", r="b1", w="ST_sb", out=ST_sb, in_=PC[:, 256:512].rearrange("p (g l) -> p g l", g=2))
        if stop == "E2":
            return fin(nc, p, ar, dbg_d)

        p.v("tensor_copy", r="cs", w="rowsrc", out=rowsrc[:, 0:8], in_=cs[:, 0:8])
        p.v("tensor_scalar", r="cs rowsrc", w="rowsrc", out=rowsrc[:, 8:16], in0=cs[:, 8:16], scalar1=-1.0, scalar2=None, op0=ALU.mult)
        p.v("tensor_scalar", r="rowsrc", w="bias16", out=bias16, in0=rowsrc, scalar1=-1.0, scalar2=None, op0=ALU.mult)
        p.t("transpose", r="rowsrc identf", w="b1", out=PC[0:16, 32:160], in_=rowsrc, identity=ident_f)
        p.s("copy", r="b1", w="RT", out=RT[0:16, :], in_=PC[0:16, 32:160])
        p.v("tensor_copy", r="RT", w="RT3", out=RT3[0:16, 0, :], in_=RT[0:16, :])
        p.v("tensor_tensor", r="RT RT3", w="RTr", out=RTr[0:16, :], in0=RT[0:16, :], in1=RT3[0:16, 0, :], op=ALU.subtract)
        p.v("tensor_copy", r="RTr RT3", w="RT3", out=RT3[0:16, 1, :], in_=RTr[0:16, :])
        p.v("tensor_tensor", r="RTr RT3", w="RTr", out=RTr[0:16, :], in0=RTr[0:16, :], in1=RT3[0:16, 1, :], op=ALU.subtract)
        p.v("tensor_copy", r="RTr RT3", w="RT3", out=RT3[0:16, 2, :], in_=RTr[0:16, :])
        if stop == "E3":
            return fin(nc, p, ar, dbg_d)

        p.v("tensor_copy", r="cs", w="e16", out=e16[:, 0:8], in_=cs[:, 0:8])
        p.v("tensor_tensor", r="cs e16", w="e16", out=e16[:, 8:16], in0=cs[:, 24:32], in1=cs[:, 8:16], op=ALU.subtract)
        p.s("activation", r="e16", w="e16", out=e16, in_=e16, func=AF.Exp)
        if stop == "E4":
            return fin(nc, p, ar, dbg_d)

        for idx in range(16):
            d, h = idx // 8, idx % 8
            g = h // 4
            pk = "b2" if idx % 2 == 0 else "b6"
            PKs = (PB(2) if idx % 2 == 0 else PB(6))[:, 0:128]
            for q in range(3):
                p.t("matmul", r="sel RT3", w=pk, out=PKs, lhsT=sel[:, idx, :], rhs=RT3[:, q, :], start=(q == 0), stop=False)
            p.t("matmul", r="ident maskF maskB", w=pk, out=PKs, lhsT=ident, rhs=(maskF if d == 0 else maskB), start=False, stop=True)
            lk = "LT%d" % (idx % 2)
            p.s("activation", r=pk + " bias16", w=lk, out=LT[idx % 2], in_=PKs, func=AF.Exp, bias=bias16[:, idx:idx + 1])
            p.v("tensor_tensor", r=lk + " ST_sb", w="SmT%d" % idx, out=SmT[:, idx, :], in0=LT[idx % 2], in1=ST_sb[:, g, :], op=ALU.mult)
        if stop == "E5a":
            return fin(nc, p, ar, dbg_d)
        for h in range(8):
            hs = slice(h * 64, (h + 1) * 64)
            p.t("matmul", r="SmT%d xdt0" % h, w="b3", out=PY[:, hs], lhsT=SmT[:, h, :], rhs=xdt[0][:, hs], start=True, stop=False)
            p.t("matmul", r="SmT%d xdt1" % (8 + h), w="b3", out=PY[:, hs], lhsT=SmT[:, 8 + h, :], rhs=xdt[1][:, hs], start=False, stop=True)
        for h in range(8):
            g = h // 4
            hs = slice(h * 64, (h + 1) * 64)
            p.t("matmul", r="xbcT%d Sbf0" % (6 + g), w="b4", out=PB(4)[:, hs], lhsT=xbcT[:, 6 + g, tok], rhs=Sbf[0][:, hs], start=True, stop=True)
            p.t("matmul", r="xbcT%d SB%d" % (6 + g, c), w="b5", out=PB(5)[:, hs], lhsT=xbcT[:, 6 + g, tok], rhs=SB_all[:, c, hs], start=True, stop=True)
        if stop == "E5b":
            return fin(nc, p, ar, dbg_d)
        p.v("tensor_tensor", r="b4 e16", w="yc", out=v3(yc), in0=v3(PB(4)), in1=bc8(e16[:, 0:8]), op=ALU.mult)
        p.v("tensor_tensor", r="b5 e16", w="yc2", out=v3(yc2), in0=v3(PB(5)), in1=bc8(e16[:, 8:16]), op=ALU.mult)
        p.g("tensor_tensor", r="yc yc2", w="yc", out=yc, in0=yc, in1=yc2, op=ALU.add)
        if stop == "E6":
            return fin(nc, p, ar, dbg_d)

        p.v("tensor_tensor", r="yc b3", w="yc", out=yc, in0=yc, in1=PY, op=ALU.add)
        p.g("tensor_tensor", r="xs_tok Drow", w="yc2", out=yc2, in0=xs_tok, in1=Drow, op=ALU.mult)
        p.v("tensor_tensor", r="yc yc2", w="yc", out=yc, in0=yc, in1=yc2, op=ALU.add)
        p.v("tensor_tensor", r="yc zst", w="gg", out=gg, in0=yc, in1=zst, op=ALU.mult)
        for g in range(2):
            p.s("activation", r="gg", w="junk2 ss2", out=junk2, in_=gg[:, g * 256:(g + 1) * 256], func=AF.Square, accum_out=ss2[:, g:g + 1])
        p.v("tensor_scalar", r="ss2", w="rs2", out=rs2, in0=ss2, scalar1=1.0 / 256, scalar2=EPS, op0=ALU.mult, op1=ALU.add)
        p.s("activation", r="rs2", w="rs2", out=rs2, in_=rs2, func=AF.Ln)
        p.s("activation", r="rs2", w="rs2", out=rs2, in_=rs2, func=AF.Exp, scale=-0.5)
        for g in range(2):
            gs = slice(g * 256, (g + 1) * 256)
            p.v("scalar_tensor_tensor", r="gg rs2 NW", w="ssm_tok", out=ssm_tok[:, gs], in0=gg[:, gs], scalar=rs2[:, g:g + 1], in1=NW[:, gs],
                op0=ALU.mult, op1=ALU.mult)
        for j in range(4):
            p.t("transpose", r="ssm_tok ident", w="b7", out=PM[:, j * 128:(j + 1) * 128], in_=ssm_tok[:, j * 128:(j + 1) * 128], identity=ident)
        p.s("copy", r="b7", w="catT", out=catT[:, 0:4, :], in_=PM[:, 0:512].rearrange("p (a b) -> p a b", a=4))
        if stop == "E7":
            return fin(nc, p, ar, dbg_d)

        ukeys = "uTt%d" % pe
        p.s("activation", r=ukeys, w="vsq", out=vsq, in_=uTc, func=AF.Square)
        for j in range(4):
            p.t("matmul", r="ones_b " + ukeys, w="b7", out=PM2[:, 256:384], lhsT=ones_b, rhs=uTc[:, j, :], start=(j == 0), stop=(j == 3))
        p.v("tensor_copy", r="vsq", w="vsqh", out=vsqh, in_=vsq)
        p.v("tensor_tensor", r="vsq vsqh", w="vsql", out=vsql, in0=vsq, in1=vsqh, op=ALU.subtract)
        for j in range(4):
            p.t("matmul", r="ones_b vsqh", w="b7", out=PM2[:, 384:512], lhsT=ones_b, rhs=vsqh[:, j, :], start=(j == 0), stop=False)
            p.t("matmul", r="ones_b vsql", w="b7", out=PM2[:, 384:512], lhsT=ones_b, rhs=vsql[:, j, :], start=False, stop=(j == 3))
        p.s("activation", r="b7", w="mean", out=mean, in_=PM2[:, 256:384], func=AF.Copy, scale=1.0 / 512)
        p.s("activation", r="b7", w="var", out=var, in_=PM2[:, 384:512], func=AF.Copy, scale=1.0 / 512)
        p.g("tensor_tensor", r="mean", w="msq", out=msq, in0=mean, in1=mean, op=ALU.mult)
        p.v("tensor_tensor", r="var msq", w="var", out=var, in0=var, in1=msq, op=ALU.subtract)
        p.v("tensor_scalar", r="var", w="var", out=var, in0=var, scalar1=EPS, scalar2=None, op0=ALU.add)
        p.s("activation", r="var", w="var", out=var, in_=var, func=AF.Ln)
        p.s("activation", r="var", w="var", out=var, in_=var, func=AF.Exp, scale=-0.5)
        p.v("tensor_tensor", r=ukeys + " mean", w="ntmp", out=ntmp, in0=uTc, in1=mean.unsqueeze(1).to_broadcast([128, 4, 128]), op=ALU.subtract)
        p.v("tensor_tensor", r="ntmp var", w="ntmp", out=ntmp, in0=ntmp, in1=var.unsqueeze(1).to_broadcast([128, 4, 128]), op=ALU.mult)
        for j in range(4):
            p.s("activation", r="ntmp cvec", w="catT", out=catT[:, 4 + j, :], in_=ntmp[:, j, :], func=AF.Silu, scale=cvec[:, 1, j:j + 1],
                bias=cvec[:, 2, j:j + 1])
        for nb in range(2):
            for kc in range(8):
                p.t("matmul", r="catT Wo", w="b%d" % (4 + nb), out=PB(4 + nb), lhsT=catT[:, kc, :], rhs=Wo[:, kc, nb * 512:(nb + 1) * 512],
                    start=(kc == 0), stop=(kc == 7))
            ns = slice(nb * 512, (nb + 1) * 512)
            tq, tk_ = (yc2, "yc2") if nb == 0 else (gg, "gg")
            p.v("tensor_tensor", r="b%d G1" % (4 + nb), w=tk_, out=tq, in0=PB(4 + nb), in1=G1[:, ns], op=ALU.mult)
            p.g("tensor_tensor", r=tk_ + " xe%d" % pe, w="xe%d" % pe, out=xe[pe][:, ns], in0=xe[pe][:, ns], in1=tq, op=ALU.add)
        p.dma(x1_s[tok, :], xe[pe], r="xe%d" % pe, w="x1_s", grp="x1w%d" % pe)
        local_states(0)
        upd(0)
    if dbg:
        dx1 = dout("x1", [L, 1024])
        p.dma(None, None, r="x1_s", w="dbg_x1", parts=[(dx1[i * 128:(i + 1) * 128, :], x1_s[i * 128:(i + 1) * 128, :]) for i in range(NT)])
    ar.release(mE)
    p.barrier()
    if stop == "E":
        return fin(nc, p, ar, dbg_d)

    ar.release(m_big)
    TB = 256
    KT = ar.a((16, 128), BF16)
    W2 = ar.a(1024); S2 = ar.a(1024); G2 = ar.a(1024); FW = ar.a(1024)
    p.dma(W2, modb_s[5], r="modb_s", w="W2")
    p.dma(S2, modb_s[6], r="modb_s", w="S2")
    p.dma(G2, modb_s[7], r="modb_s", w="G2")
    p.dma(FW, nrm_d[2], w="FW")
    xs1 = [ar.a(1024) for _ in range(2)]
    for i in range(4):
        sl = i % 2
        wv_ = xs1[sl][:, 0:512].rearrange("p (a b) -> p a b", a=4)
        p.dma(wv_, kt_d[:, i * 4:(i + 1) * 4, :], w="xs1%d" % sl)
        p.v("tensor_copy", r="xs1%d" % sl, w="KT", out=KT[:, i * 4:(i + 1) * 4, :], in_=wv_)
    iota_b = ar.a(128, BF16)
    p.v("tensor_copy", r="iota", w="iota_b", out=iota_b, in_=iota_f)
    Wqs = [ar.a((8, 128), BF16) for _ in range(3)]
    tmpF = ar.a(512)
    junkF = tmpF.bitcast(BF16)
    ssF = ar.a(2); rsF = ar.a(2)
    h2 = ar.a(1024, BF16)
    h2T = [ar.a((8, TB), BF16) for _ in range(3)]
    qT = ar.a((16, TB), BF16)
    sc = ar.a((16, 128))
    sv = ar.a((16, 16)); si = ar.a((16, 16), U32); sif = ar.a((16, 16)); wk = ar.a(128); wk2 = ar.a(256)
    best = ar.a((8, 16)); fl = ar.a((8, 16), U32); ai = ar.a((8, 16), U32); bi = ar.a((8, 16), U32)
    af = ar.a((8, 16)); bf_ = ar.a((8, 16)); gt = ar.a((8, 16)); gs = ar.a(8)
    IG = ar.a((3, 128))
    IGTb = [ar.a((2, 2, 128), BF16) for _ in range(3)]
    IGTg = [ar.a((2, 128)) for _ in range(3)]
    NG = 16
    ohA2 = [ar.a((NG, 128), BF16) for _ in range(2)]
    ohB2 = [ar.a((NG, 64), BF16) for _ in range(2)]
    ohc = [0]
    oh = ar.a((8, 16, 16))
    GmH = [ar.a((TB, 64), BF16) for _ in range(2)]
    wu = [ar.a((2, 1024), BF16) for _ in range(3)]
    wv = [ar.a((2, 1024), BF16) for _ in range(3)]
    ga = [ar.a(TB) for _ in range(2)]
    gab = [ar.a(TB, BF16) for _ in range(2)]
    cand = sc.rearrange("p a b -> p (a b)").rearrange("p (h a b) -> p h a b", h=8, a=16)
    svv = sv.rearrange("p (h t) a -> p h t a", t=2)
    sifv = sif.rearrange("p (h t) a -> p h t a", t=2)
    io16 = iota_f[:, 0:16].unsqueeze(1).unsqueeze(1).to_broadcast([128, 8, 16, 16])
    NBLK = L // TB
    wqi = [0]

    def part1(blk):
        par = blk % 3
        hT = h2T[par]
        hk = "h2T%d" % par
        for ts in range(2):
            t0 = blk * TB + ts * 128
            xk = "xs1%d" % ts
            p.dma(xs1[ts], x1_s[t0:t0 + 128, :], r="x1_s", w=xk, e="gpsimd", grp="gx%d" % ts)
            yield "sync"
            p.s("activation", r=xk, w="tmpF ssF", out=junkF, in_=xs1[ts], func=AF.Square, accum_out=ssF[:, 0:1])
            p.v("tensor_scalar", r="ssF", w="rsF", out=rsF[:, 0:1], in0=ssF[:, 0:1], scalar1=1.0 / 1024, scalar2=EPS, op0=ALU.mult, op1=ALU.add)
            p.s("activation", r="rsF", w="rsF", out=rsF[:, 0:1], in_=rsF[:, 0:1], func=AF.Ln)
            p.s("activation", r="rsF", w="rsF", out=rsF[:, 0:1], in_=rsF[:, 0:1], func=AF.Exp, scale=-0.5)
            yield
            p.v("scalar_tensor_tensor", r=xk + " rsF W2", w=xk, out=xs1[ts], in0=xs1[ts], scalar=rsF[:, 0:1], in1=W2, op0=ALU.mult, op1=ALU.mult)
            p.g("tensor_tensor", r=xk + " S2", w="h2", out=h2, in0=xs1[ts], in1=S2, op=ALU.add)
            yield "sync"
            for kc in range(8):
                p.t("transpose", r="h2 ident", w="b2", out=PBb(2)[:, kc * 128:(kc + 1) * 128], in_=h2[:, kc * 128:(kc + 1) * 128], identity=ident)
            yield "sync"
            p.s("copy", r="b2", w=hk, out=hT[:, :, ts * 128:(ts + 1) * 128], in_=PBb(2).rearrange("p (a b) -> p a b", a=8))
            yield
        def wq_load(hp_):
            ws_ = (wqi[0] + hp_) % 3
            p.dma(Wqs[ws_], wqb_s[hp_].rearrange("p (k c) -> p k c", k=8), r="wqb_s", w="Wqs%d" % ws_, e="gpsimd", grp="gwq%d" % ws_)
        wq_load(0)
        wq_load(1)
        yield "sync"
        for hp in range(17):
            if hp >= 1:
                if hp % 2 == 1:
                    p.s("copy", r="b2", w="qT", out=qT[:, hp - 1, :], in_=PB(2)[:, 0:TB])
                else:
                    p.v("tensor_copy", r="b2", w="qT", out=qT[:, hp - 1, :], in_=PB(2)[:, 0:TB])
            if hp < 16:
                ws = (wqi[0] + hp) % 3
                if hp + 2 < 16:
                    wq_load(hp + 2)
                for kc in range(8):
                    p.t("matmul", r=("Wqs%d " % ws) + hk, w="b2", out=PB(2)[:, 0:TB], lhsT=Wqs[ws][:, kc, :], rhs=hT[:, kc, :],
                        start=(kc == 0), stop=(kc == 7))
            yield "sync"
        wqi[0] += 16
        yield "sync"
        for ts in range(2):
            tsl = slice(ts * 128, (ts + 1) * 128)
            for g4 in range(5):
                if g4 >= 1:
                    p.s("copy", r="b2", w="sc", out=sc[:, (g4 - 1) * 4:g4 * 4, :], in_=PB(2).rearrange("p (a b) -> p a b", a=4))
                if g4 < 4:
                    for q4 in range(4):
                        hp = g4 * 4 + q4
                        p.t("matmul", r="qT KT", w="b2", out=PB(2)[:, q4 * 128:(q4 + 1) * 128], lhsT=qT[:, hp, tsl], rhs=KT[:, hp, :],
                            start=True, stop=True)
                yield "sync"
            yield "sync"
            for hp in range(16):
                p.v("max", r="sc", w="sv", out=sv[:, hp, 0:8], in_=sc[:, hp, :])
                p.v("max_index", r="sc sv", w="si", out=si[:, hp, 0:8], in_max=sv[:, hp, 0:8], in_values=sc[:, hp, :])
                p.v("match_replace", r="sc sv", w="wk", out=wk, in_to_replace=sv[:, hp, 0:8], in_values=sc[:, hp, :], imm_value=-1e30)
                yield
                p.v("max", r="wk", w="sv", out=sv[:, hp, 8:16], in_=wk)
                p.v("max_index", r="wk sv", w="si", out=si[:, hp, 8:16], in_max=sv[:, hp, 8:16], in_values=wk)
                yield
            p.v("tensor_copy", r="si", w="sif", out=sif, in_=si)
            p.v("tensor_tensor", r="sv sc", w="sc", out=cand, in0=svv[:, :, 0, :].unsqueeze(3).to_broadcast([128, 8, 16, 16]),
                in1=svv[:, :, 1, :].unsqueeze(2).to_broadcast([128, 8, 16, 16]), op=ALU.add)
            yield
            for h in range(8):
                ch = cand[:, h].rearrange("p a b -> p (a b)")
                p.v("max", r="sc", w="best", out=best[:, h, 0:8], in_=ch)
                p.v("max_index", r="sc best", w="fl", out=fl[:, h, 0:8], in_max=best[:, h, 0:8], in_values=ch)
                p.v("match_replace", r="sc best", w="wk2", out=wk2, in_to_replace=best[:, h, 0:8], in_values=ch, imm_value=-1e30)
                yield
                p.v("max", r="wk2", w="best", out=best[:, h, 8:16], in_=wk2)
                p.v("max_index", r="wk2 best", w="fl", out=fl[:, h, 8:16], in_max=best[:, h, 8:16], in_values=wk2)
                yield
            p.v("tensor_tensor", r="best", w="gt", out=gt, in0=best, in1=best[:, :, 0:1].to_broadcast([128, 8, 16]), op=ALU.subtract)
            yield "sync"
            p.s("activation", r="gt", w="gt", out=gt, in_=gt, func=AF.Exp)
            yield "sync"
            p.v("tensor_reduce", r="gt", w="gs", out=gs, in_=gt, axis=AX.X, op=ALU.add)
            p.v("reciprocal", r="gs", w="gs", out=gs, in_=gs)
            yield
            p.v("tensor_tensor", r="gt gs IG", w="IG", out=IG[:, 2, :].rearrange("p (h j) -> p h j", h=8), in0=gt,
                in1=gs.unsqueeze(2).to_broadcast([128, 8, 16]), op=ALU.mult)
            p.v("tensor_single_scalar", r="fl", w="ai", out=ai, in_=fl, scalar=4, op=ALU.logical_shift_right)
            p.v("tensor_single_scalar", r="fl", w="bi", out=bi, in_=fl, scalar=15, op=ALU.bitwise_and)
            p.v("tensor_copy", r="ai", w="af", out=af, in_=ai)
            p.v("tensor_copy", r="bi", w="bf", out=bf_, in_=bi)
            yield
            for t_, (xf, key) in enumerate(((af, "af"), (bf_, "bf"))):
                p.v("tensor_tensor", r=key + " iota", w="oh", out=oh, in0=xf.unsqueeze(3).to_broadcast([128, 8, 16, 16]), in1=io16, op=ALU.is_equal)
                yield
                p.v("tensor_tensor", r="oh sif", w="oh", out=oh, in0=oh, in1=sifv[:, :, t_, :].unsqueeze(2).to_broadcast([128, 8, 16, 16]), op=ALU.mult)
                yield
                p.v("tensor_reduce", r="oh IG", w="IG", out=IG[:, t_, :].rearrange("p (h j) -> p h j", h=8), in_=oh, axis=AX.X, op=ALU.add)
                yield
            yield "sync"
            for q in range(3):
                p.t("transpose", r="IG identf", w="b2", out=PB(2)[:, q * 128:(q + 1) * 128], in_=IG[:, q, :], identity=ident_f)
            yield "sync"
            p.s("copy", r="b2", w="IGTb%d" % par, out=IGTb[par][:, ts, :, :], in_=PB(2)[:, 0:256].rearrange("p (a b) -> p a b", a=2))
            p.s("copy", r="b2", w="IGTg%d" % par, out=IGTg[par][:, ts, :], in_=PB(2)[:, 256:384])
            yield

    def part2(blk, half):
        par = blk % 3
        kb, kg = "IGTb%d" % par, "IGTg%d" % par
        Gh = GmH[half]
        gk = "Gm%d" % half
        groups = [(ts, tg) for ts in range(2) for tg in range(128 // NG)]
        bufs = {}

        def gen_(n):
            ts, tg = groups[n]
            tq = slice(tg * NG, (tg + 1) * NG)
            ob = ohc[0] % 2
            ohc[0] += 1
            bufs[n] = ob
            ohA, ohB = ohA2[ob], ohB2[ob]
            ka, kbb = "ohA%d" % ob, "ohB%d" % ob
            p.v("tensor_tensor", r=kb + " iota_b", w=ka, out=ohA, in0=iota_b.unsqueeze(1).to_broadcast([128, NG, 128]),
                in1=IGTb[par][:, ts, 0, tq].unsqueeze(2).to_broadcast([128, NG, 128]), op=ALU.is_equal)
            p.v("tensor_tensor", r=kb + " iota_b", w=kbb, out=ohB, in0=iota_b[:, half * 64:(half + 1) * 64].unsqueeze(1).to_broadcast([128, NG, 64]),
                in1=IGTb[par][:, ts, 1, tq].unsqueeze(2).to_broadcast([128, NG, 64]), op=ALU.is_equal)
            p.g("tensor_tensor", r=kg + " " + kbb, w=kbb, out=ohB, in0=ohB, in1=IGTg[par][:, ts, tq].unsqueeze(2).to_broadcast([128, NG, 64]), op=ALU.mult)

        def mm_(m):
            n, t8 = m // 2, m % 2
            ob = bufs[n]
            ohA, ohB = ohA2[ob], ohB2[ob]
            ka, kbb = "ohA%d" % ob, "ohB%d" % ob
            for q8 in range(8):
                tt_ = t8 * 8 + q8
                p.t("matmul", r=ka + " " + kbb, w="b3", out=PB(3)[:, q8 * 64:(q8 + 1) * 64], lhsT=ohA[:, tt_, :], rhs=ohB[:, tt_, :],
                    start=True, stop=True)

        def ev_(m):
            n, t8 = m // 2, m % 2
            ts, tg = groups[n]
            g0 = ts * 128 + tg * NG + t8 * 8
            if m % 2 == 0:
                p.s("copy", r="b3", w=gk, out=Gh[:, g0:g0 + 8, :], in_=PB(3).rearrange("p (a b) -> p a b", a=8))
            else:
                p.v("tensor_copy", r="b3", w=gk, out=Gh[:, g0:g0 + 8, :], in_=PB(3).rearrange("p (a b) -> p a b", a=8))

        ng = len(groups)
        gen_(0)
        yield "sync"
        for m in range(2 * ng + 1):
            if m % 2 == 0 and m // 2 + 1 < ng:
                gen_(m // 2 + 1)
            if m >= 1:
                ev_(m - 1)
            if m < 2 * ng:
                mm_(m)
            yield "sync"

    widx = [0]

    def main_pair(blk, pr, between):
        par = blk % 3
        hT = h2T[par]
        sl = widx[0] % 3
        widx[0] += 1
        p.dma(wu[sl], utb_s[2 * pr:2 * pr + 2].rearrange("a p c -> p a c"), r="utb_s", w="wu%d" % sl)
        p.dma(wv[sl], vtb_s[2 * pr:2 * pr + 2].rearrange("a p c -> p a c"), r="vtb_s", w="wv%d" % sl)
        for e2 in range(2):
            if e2 == 1:
                between()
            i2 = 2 * pr + e2
            half = i2 // 64
            ab = i2 % 2
            for kc in range(8):
                p.t("matmul", r="wu%d h2T%d" % (sl, par), w="b%d" % ab, out=PB(ab)[:, 0:TB], lhsT=wu[sl][:, e2, kc * 128:(kc + 1) * 128], rhs=hT[:, kc, :],
                    start=(kc == 0), stop=(kc == 7))
            p.s("activation", r="b%d" % ab, w="ga%d" % ab, out=ga[ab], in_=PB(ab)[:, 0:TB], func=AF.Gelu)
            p.v("tensor_tensor", r="ga%d Gm%d" % (ab, half), w="gab%d" % ab, out=gab[ab], in0=ga[ab], in1=GmH[half][:, :, i2 % 64], op=ALU.mult)
            for ts in range(2):
                for nb in range(2):
                    bk = 4 + 2 * ts + nb
                    p.t("matmul", r="gab%d wv%d" % (ab, sl), w="b%d" % bk, out=PB(bk), lhsT=gab[ab][:, ts * 128:(ts + 1) * 128],
                        rhs=wv[sl][:, e2, nb * 512:(nb + 1) * 512], start=(i2 == 0), stop=(i2 == 127))

    def finalize(blk):
        for ts in range(2):
            t0 = blk * TB + ts * 128
            xk = "xs1%d" % ts
            p.dma(xs1[ts], x1_s[t0:t0 + 128, :], r="x1_s", w=xk, e="gpsimd", grp="gx%d" % ts)
            for nb in range(2):
                bk = 4 + 2 * ts + nb
                ns = slice(nb * 512, (nb + 1) * 512)
                p.v("tensor_tensor", r="b%d G2" % bk, w="tmpF", out=tmpF, in0=PB(bk), in1=G2[:, ns], op=ALU.mult)
                p.g("tensor_tensor", r="tmpF " + xk, w=xk, out=xs1[ts][:, ns], in0=xs1[ts][:, ns], in1=tmpF, op=ALU.add)
            p.s("activation", r=xk, w="tmpF ssF", out=junkF, in_=xs1[ts], func=AF.Square, accum_out=ssF[:, 1:2])
            p.v("tensor_scalar", r="ssF", w="rsF", out=rsF[:, 1:2], in0=ssF[:, 1:2], scalar1=1.0 / 1024, scalar2=EPS, op0=ALU.mult, op1=ALU.add)
            p.s("activation", r="rsF", w="rsF", out=rsF[:, 1:2], in_=rsF[:, 1:2], func=AF.Ln)
            p.s("activation", r="rsF", w="rsF", out=rsF[:, 1:2], in_=rsF[:, 1:2], func=AF.Exp, scale=-0.5)
            p.v("scalar_tensor_tensor", r=xk + " rsF FW", w=xk, out=xs1[ts], in0=xs1[ts], scalar=rsF[:, 1:2], in1=FW, op0=ALU.mult, op1=ALU.mult)
            p.dma(out_d[t0:t0 + 128, :], xs1[ts], r=xk, w="out", grp="gout%d" % ts, e="gpsimd")

    nblk = 1 if stop == "F1" else (3 if stop == "F2" else NBLK)

    def drain(g):
        if g is not None:
            for _ in g:
                pass

    def drip(g, n):
        if g is None:
            return None
        try:
            for _ in range(n):
                if next(g) == "sync":
                    break
        except StopIteration:
            return None
        return g

    drain(part1(0))
    if nblk > 1:
        drain(part1(1))
    drain(part2(0, 0))
    for blk in range(nblk):
        st = {"g1": part1(blk + 2) if blk + 2 < nblk else None, "gB": part2(blk, 1), "gA": None}

        def dr(pr, st=st, blk=blk):
            st["g1"] = drip(st["g1"], 8)
            if pr < 32:
                st["gB"] = drip(st["gB"], 1)
            else:
                st["gA"] = drip(st["gA"], 1)

        for pr in range(64):
            main_pair(blk, pr, lambda pr=pr: dr(pr))
            if pr == 31:
                drain(st["gB"])
                st["gB"] = None
                st["gA"] = part2(blk + 1, 0) if blk + 1 < nblk else None
            dr(pr)
        g1, gA = st["g1"], st["gA"]
        drain(g1)
        drain(gA)
        finalize(blk)
    return fin(nc, p, ar, dbg_d)


def fin(nc, p, ar, dbg_d):
    p.finish()
    p.emit()
    return nc, dbg_d


def host_layout(inp, b):
    f = np.float32
    A = np.ascontiguousarray
    d = {}
    d["x"] = A(inp["x"][b])
    d["ctx"] = A(inp["ctx"][b])
    cc = np.stack([inp["c"][b].reshape(8, 128).T, inp["c_ctx"].reshape(8, 128).T], axis=-1)
    d["cc"] = A(cc.astype(f))
    return d


def shared_layout(inp):
    A = np.ascontiguousarray
    d = {}
    d["ada_w"] = A(inp["ada_w"][0].reshape(8, 128, 6144).transpose(1, 0, 2))
    d["ada_b"] = A(inp["ada_b"][0].reshape(1, 6144))
    d["nrm"] = A(np.stack([np.broadcast_to(inp[k].reshape(1, 1024), (128, 1024)) for k in ("norm1_w", "norm2_w", "final_norm_w")]))
    d["w_in"] = A(inp["w_in"][0].reshape(8, 128, 2576).transpose(1, 0, 2))
    d["scw"] = A(inp["ssm_conv_w"][0].reshape(5, 8, 128).transpose(2, 1, 0))
    d["scb"] = A(inp["ssm_conv_b"][0].reshape(8, 128).T)
    d["dtb"] = A(np.broadcast_to(inp["ssm_dt_bias"][0].reshape(1, 16), (128, 16)))
    d["alog"] = A(np.broadcast_to(inp["ssm_a_log"][0].reshape(1, 16), (128, 16)))
    d["drow"] = A(np.broadcast_to(np.repeat(inp["ssm_d"][0], 64).reshape(1, 512), (128, 512)))
    d["snw"] = A(np.broadcast_to(inp["ssm_norm_w"][0].reshape(1, 512), (128, 512)))
    d["ccw"] = A(inp["cfm_conv_w"][0].reshape(31, 4, 128).transpose(2, 1, 0))
    d["cvec"] = A(np.stack([inp[k][0].reshape(4, 128).T for k in ("cfm_conv_b", "cfm_ln_w", "cfm_ln_b")], axis=1))
    d["w_out"] = A(inp["w_out"][0].reshape(8, 128, 1024).transpose(1, 0, 2))
    d["wq"] = A(inp["peer_wq"][0].reshape(8, 128, 2048).transpose(1, 0, 2))
    d["kt"] = A(inp["peer_subkeys"][0].reshape(16, 128, 128).transpose(2, 0, 1))
    d["ut"] = A(inp["peer_u"][0].reshape(128, 128, 8, 128).transpose(1, 3, 2, 0))
    d["vt"] = A(inp["peer_v"][0].reshape(128, 128, 1024).transpose(1, 0, 2))
    return d


_CACHE = {}


def kernel(**inputs):
    inp = {k: np.asarray(v) for k, v in inputs.items()}
    if "nc" not in _CACHE:
        _CACHE["nc"] = build()[0]
    nc = _CACHE["nc"]
    sh = shared_layout(inp)
    in_maps = []
    for b in range(8):
        d = dict(sh)
        d.update(host_layout(inp, b))
        in_maps.append(d)
    res = run_bass_kernel_spmd(nc, in_maps, core_ids=list(range(8)))
    return np.stack([np.asarray(r["out"], dtype=np.float32) for r in res.results], axis=0)
```

```python
import re
import numpy as np
import concourse.bass as bass
import concourse.mybir as mybir
from concourse.bass_utils import run_bass_kernel_spmd

F32 = mybir.dt.float32
BF16 = mybir.dt.bfloat16
I32 = mybir.dt.int32
U32 = mybir.dt.uint32
AF = mybir.ActivationFunctionType
ALU = mybir.AluOpType
AX = mybir.AxisListType
ENGS = ("sync", "scalar", "vector", "gpsimd", "tensor")
CH = 4096
EPS = 1e-6
DSZ = {F32: 4, BF16: 2, I32: 4, U32: 4}


class Prog:
    def __init__(self, nc):
        self.nc = nc
        self.q = {e: [] for e in ENGS}
        self.cnt = {e: 0 for e in ENGS}
        self.known = {e: {} for e in ENGS}
        self.lastw = {}
        self.readers = {}
        self.sems = {}
        self.dmacum = {}
        self.pend = {e: {} for e in ENGS}
        self.lasttok = {}

    def sem(self, key):
        s = self.sems.get(key)
        if s is None:
            s = self.nc.alloc_semaphore(name="s%d" % len(self.sems))
            self.sems[key] = s
        return s

    def _waits(self, e, reads, writes):
        toks = dict(self.pend[e])
        self.pend[e] = {}

        def add(t):
            if t is None:
                return
            k, v = t
            if k[0] == "eng" and k[1] == e and e == "tensor":
                return
            if toks.get(k, 0) < v:
                toks[k] = v
        for r in reads:
            add(self.lastw.get(r))
        for w in writes:
            add(self.lastw.get(w))
            for t in self.readers.get(w, ()):
                add(t)
        out = []
        for k, v in toks.items():
            if self.known[e].get(k, 0) >= v:
                continue
            self.known[e][k] = v
            out.append((self.sem(k), v))
        return out

    def _commit(self, tok, reads, writes):
        self.lasttok[tok[0]] = tok[1]
        for w in writes:
            self.lastw[w] = tok
            self.readers[w] = []
        for r in reads:
            self.readers.setdefault(r, []).append(tok)

    def op(self, e, name, r="", w="", **kw):
        reads, writes = r.split(), w.split()
        ex = [k for k in reads if re.fullmatch(r"(ps|b)\d", k)]
        reads = [k for k in reads if k not in ex]
        writes = writes + [k for k in ex if k not in writes]
        waits = self._waits(e, reads, writes)
        idx = self.cnt[e]
        self.cnt[e] += 1
        k = ("eng", e, idx // CH)
        self.q[e].append((waits, name, kw, self.sem(k), 1))
        self._commit((k, idx % CH + 1), reads, writes)

    def v(self, name, r="", w="", **kw):
        self.op("vector", name, r, w, **kw)

    def s(self, name, r="", w="", **kw):
        self.op("scalar", name, r, w, **kw)

    def g(self, name, r="", w="", **kw):
        self.op("gpsimd", name, r, w, **kw)

    def t(self, name, r="", w="", **kw):
        self.op("tensor", name, r, w, **kw)

    def dma(self, out, in_, r="", w="", e="sync", grp=None, parts=None, **kw):
        reads, writes = r.split(), w.split()
        sk = ("dma", grp if grp is not None else writes[0])
        waits = self._waits(e, reads, writes)
        pieces = parts if parts is not None else [(out, in_)]
        for i, (o, a) in enumerate(pieces):
            self.dmacum[sk] = self.dmacum.get(sk, 0) + 16
            kk = dict(kw)
            kk.update(out=o, in_=a)
            self.q[e].append((waits if i == 0 else [], "dma_start", kk, self.sem(sk), 16))
        self._commit((sk, self.dmacum[sk]), reads, writes)

    def barrier(self):
        for e in ENGS:
            for k, v in self.lasttok.items():
                if self.pend[e].get(k, 0) < v:
                    self.pend[e][k] = v

    def finish(self):
        self.barrier()
        for e in ("sync", "gpsimd"):
            waits = self._waits(e, [], [])
            self.q[e].append((waits, None, None, None, 0))

    def emit(self):
        nc = self.nc
        with nc.Block() as block:
            def mk(e):
                def body(eng):
                    for waits, name, kw, s, inc in self.q[e]:
                        for ws, wv in waits:
                            eng.wait_ge(ws, wv)
                        if name is not None:
                            getattr(eng, name)(**kw).then_inc(s, inc)
                return body
            block.sync(mk("sync"))
            block.scalar(mk("scalar"))
            block.vector(mk("vector"))
            block.gpsimd(mk("gpsimd"))
            block.tensor(mk("tensor"))


class Arena:
    def __init__(self, nc, nbytes):
        self.t = nc.alloc_sbuf_tensor("arena", [128, nbytes // 4], F32)
        self.cap = nbytes
        self.off = 0
        self.peak = 0

    def a(self, free, dt=F32, parts=128):
        if isinstance(free, int):
            free = (free,)
        n = int(np.prod(free)) * DSZ[dt]
        n = (n + 63) // 64 * 64
        assert self.off + n <= self.cap, ("SBUF arena overflow", self.off, n, self.cap)
        ap = self.t[0:parts, self.off // 4:(self.off + n) // 4]
        self.off += n
        self.peak = max(self.peak, self.off)
        if dt != F32:
            ap = ap.bitcast(dt)
        ap = ap[:, 0:int(np.prod(free))]
        if len(free) == 2:
            ap = ap.rearrange("p (a b) -> p a b", a=free[0])
        elif len(free) == 3:
            ap = ap.rearrange("p (a b c) -> p a b c", a=free[0], b=free[1])
        return ap

    def mark(self):
        return self.off

    def release(self, m):
        self.off = m


L = 4096
LC = 256
NT = L // 128


def build(stop=None, dbg=False, npeer=128):
    nc = bass.Bass("TRN2", target_bir_lowering=False)
    D = nc.dram_tensor

    def din(name, shape, dt=F32):
        return D(name, list(shape), dt, kind="ExternalInput").ap()
    x_d = din("x", [L, 1024])
    ctx_d = din("ctx", [LC, 1024])
    cc_d = din("cc", [128, 8, 2])
    adaw_d = din("ada_w", [128, 8, 6144])
    adab_d = din("ada_b", [1, 6144])
    nrm_d = din("nrm", [3, 128, 1024])
    win_d = din("w_in", [128, 8, 2576])
    scw_d = din("scw", [128, 8, 5])
    scb_d = din("scb", [128, 8])
    dtb_d = din("dtb", [128, 16])
    alog_d = din("alog", [128, 16])
    drow_d = din("drow", [128, 512])
    snw_d = din("snw", [128, 512])
    ccw_d = din("ccw", [128, 4, 31])
    cvec_d = din("cvec", [128, 3, 4])
    wout_d = din("w_out", [128, 8, 1024])
    wq_d = din("wq", [128, 8, 2048])
    kt_d = din("kt", [128, 16, 128])
    ut_d = din("ut", [npeer, 128, 8, 128])
    vt_d = din("vt", [npeer, 128, 1024])
    out_d = D("out", [L, 1024], F32, kind="ExternalOutput").ap()
    dbg_d = {}

    def dout(name, shape):
        dbg_d[name] = D("dbg_" + name, list(shape), F32, kind="ExternalOutput").ap()
        return dbg_d[name]
    modb_s = D("modb_s", [8, 128, 1024], F32).ap()
    zs_s = D("zs_s", [L, 512], BF16).ap()
    x1_s = D("x1_s", [L, 1024], F32).ap()
    utb_s = D("utb_s", [128, 128, 1024], BF16).ap()
    vtb_s = D("vtb_s", [128, 128, 1024], BF16).ap()
    uts_s = D("uts_s", [128, 4, L], BF16).ap()
    wqb_s = D("wqb_s", [16, 128, 1024], BF16).ap()

    p = Prog(nc)
    ar = Arena(nc, 212000)
    banks = [nc.alloc_psum_tensor("pb%d" % i, [128, 512], F32) for i in range(8)]

    def PB(i):
        return banks[i][:]

    def PBb(i):
        return banks[i][:].bitcast(BF16)

    ident_f = ar.a(128)
    ident = ar.a(128, BF16)
    iota_f = ar.a(128)
    pidx = ar.a(128)
    ones_f = ar.a(128)
    ones_b = ar.a(128, BF16)
    triU = ar.a(128)
    triS = ar.a(128)
    maskF = ar.a(128, BF16)
    maskB = ar.a(128, BF16)
    p.g("iota", w="iota", out=iota_f, pattern=[[1, 128]], base=0, channel_multiplier=0, allow_small_or_imprecise_dtypes=True)
    p.g("iota", w="pidx", out=pidx, pattern=[[0, 128]], base=0, channel_multiplier=1, allow_small_or_imprecise_dtypes=True)
    p.v("tensor_tensor", r="iota pidx", w="identf", out=ident_f, in0=iota_f, in1=pidx, op=ALU.is_equal)
    p.v("tensor_copy", r="identf", w="ident", out=ident, in_=ident_f)
    p.v("memset", w="ones_f", ap=ones_f, constant=1.0)
    p.v("memset", w="ones_b", ap=ones_b, constant=1.0)
    p.v("tensor_tensor", r="iota pidx", w="triU", out=triU, in0=iota_f, in1=pidx, op=ALU.is_ge)
    p.v("tensor_tensor", r="iota pidx", w="triS", out=triS, in0=iota_f, in1=pidx, op=ALU.is_gt)
    p.v("tensor_scalar", r="triU", w="maskF", out=maskF, in0=triU, scalar1=-1.0, scalar2=30000.0, op0=ALU.add, op1=ALU.mult)
    tmpc = ar.a(128)
    p.v("tensor_tensor", r="iota pidx", w="tmpc", out=tmpc, in0=iota_f, in1=pidx, op=ALU.is_le)
    p.v("tensor_scalar", r="tmpc", w="maskB", out=maskB, in0=tmpc, scalar1=-1.0, scalar2=30000.0, op0=ALU.add, op1=ALU.mult)
    sel = ar.a((16, 128), BF16)
    p.v("memset", w="sel", ap=sel, constant=0.0)
    p.v("tensor_copy", r="identf sel", w="sel", out=sel[0:16], in_=ident_f[0:16, 0:16].unsqueeze(2).to_broadcast([16, 16, 128]))
    scw = ar.a((8, 5)); scb = ar.a(8); dtbias = ar.a(16); alog = ar.a(16); ccw = ar.a((4, 31)); cvec = ar.a((3, 4))
    Aneg = ar.a(16)
    p.dma(scw, scw_d, w="scw", grp="small")
    p.dma(scb, scb_d, w="scb", grp="small")
    p.dma(dtbias, dtb_d, w="dtbias", grp="small")
    p.dma(alog, alog_d, w="alog", grp="small")
    p.dma(ccw, ccw_d, w="ccw", grp="small")
    p.dma(cvec, cvec_d, w="cvec", grp="small")
    p.s("activation", r="alog", w="Aneg", out=Aneg, in_=alog, func=AF.Exp)
    p.v("tensor_scalar", r="Aneg", w="Aneg", out=Aneg, in0=Aneg, scalar1=-1.0, scalar2=None, op0=ALU.mult)
    dt_all = ar.a((NT + 2, 16))
    m_persist = ar.mark()

    m0 = ar.mark()
    cc = ar.a((8, 2)); scT = ar.a((8, 2))
    adab = ar.a(6144, parts=1)
    modrow = ar.a(6144, parts=1)
    modrow_c = ar.a(2048, parts=1)
    awb = [ar.a((8, 512)) for _ in range(2)]
    nrm = [ar.a(1024) for _ in range(2)]
    bct = [ar.a(1024) for _ in range(2)]
    p.dma(cc, cc_d, w="cc")
    p.dma(adab, adab_d, w="adab")
    p.dma(nrm[0], nrm_d[0], w="nrm0")
    p.dma(nrm[1], nrm_d[1], w="nrm1")
    p.s("activation", r="cc", w="scT", out=scT, in_=cc, func=AF.Silu)
    for blk in range(12):
        sl = blk % 2
        p.dma(awb[sl], adaw_d[:, :, blk * 512:(blk + 1) * 512], w="awb%d" % sl)
        for kc in range(8):
            p.t("matmul", r="scT awb%d" % sl, w="ps0", out=PB(0)[0:1, :], lhsT=scT[:, kc, 0:1], rhs=awb[sl][:, kc, :],
                start=(kc == 0), stop=(kc == 7))
        if blk < 4:
            for kc in range(8):
                p.t("matmul", r="scT awb%d" % sl, w="ps1", out=PB(1)[0:1, :], lhsT=scT[:, kc, 1:2], rhs=awb[sl][:, kc, :],
                    start=(kc == 0), stop=(kc == 7))
            p.v("tensor_tensor", r="ps1 adab", w="modrow_c", out=modrow_c[:, blk * 512:(blk + 1) * 512], in0=PB(1)[0:1, :],
                in1=adab[:, blk * 512:(blk + 1) * 512], op=ALU.add)
        p.v("tensor_tensor", r="ps0 adab", w="modrow", out=modrow[:, blk * 512:(blk + 1) * 512], in0=PB(0)[0:1, :],
            in1=adab[:, blk * 512:(blk + 1) * 512], op=ALU.add)
    plan = [(0, modrow, 1, 0), (1, modrow, 0, None), (2, modrow_c, 1, 0), (3, modrow_c, 0, None), (4, modrow, 2, None),
            (5, modrow, 4, 1), (6, modrow, 3, None), (7, modrow, 5, None)]
    for i, (ti, src, mi, ni) in enumerate(plan):
        bt = bct[i % 2]
        bk = "bct%d" % (i % 2)
        for hf in range(2):
            p.t("matmul", r="ones_f modrow modrow_c", w="ps%d" % (2 + hf), out=PB(2 + hf), lhsT=ones_f[0:1, :],
                rhs=src[:, mi * 1024 + hf * 512: mi * 1024 + (hf + 1) * 512], start=True, stop=True)
            if ni is None:
                p.v("tensor_copy", r="ps%d" % (2 + hf), w=bk, out=bt[:, hf * 512:(hf + 1) * 512], in_=PB(2 + hf))
            else:
                p.v("scalar_tensor_tensor", r="ps%d nrm%d" % (2 + hf, ni), w=bk, out=bt[:, hf * 512:(hf + 1) * 512], in0=PB(2 + hf),
                    scalar=1.0, in1=nrm[ni][:, hf * 512:(hf + 1) * 512], op0=ALU.add, op1=ALU.mult)
        p.dma(modb_s[ti], bt, r=bk, w="modb_s", grp="modb_w")
    if dbg:
        p.dma(dout("modrow", [1, 6144]), modrow, r="modrow", w="dbg_modrow")
    ar.release(m0)
    p.barrier()
    if stop == "0":
        return fin(nc, p, ar, dbg_d)

    m_big = ar.mark()
    xbcT = ar.a((8, L), BF16)
    xbcTc = ar.a((8, LC), BF16)
    m_uT = ar.mark()
    uT = ar.a((4, L), BF16)

    mA = ar.mark()
    Wb = ar.a((8, 2576), BF16)
    wst = [ar.a((8, 128)) for _ in range(2)]
    W1 = ar.a(1024); S1 = ar.a(1024)

    def load_mod(i0):
        p.dma(W1, modb_s[i0], r="modb_s", w="modA0", grp="modA")
        p.dma(S1, modb_s[i0 + 1], r="modb_s", w="modA1", grp="modA")
    ncol = 2576
    pieces = [(c0, min(128, ncol - c0)) for c0 in range(0, ncol, 128)]
    for i, (c0, cw) in enumerate(pieces):
        sl = i % 2
        p.dma(wst[sl][:, :, 0:cw], win_d[:, :, c0:c0 + cw], w="wst%d" % sl)
        if i % 2 == 0:
            p.v("tensor_copy", r="wst%d" % sl, w="Wb", out=Wb[:, :, c0:c0 + cw], in_=wst[sl][:, :, 0:cw])
        else:
            p.s("copy", r="wst%d" % sl, w="Wb", out=Wb[:, :, c0:c0 + cw], in_=wst[sl][:, :, 0:cw])
    xt = [ar.a(1024) for _ in range(2)]
    junk = ar.a(1024, BF16)
    hb = [ar.a(1024, BF16) for _ in range(2)]
    hT = ar.a((8, 512), BF16)
    ss = ar.a(2); rs = ar.a(2)
    sig = ar.a(512)
    zsb = [ar.a(512, BF16) for _ in range(2)]
    dtt = ar.a(16)

    def inproj_block(src_d, t0, ntile, Wm, Sm, is_ctx, tile0):
        ntok = ntile * 128
        for ts in range(ntile):
            sl = ts % 2
            p.dma(xt[sl], src_d[t0 + ts * 128:t0 + (ts + 1) * 128, :], w="xt%d" % sl)
            p.s("activation", r="xt%d" % sl, w="junk ss", out=junk, in_=xt[sl], func=AF.Square, accum_out=ss[:, 0:1])
            p.v("tensor_scalar", r="ss", w="rs", out=rs[:, 0:1], in0=ss[:, 0:1], scalar1=1.0 / 1024, scalar2=EPS, op0=ALU.mult, op1=ALU.add)
            p.s("activation", r="rs", w="rs", out=rs[:, 0:1], in_=rs[:, 0:1], func=AF.Ln)
            p.s("activation", r="rs", w="rs", out=rs[:, 0:1], in_=rs[:, 0:1], func=AF.Exp, scale=-0.5)
            p.v("scalar_tensor_tensor", r="xt%d rs modA0" % sl, w="xt%d" % sl, out=xt[sl], in0=xt[sl], scalar=rs[:, 0:1], in1=Wm,
                op0=ALU.mult, op1=ALU.mult)
            p.g("tensor_tensor", r="xt%d modA1" % sl, w="hb%d" % sl, out=hb[sl], in0=xt[sl], in1=Sm, op=ALU.add)
            for kc in range(8):
                p.t("transpose", r="hb%d ident" % sl, w="ps%d" % (4 + sl), out=PBb(4 + sl)[:, kc * 128:(kc + 1) * 128],
                    in_=hb[sl][:, kc * 128:(kc + 1) * 128], identity=ident)
            p.s("copy", r="ps%d" % (4 + sl), w="hT", out=hT[:, :, ts * 128:(ts + 1) * 128],
                in_=PBb(4 + sl).rearrange("p (a b) -> p a b", a=8))
        for j in range(8):
            pk = j % 2
            c0 = 512 + j * 128
            for kc in range(8):
                p.t("matmul", r="Wb hT", w="ps%d" % pk, out=PB(pk)[:, 0:ntok], lhsT=Wb[:, kc, c0:c0 + 128], rhs=hT[:, kc, 0:ntok],
                    start=(kc == 0), stop=(kc == 7))
            dst = xbcTc[:, j, 0:ntok] if is_ctx else xbcT[:, j, t0:t0 + ntok]
            if j % 2 == 0:
                p.s("copy", r="ps%d" % pk, w="xbcT", out=dst, in_=PB(pk)[:, 0:ntok])
            else:
                p.v("tensor_copy", r="ps%d" % pk, w="xbcT", out=dst, in_=PB(pk)[:, 0:ntok])
        if not is_ctx:
            for j in range(4):
                ca = 1552 + j * 128
                cb = 1552 + 512 + j * 128
                for kc in range(8):
                    p.t("matmul", r="Wb hT", w="ps2", out=PB(2)[:, 0:ntok], lhsT=Wb[:, kc, ca:ca + 128], rhs=hT[:, kc, 0:ntok],
                        start=(kc == 0), stop=(kc == 7))
                for kc in range(8):
                    p.t("matmul", r="Wb hT", w="ps3", out=PB(3)[:, 0:ntok], lhsT=Wb[:, kc, cb:cb + 128], rhs=hT[:, kc, 0:ntok],
                        start=(kc == 0), stop=(kc == 7))
                p.s("activation", r="ps3", w="sig", out=sig[:, 0:ntok], in_=PB(3)[:, 0:ntok], func=AF.Sigmoid)
                p.v("tensor_tensor", r="ps2 sig", w="uT", out=uT[:, j, t0:t0 + ntok], in0=PB(2)[:, 0:ntok], in1=sig[:, 0:ntok], op=ALU.mult)
        for ts in range(ntile):
            sl = ts % 2
            if not is_ctx:
                for kc in range(8):
                    p.t("matmul", r="Wb hT", w="ps%d" % (6 + sl), out=PB(6 + sl), lhsT=hT[:, kc, ts * 128:(ts + 1) * 128], rhs=Wb[:, kc, 0:512],
                        start=(kc == 0), stop=(kc == 7))
                p.s("activation", r="ps%d" % (6 + sl), w="zsb%d" % sl, out=zsb[sl], in_=PB(6 + sl), func=AF.Silu)
                p.dma(zs_s[t0 + ts * 128:t0 + (ts + 1) * 128, :], zsb[sl], r="zsb%d" % sl, w="zs_s", e="sync", grp="zsw%d" % sl)
            for kc in range(8):
                p.t("matmul", r="Wb hT", w="ps0", out=PB(0)[:, 0:16], lhsT=hT[:, kc, ts * 128:(ts + 1) * 128], rhs=Wb[:, kc, 1536:1552],
                    start=(kc == 0), stop=(kc == 7))
            p.v("tensor_tensor", r="ps0 dtbias", w="dtt", out=dtt, in0=PB(0)[:, 0:16], in1=dtbias, op=ALU.add)
            p.s("activation", r="dtt", w="dtt", out=dtt, in_=dtt, func=AF.Exp)
            p.s("activation", r="dtt", w="dt_all", out=dt_all[:, tile0 + ts, :], in_=dtt, func=AF.Ln, bias=1.0)

    load_mod(2)
    inproj_block(ctx_d, 0, 2, W1, S1, True, NT)
    if stop == "A1":
        p.dma(dout("dt_all", [128, NT + 2, 16]), dt_all, r="dt_all", w="dbg_dt")
        return fin(nc, p, ar, dbg_d)
    load_mod(0)
    for b in range(L // 512):
        inproj_block(x_d, b * 512, 4, W1, S1, False, b * 4)
    if dbg:
        p.dma(dout("dt_all", [128, NT + 2, 16]), dt_all, r="dt_all", w="dbg_dt")
        dxb = dout("xbc_pre", [128, 8, 128])
        tdb = ar.a((8, 128))
        p.v("tensor_copy", r="xbcT", w="tdb", out=tdb, in_=xbcT[:, :, 512:640])
        p.dma(dxb, tdb, r="tdb", w="dbg_xb")
        dub = dout("u_pre", [128, 4, 128])
        tdb2 = ar.a((4, 128))
        p.v("tensor_copy", r="uT", w="tdb2", out=tdb2, in_=uT[:, :, 512:640])
        p.dma(dub, tdb2, r="tdb2", w="dbg_ub")
    ar.release(mA)
    p.barrier()
    if stop == "A":
        return fin(nc, p, ar, dbg_d)

    mB = ar.mark()
    acc = [ar.a(L) for _ in range(2)]
    EN = [p.v, p.v]

    def ssm_conv(src, n, ei, j, key):
        E = EN[ei]
        ak = "acc%d" % ei
        a_ = acc[ei][:, 0:n]
        x_ = src[:, j, 0:n]
        E("tensor_scalar", r=key + " scw", w=ak, out=a_, in0=x_, scalar1=scw[:, j, 2:3], scalar2=None, op0=ALU.mult)
        for k in (0, 1, 3, 4):
            o = k - 2
            lo, hi = max(0, -o), min(n, n - o)
            E("scalar_tensor_tensor", r=key + " scw " + ak, w=ak, out=a_[:, lo:hi], in0=x_[:, lo + o:hi + o], scalar=scw[:, j, k:k + 1],
              in1=a_[:, lo:hi], op0=ALU.mult, op1=ALU.add)
        p.s("activation", r=ak + " scb", w=key, out=x_, in_=a_, func=AF.Silu, bias=scb[:, j:j + 1])

    tmpc2 = [ar.a(L) for _ in range(2)]

    def cfm_conv(j, ei):
        ak = "acc%d" % ei
        key = "uT%d" % j
        a_ = acc[ei]
        u_ = uT[:, j, :]
        a3 = a_.rearrange("p (r c) -> p r c", c=64)
        u3 = u_.rearrange("p (r c) -> p r c", c=64)
        if ei == 0:
            p.v("tensor_scalar", r=key + " ccw", w=ak, out=a_, in0=u_, scalar1=ccw[:, j, 15:16], scalar2=None, op0=ALU.mult)
        else:
            p.s("activation", r=key + " ccw", w=ak, out=a_, in_=u_, func=AF.Copy, scale=ccw[:, j, 15:16])
        n = 0
        for k in range(31):
            o = k - 15
            if o == 0:
                continue
            lo, hi = max(0, -o), min(64, 64 - o)
            if j < 2:
                oo, i0 = a3[:, :, lo:hi], u3[:, :, lo + o:hi + o]
            else:
                oo, i0 = a_[:, lo * 64:hi * 64], u_[:, (lo + o) * 64:(hi + o) * 64]
            if ei == 0:
                p.v("scalar_tensor_tensor", r=key + " ccw " + ak, w=ak, out=oo, in0=i0, scalar=ccw[:, j, k:k + 1], in1=oo, op0=ALU.mult, op1=ALU.add)
            else:
                tk = "tmpc2%d" % (n % 2)
                tt = tmpc2[n % 2]
                if j < 2:
                    t_ = tt.rearrange("p (r c) -> p r c", c=64)[:, :, lo:hi]
                else:
                    t_ = tt[:, lo * 64:hi * 64]
                p.s("activation", r=key + " ccw", w=tk, out=t_, in_=i0, func=AF.Copy, scale=ccw[:, j, k:k + 1])
                p.g("tensor_tensor", r=tk + " " + ak, w=ak, out=oo, in0=oo, in1=t_, op=ALU.add)
                n += 1
        p.s("activation", r=ak + " cvec", w=key, out=u_, in_=a_, func=AF.Identity, bias=cvec[:, 0, j:j + 1])

    for j in range(8):
        ssm_conv(xbcTc, LC, 0, j, "xbcTc%d" % j)
    cfm_conv(2, 1)
    for j in range(8):
        ssm_conv(xbcT, L, 0, j, "xbcT%d" % j)
    cfm_conv(3, 1)
    cfm_conv(0, 0)
    cfm_conv(1, 0)
    if dbg:
        tdb = ar.a((8, 128))
        p.v("tensor_copy", r=" ".join("xbcT%d" % j for j in range(8)), w="tdb", out=tdb, in_=xbcT[:, :, 512:640])
        p.dma(dout("xbc_post", [128, 8, 128]), tdb, r="tdb", w="dbg_xb2")
        tdb2 = ar.a((4, 128))
        p.v("tensor_copy", r=" ".join("uT%d" % j for j in range(4)), w="tdb2", out=tdb2, in_=uT[:, :, 512:640])
        p.dma(dout("u_post", [128, 4, 128]), tdb2, r="tdb2", w="dbg_ub2")
    for j in range(4):
        p.dma(uts_s[:, j, :], uT[:, j, :], r="uT%d" % j, w="uts_s", grp="utsw")
    ar.release(m_uT)
    p.barrier()
    if stop == "B":
        return fin(nc, p, ar, dbg_d)

    mE = ar.mark()
    SB_all = ar.a((NT, 512), BF16)
    Wo = ar.a((8, 1024), BF16)
    G1 = ar.a(1024); NW = ar.a(512); Drow = ar.a(512)
    p.dma(G1, modb_s[4], r="modb_s", w="G1")
    p.dma(NW, snw_d, w="NW")
    p.dma(Drow, drow_d, w="Drow")
    a16 = ar.a(16); cs = ar.a(32); dec = ar.a(16); tmp16 = ar.a(16); w16 = ar.a(16)
    rowsrc = ar.a(16); bias16 = ar.a(16); e16 = ar.a(16); RT = ar.a(128); RT3 = ar.a((3, 128), BF16); RTr = ar.a(128)
    xs_tok = ar.a(512, BF16); Btok = ar.a(256, BF16)
    xw = [ar.a(512, BF16) for _ in range(2)]
    xdt = [ar.a(512, BF16) for _ in range(2)]
    ST_sb = ar.a((2, 128))
    LT = [ar.a(128) for _ in range(2)]
    SmT = ar.a((16, 128), BF16)
    Sst = [ar.a(512) for _ in range(2)]
    Sbf = [ar.a(512, BF16) for _ in range(2)]
    yc = ar.a(512); yc2 = ar.a(512); zst = ar.a(512, BF16); gg = ar.a(512)
    ss2 = ar.a(2); rs2 = ar.a(2)
    ssm_tok = ar.a(512, BF16); catT = ar.a((8, 128), BF16)
    vsq = ar.a((4, 128)); vsqh = ar.a((4, 128), BF16); vsql = ar.a((4, 128), BF16); mean = ar.a(128); msq = ar.a(128); var = ar.a(128); ntmp = ar.a((4, 128))
    xe = [ar.a(1024) for _ in range(2)]
    for i in range(8):
        sl = i % 2
        wv_ = xe[sl].rearrange("p (a b) -> p a b", a=8)
        p.dma(wv_, wout_d[:, :, i * 128:(i + 1) * 128], w="xe%d" % sl)
        if i % 2 == 0:
            p.v("tensor_copy", r="xe%d" % sl, w="Wo", out=Wo[:, :, i * 128:(i + 1) * 128], in_=wv_)
        else:
            p.s("copy", r="xe%d" % sl, w="Wo", out=Wo[:, :, i * 128:(i + 1) * 128], in_=wv_)
    junk2 = ar.a(256, BF16)
    uTt = [ar.a((4, 128), BF16) for _ in range(2)]
    PT = PBb(0)
    PC = PB(1)
    PK = PB(2)
    PY = PB(3)
    PLS = PB(6)
    PM = PBb(7)
    PM2 = PB(7)

    def bc8(ap8):
        return ap8.unsqueeze(2).to_broadcast([128, 8, 64])

    def v3(ap512):
        return ap512.rearrange("p (h q) -> p h q", h=8)

    def prep(is_ctx, c, dirs):
        src = xbcTc if is_ctx else xbcT
        kx = "xbcTc%d" if is_ctx else "xbcT%d"
        tl = (NT + c) if is_ctx else c
        tok = slice(c * 128, (c + 1) * 128)
        p.v("tensor_tensor", r="dt_all Aneg", w="a16", out=a16, in0=dt_all[:, tl, :], in1=Aneg, op=ALU.mult)
        p.t("matmul", r="triU a16", w="b1", out=PC[:, 0:8], lhsT=triU, rhs=a16[:, 0:8], start=True, stop=True)
        p.t("matmul", r="triS a16", w="b1", out=PC[:, 8:16], lhsT=triS, rhs=a16[:, 8:16], start=True, stop=True)
        p.t("matmul", r="ones_f a16", w="b1", out=PC[:, 16:32], lhsT=ones_f, rhs=a16, start=True, stop=True)
        p.s("copy", r="b1", w="cs", out=cs, in_=PC[:, 0:32])
        p.s("activation", r="cs", w="dec", out=dec, in_=cs[:, 16:32], func=AF.Exp)
        p.v("tensor_tensor", r="cs", w="tmp16", out=tmp16[:, 0:8], in0=cs[:, 16:24], in1=cs[:, 0:8], op=ALU.subtract)
        p.v("tensor_copy", r="cs tmp16", w="tmp16", out=tmp16[:, 8:16], in_=cs[:, 8:16])
        p.s("activation", r="tmp16", w="tmp16", out=tmp16, in_=tmp16, func=AF.Exp)
        p.v("tensor_tensor", r="tmp16 dt_all", w="w16", out=w16, in0=tmp16, in1=dt_all[:, tl, :], op=ALU.mult)
        for j in range(4):
            p.t("transpose", r=(kx % j) + " ident", w="b0", out=PT[:, j * 128:(j + 1) * 128], in_=src[:, j, tok], identity=ident)
        for g in range(2):
            p.t("transpose", r=(kx % (4 + g)) + " ident", w="b0", out=PT[:, 512 + g * 128:512 + (g + 1) * 128], in_=src[:, 4 + g, tok], identity=ident)
        p.v("tensor_copy", r="b0", w="Btok", out=Btok, in_=PT[:, 512:768])
        for d in dirs:
            p.v("tensor_tensor", r="b0 w16", w="xw%d" % d, out=v3(xw[d]), in0=v3(PT[:, 0:512]), in1=bc8(w16[:, d * 8:(d + 1) * 8]), op=ALU.mult)

    def local_states(d):
        for h in range(8):
            g = h // 4
            p.t("matmul", r="Btok xw%d" % d, w="b6", out=PLS[:, h * 64:(h + 1) * 64], lhsT=Btok[:, g * 128:(g + 1) * 128],
                rhs=xw[d][:, h * 64:(h + 1) * 64], start=True, stop=True)

    def upd(d):
        p.v("tensor_tensor", r="S%d dec" % d, w="S%d" % d, out=v3(Sst[d]), in0=v3(Sst[d]), in1=bc8(dec[:, d * 8:(d + 1) * 8]), op=ALU.mult)
        p.v("tensor_tensor", r="S%d b6" % d, w="S%d" % d, out=Sst[d], in0=Sst[d], in1=PLS, op=ALU.add)
        p.s("copy", r="S%d" % d, w="Sbf%d" % d, out=Sbf[d], in_=Sst[d])

    p.v("memset", w="RT3", ap=RT3, constant=0.0)
    for d in range(2):
        p.v("memset", w="S%d" % d, ap=Sst[d], constant=0.0)
        p.v("memset", w="Sbf%d" % d, ap=Sbf[d], constant=0.0)
    prep(True, 0, (0,)); local_states(0); upd(0)
    prep(True, 1, (0, 1)); local_states(0); upd(0); local_states(1); upd(1)
    prep(True, 0, (1,)); local_states(1); upd(1)
    if stop == "C":
        p.dma(dout("S_f", [128, 512]), Sst[0], r="S0", w="dbg_sf")
        p.dma(dout("S_b", [128, 512]), Sst[1], r="S1", w="dbg_sb")
        return fin(nc, p, ar, dbg_d)
    for c in range(NT - 1, -1, -1):
        p.s("copy", r="Sbf1", w="SB%d" % c, out=SB_all[:, c, :], in_=Sbf[1])
        prep(False, c, (1,)); local_states(1); upd(1)
        if npeer == 128:
            if c >= 16:
                hp_ = c - 16
                p.dma(wqb_s[hp_].rearrange("p (k c) -> p k c", k=8), wq_d[:, :, hp_ * 128:(hp_ + 1) * 128], w="wqb_s", e="gpsimd", grp="castq")
            for i2 in range(c * 4, c * 4 + 4):
                p.dma(utb_s[i2], ut_d[i2].rearrange("p k i -> p (k i)"), w="utb_s", e="gpsimd", grp="castu")
                p.dma(vtb_s[i2], vt_d[i2], w="vtb_s", e="gpsimd", grp="castv")
    if dbg:
        p.dma(dout("S_b0", [128, 512]), Sst[1], r="S1", w="dbg_sb0")
    if stop == "D":
        return fin(nc, p, ar, dbg_d)
    for c in range(NT):
        tok = slice(c * 128, (c + 1) * 128)
        pe = c % 2
        p.dma(xe[pe], x_d[tok, :], w="xe%d" % pe)
        p.dma(zst, zs_s[tok, :], r="zs_s", w="zst")
        p.dma(uTt[pe], uts_s[:, :, tok], r="uts_s", w="uTt%d" % pe)
        uTc = uTt[pe]
        if stop == "E0":
            return fin(nc, p, ar, dbg_d)
        prep(False, c, (0,))
        if stop == "E1":
            return fin(nc, p, ar, dbg_d)
        p.v("tensor_copy", r="b0", w="xs_tok", out=xs_tok, in_=PT[:, 0:512])
        for d in range(2):
            p.v("tensor_tensor", r="b0 dt_all", w="xdt%d" % d, out=v3(xdt[d]), in0=v3(PT[:, 0:512]),
                in1=bc8(dt_all[:, c, d * 8:(d + 1) * 8]), op=ALU.mult)
        if stop == "E1a":
            return fin(nc, p, ar, dbg_d)
        for g in range(2):
            p.t("matmul", r="xbcT%d xbcT%d" % (4 + g, 6 + g), w="b1", out=PC[:, 256 + g * 128:256 + (g + 1) * 128],
                lhsT=xbcT[:, 4 + g, tok], rhs=xbcT[:, 6 + g, tok], start=True, stop=True)
        if stop == "E1b":
            return fin(nc, p, ar, dbg_d)
        p.s("copy", r="b1", w="ST_sb", out=ST_sb, in_=PC[:, 256:512].rearrange("p (g l) -> p g l", g=2))
        if stop == "E2":
            return fin(nc, p, ar, dbg_d)

        p.v("tensor_copy", r="cs", w="rowsrc", out=rowsrc[:, 0:8], in_=cs[:, 0:8])
        p.v("tensor_scalar", r="cs rowsrc", w="rowsrc", out=rowsrc[:, 8:16], in0=cs[:, 8:16], scalar1=-1.0, scalar2=None, op0=ALU.mult)
        p.v("tensor_scalar", r="rowsrc", w="bias16", out=bias16, in0=rowsrc, scalar1=-1.0, scalar2=None, op0=ALU.mult)
        p.t("transpose", r="rowsrc identf", w="b1", out=PC[0:16, 32:160], in_=rowsrc, identity=ident_f)
        p.s("copy", r="b1", w="RT", out=RT[0:16, :], in_=PC[0:16, 32:160])
        p.v("tensor_copy", r="RT", w="RT3", out=RT3[0:16, 0, :], in_=RT[0:16, :])
        p.v("tensor_tensor", r="RT RT3", w="RTr", out=RTr[0:16, :], in0=RT[0:16, :], in1=RT3[0:16, 0, :], op=ALU.subtract)
        p.v("tensor_copy", r="RTr RT3", w="RT3", out=RT3[0:16, 1, :], in_=RTr[0:16, :])
        p.v("tensor_tensor", r="RTr RT3", w="RTr", out=RTr[0:16, :], in0=RTr[0:16, :], in1=RT3[0:16, 1, :], op=ALU.subtract)
        p.v("tensor_copy", r="RTr RT3", w="RT3", out=RT3[0:16, 2, :], in_=RTr[0:16, :])
        if stop == "E3":
            return fin(nc, p, ar, dbg_d)

        p.v("tensor_copy", r="cs", w="e16", out=e16[:, 0:8], in_=cs[:, 0:8])
        p.v("tensor_tensor", r="cs e16", w="e16", out=e16[:, 8:16], in0=cs[:, 24:32], in1=cs[:, 8:16], op=ALU.subtract)
        p.s("activation", r="e16", w="e16", out=e16, in_=e16, func=AF.Exp)
        if stop == "E4":
            return fin(nc, p, ar, dbg_d)

        for idx in range(16):
            d, h = idx // 8, idx % 8
            g = h // 4
            pk = "b2" if idx % 2 == 0 else "b6"
            PKs = (PB(2) if idx % 2 == 0 else PB(6))[:, 0:128]
            for q in range(3):
                p.t("matmul", r="sel RT3", w=pk, out=PKs, lhsT=sel[:, idx, :], rhs=RT3[:, q, :], start=(q == 0), stop=False)
            p.t("matmul", r="ident maskF maskB", w=pk, out=PKs, lhsT=ident, rhs=(maskF if d == 0 else maskB), start=False, stop=True)
            lk = "LT%d" % (idx % 2)
            p.s("activation", r=pk + " bias16", w=lk, out=LT[idx % 2], in_=PKs, func=AF.Exp, bias=bias16[:, idx:idx + 1])
            p.v("tensor_tensor", r=lk + " ST_sb", w="SmT%d" % idx, out=SmT[:, idx, :], in0=LT[idx % 2], in1=ST_sb[:, g, :], op=ALU.mult)
        if stop == "E5a":
            return fin(nc, p, ar, dbg_d)
        for h in range(8):
            hs = slice(h * 64, (h + 1) * 64)
            p.t("matmul", r="SmT%d xdt0" % h, w="b3", out=PY[:, hs], lhsT=SmT[:, h, :], rhs=xdt[0][:, hs], start=True, stop=False)
            p.t("matmul", r="SmT%d xdt1" % (8 + h), w="b3", out=PY[:, hs], lhsT=SmT[:, 8 + h, :], rhs=xdt[1][:, hs], start=False, stop=True)
        for h in range(8):
            g = h // 4
            hs = slice(h * 64, (h + 1) * 64)
            p.t("matmul", r="xbcT%d Sbf0" % (6 + g), w="b4", out=PB(4)[:, hs], lhsT=xbcT[:, 6 + g, tok], rhs=Sbf[0][:, hs], start=True, stop=True)
            p.t("matmul", r="xbcT%d SB%d" % (6 + g, c), w="b5", out=PB(5)[:, hs], lhsT=xbcT[:, 6 + g, tok], rhs=SB_all[:, c, hs], start=True, stop=True)
        if stop == "E5b":
            return fin(nc, p, ar, dbg_d)
        p.v("tensor_tensor", r="b4 e16", w="yc", out=v3(yc), in0=v3(PB(4)), in1=bc8(e16[:, 0:8]), op=ALU.mult)
        p.v("tensor_tensor", r="b5 e16", w="yc2", out=v3(yc2), in0=v3(PB(5)), in1=bc8(e16[:, 8:16]), op=ALU.mult)
        p.g("tensor_tensor", r="yc yc2", w="yc", out=yc, in0=yc, in1=yc2, op=ALU.add)
        if stop == "E6":
            return fin(nc, p, ar, dbg_d)

        p.v("tensor_tensor", r="yc b3", w="yc", out=yc, in0=yc, in1=PY, op=ALU.add)
        p.g("tensor_tensor", r="xs_tok Drow", w="yc2", out=yc2, in0=xs_tok, in1=Drow, op=ALU.mult)
        p.v("tensor_tensor", r="yc yc2", w="yc", out=yc, in0=yc, in1=yc2, op=ALU.add)
        p.v("tensor_tensor", r="yc zst", w="gg", out=gg, in0=yc, in1=zst, op=ALU.mult)
        for g in range(2):
            p.s("activation", r="gg", w="junk2 ss2", out=junk2, in_=gg[:, g * 256:(g + 1) * 256], func=AF.Square, accum_out=ss2[:, g:g + 1])
        p.v("tensor_scalar", r="ss2", w="rs2", out=rs2, in0=ss2, scalar1=1.0 / 256, scalar2=EPS, op0=ALU.mult, op1=ALU.add)
        p.s("activation", r="rs2", w="rs2", out=rs2, in_=rs2, func=AF.Ln)
        p.s("activation", r="rs2", w="rs2", out=rs2, in_=rs2, func=AF.Exp, scale=-0.5)
        for g in range(2):
            gs = slice(g * 256, (g + 1) * 256)
            p.v("scalar_tensor_tensor", r="gg rs2 NW", w="ssm_tok", out=ssm_tok[:, gs], in0=gg[:, gs], scalar=rs2[:, g:g + 1], in1=NW[:, gs],
                op0=ALU.mult, op1=ALU.mult)
        for j in range(4):
            p.t("transpose", r="ssm_tok ident", w="b7", out=PM[:, j * 128:(j + 1) * 128], in_=ssm_tok[:, j * 128:(j + 1) * 128], identity=ident)
        p.s("copy", r="b7", w="catT", out=catT[:, 0:4, :], in_=PM[:, 0:512].rearrange("p (a b) -> p a b", a=4))
        if stop == "E7":
            return fin(nc, p, ar, dbg_d)

        ukeys = "uTt%d" % pe
        p.s("activation", r=ukeys, w="vsq", out=vsq, in_=uTc, func=AF.Square)
        for j in range(4):
            p.t("matmul", r="ones_b " + ukeys, w="b7", out=PM2[:, 256:384], lhsT=ones_b, rhs=uTc[:, j, :], start=(j == 0), stop=(j == 3))
        p.v("tensor_copy", r="vsq", w="vsqh", out=vsqh, in_=vsq)
        p.v("tensor_tensor", r="vsq vsqh", w="vsql", out=vsql, in0=vsq, in1=vsqh, op=ALU.subtract)
        for j in range(4):
            p.t("matmul", r="ones_b vsqh", w="b7", out=PM2[:, 384:512], lhsT=ones_b, rhs=vsqh[:, j, :], start=(j == 0), stop=False)
            p.t("matmul", r="ones_b vsql", w="b7", out=PM2[:, 384:512], lhsT=ones_b, rhs=vsql[:, j, :], start=False, stop=(j == 3))
        p.s("activation", r="b7", w="mean", out=mean, in_=PM2[:, 256:384], func=AF.Copy, scale=1.0 / 512)
        p.s("activation", r="b7", w="var", out=var, in_=PM2[:, 384:512], func=AF.Copy, scale=1.0 / 512)
        p.g("tensor_tensor", r="mean", w="msq", out=msq, in0=mean, in1=mean, op=ALU.mult)
        p.v("tensor_tensor", r="var msq", w="var", out=var, in0=var, in1=msq, op=ALU.subtract)
        p.v("tensor_scalar", r="var", w="var", out=var, in0=var, scalar1=EPS, scalar2=None, op0=ALU.add)
        p.s("activation", r="var", w="var", out=var, in_=var, func=AF.Ln)
        p.s("activation", r="var", w="var", out=var, in_=var, func=AF.Exp, scale=-0.5)
        p.v("tensor_tensor", r=ukeys + " mean", w="ntmp", out=ntmp, in0=uTc, in1=mean.unsqueeze(1).to_broadcast([128, 4, 128]), op=ALU.subtract)
        p.v("tensor_tensor", r="ntmp var", w="ntmp", out=ntmp, in0=ntmp, in1=var.unsqueeze(1).to_broadcast([128, 4, 128]), op=ALU.mult)
        for j in range(4):
            p.s("activation", r="ntmp cvec", w="catT", out=catT[:, 4 + j, :], in_=ntmp[:, j, :], func=AF.Silu, scale=cvec[:, 1, j:j + 1],
                bias=cvec[:, 2, j:j + 1])
        for nb in range(2):
            for kc in range(8):
                p.t("matmul", r="catT Wo", w="b%d" % (4 + nb), out=PB(4 + nb), lhsT=catT[:, kc, :], rhs=Wo[:, kc, nb * 512:(nb + 1) * 512],
                    start=(kc == 0), stop=(kc == 7))
            ns = slice(nb * 512, (nb + 1) * 512)
            tq, tk_ = (yc2, "yc2") if nb == 0 else (gg, "gg")
            p.v("tensor_tensor", r="b%d G1" % (4 + nb), w=tk_, out=tq, in0=PB(4 + nb), in1=G1[:, ns], op=ALU.mult)
            p.g("tensor_tensor", r=tk_ + " xe%d" % pe, w="xe%d" % pe, out=xe[pe][:, ns], in0=xe[pe][:, ns], in1=tq, op=ALU.add)
        p.dma(x1_s[tok, :], xe[pe], r="xe%d" % pe, w="x1_s", grp="x1w%d" % pe)
        local_states(0)
        upd(0)
    if dbg:
        dx1 = dout("x1", [L, 1024])
        p.dma(None, None, r="x1_s", w="dbg_x1", parts=[(dx1[i * 128:(i + 1) * 128, :], x1_s[i * 128:(i + 1) * 128, :]) for i in range(NT)])
    ar.release(mE)
    p.barrier()
    if stop == "E":
        return fin(nc, p, ar, dbg_d)

    ar.release(m_big)
    TB = 256
    KT = ar.a((16, 128), BF16)
    W2 = ar.a(1024); S2 = ar.a(1024); G2 = ar.a(1024); FW = ar.a(1024)
    p.dma(W2, modb_s[5], r="modb_s", w="W2")
    p.dma(S2, modb_s[6], r="modb_s", w="S2")
    p.dma(G2, modb_s[7], r="modb_s", w="G2")
    p.dma(FW, nrm_d[2], w="FW")
    xs1 = [ar.a(1024) for _ in range(2)]
    for i in range(4):
        sl = i % 2
        wv_ = xs1[sl][:, 0:512].rearrange("p (a b) -> p a b", a=4)
        p.dma(wv_, kt_d[:, i * 4:(i + 1) * 4, :], w="xs1%d" % sl)
        p.v("tensor_copy", r="xs1%d" % sl, w="KT", out=KT[:, i * 4:(i + 1) * 4, :], in_=wv_)
    iota_b = ar.a(128, BF16)
    p.v("tensor_copy", r="iota", w="iota_b", out=iota_b, in_=iota_f)
    Wqs = [ar.a((8, 128), BF16) for _ in range(3)]
    tmpF = ar.a(512)
    junkF = tmpF.bitcast(BF16)
    ssF = ar.a(2); rsF = ar.a(2)
    h2 = ar.a(1024, BF16)
    h2T = [ar.a((8, TB), BF16) for _ in range(3)]
    qT = ar.a((16, TB), BF16)
    sc = ar.a((16, 128))
    sv = ar.a((16, 16)); si = ar.a((16, 16), U32); sif = ar.a((16, 16)); wk = ar.a(128); wk2 = ar.a(256)
    best = ar.a((8, 16)); fl = ar.a((8, 16), U32); ai = ar.a((8, 16), U32); bi = ar.a((8, 16), U32)
    af = ar.a((8, 16)); bf_ = ar.a((8, 16)); gt = ar.a((8, 16)); gs = ar.a(8)
    IG = ar.a((3, 128))
    IGTb = [ar.a((2, 2, 128), BF16) for _ in range(3)]
    IGTg = [ar.a((2, 128)) for _ in range(3)]
    NG = 16
    ohA2 = [ar.a((NG, 128), BF16) for _ in range(2)]
    ohB2 = [ar.a((NG, 64), BF16) for _ in range(2)]
    ohc = [0]
    oh = ar.a((8, 16, 16))
    GmH = [ar.a((TB, 64), BF16) for _ in range(2)]
    wu = [ar.a((2, 1024), BF16) for _ in range(3)]
    wv = [ar.a((2, 1024), BF16) for _ in range(3)]
    ga = [ar.a(TB) for _ in range(2)]
    gab = [ar.a(TB, BF16) for _ in range(2)]
    cand = sc.rearrange("p a b -> p (a b)").rearrange("p (h a b) -> p h a b", h=8, a=16)
    svv = sv.rearrange("p (h t) a -> p h t a", t=2)
    sifv = sif.rearrange("p (h t) a -> p h t a", t=2)
    io16 = iota_f[:, 0:16].unsqueeze(1).unsqueeze(1).to_broadcast([128, 8, 16, 16])
    NBLK = L // TB
    wqi = [0]

    def part1(blk):
        par = blk % 3
        hT = h2T[par]
        hk = "h2T%d" % par
        for ts in range(2):
            t0 = blk * TB + ts * 128
            xk = "xs1%d" % ts
            p.dma(xs1[ts], x1_s[t0:t0 + 128, :], r="x1_s", w=xk, e="gpsimd", grp="gx%d" % ts)
            yield "sync"
            p.s("activation", r=xk, w="tmpF ssF", out=junkF, in_=xs1[ts], func=AF.Square, accum_out=ssF[:, 0:1])
            p.v("tensor_scalar", r="ssF", w="rsF", out=rsF[:, 0:1], in0=ssF[:, 0:1], scalar1=1.0 / 1024, scalar2=EPS, op0=ALU.mult, op1=ALU.add)
            p.s("activation", r="rsF", w="rsF", out=rsF[:, 0:1], in_=rsF[:, 0:1], func=AF.Ln)
            p.s("activation", r="rsF", w="rsF", out=rsF[:, 0:1], in_=rsF[:, 0:1], func=AF.Exp, scale=-0.5)
            yield
            p.v("scalar_tensor_tensor", r=xk + " rsF W2", w=xk, out=xs1[ts], in0=xs1[ts], scalar=rsF[:, 0:1], in1=W2, op0=ALU.mult, op1=ALU.mult)
            p.g("tensor_tensor", r=xk + " S2", w="h2", out=h2, in0=xs1[ts], in1=S2, op=ALU.add)
            yield "sync"
            for kc in range(8):
                p.t("transpose", r="h2 ident", w="b2", out=PBb(2)[:, kc * 128:(kc + 1) * 128], in_=h2[:, kc * 128:(kc + 1) * 128], identity=ident)
            yield "sync"
            p.s("copy", r="b2", w=hk, out=hT[:, :, ts * 128:(ts + 1) * 128], in_=PBb(2).rearrange("p (a b) -> p a b", a=8))
            yield
        def wq_load(hp_):
            ws_ = (wqi[0] + hp_) % 3
            p.dma(Wqs[ws_], wqb_s[hp_].rearrange("p (k c) -> p k c", k=8), r="wqb_s", w="Wqs%d" % ws_, e="gpsimd", grp="gwq%d" % ws_)
        wq_load(0)
        wq_load(1)
        yield "sync"
        for hp in range(17):
            if hp >= 1:
                if hp % 2 == 1:
                    p.s("copy", r="b2", w="qT", out=qT[:, hp - 1, :], in_=PB(2)[:, 0:TB])
                else:
                    p.v("tensor_copy", r="b2", w="qT", out=qT[:, hp - 1, :], in_=PB(2)[:, 0:TB])
            if hp < 16:
                ws = (wqi[0] + hp) % 3
                if hp + 2 < 16:
                    wq_load(hp + 2)
                for kc in range(8):
                    p.t("matmul", r=("Wqs%d " % ws) + hk, w="b2", out=PB(2)[:, 0:TB], lhsT=Wqs[ws][:, kc, :], rhs=hT[:, kc, :],
                        start=(kc == 0), stop=(kc == 7))
            yield "sync"
        wqi[0] += 16
        yield "sync"
        for ts in range(2):
            tsl = slice(ts * 128, (ts + 1) * 128)
            for g4 in range(5):
                if g4 >= 1:
                    p.s("copy", r="b2", w="sc", out=sc[:, (g4 - 1) * 4:g4 * 4, :], in_=PB(2).rearrange("p (a b) -> p a b", a=4))
                if g4 < 4:
                    for q4 in range(4):
                        hp = g4 * 4 + q4
                        p.t("matmul", r="qT KT", w="b2", out=PB(2)[:, q4 * 128:(q4 + 1) * 128], lhsT=qT[:, hp, tsl], rhs=KT[:, hp, :],
                            start=True, stop=True)
                yield "sync"
            yield "sync"
            for hp in range(16):
                p.v("max", r="sc", w="sv", out=sv[:, hp, 0:8], in_=sc[:, hp, :])
                p.v("max_index", r="sc sv", w="si", out=si[:, hp, 0:8], in_max=sv[:, hp, 0:8], in_values=sc[:, hp, :])
                p.v("match_replace", r="sc sv", w="wk", out=wk, in_to_replace=sv[:, hp, 0:8], in_values=sc[:, hp, :], imm_value=-1e30)
                yield
                p.v("max", r="wk", w="sv", out=sv[:, hp, 8:16], in_=wk)
                p.v("max_index", r="wk sv", w="si", out=si[:, hp, 8:16], in_max=sv[:, hp, 8:16], in_values=wk)
                yield
            p.v("tensor_copy", r="si", w="sif", out=sif, in_=si)
            p.v("tensor_tensor", r="sv sc", w="sc", out=cand, in0=svv[:, :, 0, :].unsqueeze(3).to_broadcast([128, 8, 16, 16]),
                in1=svv[:, :, 1, :].unsqueeze(2).to_broadcast([128, 8, 16, 16]), op=ALU.add)
            yield
            for h in range(8):
                ch = cand[:, h].rearrange("p a b -> p (a b)")
                p.v("max", r="sc", w="best", out=best[:, h, 0:8], in_=ch)
                p.v("max_index", r="sc best", w="fl", out=fl[:, h, 0:8], in_max=best[:, h, 0:8], in_values=ch)
                p.v("match_replace", r="sc best", w="wk2", out=wk2, in_to_replace=best[:, h, 0:8], in_values=ch, imm_value=-1e30)
                yield
                p.v("max", r="wk2", w="best", out=best[:, h, 8:16], in_=wk2)
                p.v("max_index", r="wk2 best", w="fl", out=fl[:, h, 8:16], in_max=best[:, h, 8:16], in_values=wk2)
                yield
            p.v("tensor_tensor", r="best", w="gt", out=gt, in0=best, in1=best[:, :, 0:1].to_broadcast([128, 8, 16]), op=ALU.subtract)
            yield "sync"
            p.s("activation", r="gt", w="gt", out=gt, in_=gt, func=AF.Exp)
            yield "sync"
            p.v("tensor_reduce", r="gt", w="gs", out=gs, in_=gt, axis=AX.X, op=ALU.add)
            p.v("reciprocal", r="gs", w="gs", out=gs, in_=gs)
            yield
            p.v("tensor_tensor", r="gt gs IG", w="IG", out=IG[:, 2, :].rearrange("p (h j) -> p h j", h=8), in0=gt,
                in1=gs.unsqueeze(2).to_broadcast([128, 8, 16]), op=ALU.mult)
            p.v("tensor_single_scalar", r="fl", w="ai", out=ai, in_=fl, scalar=4, op=ALU.logical_shift_right)
            p.v("tensor_single_scalar", r="fl", w="bi", out=bi, in_=fl, scalar=15, op=ALU.bitwise_and)
            p.v("tensor_copy", r="ai", w="af", out=af, in_=ai)
            p.v("tensor_copy", r="bi", w="bf", out=bf_, in_=bi)
            yield
            for t_, (xf, key) in enumerate(((af, "af"), (bf_, "bf"))):
                p.v("tensor_tensor", r=key + " iota", w="oh", out=oh, in0=xf.unsqueeze(3).to_broadcast([128, 8, 16, 16]), in1=io16, op=ALU.is_equal)
                yield
                p.v("tensor_tensor", r="oh sif", w="oh", out=oh, in0=oh, in1=sifv[:, :, t_, :].unsqueeze(2).to_broadcast([128, 8, 16, 16]), op=ALU.mult)
                yield
                p.v("tensor_reduce", r="oh IG", w="IG", out=IG[:, t_, :].rearrange("p (h j) -> p h j", h=8), in_=oh, axis=AX.X, op=ALU.add)
                yield
            yield "sync"
            for q in range(3):
                p.t("transpose", r="IG identf", w="b2", out=PB(2)[:, q * 128:(q + 1) * 128], in_=IG[:, q, :], identity=ident_f)
            yield "sync"
            p.s("copy", r="b2", w="IGTb%d" % par, out=IGTb[par][:, ts, :, :], in_=PB(2)[:, 0:256].rearrange("p (a b) -> p a b", a=2))
            p.s("copy", r="b2", w="IGTg%d" % par, out=IGTg[par][:, ts, :], in_=PB(2)[:, 256:384])
            yield

    def part2(blk, half):
        par = blk % 3
        kb, kg = "IGTb%d" % par, "IGTg%d" % par
        Gh = GmH[half]
        gk = "Gm%d" % half
        groups = [(ts, tg) for ts in range(2) for tg in range(128 // NG)]
        bufs = {}

        def gen_(n):
            ts, tg = groups[n]
            tq = slice(tg * NG, (tg + 1) * NG)
            ob = ohc[0] % 2
            ohc[0] += 1
            bufs[n] = ob
            ohA, ohB = ohA2[ob], ohB2[ob]
            ka, kbb = "ohA%d" % ob, "ohB%d" % ob
            p.v("tensor_tensor", r=kb + " iota_b", w=ka, out=ohA, in0=iota_b.unsqueeze(1).to_broadcast([128, NG, 128]),
                in1=IGTb[par][:, ts, 0, tq].unsqueeze(2).to_broadcast([128, NG, 128]), op=ALU.is_equal)
            p.v("tensor_tensor", r=kb + " iota_b", w=kbb, out=ohB, in0=iota_b[:, half * 64:(half + 1) * 64].unsqueeze(1).to_broadcast([128, NG, 64]),
                in1=IGTb[par][:, ts, 1, tq].unsqueeze(2).to_broadcast([128, NG, 64]), op=ALU.is_equal)
            p.g("tensor_tensor", r=kg + " " + kbb, w=kbb, out=ohB, in0=ohB, in1=IGTg[par][:, ts, tq].unsqueeze(2).to_broadcast([128, NG, 64]), op=ALU.mult)

        def mm_(m):
            n, t8 = m // 2, m % 2
            ob = bufs[n]
            ohA, ohB = ohA2[ob], ohB2[ob]
            ka, kbb = "ohA%d" % ob, "ohB%d" % ob
            for q8 in range(8):
                tt_ = t8 * 8 + q8
                p.t("matmul", r=ka + " " + kbb, w="b3", out=PB(3)[:, q8 * 64:(q8 + 1) * 64], lhsT=ohA[:, tt_, :], rhs=ohB[:, tt_, :],
                    start=True, stop=True)

        def ev_(m):
            n, t8 = m // 2, m % 2
            ts, tg = groups[n]
            g0 = ts * 128 + tg * NG + t8 * 8
            if m % 2 == 0:
                p.s("copy", r="b3", w=gk, out=Gh[:, g0:g0 + 8, :], in_=PB(3).rearrange("p (a b) -> p a b", a=8))
            else:
                p.v("tensor_copy", r="b3", w=gk, out=Gh[:, g0:g0 + 8, :], in_=PB(3).rearrange("p (a b) -> p a b", a=8))

        ng = len(groups)
        gen_(0)
        yield "sync"
        for m in range(2 * ng + 1):
            if m % 2 == 0 and m // 2 + 1 < ng:
                gen_(m // 2 + 1)
            if m >= 1:
                ev_(m - 1)
            if m < 2 * ng:
                mm_(m)
            yield "sync"

    widx = [0]

    wslot = {}

    def main_step(blk, s_):
        par = blk % 3
        hT = h2T[par]
        if s_ < 128:
            pr, e2 = s_ // 2, s_ % 2
            if e2 == 0:
                sl = widx[0] % 3
                widx[0] += 1
                wslot[pr % 4] = sl
                p.dma(wu[sl], utb_s[2 * pr:2 * pr + 2].rearrange("a p c -> p a c"), r="utb_s", w="wu%d" % sl)
                p.dma(wv[sl], vtb_s[2 * pr:2 * pr + 2].rearrange("a p c -> p a c"), r="vtb_s", w="wv%d" % sl)
            sl = wslot[pr % 4]
            half = s_ // 64
            ab = s_ % 2
            for kc in range(8):
                p.t("matmul", r="wu%d h2T%d" % (sl, par), w="b%d" % ab, out=PB(ab)[:, 0:TB], lhsT=wu[sl][:, e2, kc * 128:(kc + 1) * 128], rhs=hT[:, kc, :],
                    start=(kc == 0), stop=(kc == 7))
            p.s("activation", r="b%d" % ab, w="ga%d" % ab, out=ga[ab], in_=PB(ab)[:, 0:TB], func=AF.Gelu)
            p.v("tensor_tensor", r="ga%d Gm%d" % (ab, half), w="gab%d" % ab, out=gab[ab], in0=ga[ab], in1=GmH[half][:, :, s_ % 64], op=ALU.mult)
        if s_ >= 1:
            o_ = s_ - 1
            pr, e2 = o_ // 2, o_ % 2
            sl = wslot[pr % 4]
            ab = o_ % 2
            for ts in range(2):
                for nb in range(2):
                    bk = 4 + 2 * ts + nb
                    p.t("matmul", r="gab%d wv%d" % (ab, sl), w="b%d" % bk, out=PB(bk), lhsT=gab[ab][:, ts * 128:(ts + 1) * 128],
                        rhs=wv[sl][:, e2, nb * 512:(nb + 1) * 512], start=(o_ == 0), stop=(o_ == 127))

    def finalize(blk):
        for ts in range(2):
            t0 = blk * TB + ts * 128
            xk = "xs1%d" % ts
            p.dma(xs1[ts], x1_s[t0:t0 + 128, :], r="x1_s", w=xk, e="gpsimd", grp="gx%d" % ts)
            for nb in range(2):
                bk = 4 + 2 * ts + nb
                ns = slice(nb * 512, (nb + 1) * 512)
                p.v("tensor_tensor", r="b%d G2" % bk, w="tmpF", out=tmpF, in0=PB(bk), in1=G2[:, ns], op=ALU.mult)
                p.g("tensor_tensor", r="tmpF " + xk, w=xk, out=xs1[ts][:, ns], in0=xs1[ts][:, ns], in1=tmpF, op=ALU.add)
            p.s("activation", r=xk, w="tmpF ssF", out=junkF, in_=xs1[ts], func=AF.Square, accum_out=ssF[:, 1:2])
            p.v("tensor_scalar", r="ssF", w="rsF", out=rsF[:, 1:2], in0=ssF[:, 1:2], scalar1=1.0 / 1024, scalar2=EPS, op0=ALU.mult, op1=ALU.add)
            p.s("activation", r="rsF", w="rsF", out=rsF[:, 1:2], in_=rsF[:, 1:2], func=AF.Ln)
            p.s("activation", r="rsF", w="rsF", out=rsF[:, 1:2], in_=rsF[:, 1:2], func=AF.Exp, scale=-0.5)
            p.v("scalar_tensor_tensor", r=xk + " rsF FW", w=xk, out=xs1[ts], in0=xs1[ts], scalar=rsF[:, 1:2], in1=FW, op0=ALU.mult, op1=ALU.mult)
            p.dma(out_d[t0:t0 + 128, :], xs1[ts], r=xk, w="out", grp="gout%d" % ts, e="gpsimd")

    nblk = 1 if stop == "F1" else (3 if stop == "F2" else NBLK)

    def drain(g):
        if g is not None:
            for _ in g:
                pass

    def drip(g, n):
        if g is None:
            return None
        try:
            for _ in range(n):
                if next(g) == "sync":
                    break
        except StopIteration:
            return None
        return g

    drain(part1(0))
    if nblk > 1:
        drain(part1(1))
    drain(part2(0, 0))
    for blk in range(nblk):
        st = {"g1": part1(blk + 2) if blk + 2 < nblk else None, "gB": part2(blk, 1), "gA": None}

        import os
        DM = os.environ.get("DRIPMODE", "3")
        if DM in ("0", "2"):
            drain(st["g1"]); st["g1"] = None
        if DM in ("0", "1"):
            drain(st["gB"]); st["gB"] = None

        def dr(s_, st=st, blk=blk):
            st["g1"] = drip(st["g1"], 8)
            if s_ < 64:
                st["gB"] = drip(st["gB"], 1)
            elif DM in ("2", "3"):
                st["gA"] = drip(st["gA"], 1)

        for s_ in range(129 if os.environ.get("SKIPMAIN") != "1" else 0):
            main_step(blk, s_)
            if s_ == 63:
                drain(st["gB"])
                st["gB"] = None
                st["gA"] = part2(blk + 1, 0) if blk + 1 < nblk else None
            dr(s_)
        g1, gA = st["g1"], st["gA"]
        drain(g1)
        drain(gA)
        finalize(blk)
    return fin(nc, p, ar, dbg_d)


def fin(nc, p, ar, dbg_d):
    p.finish()
    p.emit()
    return nc, dbg_d


def host_layout(inp, b):
    f = np.float32
    A = np.ascontiguousarray
    d = {}
    d["x"] = A(inp["x"][b])
    d["ctx"] = A(inp["ctx"][b])
    cc = np.stack([inp["c"][b].reshape(8, 128).T, inp["c_ctx"].reshape(8, 128).T], axis=-1)
    d["cc"] = A(cc.astype(f))
    return d


def shared_layout(inp):
    A = np.ascontiguousarray
    d = {}
    d["ada_w"] = A(inp["ada_w"][0].reshape(8, 128, 6144).transpose(1, 0, 2))
    d["ada_b"] = A(inp["ada_b"][0].reshape(1, 6144))
    d["nrm"] = A(np.stack([np.broadcast_to(inp[k].reshape(1, 1024), (128, 1024)) for k in ("norm1_w", "norm2_w", "final_norm_w")]))
    d["w_in"] = A(inp["w_in"][0].reshape(8, 128, 2576).transpose(1, 0, 2))
    d["scw"] = A(inp["ssm_conv_w"][0].reshape(5, 8, 128).transpose(2, 1, 0))
    d["scb"] = A(inp["ssm_conv_b"][0].reshape(8, 128).T)
    d["dtb"] = A(np.broadcast_to(inp["ssm_dt_bias"][0].reshape(1, 16), (128, 16)))
    d["alog"] = A(np.broadcast_to(inp["ssm_a_log"][0].reshape(1, 16), (128, 16)))
    d["drow"] = A(np.broadcast_to(np.repeat(inp["ssm_d"][0], 64).reshape(1, 512), (128, 512)))
    d["snw"] = A(np.broadcast_to(inp["ssm_norm_w"][0].reshape(1, 512), (128, 512)))
    d["ccw"] = A(inp["cfm_conv_w"][0].reshape(31, 4, 128).transpose(2, 1, 0))
    d["cvec"] = A(np.stack([inp[k][0].reshape(4, 128).T for k in ("cfm_conv_b", "cfm_ln_w", "cfm_ln_b")], axis=1))
    d["w_out"] = A(inp["w_out"][0].reshape(8, 128, 1024).transpose(1, 0, 2))
    d["wq"] = A(inp["peer_wq"][0].reshape(8, 128, 2048).transpose(1, 0, 2))
    d["kt"] = A(inp["peer_subkeys"][0].reshape(16, 128, 128).transpose(2, 0, 1))
    d["ut"] = A(inp["peer_u"][0].reshape(128, 128, 8, 128).transpose(1, 3, 2, 0))
    d["vt"] = A(inp["peer_v"][0].reshape(128, 128, 1024).transpose(1, 0, 2))
    return d


_CACHE = {}


def kernel(**inputs):
    inp = {k: np.asarray(v) for k, v in inputs.items()}
    if "nc" not in _CACHE:
        _CACHE["nc"] = build()[0]
    nc = _CACHE["nc"]
    sh = shared_layout(inp)
    in_maps = []
    for b in range(8):
        d = dict(sh)
        d.update(host_layout(inp, b))
        in_maps.append(d)
    res = run_bass_kernel_spmd(nc, in_maps, core_ids=list(range(8)))
    return np.stack([np.asarray(r["out"], dtype=np.float32) for r in res.results], axis=0)
```

```python
import re
import numpy as np
import concourse.bass as bass
import concourse.mybir as mybir
from concourse.bass_utils import run_bass_kernel_spmd

F32 = mybir.dt.float32
BF16 = mybir.dt.bfloat16
I32 = mybir.dt.int32
U32 = mybir.dt.uint32
AF = mybir.ActivationFunctionType
ALU = mybir.AluOpType
AX = mybir.AxisListType
ENGS = ("sync", "scalar", "vector", "gpsimd", "tensor")
CH = 4096
EPS = 1e-6
DSZ = {F32: 4, BF16: 2, I32: 4, U32: 4}


class Prog:
    def __init__(self, nc):
        self.nc = nc
        self.q = {e: [] for e in ENGS}
        self.cnt = {e: 0 for e in ENGS}
        self.known = {e: {} for e in ENGS}
        self.lastw = {}
        self.readers = {}
        self.sems = {}
        self.dmacum = {}
        self.pend = {e: {} for e in ENGS}
        self.lasttok = {}

    def sem(self, key):
        s = self.sems.get(key)
        if s is None:
            s = self.nc.alloc_semaphore(name="s%d" % len(self.sems))
            self.sems[key] = s
        return s

    def _waits(self, e, reads, writes):
        toks = dict(self.pend[e])
        self.pend[e] = {}

        def add(t):
            if t is None:
                return
            k, v = t
            if k[0] == "eng" and k[1] == e and e == "tensor":
                return
            if toks.get(k, 0) < v:
                toks[k] = v
        for r in reads:
            add(self.lastw.get(r))
        for w in writes:
            add(self.lastw.get(w))
            for t in self.readers.get(w, ()):
                add(t)
        out = []
        for k, v in toks.items():
            if self.known[e].get(k, 0) >= v:
                continue
            self.known[e][k] = v
            out.append((self.sem(k), v))
        return out

    def _commit(self, tok, reads, writes):
        self.lasttok[tok[0]] = tok[1]
        for w in writes:
            self.lastw[w] = tok
            self.readers[w] = []
        for r in reads:
            self.readers.setdefault(r, []).append(tok)

    def op(self, e, name, r="", w="", **kw):
        reads, writes = r.split(), w.split()
        ex = [k for k in reads if re.fullmatch(r"(ps|b)\d", k)]
        reads = [k for k in reads if k not in ex]
        writes = writes + [k for k in ex if k not in writes]
        waits = self._waits(e, reads, writes)
        idx = self.cnt[e]
        self.cnt[e] += 1
        k = ("eng", e, idx // CH)
        self.q[e].append((waits, name, kw, self.sem(k), 1))
        self._commit((k, idx % CH + 1), reads, writes)

    def v(self, name, r="", w="", **kw):
        self.op("vector", name, r, w, **kw)

    def s(self, name, r="", w="", **kw):
        self.op("scalar", name, r, w, **kw)

    def g(self, name, r="", w="", **kw):
        self.op("gpsimd", name, r, w, **kw)

    def t(self, name, r="", w="", **kw):
        self.op("tensor", name, r, w, **kw)

    def dma(self, out, in_, r="", w="", e="sync", grp=None, parts=None, **kw):
        reads, writes = r.split(), w.split()
        sk = ("dma", grp if grp is not None else writes[0])
        waits = self._waits(e, reads, writes)
        pieces = parts if parts is not None else [(out, in_)]
        for i, (o, a) in enumerate(pieces):
            self.dmacum[sk] = self.dmacum.get(sk, 0) + 16
            kk = dict(kw)
            kk.update(out=o, in_=a)
            self.q[e].append((waits if i == 0 else [], "dma_start", kk, self.sem(sk), 16))
        self._commit((sk, self.dmacum[sk]), reads, writes)

    def barrier(self):
        for e in ENGS:
            for k, v in self.lasttok.items():
                if self.pend[e].get(k, 0) < v:
                    self.pend[e][k] = v

    def finish(self):
        self.barrier()
        for e in ("sync", "gpsimd"):
            waits = self._waits(e, [], [])
            self.q[e].append((waits, None, None, None, 0))

    def emit(self):
        nc = self.nc
        with nc.Block() as block:
            def mk(e):
                def body(eng):
                    for waits, name, kw, s, inc in self.q[e]:
                        for ws, wv in waits:
                            eng.wait_ge(ws, wv)
                        if name is not None:
                            getattr(eng, name)(**kw).then_inc(s, inc)
                return body
            block.sync(mk("sync"))
            block.scalar(mk("scalar"))
            block.vector(mk("vector"))
            block.gpsimd(mk("gpsimd"))
            block.tensor(mk("tensor"))


class Arena:
    def __init__(self, nc, nbytes):
        self.t = nc.alloc_sbuf_tensor("arena", [128, nbytes // 4], F32)
        self.cap = nbytes
        self.off = 0
        self.peak = 0

    def a(self, free, dt=F32, parts=128):
        if isinstance(free, int):
            free = (free,)
        n = int(np.prod(free)) * DSZ[dt]
        n = (n + 63) // 64 * 64
        assert self.off + n <= self.cap, ("SBUF arena overflow", self.off, n, self.cap)
        ap = self.t[0:parts, self.off // 4:(self.off + n) // 4]
        self.off += n
        self.peak = max(self.peak, self.off)
        if dt != F32:
            ap = ap.bitcast(dt)
        ap = ap[:, 0:int(np.prod(free))]
        if len(free) == 2:
            ap = ap.rearrange("p (a b) -> p a b", a=free[0])
        elif len(free) == 3:
            ap = ap.rearrange("p (a b c) -> p a b c", a=free[0], b=free[1])
        return ap

    def mark(self):
        return self.off

    def release(self, m):
        self.off = m


L = 4096
LC = 256
NT = L // 128


def build(stop=None, dbg=False, npeer=128):
    nc = bass.Bass("TRN2", target_bir_lowering=False)
    D = nc.dram_tensor

    def din(name, shape, dt=F32):
        return D(name, list(shape), dt, kind="ExternalInput").ap()
    x_d = din("x", [L, 1024])
    ctx_d = din("ctx", [LC, 1024])
    cc_d = din("cc", [128, 8, 2])
    adaw_d = din("ada_w", [128, 8, 6144])
    adab_d = din("ada_b", [1, 6144])
    nrm_d = din("nrm", [3, 128, 1024])
    win_d = din("w_in", [128, 8, 2576])
    scw_d = din("scw", [128, 8, 5])
    scb_d = din("scb", [128, 8])
    dtb_d = din("dtb", [128, 16])
    alog_d = din("alog", [128, 16])
    drow_d = din("drow", [128, 512])
    snw_d = din("snw", [128, 512])
    ccw_d = din("ccw", [128, 4, 31])
    cvec_d = din("cvec", [128, 3, 4])
    wout_d = din("w_out", [128, 8, 1024])
    wq_d = din("wq", [128, 8, 2048])
    kt_d = din("kt", [128, 16, 128])
    ut_d = din("ut", [npeer, 128, 8, 128])
    vt_d = din("vt", [npeer, 128, 1024])
    out_d = D("out", [L, 1024], F32, kind="ExternalOutput").ap()
    dbg_d = {}

    def dout(name, shape):
        dbg_d[name] = D("dbg_" + name, list(shape), F32, kind="ExternalOutput").ap()
        return dbg_d[name]
    modb_s = D("modb_s", [8, 128, 1024], F32).ap()
    zs_s = D("zs_s", [L, 512], BF16).ap()
    x1_s = D("x1_s", [L, 1024], F32).ap()
    utb_s = D("utb_s", [128, 128, 1024], BF16).ap()
    vtb_s = D("vtb_s", [128, 128, 1024], BF16).ap()
    uts_s = D("uts_s", [128, 4, L], BF16).ap()
    wqb_s = D("wqb_s", [16, 128, 1024], BF16).ap()

    p = Prog(nc)
    ar = Arena(nc, 212000)
    banks = [nc.alloc_psum_tensor("pb%d" % i, [128, 512], F32) for i in range(8)]

    def PB(i):
        return banks[i][:]

    def PBb(i):
        return banks[i][:].bitcast(BF16)

    ident_f = ar.a(128)
    ident = ar.a(128, BF16)
    iota_f = ar.a(128)
    pidx = ar.a(128)
    ones_f = ar.a(128)
    ones_b = ar.a(128, BF16)
    triU = ar.a(128)
    triS = ar.a(128)
    maskF = ar.a(128, BF16)
    maskB = ar.a(128, BF16)
    p.g("iota", w="iota", out=iota_f, pattern=[[1, 128]], base=0, channel_multiplier=0, allow_small_or_imprecise_dtypes=True)
    p.g("iota", w="pidx", out=pidx, pattern=[[0, 128]], base=0, channel_multiplier=1, allow_small_or_imprecise_dtypes=True)
    p.v("tensor_tensor", r="iota pidx", w="identf", out=ident_f, in0=iota_f, in1=pidx, op=ALU.is_equal)
    p.v("tensor_copy", r="identf", w="ident", out=ident, in_=ident_f)
    p.v("memset", w="ones_f", ap=ones_f, constant=1.0)
    p.v("memset", w="ones_b", ap=ones_b, constant=1.0)
    p.v("tensor_tensor", r="iota pidx", w="triU", out=triU, in0=iota_f, in1=pidx, op=ALU.is_ge)
    p.v("tensor_tensor", r="iota pidx", w="triS", out=triS, in0=iota_f, in1=pidx, op=ALU.is_gt)
    p.v("tensor_scalar", r="triU", w="maskF", out=maskF, in0=triU, scalar1=-1.0, scalar2=30000.0, op0=ALU.add, op1=ALU.mult)
    tmpc = ar.a(128)
    p.v("tensor_tensor", r="iota pidx", w="tmpc", out=tmpc, in0=iota_f, in1=pidx, op=ALU.is_le)
    p.v("tensor_scalar", r="tmpc", w="maskB", out=maskB, in0=tmpc, scalar1=-1.0, scalar2=30000.0, op0=ALU.add, op1=ALU.mult)
    sel = ar.a((16, 128), BF16)
    p.v("memset", w="sel", ap=sel, constant=0.0)
    p.v("tensor_copy", r="identf sel", w="sel", out=sel[0:16], in_=ident_f[0:16, 0:16].unsqueeze(2).to_broadcast([16, 16, 128]))
    scw = ar.a((8, 5)); scb = ar.a(8); dtbias = ar.a(16); alog = ar.a(16); ccw = ar.a((4, 31)); cvec = ar.a((3, 4))
    Aneg = ar.a(16)
    p.dma(scw, scw_d, w="scw", grp="small")
    p.dma(scb, scb_d, w="scb", grp="small")
    p.dma(dtbias, dtb_d, w="dtbias", grp="small")
    p.dma(alog, alog_d, w="alog", grp="small")
    p.dma(ccw, ccw_d, w="ccw", grp="small")
    p.dma(cvec, cvec_d, w="cvec", grp="small")
    p.s("activation", r="alog", w="Aneg", out=Aneg, in_=alog, func=AF.Exp)
    p.v("tensor_scalar", r="Aneg", w="Aneg", out=Aneg, in0=Aneg, scalar1=-1.0, scalar2=None, op0=ALU.mult)
    dt_all = ar.a((NT + 2, 16))
    m_persist = ar.mark()

    m0 = ar.mark()
    cc = ar.a((8, 2)); scT = ar.a((8, 2))
    adab = ar.a(6144, parts=1)
    modrow = ar.a(6144, parts=1)
    modrow_c = ar.a(2048, parts=1)
    awb = [ar.a((8, 512)) for _ in range(2)]
    nrm = [ar.a(1024) for _ in range(2)]
    bct = [ar.a(1024) for _ in range(2)]
    p.dma(cc, cc_d, w="cc")
    p.dma(adab, adab_d, w="adab")
    p.dma(nrm[0], nrm_d[0], w="nrm0")
    p.dma(nrm[1], nrm_d[1], w="nrm1")
    p.s("activation", r="cc", w="scT", out=scT, in_=cc, func=AF.Silu)
    for blk in range(12):
        sl = blk % 2
        p.dma(awb[sl], adaw_d[:, :, blk * 512:(blk + 1) * 512], w="awb%d" % sl)
        for kc in range(8):
            p.t("matmul", r="scT awb%d" % sl, w="ps0", out=PB(0)[0:1, :], lhsT=scT[:, kc, 0:1], rhs=awb[sl][:, kc, :],
                start=(kc == 0), stop=(kc == 7))
        if blk < 4:
            for kc in range(8):
                p.t("matmul", r="scT awb%d" % sl, w="ps1", out=PB(1)[0:1, :], lhsT=scT[:, kc, 1:2], rhs=awb[sl][:, kc, :],
                    start=(kc == 0), stop=(kc == 7))
            p.v("tensor_tensor", r="ps1 adab", w="modrow_c", out=modrow_c[:, blk * 512:(blk + 1) * 512], in0=PB(1)[0:1, :],
                in1=adab[:, blk * 512:(blk + 1) * 512], op=ALU.add)
        p.v("tensor_tensor", r="ps0 adab", w="modrow", out=modrow[:, blk * 512:(blk + 1) * 512], in0=PB(0)[0:1, :],
            in1=adab[:, blk * 512:(blk + 1) * 512], op=ALU.add)
    plan = [(0, modrow, 1, 0), (1, modrow, 0, None), (2, modrow_c, 1, 0), (3, modrow_c, 0, None), (4, modrow, 2, None),
            (5, modrow, 4, 1), (6, modrow, 3, None), (7, modrow, 5, None)]
    for i, (ti, src, mi, ni) in enumerate(plan):
        bt = bct[i % 2]
        bk = "bct%d" % (i % 2)
        for hf in range(2):
            p.t("matmul", r="ones_f modrow modrow_c", w="ps%d" % (2 + hf), out=PB(2 + hf), lhsT=ones_f[0:1, :],
                rhs=src[:, mi * 1024 + hf * 512: mi * 1024 + (hf + 1) * 512], start=True, stop=True)
            if ni is None:
                p.v("tensor_copy", r="ps%d" % (2 + hf), w=bk, out=bt[:, hf * 512:(hf + 1) * 512], in_=PB(2 + hf))
            else:
                p.v("scalar_tensor_tensor", r="ps%d nrm%d" % (2 + hf, ni), w=bk, out=bt[:, hf * 512:(hf + 1) * 512], in0=PB(2 + hf),
                    scalar=1.0, in1=nrm[ni][:, hf * 512:(hf + 1) * 512], op0=ALU.add, op1=ALU.mult)
        p.dma(modb_s[ti], bt, r=bk, w="modb_s", grp="modb_w")
    if dbg:
        p.dma(dout("modrow", [1, 6144]), modrow, r="modrow", w="dbg_modrow")
    ar.release(m0)
    p.barrier()
    if stop == "0":
        return fin(nc, p, ar, dbg_d)

    m_big = ar.mark()
    xbcT = ar.a((8, L), BF16)
    xbcTc = ar.a((8, LC), BF16)
    m_uT = ar.mark()
    uT = ar.a((4, L), BF16)

    mA = ar.mark()
    Wb = ar.a((8, 2576), BF16)
    wst = [ar.a((8, 128)) for _ in range(2)]
    W1 = ar.a(1024); S1 = ar.a(1024)

    def load_mod(i0):
        p.dma(W1, modb_s[i0], r="modb_s", w="modA0", grp="modA")
        p.dma(S1, modb_s[i0 + 1], r="modb_s", w="modA1", grp="modA")
    ncol = 2576
    pieces = [(c0, min(128, ncol - c0)) for c0 in range(0, ncol, 128)]
    for i, (c0, cw) in enumerate(pieces):
        sl = i % 2
        p.dma(wst[sl][:, :, 0:cw], win_d[:, :, c0:c0 + cw], w="wst%d" % sl)
        if i % 2 == 0:
            p.v("tensor_copy", r="wst%d" % sl, w="Wb", out=Wb[:, :, c0:c0 + cw], in_=wst[sl][:, :, 0:cw])
        else:
            p.s("copy", r="wst%d" % sl, w="Wb", out=Wb[:, :, c0:c0 + cw], in_=wst[sl][:, :, 0:cw])
    xt = [ar.a(1024) for _ in range(2)]
    junk = ar.a(1024, BF16)
    hb = [ar.a(1024, BF16) for _ in range(2)]
    hT = ar.a((8, 512), BF16)
    ss = ar.a(2); rs = ar.a(2)
    sig = ar.a(512)
    zsb = [ar.a(512, BF16) for _ in range(2)]
    dtt = ar.a(16)

    def inproj_block(src_d, t0, ntile, Wm, Sm, is_ctx, tile0):
        ntok = ntile * 128
        for ts in range(ntile):
            sl = ts % 2
            p.dma(xt[sl], src_d[t0 + ts * 128:t0 + (ts + 1) * 128, :], w="xt%d" % sl)
            p.s("activation", r="xt%d" % sl, w="junk ss", out=junk, in_=xt[sl], func=AF.Square, accum_out=ss[:, 0:1])
            p.v("tensor_scalar", r="ss", w="rs", out=rs[:, 0:1], in0=ss[:, 0:1], scalar1=1.0 / 1024, scalar2=EPS, op0=ALU.mult, op1=ALU.add)
            p.s("activation", r="rs", w="rs", out=rs[:, 0:1], in_=rs[:, 0:1], func=AF.Ln)
            p.s("activation", r="rs", w="rs", out=rs[:, 0:1], in_=rs[:, 0:1], func=AF.Exp, scale=-0.5)
            p.v("scalar_tensor_tensor", r="xt%d rs modA0" % sl, w="xt%d" % sl, out=xt[sl], in0=xt[sl], scalar=rs[:, 0:1], in1=Wm,
                op0=ALU.mult, op1=ALU.mult)
            p.g("tensor_tensor", r="xt%d modA1" % sl, w="hb%d" % sl, out=hb[sl], in0=xt[sl], in1=Sm, op=ALU.add)
            for kc in range(8):
                p.t("transpose", r="hb%d ident" % sl, w="ps%d" % (4 + sl), out=PBb(4 + sl)[:, kc * 128:(kc + 1) * 128],
                    in_=hb[sl][:, kc * 128:(kc + 1) * 128], identity=ident)
            p.s("copy", r="ps%d" % (4 + sl), w="hT", out=hT[:, :, ts * 128:(ts + 1) * 128],
                in_=PBb(4 + sl).rearrange("p (a b) -> p a b", a=8))
        for j in range(8):
            pk = j % 2
            c0 = 512 + j * 128
            for kc in range(8):
                p.t("matmul", r="Wb hT", w="ps%d" % pk, out=PB(pk)[:, 0:ntok], lhsT=Wb[:, kc, c0:c0 + 128], rhs=hT[:, kc, 0:ntok],
                    start=(kc == 0), stop=(kc == 7))
            dst = xbcTc[:, j, 0:ntok] if is_ctx else xbcT[:, j, t0:t0 + ntok]
            if j % 2 == 0:
                p.s("copy", r="ps%d" % pk, w="xbcT", out=dst, in_=PB(pk)[:, 0:ntok])
            else:
                p.v("tensor_copy", r="ps%d" % pk, w="xbcT", out=dst, in_=PB(pk)[:, 0:ntok])
        if not is_ctx:
            for j in range(4):
                ca = 1552 + j * 128
                cb = 1552 + 512 + j * 128
                for kc in range(8):
                    p.t("matmul", r="Wb hT", w="ps2", out=PB(2)[:, 0:ntok], lhsT=Wb[:, kc, ca:ca + 128], rhs=hT[:, kc, 0:ntok],
                        start=(kc == 0), stop=(kc == 7))
                for kc in range(8):
                    p.t("matmul", r="Wb hT", w="ps3", out=PB(3)[:, 0:ntok], lhsT=Wb[:, kc, cb:cb + 128], rhs=hT[:, kc, 0:ntok],
                        start=(kc == 0), stop=(kc == 7))
                p.s("activation", r="ps3", w="sig", out=sig[:, 0:ntok], in_=PB(3)[:, 0:ntok], func=AF.Sigmoid)
                p.v("tensor_tensor", r="ps2 sig", w="uT", out=uT[:, j, t0:t0 + ntok], in0=PB(2)[:, 0:ntok], in1=sig[:, 0:ntok], op=ALU.mult)
        for ts in range(ntile):
            sl = ts % 2
            if not is_ctx:
                for kc in range(8):
                    p.t("matmul", r="Wb hT", w="ps%d" % (6 + sl), out=PB(6 + sl), lhsT=hT[:, kc, ts * 128:(ts + 1) * 128], rhs=Wb[:, kc, 0:512],
                        start=(kc == 0), stop=(kc == 7))
                p.s("activation", r="ps%d" % (6 + sl), w="zsb%d" % sl, out=zsb[sl], in_=PB(6 + sl), func=AF.Silu)
                p.dma(zs_s[t0 + ts * 128:t0 + (ts + 1) * 128, :], zsb[sl], r="zsb%d" % sl, w="zs_s", e="sync", grp="zsw%d" % sl)
            for kc in range(8):
                p.t("matmul", r="Wb hT", w="ps0", out=PB(0)[:, 0:16], lhsT=hT[:, kc, ts * 128:(ts + 1) * 128], rhs=Wb[:, kc, 1536:1552],
                    start=(kc == 0), stop=(kc == 7))
            p.v("tensor_tensor", r="ps0 dtbias", w="dtt", out=dtt, in0=PB(0)[:, 0:16], in1=dtbias, op=ALU.add)
            p.s("activation", r="dtt", w="dtt", out=dtt, in_=dtt, func=AF.Exp)
            p.s("activation", r="dtt", w="dt_all", out=dt_all[:, tile0 + ts, :], in_=dtt, func=AF.Ln, bias=1.0)

    load_mod(2)
    inproj_block(ctx_d, 0, 2, W1, S1, True, NT)
    if stop == "A1":
        p.dma(dout("dt_all", [128, NT + 2, 16]), dt_all, r="dt_all", w="dbg_dt")
        return fin(nc, p, ar, dbg_d)
    load_mod(0)
    for b in range(L // 512):
        inproj_block(x_d, b * 512, 4, W1, S1, False, b * 4)
    if dbg:
        p.dma(dout("dt_all", [128, NT + 2, 16]), dt_all, r="dt_all", w="dbg_dt")
        dxb = dout("xbc_pre", [128, 8, 128])
        tdb = ar.a((8, 128))
        p.v("tensor_copy", r="xbcT", w="tdb", out=tdb, in_=xbcT[:, :, 512:640])
        p.dma(dxb, tdb, r="tdb", w="dbg_xb")
        dub = dout("u_pre", [128, 4, 128])
        tdb2 = ar.a((4, 128))
        p.v("tensor_copy", r="uT", w="tdb2", out=tdb2, in_=uT[:, :, 512:640])
        p.dma(dub, tdb2, r="tdb2", w="dbg_ub")
    ar.release(mA)
    p.barrier()
    if stop == "A":
        return fin(nc, p, ar, dbg_d)

    mB = ar.mark()
    acc = [ar.a(L) for _ in range(2)]
    EN = [p.v, p.v]

    def ssm_conv(src, n, ei, j, key):
        E = EN[ei]
        ak = "acc%d" % ei
        a_ = acc[ei][:, 0:n]
        x_ = src[:, j, 0:n]
        E("tensor_scalar", r=key + " scw", w=ak, out=a_, in0=x_, scalar1=scw[:, j, 2:3], scalar2=None, op0=ALU.mult)
        for k in (0, 1, 3, 4):
            o = k - 2
            lo, hi = max(0, -o), min(n, n - o)
            E("scalar_tensor_tensor", r=key + " scw " + ak, w=ak, out=a_[:, lo:hi], in0=x_[:, lo + o:hi + o], scalar=scw[:, j, k:k + 1],
              in1=a_[:, lo:hi], op0=ALU.mult, op1=ALU.add)
        p.s("activation", r=ak + " scb", w=key, out=x_, in_=a_, func=AF.Silu, bias=scb[:, j:j + 1])

    tmpc2 = [ar.a(L) for _ in range(2)]

    def cfm_conv(j, ei):
        ak = "acc%d" % ei
        key = "uT%d" % j
        a_ = acc[ei]
        u_ = uT[:, j, :]
        a3 = a_.rearrange("p (r c) -> p r c", c=64)
        u3 = u_.rearrange("p (r c) -> p r c", c=64)
        if ei == 0:
            p.v("tensor_scalar", r=key + " ccw", w=ak, out=a_, in0=u_, scalar1=ccw[:, j, 15:16], scalar2=None, op0=ALU.mult)
        else:
            p.s("activation", r=key + " ccw", w=ak, out=a_, in_=u_, func=AF.Copy, scale=ccw[:, j, 15:16])
        n = 0
        for k in range(31):
            o = k - 15
            if o == 0:
                continue
            lo, hi = max(0, -o), min(64, 64 - o)
            if j < 2:
                oo, i0 = a3[:, :, lo:hi], u3[:, :, lo + o:hi + o]
            else:
                oo, i0 = a_[:, lo * 64:hi * 64], u_[:, (lo + o) * 64:(hi + o) * 64]
            if ei == 0:
                p.v("scalar_tensor_tensor", r=key + " ccw " + ak, w=ak, out=oo, in0=i0, scalar=ccw[:, j, k:k + 1], in1=oo, op0=ALU.mult, op1=ALU.add)
            else:
                tk = "tmpc2%d" % (n % 2)
                tt = tmpc2[n % 2]
                if j < 2:
                    t_ = tt.rearrange("p (r c) -> p r c", c=64)[:, :, lo:hi]
                else:
                    t_ = tt[:, lo * 64:hi * 64]
                p.s("activation", r=key + " ccw", w=tk, out=t_, in_=i0, func=AF.Copy, scale=ccw[:, j, k:k + 1])
                p.g("tensor_tensor", r=tk + " " + ak, w=ak, out=oo, in0=oo, in1=t_, op=ALU.add)
                n += 1
        p.s("activation", r=ak + " cvec", w=key, out=u_, in_=a_, func=AF.Identity, bias=cvec[:, 0, j:j + 1])

    import os
    NCD = int(os.environ.get("CFMDVE", "4"))
    for j in range(8):
        ssm_conv(xbcTc, LC, 0, j, "xbcTc%d" % j)
    pool_chunks = [2, 3, 0, 1][:4 - NCD]
    dve_chunks = [2, 3, 0, 1][4 - NCD:]
    if pool_chunks:
        cfm_conv(pool_chunks[0], 1)
    for j in range(8):
        ssm_conv(xbcT, L, 0, j, "xbcT%d" % j)
    for j in pool_chunks[1:]:
        cfm_conv(j, 1)
    for j in dve_chunks:
        cfm_conv(j, 0)
    if dbg:
        tdb = ar.a((8, 128))
        p.v("tensor_copy", r=" ".join("xbcT%d" % j for j in range(8)), w="tdb", out=tdb, in_=xbcT[:, :, 512:640])
        p.dma(dout("xbc_post", [128, 8, 128]), tdb, r="tdb", w="dbg_xb2")
        tdb2 = ar.a((4, 128))
        p.v("tensor_copy", r=" ".join("uT%d" % j for j in range(4)), w="tdb2", out=tdb2, in_=uT[:, :, 512:640])
        p.dma(dout("u_post", [128, 4, 128]), tdb2, r="tdb2", w="dbg_ub2")
    for j in range(4):
        p.dma(uts_s[:, j, :], uT[:, j, :], r="uT%d" % j, w="uts_s", grp="utsw")
    ar.release(m_uT)
    p.barrier()
    if stop == "B":
        return fin(nc, p, ar, dbg_d)

    mE = ar.mark()
    SB_all = ar.a((NT, 512), BF16)
    Wo = ar.a((8, 1024), BF16)
    G1 = ar.a(1024); NW = ar.a(512); Drow = ar.a(512)
    p.dma(G1, modb_s[4], r="modb_s", w="G1")
    p.dma(NW, snw_d, w="NW")
    p.dma(Drow, drow_d, w="Drow")
    a16 = ar.a(16); cs = ar.a(32); dec = ar.a(16); tmp16 = ar.a(16); w16 = ar.a(16)
    rowsrc = ar.a(16); bias16 = ar.a(16); e16 = ar.a(16); RT = ar.a(128); RT3 = ar.a((3, 128), BF16); RTr = ar.a(128)
    xs_tok = ar.a(512, BF16); Btok = ar.a(256, BF16)
    xw = [ar.a(512, BF16) for _ in range(2)]
    xdt = [ar.a(512, BF16) for _ in range(2)]
    ST_sb = ar.a((2, 128))
    LT = [ar.a(128) for _ in range(2)]
    SmT = ar.a((16, 128), BF16)
    Sst = [ar.a(512) for _ in range(2)]
    Sbf = [ar.a(512, BF16) for _ in range(2)]
    yc = ar.a(512); yc2 = ar.a(512); zst = ar.a(512, BF16); gg = ar.a(512)
    ss2 = ar.a(2); rs2 = ar.a(2)
    ssm_tok = ar.a(512, BF16); catT = ar.a((8, 128), BF16)
    vsq = ar.a((4, 128)); vsqh = ar.a((4, 128), BF16); vsql = ar.a((4, 128), BF16); mean = ar.a(128); msq = ar.a(128); var = ar.a(128); ntmp = ar.a((4, 128))
    xe = [ar.a(1024) for _ in range(2)]
    for i in range(8):
        sl = i % 2
        wv_ = xe[sl].rearrange("p (a b) -> p a b", a=8)
        p.dma(wv_, wout_d[:, :, i * 128:(i + 1) * 128], w="xe%d" % sl)
        if i % 2 == 0:
            p.v("tensor_copy", r="xe%d" % sl, w="Wo", out=Wo[:, :, i * 128:(i + 1) * 128], in_=wv_)
        else:
            p.s("copy", r="xe%d" % sl, w="Wo", out=Wo[:, :, i * 128:(i + 1) * 128], in_=wv_)
    junk2 = ar.a(256, BF16)
    uTt = [ar.a((4, 128), BF16) for _ in range(2)]
    PT = PBb(0)
    PC = PB(1)
    PK = PB(2)
    PY = PB(3)
    PLS = PB(6)
    PM = PBb(7)
    PM2 = PB(7)

    def bc8(ap8):
        return ap8.unsqueeze(2).to_broadcast([128, 8, 64])

    def v3(ap512):
        return ap512.rearrange("p (h q) -> p h q", h=8)

    def prep(is_ctx, c, dirs):
        src = xbcTc if is_ctx else xbcT
        kx = "xbcTc%d" if is_ctx else "xbcT%d"
        tl = (NT + c) if is_ctx else c
        tok = slice(c * 128, (c + 1) * 128)
        p.v("tensor_tensor", r="dt_all Aneg", w="a16", out=a16, in0=dt_all[:, tl, :], in1=Aneg, op=ALU.mult)
        p.t("matmul", r="triU a16", w="b1", out=PC[:, 0:8], lhsT=triU, rhs=a16[:, 0:8], start=True, stop=True)
        p.t("matmul", r="triS a16", w="b1", out=PC[:, 8:16], lhsT=triS, rhs=a16[:, 8:16], start=True, stop=True)
        p.t("matmul", r="ones_f a16", w="b1", out=PC[:, 16:32], lhsT=ones_f, rhs=a16, start=True, stop=True)
        p.s("copy", r="b1", w="cs", out=cs, in_=PC[:, 0:32])
        p.s("activation", r="cs", w="dec", out=dec, in_=cs[:, 16:32], func=AF.Exp)
        p.v("tensor_tensor", r="cs", w="tmp16", out=tmp16[:, 0:8], in0=cs[:, 16:24], in1=cs[:, 0:8], op=ALU.subtract)
        p.v("tensor_copy", r="cs tmp16", w="tmp16", out=tmp16[:, 8:16], in_=cs[:, 8:16])
        p.s("activation", r="tmp16", w="tmp16", out=tmp16, in_=tmp16, func=AF.Exp)
        p.v("tensor_tensor", r="tmp16 dt_all", w="w16", out=w16, in0=tmp16, in1=dt_all[:, tl, :], op=ALU.mult)
        for j in range(4):
            p.t("transpose", r=(kx % j) + " ident", w="b0", out=PT[:, j * 128:(j + 1) * 128], in_=src[:, j, tok], identity=ident)
        for g in range(2):
            p.t("transpose", r=(kx % (4 + g)) + " ident", w="b0", out=PT[:, 512 + g * 128:512 + (g + 1) * 128], in_=src[:, 4 + g, tok], identity=ident)
        p.v("tensor_copy", r="b0", w="Btok", out=Btok, in_=PT[:, 512:768])
        for d in dirs:
            p.v("tensor_tensor", r="b0 w16", w="xw%d" % d, out=v3(xw[d]), in0=v3(PT[:, 0:512]), in1=bc8(w16[:, d * 8:(d + 1) * 8]), op=ALU.mult)

    def local_states(d):
        for h in range(8):
            g = h // 4
            p.t("matmul", r="Btok xw%d" % d, w="b6", out=PLS[:, h * 64:(h + 1) * 64], lhsT=Btok[:, g * 128:(g + 1) * 128],
                rhs=xw[d][:, h * 64:(h + 1) * 64], start=True, stop=True)

    def upd(d):
        p.v("tensor_tensor", r="S%d dec" % d, w="S%d" % d, out=v3(Sst[d]), in0=v3(Sst[d]), in1=bc8(dec[:, d * 8:(d + 1) * 8]), op=ALU.mult)
        p.v("tensor_tensor", r="S%d b6" % d, w="S%d" % d, out=Sst[d], in0=Sst[d], in1=PLS, op=ALU.add)
        p.s("copy", r="S%d" % d, w="Sbf%d" % d, out=Sbf[d], in_=Sst[d])

    p.v("memset", w="RT3", ap=RT3, constant=0.0)
    for d in range(2):
        p.v("memset", w="S%d" % d, ap=Sst[d], constant=0.0)
        p.v("memset", w="Sbf%d" % d, ap=Sbf[d], constant=0.0)
    prep(True, 0, (0,)); local_states(0); upd(0)
    prep(True, 1, (0, 1)); local_states(0); upd(0); local_states(1); upd(1)
    prep(True, 0, (1,)); local_states(1); upd(1)
    if stop == "C":
        p.dma(dout("S_f", [128, 512]), Sst[0], r="S0", w="dbg_sf")
        p.dma(dout("S_b", [128, 512]), Sst[1], r="S1", w="dbg_sb")
        return fin(nc, p, ar, dbg_d)
    for c in range(NT - 1, -1, -1):
        p.s("copy", r="Sbf1", w="SB%d" % c, out=SB_all[:, c, :], in_=Sbf[1])
        prep(False, c, (1,)); local_states(1); upd(1)
        if npeer == 128:
            if c >= 16:
                hp_ = c - 16
                p.dma(wqb_s[hp_].rearrange("p (k c) -> p k c", k=8), wq_d[:, :, hp_ * 128:(hp_ + 1) * 128], w="wqb_s", e="gpsimd", grp="castq")
            for i2 in range(c * 4, c * 4 + 4):
                p.dma(utb_s[i2], ut_d[i2].rearrange("p k i -> p (k i)"), w="utb_s", e="gpsimd", grp="castu")
                p.dma(vtb_s[i2], vt_d[i2], w="vtb_s", e="gpsimd", grp="castv")
    if dbg:
        p.dma(dout("S_b0", [128, 512]), Sst[1], r="S1", w="dbg_sb0")
    if stop == "D":
        return fin(nc, p, ar, dbg_d)
    for c in range(NT):
        tok = slice(c * 128, (c + 1) * 128)
        pe = c % 2
        p.dma(xe[pe], x_d[tok, :], w="xe%d" % pe)
        p.dma(zst, zs_s[tok, :], r="zs_s", w="zst")
        p.dma(uTt[pe], uts_s[:, :, tok], r="uts_s", w="uTt%d" % pe)
        uTc = uTt[pe]
        if stop == "E0":
            return fin(nc, p, ar, dbg_d)
        prep(False, c, (0,))
        if stop == "E1":
            return fin(nc, p, ar, dbg_d)
        p.v("tensor_copy", r="b0", w="xs_tok", out=xs_tok, in_=PT[:, 0:512])
        for d in range(2):
            p.v("tensor_tensor", r="b0 dt_all", w="xdt%d" % d, out=v3(xdt[d]), in0=v3(PT[:, 0:512]),
                in1=bc8(dt_all[:, c, d * 8:(d + 1) * 8]), op=ALU.mult)
        if stop == "E1a":
            return fin(nc, p, ar, dbg_d)
        for g in range(2):
            p.t("matmul", r="xbcT%d xbcT%d" % (4 + g, 6 + g), w="b1", out=PC[:, 256 + g * 128:256 + (g + 1) * 128],
                lhsT=xbcT[:, 4 + g, tok], rhs=xbcT[:, 6 + g, tok], start=True, stop=True)
        if stop == "E1b":
            return fin(nc, p, ar, dbg_d)
        p.s("copy", r="b1", w="ST_sb", out=ST_sb, in_=PC[:, 256:512].rearrange("p (g l) -> p g l", g=2))
        if stop == "E2":
            return fin(nc, p, ar, dbg_d)

        p.v("tensor_copy", r="cs", w="rowsrc", out=rowsrc[:, 0:8], in_=cs[:, 0:8])
        p.v("tensor_scalar", r="cs rowsrc", w="rowsrc", out=rowsrc[:, 8:16], in0=cs[:, 8:16], scalar1=-1.0, scalar2=None, op0=ALU.mult)
        p.v("tensor_scalar", r="rowsrc", w="bias16", out=bias16, in0=rowsrc, scalar1=-1.0, scalar2=None, op0=ALU.mult)
        p.t("transpose", r="rowsrc identf", w="b1", out=PC[0:16, 32:160], in_=rowsrc, identity=ident_f)
        p.s("copy", r="b1", w="RT", out=RT[0:16, :], in_=PC[0:16, 32:160])
        p.v("tensor_copy", r="RT", w="RT3", out=RT3[0:16, 0, :], in_=RT[0:16, :])
        p.v("tensor_tensor", r="RT RT3", w="RTr", out=RTr[0:16, :], in0=RT[0:16, :], in1=RT3[0:16, 0, :], op=ALU.subtract)
        p.v("tensor_copy", r="RTr RT3", w="RT3", out=RT3[0:16, 1, :], in_=RTr[0:16, :])
        p.v("tensor_tensor", r="RTr RT3", w="RTr", out=RTr[0:16, :], in0=RTr[0:16, :], in1=RT3[0:16, 1, :], op=ALU.subtract)
        p.v("tensor_copy", r="RTr RT3", w="RT3", out=RT3[0:16, 2, :], in_=RTr[0:16, :])
        if stop == "E3":
            return fin(nc, p, ar, dbg_d)

        p.v("tensor_copy", r="cs", w="e16", out=e16[:, 0:8], in_=cs[:, 0:8])
        p.v("tensor_tensor", r="cs e16", w="e16", out=e16[:, 8:16], in0=cs[:, 24:32], in1=cs[:, 8:16], op=ALU.subtract)
        p.s("activation", r="e16", w="e16", out=e16, in_=e16, func=AF.Exp)
        if stop == "E4":
            return fin(nc, p, ar, dbg_d)

        for idx in range(16):
            d, h = idx // 8, idx % 8
            g = h // 4
            pk = "b2" if idx % 2 == 0 else "b6"
            PKs = (PB(2) if idx % 2 == 0 else PB(6))[:, 0:128]
            for q in range(3):
                p.t("matmul", r="sel RT3", w=pk, out=PKs, lhsT=sel[:, idx, :], rhs=RT3[:, q, :], start=(q == 0), stop=False)
            p.t("matmul", r="ident maskF maskB", w=pk, out=PKs, lhsT=ident, rhs=(maskF if d == 0 else maskB), start=False, stop=True)
            lk = "LT%d" % (idx % 2)
            p.s("activation", r=pk + " bias16", w=lk, out=LT[idx % 2], in_=PKs, func=AF.Exp, bias=bias16[:, idx:idx + 1])
            p.v("tensor_tensor", r=lk + " ST_sb", w="SmT%d" % idx, out=SmT[:, idx, :], in0=LT[idx % 2], in1=ST_sb[:, g, :], op=ALU.mult)
        if stop == "E5a":
            return fin(nc, p, ar, dbg_d)
        for h in range(8):
            hs = slice(h * 64, (h + 1) * 64)
            p.t("matmul", r="SmT%d xdt0" % h, w="b3", out=PY[:, hs], lhsT=SmT[:, h, :], rhs=xdt[0][:, hs], start=True, stop=False)
            p.t("matmul", r="SmT%d xdt1" % (8 + h), w="b3", out=PY[:, hs], lhsT=SmT[:, 8 + h, :], rhs=xdt[1][:, hs], start=False, stop=True)
        for h in range(8):
            g = h // 4
            hs = slice(h * 64, (h + 1) * 64)
            p.t("matmul", r="xbcT%d Sbf0" % (6 + g), w="b4", out=PB(4)[:, hs], lhsT=xbcT[:, 6 + g, tok], rhs=Sbf[0][:, hs], start=True, stop=True)
            p.t("matmul", r="xbcT%d SB%d" % (6 + g, c), w="b5", out=PB(5)[:, hs], lhsT=xbcT[:, 6 + g, tok], rhs=SB_all[:, c, hs], start=True, stop=True)
        if stop == "E5b":
            return fin(nc, p, ar, dbg_d)
        p.v("tensor_tensor", r="b4 e16", w="yc", out=v3(yc), in0=v3(PB(4)), in1=bc8(e16[:, 0:8]), op=ALU.mult)
        p.v("tensor_tensor", r="b5 e16", w="yc2", out=v3(yc2), in0=v3(PB(5)), in1=bc8(e16[:, 8:16]), op=ALU.mult)
        p.g("tensor_tensor", r="yc yc2", w="yc", out=yc, in0=yc, in1=yc2, op=ALU.add)
        if stop == "E6":
            return fin(nc, p, ar, dbg_d)

        p.v("tensor_tensor", r="yc b3", w="yc", out=yc, in0=yc, in1=PY, op=ALU.add)
        p.g("tensor_tensor", r="xs_tok Drow", w="yc2", out=yc2, in0=xs_tok, in1=Drow, op=ALU.mult)
        p.v("tensor_tensor", r="yc yc2", w="yc", out=yc, in0=yc, in1=yc2, op=ALU.add)
        p.v("tensor_tensor", r="yc zst", w="gg", out=gg, in0=yc, in1=zst, op=ALU.mult)
        for g in range(2):
            p.s("activation", r="gg", w="junk2 ss2", out=junk2, in_=gg[:, g * 256:(g + 1) * 256], func=AF.Square, accum_out=ss2[:, g:g + 1])
        p.v("tensor_scalar", r="ss2", w="rs2", out=rs2, in0=ss2, scalar1=1.0 / 256, scalar2=EPS, op0=ALU.mult, op1=ALU.add)
        p.s("activation", r="rs2", w="rs2", out=rs2, in_=rs2, func=AF.Ln)
        p.s("activation", r="rs2", w="rs2", out=rs2, in_=rs2, func=AF.Exp, scale=-0.5)
        for g in range(2):
            gs = slice(g * 256, (g + 1) * 256)
            p.v("scalar_tensor_tensor", r="gg rs2 NW", w="ssm_tok", out=ssm_tok[:, gs], in0=gg[:, gs], scalar=rs2[:, g:g + 1], in1=NW[:, gs],
                op0=ALU.mult, op1=ALU.mult)
        for j in range(4):
            p.t("transpose", r="ssm_tok ident", w="b7", out=PM[:, j * 128:(j + 1) * 128], in_=ssm_tok[:, j * 128:(j + 1) * 128], identity=ident)
        p.s("copy", r="b7", w="catT", out=catT[:, 0:4, :], in_=PM[:, 0:512].rearrange("p (a b) -> p a b", a=4))
        if stop == "E7":
            return fin(nc, p, ar, dbg_d)

        ukeys = "uTt%d" % pe
        p.s("activation", r=ukeys, w="vsq", out=vsq, in_=uTc, func=AF.Square)
        for j in range(4):
            p.t("matmul", r="ones_b " + ukeys, w="b7", out=PM2[:, 256:384], lhsT=ones_b, rhs=uTc[:, j, :], start=(j == 0), stop=(j == 3))
        p.v("tensor_copy", r="vsq", w="vsqh", out=vsqh, in_=vsq)
        p.v("tensor_tensor", r="vsq vsqh", w="vsql", out=vsql, in0=vsq, in1=vsqh, op=ALU.subtract)
        for j in range(4):
            p.t("matmul", r="ones_b vsqh", w="b7", out=PM2[:, 384:512], lhsT=ones_b, rhs=vsqh[:, j, :], start=(j == 0), stop=False)
            p.t("matmul", r="ones_b vsql", w="b7", out=PM2[:, 384:512], lhsT=ones_b, rhs=vsql[:, j, :], start=False, stop=(j == 3))
        p.s("activation", r="b7", w="mean", out=mean, in_=PM2[:, 256:384], func=AF.Copy, scale=1.0 / 512)
        p.s("activation", r="b7", w="var", out=var, in_=PM2[:, 384:512], func=AF.Copy, scale=1.0 / 512)
        p.g("tensor_tensor", r="mean", w="msq", out=msq, in0=mean, in1=mean, op=ALU.mult)
        p.v("tensor_tensor", r="var msq", w="var", out=var, in0=var, in1=msq, op=ALU.subtract)
        p.v("tensor_scalar", r="var", w="var", out=var, in0=var, scalar1=EPS, scalar2=None, op0=ALU.add)
        p.s("activation", r="var", w="var", out=var, in_=var, func=AF.Ln)
        p.s("activation", r="var", w="var", out=var, in_=var, func=AF.Exp, scale=-0.5)
        p.v("tensor_tensor", r=ukeys + " mean", w="ntmp", out=ntmp, in0=uTc, in1=mean.unsqueeze(1).to_broadcast([128, 4, 128]), op=ALU.subtract)
        p.v("tensor_tensor", r="ntmp var", w="ntmp", out=ntmp, in0=ntmp, in1=var.unsqueeze(1).to_broadcast([128, 4, 128]), op=ALU.mult)
        for j in range(4):
            p.s("activation", r="ntmp cvec", w="catT", out=catT[:, 4 + j, :], in_=ntmp[:, j, :], func=AF.Silu, scale=cvec[:, 1, j:j + 1],
                bias=cvec[:, 2, j:j + 1])
        for nb in range(2):
            for kc in range(8):
                p.t("matmul", r="catT Wo", w="b%d" % (4 + nb), out=PB(4 + nb), lhsT=catT[:, kc, :], rhs=Wo[:, kc, nb * 512:(nb + 1) * 512],
                    start=(kc == 0), stop=(kc == 7))
            ns = slice(nb * 512, (nb + 1) * 512)
            tq, tk_ = (yc2, "yc2") if nb == 0 else (gg, "gg")
            p.v("tensor_tensor", r="b%d G1" % (4 + nb), w=tk_, out=tq, in0=PB(4 + nb), in1=G1[:, ns], op=ALU.mult)
            p.g("tensor_tensor", r=tk_ + " xe%d" % pe, w="xe%d" % pe, out=xe[pe][:, ns], in0=xe[pe][:, ns], in1=tq, op=ALU.add)
        p.dma(x1_s[tok, :], xe[pe], r="xe%d" % pe, w="x1_s", grp="x1w%d" % pe)
        local_states(0)
        upd(0)
    if dbg:
        dx1 = dout("x1", [L, 1024])
        p.dma(None, None, r="x1_s", w="dbg_x1", parts=[(dx1[i * 128:(i + 1) * 128, :], x1_s[i * 128:(i + 1) * 128, :]) for i in range(NT)])
    ar.release(mE)
    p.barrier()
    if stop == "E":
        return fin(nc, p, ar, dbg_d)

    ar.release(m_big)
    TB = 256
    KT = ar.a((16, 128), BF16)
    W2 = ar.a(1024); S2 = ar.a(1024); G2 = ar.a(1024); FW = ar.a(1024)
    p.dma(W2, modb_s[5], r="modb_s", w="W2")
    p.dma(S2, modb_s[6], r="modb_s", w="S2")
    p.dma(G2, modb_s[7], r="modb_s", w="G2")
    p.dma(FW, nrm_d[2], w="FW")
    xs1 = [ar.a(1024) for _ in range(2)]
    for i in range(4):
        sl = i % 2
        wv_ = xs1[sl][:, 0:512].rearrange("p (a b) -> p a b", a=4)
        p.dma(wv_, kt_d[:, i * 4:(i + 1) * 4, :], w="xs1%d" % sl)
        p.v("tensor_copy", r="xs1%d" % sl, w="KT", out=KT[:, i * 4:(i + 1) * 4, :], in_=wv_)
    iota_b = ar.a(128, BF16)
    p.v("tensor_copy", r="iota", w="iota_b", out=iota_b, in_=iota_f)
    Wqs = [ar.a((8, 128), BF16) for _ in range(3)]
    tmpF = ar.a(512)
    junkF = tmpF.bitcast(BF16)
    ssF = ar.a(2); rsF = ar.a(2)
    h2 = ar.a(1024, BF16)
    h2T = [ar.a((8, TB), BF16) for _ in range(3)]
    qT = ar.a((16, TB), BF16)
    sc = ar.a((16, 128))
    sv = ar.a((16, 16)); si = ar.a((16, 16), U32); sif = ar.a((16, 16)); wk = ar.a(128); wk2 = ar.a(256)
    best = ar.a((8, 16)); fl = ar.a((8, 16), U32); ai = ar.a((8, 16), U32); bi = ar.a((8, 16), U32)
    af = ar.a((8, 16)); bf_ = ar.a((8, 16)); gt = ar.a((8, 16)); gs = ar.a(8)
    IG = ar.a((3, 128))
    IGTb = [ar.a((2, 2, 128), BF16) for _ in range(3)]
    IGTg = [ar.a((2, 128)) for _ in range(3)]
    NG = 16
    ohA2 = [ar.a((NG, 128), BF16) for _ in range(2)]
    ohB2 = [ar.a((NG, 64), BF16) for _ in range(2)]
    ohc = [0]
    oh = ar.a((8, 16, 16))
    GmH = [ar.a((TB, 64), BF16) for _ in range(2)]
    wu = [ar.a((2, 1024), BF16) for _ in range(3)]
    wv = [ar.a((2, 1024), BF16) for _ in range(3)]
    ga = [ar.a(TB) for _ in range(2)]
    gab = [ar.a(TB, BF16) for _ in range(2)]
    cand = sc.rearrange("p a b -> p (a b)").rearrange("p (h a b) -> p h a b", h=8, a=16)
    svv = sv.rearrange("p (h t) a -> p h t a", t=2)
    sifv = sif.rearrange("p (h t) a -> p h t a", t=2)
    io16 = iota_f[:, 0:16].unsqueeze(1).unsqueeze(1).to_broadcast([128, 8, 16, 16])
    NBLK = L // TB
    wqi = [0]

    def part1(blk):
        par = blk % 3
        hT = h2T[par]
        hk = "h2T%d" % par
        for ts in range(2):
            t0 = blk * TB + ts * 128
            xk = "xs1%d" % ts
            p.dma(xs1[ts], x1_s[t0:t0 + 128, :], r="x1_s", w=xk, e="gpsimd", grp="gx%d" % ts)
            yield "sync"
            p.s("activation", r=xk, w="tmpF ssF", out=junkF, in_=xs1[ts], func=AF.Square, accum_out=ssF[:, 0:1])
            p.v("tensor_scalar", r="ssF", w="rsF", out=rsF[:, 0:1], in0=ssF[:, 0:1], scalar1=1.0 / 1024, scalar2=EPS, op0=ALU.mult, op1=ALU.add)
            p.s("activation", r="rsF", w="rsF", out=rsF[:, 0:1], in_=rsF[:, 0:1], func=AF.Ln)
            p.s("activation", r="rsF", w="rsF", out=rsF[:, 0:1], in_=rsF[:, 0:1], func=AF.Exp, scale=-0.5)
            yield
            p.v("scalar_tensor_tensor", r=xk + " rsF W2", w=xk, out=xs1[ts], in0=xs1[ts], scalar=rsF[:, 0:1], in1=W2, op0=ALU.mult, op1=ALU.mult)
            p.g("tensor_tensor", r=xk + " S2", w="h2", out=h2, in0=xs1[ts], in1=S2, op=ALU.add)
            yield "sync"
            for kc in range(8):
                p.t("transpose", r="h2 ident", w="b2", out=PBb(2)[:, kc * 128:(kc + 1) * 128], in_=h2[:, kc * 128:(kc + 1) * 128], identity=ident)
            yield "sync"
            p.s("copy", r="b2", w=hk, out=hT[:, :, ts * 128:(ts + 1) * 128], in_=PBb(2).rearrange("p (a b) -> p a b", a=8))
            yield
        def wq_load(hp_):
            ws_ = (wqi[0] + hp_) % 3
            p.dma(Wqs[ws_], wqb_s[hp_].rearrange("p (k c) -> p k c", k=8), r="wqb_s", w="Wqs%d" % ws_, e="gpsimd", grp="gwq%d" % ws_)
        wq_load(0)
        wq_load(1)
        yield "sync"
        for hp in range(17):
            if hp >= 1:
                if hp % 2 == 1:
                    p.s("copy", r="b2", w="qT", out=qT[:, hp - 1, :], in_=PB(2)[:, 0:TB])
                else:
                    p.v("tensor_copy", r="b2", w="qT", out=qT[:, hp - 1, :], in_=PB(2)[:, 0:TB])
            if hp < 16:
                ws = (wqi[0] + hp) % 3
                if hp + 2 < 16:
                    wq_load(hp + 2)
                for kc in range(8):
                    p.t("matmul", r=("Wqs%d " % ws) + hk, w="b2", out=PB(2)[:, 0:TB], lhsT=Wqs[ws][:, kc, :], rhs=hT[:, kc, :],
                        start=(kc == 0), stop=(kc == 7))
            yield "sync"
        wqi[0] += 16
        yield "sync"
        for ts in range(2):
            tsl = slice(ts * 128, (ts + 1) * 128)
            for g4 in range(5):
                if g4 >= 1:
                    p.s("copy", r="b2", w="sc", out=sc[:, (g4 - 1) * 4:g4 * 4, :], in_=PB(2).rearrange("p (a b) -> p a b", a=4))
                if g4 < 4:
                    for q4 in range(4):
                        hp = g4 * 4 + q4
                        p.t("matmul", r="qT KT", w="b2", out=PB(2)[:, q4 * 128:(q4 + 1) * 128], lhsT=qT[:, hp, tsl], rhs=KT[:, hp, :],
                            start=True, stop=True)
                yield "sync"
            yield "sync"
            for hp in range(16):
                p.v("max", r="sc", w="sv", out=sv[:, hp, 0:8], in_=sc[:, hp, :])
                p.v("max_index", r="sc sv", w="si", out=si[:, hp, 0:8], in_max=sv[:, hp, 0:8], in_values=sc[:, hp, :])
                p.v("match_replace", r="sc sv", w="wk", out=wk, in_to_replace=sv[:, hp, 0:8], in_values=sc[:, hp, :], imm_value=-1e30)
                yield
                p.v("max", r="wk", w="sv", out=sv[:, hp, 8:16], in_=wk)
                p.v("max_index", r="wk sv", w="si", out=si[:, hp, 8:16], in_max=sv[:, hp, 8:16], in_values=wk)
                yield
            p.v("tensor_copy", r="si", w="sif", out=sif, in_=si)
            p.v("tensor_tensor", r="sv sc", w="sc", out=cand, in0=svv[:, :, 0, :].unsqueeze(3).to_broadcast([128, 8, 16, 16]),
                in1=svv[:, :, 1, :].unsqueeze(2).to_broadcast([128, 8, 16, 16]), op=ALU.add)
            yield
            for h in range(8):
                ch = cand[:, h].rearrange("p a b -> p (a b)")
                p.v("max", r="sc", w="best", out=best[:, h, 0:8], in_=ch)
                p.v("max_index", r="sc best", w="fl", out=fl[:, h, 0:8], in_max=best[:, h, 0:8], in_values=ch)
                p.v("match_replace", r="sc best", w="wk2", out=wk2, in_to_replace=best[:, h, 0:8], in_values=ch, imm_value=-1e30)
                yield
                p.v("max", r="wk2", w="best", out=best[:, h, 8:16], in_=wk2)
                p.v("max_index", r="wk2 best", w="fl", out=fl[:, h, 8:16], in_max=best[:, h, 8:16], in_values=wk2)
                yield
            p.v("tensor_tensor", r="best", w="gt", out=gt, in0=best, in1=best[:, :, 0:1].to_broadcast([128, 8, 16]), op=ALU.subtract)
            yield "sync"
            p.s("activation", r="gt", w="gt", out=gt, in_=gt, func=AF.Exp)
            yield "sync"
            p.v("tensor_reduce", r="gt", w="gs", out=gs, in_=gt, axis=AX.X, op=ALU.add)
            p.v("reciprocal", r="gs", w="gs", out=gs, in_=gs)
            yield
            p.v("tensor_tensor", r="gt gs IG", w="IG", out=IG[:, 2, :].rearrange("p (h j) -> p h j", h=8), in0=gt,
                in1=gs.unsqueeze(2).to_broadcast([128, 8, 16]), op=ALU.mult)
            p.v("tensor_single_scalar", r="fl", w="ai", out=ai, in_=fl, scalar=4, op=ALU.logical_shift_right)
            p.v("tensor_single_scalar", r="fl", w="bi", out=bi, in_=fl, scalar=15, op=ALU.bitwise_and)
            p.v("tensor_copy", r="ai", w="af", out=af, in_=ai)
            p.v("tensor_copy", r="bi", w="bf", out=bf_, in_=bi)
            yield
            for t_, (xf, key) in enumerate(((af, "af"), (bf_, "bf"))):
                p.v("tensor_tensor", r=key + " iota", w="oh", out=oh, in0=xf.unsqueeze(3).to_broadcast([128, 8, 16, 16]), in1=io16, op=ALU.is_equal)
                yield
                p.v("tensor_tensor", r="oh sif", w="oh", out=oh, in0=oh, in1=sifv[:, :, t_, :].unsqueeze(2).to_broadcast([128, 8, 16, 16]), op=ALU.mult)
                yield
                p.v("tensor_reduce", r="oh IG", w="IG", out=IG[:, t_, :].rearrange("p (h j) -> p h j", h=8), in_=oh, axis=AX.X, op=ALU.add)
                yield
            yield "sync"
            for q in range(3):
                p.t("transpose", r="IG identf", w="b2", out=PB(2)[:, q * 128:(q + 1) * 128], in_=IG[:, q, :], identity=ident_f)
            yield "sync"
            p.s("copy", r="b2", w="IGTb%d" % par, out=IGTb[par][:, ts, :, :], in_=PB(2)[:, 0:256].rearrange("p (a b) -> p a b", a=2))
            p.s("copy", r="b2", w="IGTg%d" % par, out=IGTg[par][:, ts, :], in_=PB(2)[:, 256:384])
            yield

    def part2(blk, half):
        par = blk % 3
        kb, kg = "IGTb%d" % par, "IGTg%d" % par
        Gh = GmH[half]
        gk = "Gm%d" % half
        groups = [(ts, tg) for ts in range(2) for tg in range(128 // NG)]
        bufs = {}

        def gen_(n):
            ts, tg = groups[n]
            tq = slice(tg * NG, (tg + 1) * NG)
            ob = ohc[0] % 2
            ohc[0] += 1
            bufs[n] = ob
            ohA, ohB = ohA2[ob], ohB2[ob]
            ka, kbb = "ohA%d" % ob, "ohB%d" % ob
            p.v("tensor_tensor", r=kb + " iota_b", w=ka, out=ohA, in0=iota_b.unsqueeze(1).to_broadcast([128, NG, 128]),
                in1=IGTb[par][:, ts, 0, tq].unsqueeze(2).to_broadcast([128, NG, 128]), op=ALU.is_equal)
            p.v("tensor_tensor", r=kb + " iota_b", w=kbb, out=ohB, in0=iota_b[:, half * 64:(half + 1) * 64].unsqueeze(1).to_broadcast([128, NG, 64]),
                in1=IGTb[par][:, ts, 1, tq].unsqueeze(2).to_broadcast([128, NG, 64]), op=ALU.is_equal)
            p.g("tensor_tensor", r=kg + " " + kbb, w=kbb, out=ohB, in0=ohB, in1=IGTg[par][:, ts, tq].unsqueeze(2).to_broadcast([128, NG, 64]), op=ALU.mult)

        def mm_(m):
            n, t8 = m // 2, m % 2
            ob = bufs[n]
            ohA, ohB = ohA2[ob], ohB2[ob]
            ka, kbb = "ohA%d" % ob, "ohB%d" % ob
            for q8 in range(8):
                tt_ = t8 * 8 + q8
                p.t("matmul", r=ka + " " + kbb, w="b3", out=PB(3)[:, q8 * 64:(q8 + 1) * 64], lhsT=ohA[:, tt_, :], rhs=ohB[:, tt_, :],
                    start=True, stop=True)

        def ev_(m):
            n, t8 = m // 2, m % 2
            ts, tg = groups[n]
            g0 = ts * 128 + tg * NG + t8 * 8
            if m % 2 == 0:
                p.s("copy", r="b3", w=gk, out=Gh[:, g0:g0 + 8, :], in_=PB(3).rearrange("p (a b) -> p a b", a=8))
            else:
                p.v("tensor_copy", r="b3", w=gk, out=Gh[:, g0:g0 + 8, :], in_=PB(3).rearrange("p (a b) -> p a b", a=8))

        ng = len(groups)
        gen_(0)
        yield "sync"
        for m in range(2 * ng + 1):
            if m % 2 == 0 and m // 2 + 1 < ng:
                gen_(m // 2 + 1)
            if m >= 1:
                ev_(m - 1)
            if m < 2 * ng:
                mm_(m)
            yield "sync"

    widx = [0]

    wslot = {}

    def main_step(blk, s_):
        par = blk % 3
        hT = h2T[par]
        if s_ < 128:
            pr, e2 = s_ // 2, s_ % 2
            if e2 == 0:
                sl = widx[0] % 3
                widx[0] += 1
                wslot[pr % 4] = sl
                p.dma(wu[sl], utb_s[2 * pr:2 * pr + 2].rearrange("a p c -> p a c"), r="utb_s", w="wu%d" % sl)
                p.dma(wv[sl], vtb_s[2 * pr:2 * pr + 2].rearrange("a p c -> p a c"), r="vtb_s", w="wv%d" % sl)
            sl = wslot[pr % 4]
            half = s_ // 64
            ab = s_ % 2
            for kc in range(8):
                p.t("matmul", r="wu%d h2T%d" % (sl, par), w="b%d" % ab, out=PB(ab)[:, 0:TB], lhsT=wu[sl][:, e2, kc * 128:(kc + 1) * 128], rhs=hT[:, kc, :],
                    start=(kc == 0), stop=(kc == 7))
            p.s("activation", r="b%d" % ab, w="ga%d" % ab, out=ga[ab], in_=PB(ab)[:, 0:TB], func=AF.Gelu)
            p.v("tensor_tensor", r="ga%d Gm%d" % (ab, half), w="gab%d" % ab, out=gab[ab], in0=ga[ab], in1=GmH[half][:, :, s_ % 64], op=ALU.mult)
        if s_ >= 1:
            o_ = s_ - 1
            pr, e2 = o_ // 2, o_ % 2
            sl = wslot[pr % 4]
            ab = o_ % 2
            for ts in range(2):
                for nb in range(2):
                    bk = 4 + 2 * ts + nb
                    p.t("matmul", r="gab%d wv%d" % (ab, sl), w="b%d" % bk, out=PB(bk), lhsT=gab[ab][:, ts * 128:(ts + 1) * 128],
                        rhs=wv[sl][:, e2, nb * 512:(nb + 1) * 512], start=(o_ == 0), stop=(o_ == 127))

    def finalize(blk):
        for ts in range(2):
            t0 = blk * TB + ts * 128
            xk = "xs1%d" % ts
            p.dma(xs1[ts], x1_s[t0:t0 + 128, :], r="x1_s", w=xk, e="gpsimd", grp="gx%d" % ts)
            for nb in range(2):
                bk = 4 + 2 * ts + nb
                ns = slice(nb * 512, (nb + 1) * 512)
                p.v("tensor_tensor", r="b%d G2" % bk, w="tmpF", out=tmpF, in0=PB(bk), in1=G2[:, ns], op=ALU.mult)
                p.g("tensor_tensor", r="tmpF " + xk, w=xk, out=xs1[ts][:, ns], in0=xs1[ts][:, ns], in1=tmpF, op=ALU.add)
            p.s("activation", r=xk, w="tmpF ssF", out=junkF, in_=xs1[ts], func=AF.Square, accum_out=ssF[:, 1:2])
            p.v("tensor_scalar", r="ssF", w="rsF", out=rsF[:, 1:2], in0=ssF[:, 1:2], scalar1=1.0 / 1024, scalar2=EPS, op0=ALU.mult, op1=ALU.add)
            p.s("activation", r="rsF", w="rsF", out=rsF[:, 1:2], in_=rsF[:, 1:2], func=AF.Ln)
            p.s("activation", r="rsF", w="rsF", out=rsF[:, 1:2], in_=rsF[:, 1:2], func=AF.Exp, scale=-0.5)
            p.v("scalar_tensor_tensor", r=xk + " rsF FW", w=xk, out=xs1[ts], in0=xs1[ts], scalar=rsF[:, 1:2], in1=FW, op0=ALU.mult, op1=ALU.mult)
            p.dma(out_d[t0:t0 + 128, :], xs1[ts], r=xk, w="out", grp="gout%d" % ts, e="gpsimd")

    nblk = 1 if stop == "F1" else (3 if stop == "F2" else NBLK)

    def drain(g):
        if g is not None:
            for _ in g:
                pass

    def drip(g, n):
        if g is None:
            return None
        try:
            for _ in range(n):
                if next(g) == "sync":
                    break
        except StopIteration:
            return None
        return g

    drain(part1(0))
    if nblk > 1:
        drain(part1(1))
    drain(part2(0, 0))
    for blk in range(nblk):
        st = {"g1": part1(blk + 2) if blk + 2 < nblk else None, "gB": part2(blk, 1), "gA": None}

        import os
        DM = os.environ.get("DRIPMODE", "3")
        if DM in ("0", "2"):
            drain(st["g1"]); st["g1"] = None
        if DM in ("0", "1"):
            drain(st["gB"]); st["gB"] = None

        def dr(s_, st=st, blk=blk):
            st["g1"] = drip(st["g1"], 8)
            if s_ < 64:
                st["gB"] = drip(st["gB"], 1)
            elif DM in ("2", "3"):
                st["gA"] = drip(st["gA"], 1)

        for s_ in range(129 if os.environ.get("SKIPMAIN") != "1" else 0):
            main_step(blk, s_)
            if s_ == 63:
                drain(st["gB"])
                st["gB"] = None
                st["gA"] = part2(blk + 1, 0) if blk + 1 < nblk else None
            dr(s_)
        g1, gA = st["g1"], st["gA"]
        drain(g1)
        drain(gA)
        finalize(blk)
    return fin(nc, p, ar, dbg_d)


def fin(nc, p, ar, dbg_d):
    p.finish()
    p.emit()
    return nc, dbg_d


def host_layout(inp, b):
    f = np.float32
    A = np.ascontiguousarray
    d = {}
    d["x"] = A(inp["x"][b])
    d["ctx"] = A(inp["ctx"][b])
    cc = np.stack([inp["c"][b].reshape(8, 128).T, inp["c_ctx"].reshape(8, 128).T], axis=-1)
    d["cc"] = A(cc.astype(f))
    return d


def shared_layout(inp):
    A = np.ascontiguousarray
    d = {}
    d["ada_w"] = A(inp["ada_w"][0].reshape(8, 128, 6144).transpose(1, 0, 2))
    d["ada_b"] = A(inp["ada_b"][0].reshape(1, 6144))
    d["nrm"] = A(np.stack([np.broadcast_to(inp[k].reshape(1, 1024), (128, 1024)) for k in ("norm1_w", "norm2_w", "final_norm_w")]))
    d["w_in"] = A(inp["w_in"][0].reshape(8, 128, 2576).transpose(1, 0, 2))
    d["scw"] = A(inp["ssm_conv_w"][0].reshape(5, 8, 128).transpose(2, 1, 0))
    d["scb"] = A(inp["ssm_conv_b"][0].reshape(8, 128).T)
    d["dtb"] = A(np.broadcast_to(inp["ssm_dt_bias"][0].reshape(1, 16), (128, 16)))
    d["alog"] = A(np.broadcast_to(inp["ssm_a_log"][0].reshape(1, 16), (128, 16)))
    d["drow"] = A(np.broadcast_to(np.repeat(inp["ssm_d"][0], 64).reshape(1, 512), (128, 512)))
    d["snw"] = A(np.broadcast_to(inp["ssm_norm_w"][0].reshape(1, 512), (128, 512)))
    d["ccw"] = A(inp["cfm_conv_w"][0].reshape(31, 4, 128).transpose(2, 1, 0))
    d["cvec"] = A(np.stack([inp[k][0].reshape(4, 128).T for k in ("cfm_conv_b", "cfm_ln_w", "cfm_ln_b")], axis=1))
    d["w_out"] = A(inp["w_out"][0].reshape(8, 128, 1024).transpose(1, 0, 2))
    d["wq"] = A(inp["peer_wq"][0].reshape(8, 128, 2048).transpose(1, 0, 2))
    d["kt"] = A(inp["peer_subkeys"][0].reshape(16, 128, 128).transpose(2, 0, 1))
    d["ut"] = A(inp["peer_u"][0].reshape(128, 128, 8, 128).transpose(1, 3, 2, 0))
    d["vt"] = A(inp["peer_v"][0].reshape(128, 128, 1024).transpose(1, 0, 2))
    return d


_CACHE = {}


def kernel(**inputs):
    inp = {k: np.asarray(v) for k, v in inputs.items()}
    if "nc" not in _CACHE:
        _CACHE["nc"] = build()[0]
    nc = _CACHE["nc"]
    sh = shared_layout(inp)
    in_maps = []
    for b in range(8):
        d = dict(sh)
        d.update(host_layout(inp, b))
        in_maps.append(d)
    res = run_bass_kernel_spmd(nc, in_maps, core_ids=list(range(8)))
    return np.stack([np.asarray(r["out"], dtype=np.float32) for r in res.results], axis=0)
```

```python
import re
import numpy as np
import concourse.bass as bass
import concourse.mybir as mybir
from concourse.bass_utils import run_bass_kernel_spmd

F32 = mybir.dt.float32
BF16 = mybir.dt.bfloat16
I32 = mybir.dt.int32
U32 = mybir.dt.uint32
AF = mybir.ActivationFunctionType
ALU = mybir.AluOpType
AX = mybir.AxisListType
ENGS = ("sync", "scalar", "vector", "gpsimd", "tensor")
CH = 4096
EPS = 1e-6
DSZ = {F32: 4, BF16: 2, I32: 4, U32: 4}


class Prog:
    def __init__(self, nc):
        self.nc = nc
        self.q = {e: [] for e in ENGS}
        self.cnt = {e: 0 for e in ENGS}
        self.known = {e: {} for e in ENGS}
        self.lastw = {}
        self.readers = {}
        self.sems = {}
        self.dmacum = {}
        self.pend = {e: {} for e in ENGS}
        self.lasttok = {}

    def sem(self, key):
        s = self.sems.get(key)
        if s is None:
            s = self.nc.alloc_semaphore(name="s%d" % len(self.sems))
            self.sems[key] = s
        return s

    def _waits(self, e, reads, writes):
        toks = dict(self.pend[e])
        self.pend[e] = {}

        def add(t):
            if t is None:
                return
            k, v = t
            if k[0] == "eng" and k[1] == e and e == "tensor":
                return
            if toks.get(k, 0) < v:
                toks[k] = v
        for r in reads:
            add(self.lastw.get(r))
        for w in writes:
            add(self.lastw.get(w))
            for t in self.readers.get(w, ()):
                add(t)
        out = []
        for k, v in toks.items():
            if self.known[e].get(k, 0) >= v:
                continue
            self.known[e][k] = v
            out.append((self.sem(k), v))
        return out

    def _commit(self, tok, reads, writes):
        self.lasttok[tok[0]] = tok[1]
        for w in writes:
            self.lastw[w] = tok
            self.readers[w] = []
        for r in reads:
            self.readers.setdefault(r, []).append(tok)

    def op(self, e, name, r="", w="", **kw):
        reads, writes = r.split(), w.split()
        ex = [k for k in reads if re.fullmatch(r"(ps|b)\d", k)]
        reads = [k for k in reads if k not in ex]
        writes = writes + [k for k in ex if k not in writes]
        waits = self._waits(e, reads, writes)
        idx = self.cnt[e]
        self.cnt[e] += 1
        k = ("eng", e, idx // CH)
        self.q[e].append((waits, name, kw, self.sem(k), 1))
        self._commit((k, idx % CH + 1), reads, writes)

    def v(self, name, r="", w="", **kw):
        self.op("vector", name, r, w, **kw)

    def s(self, name, r="", w="", **kw):
        self.op("scalar", name, r, w, **kw)

    def g(self, name, r="", w="", **kw):
        self.op("gpsimd", name, r, w, **kw)

    def t(self, name, r="", w="", **kw):
        self.op("tensor", name, r, w, **kw)

    def dma(self, out, in_, r="", w="", e="sync", grp=None, parts=None, **kw):
        reads, writes = r.split(), w.split()
        sk = ("dma", grp if grp is not None else writes[0])
        waits = self._waits(e, reads, writes)
        pieces = parts if parts is not None else [(out, in_)]
        for i, (o, a) in enumerate(pieces):
            self.dmacum[sk] = self.dmacum.get(sk, 0) + 16
            kk = dict(kw)
            kk.update(out=o, in_=a)
            self.q[e].append((waits if i == 0 else [], "dma_start", kk, self.sem(sk), 16))
        self._commit((sk, self.dmacum[sk]), reads, writes)

    def barrier(self):
        for e in ENGS:
            for k, v in self.lasttok.items():
                if self.pend[e].get(k, 0) < v:
                    self.pend[e][k] = v

    def finish(self):
        self.barrier()
        for e in ("sync", "gpsimd"):
            waits = self._waits(e, [], [])
            self.q[e].append((waits, None, None, None, 0))

    def emit(self):
        nc = self.nc
        with nc.Block() as block:
            def mk(e):
                def body(eng):
                    for waits, name, kw, s, inc in self.q[e]:
                        for ws, wv in waits:
                            eng.wait_ge(ws, wv)
                        if name is not None:
                            getattr(eng, name)(**kw).then_inc(s, inc)
                return body
            block.sync(mk("sync"))
            block.scalar(mk("scalar"))
            block.vector(mk("vector"))
            block.gpsimd(mk("gpsimd"))
            block.tensor(mk("tensor"))


class Arena:
    def __init__(self, nc, nbytes):
        self.t = nc.alloc_sbuf_tensor("arena", [128, nbytes // 4], F32)
        self.cap = nbytes
        self.off = 0
        self.peak = 0

    def a(self, free, dt=F32, parts=128):
        if isinstance(free, int):
            free = (free,)
        n = int(np.prod(free)) * DSZ[dt]
        n = (n + 63) // 64 * 64
        assert self.off + n <= self.cap, ("SBUF arena overflow", self.off, n, self.cap)
        ap = self.t[0:parts, self.off // 4:(self.off + n) // 4]
        self.off += n
        self.peak = max(self.peak, self.off)
        if dt != F32:
            ap = ap.bitcast(dt)
        ap = ap[:, 0:int(np.prod(free))]
        if len(free) == 2:
            ap = ap.rearrange("p (a b) -> p a b", a=free[0])
        elif len(free) == 3:
            ap = ap.rearrange("p (a b c) -> p a b c", a=free[0], b=free[1])
        return ap

    def mark(self):
        return self.off

    def release(self, m):
        self.off = m


L = 4096
LC = 256
NT = L // 128


def build(stop=None, dbg=False, npeer=128):
    nc = bass.Bass("TRN2", target_bir_lowering=False)
    D = nc.dram_tensor

    def din(name, shape, dt=F32):
        return D(name, list(shape), dt, kind="ExternalInput").ap()
    x_d = din("x", [L, 1024])
    ctx_d = din("ctx", [LC, 1024])
    cc_d = din("cc", [128, 8, 2])
    adaw_d = din("ada_w", [128, 8, 6144])
    adab_d = din("ada_b", [1, 6144])
    nrm_d = din("nrm", [3, 128, 1024])
    win_d = din("w_in", [128, 8, 2576])
    scw_d = din("scw", [128, 8, 5])
    scb_d = din("scb", [128, 8])
    dtb_d = din("dtb", [128, 16])
    alog_d = din("alog", [128, 16])
    drow_d = din("drow", [128, 512])
    snw_d = din("snw", [128, 512])
    ccw_d = din("ccw", [128, 4, 31])
    cvec_d = din("cvec", [128, 3, 4])
    wout_d = din("w_out", [128, 8, 1024])
    wq_d = din("wq", [128, 8, 2048])
    kt_d = din("kt", [128, 16, 128])
    ut_d = din("ut", [npeer, 128, 8, 128])
    vt_d = din("vt", [npeer, 128, 1024])
    out_d = D("out", [L, 1024], F32, kind="ExternalOutput").ap()
    dbg_d = {}

    def dout(name, shape):
        dbg_d[name] = D("dbg_" + name, list(shape), F32, kind="ExternalOutput").ap()
        return dbg_d[name]
    modb_s = D("modb_s", [8, 128, 1024], F32).ap()
    zs_s = D("zs_s", [L, 512], BF16).ap()
    x1_s = D("x1_s", [L, 1024], F32).ap()
    utb_s = D("utb_s", [128, 128, 1024], BF16).ap()
    vtb_s = D("vtb_s", [128, 128, 1024], BF16).ap()
    uts_s = D("uts_s", [128, 4, L], BF16).ap()
    wqb_s = D("wqb_s", [16, 128, 1024], BF16).ap()

    p = Prog(nc)
    ar = Arena(nc, 212000)
    banks = [nc.alloc_psum_tensor("pb%d" % i, [128, 512], F32) for i in range(8)]

    def PB(i):
        return banks[i][:]

    def PBb(i):
        return banks[i][:].bitcast(BF16)

    ident_f = ar.a(128)
    ident = ar.a(128, BF16)
    iota_f = ar.a(128)
    pidx = ar.a(128)
    ones_f = ar.a(128)
    ones_b = ar.a(128, BF16)
    triU = ar.a(128)
    triS = ar.a(128)
    maskF = ar.a(128, BF16)
    maskB = ar.a(128, BF16)
    p.g("iota", w="iota", out=iota_f, pattern=[[1, 128]], base=0, channel_multiplier=0, allow_small_or_imprecise_dtypes=True)
    p.g("iota", w="pidx", out=pidx, pattern=[[0, 128]], base=0, channel_multiplier=1, allow_small_or_imprecise_dtypes=True)
    p.v("tensor_tensor", r="iota pidx", w="identf", out=ident_f, in0=iota_f, in1=pidx, op=ALU.is_equal)
    p.v("tensor_copy", r="identf", w="ident", out=ident, in_=ident_f)
    p.v("memset", w="ones_f", ap=ones_f, constant=1.0)
    p.v("memset", w="ones_b", ap=ones_b, constant=1.0)
    p.v("tensor_tensor", r="iota pidx", w="triU", out=triU, in0=iota_f, in1=pidx, op=ALU.is_ge)
    p.v("tensor_tensor", r="iota pidx", w="triS", out=triS, in0=iota_f, in1=pidx, op=ALU.is_gt)
    p.v("tensor_scalar", r="triU", w="maskF", out=maskF, in0=triU, scalar1=-1.0, scalar2=30000.0, op0=ALU.add, op1=ALU.mult)
    tmpc = ar.a(128)
    p.v("tensor_tensor", r="iota pidx", w="tmpc", out=tmpc, in0=iota_f, in1=pidx, op=ALU.is_le)
    p.v("tensor_scalar", r="tmpc", w="maskB", out=maskB, in0=tmpc, scalar1=-1.0, scalar2=30000.0, op0=ALU.add, op1=ALU.mult)
    sel = ar.a((16, 128), BF16)
    p.v("memset", w="sel", ap=sel, constant=0.0)
    p.v("tensor_copy", r="identf sel", w="sel", out=sel[0:16], in_=ident_f[0:16, 0:16].unsqueeze(2).to_broadcast([16, 16, 128]))
    scw = ar.a((8, 5)); scb = ar.a(8); dtbias = ar.a(16); alog = ar.a(16); ccw = ar.a((4, 31)); cvec = ar.a((3, 4))
    Aneg = ar.a(16)
    p.dma(scw, scw_d, w="scw", grp="small")
    p.dma(scb, scb_d, w="scb", grp="small")
    p.dma(dtbias, dtb_d, w="dtbias", grp="small")
    p.dma(alog, alog_d, w="alog", grp="small")
    p.dma(ccw, ccw_d, w="ccw", grp="small")
    p.dma(cvec, cvec_d, w="cvec", grp="small")
    p.s("activation", r="alog", w="Aneg", out=Aneg, in_=alog, func=AF.Exp)
    p.v("tensor_scalar", r="Aneg", w="Aneg", out=Aneg, in0=Aneg, scalar1=-1.0, scalar2=None, op0=ALU.mult)
    dt_all = ar.a((NT + 2, 16))
    m_persist = ar.mark()

    m0 = ar.mark()
    cc = ar.a((8, 2)); scT = ar.a((8, 2))
    adab = ar.a(6144, parts=1)
    modrow = ar.a(6144, parts=1)
    modrow_c = ar.a(2048, parts=1)
    awb = [ar.a((8, 512)) for _ in range(2)]
    nrm = [ar.a(1024) for _ in range(2)]
    bct = [ar.a(1024) for _ in range(2)]
    p.dma(cc, cc_d, w="cc")
    p.dma(adab, adab_d, w="adab")
    p.dma(nrm[0], nrm_d[0], w="nrm0")
    p.dma(nrm[1], nrm_d[1], w="nrm1")
    p.s("activation", r="cc", w="scT", out=scT, in_=cc, func=AF.Silu)
    for blk in range(12):
        sl = blk % 2
        p.dma(awb[sl], adaw_d[:, :, blk * 512:(blk + 1) * 512], w="awb%d" % sl)
        for kc in range(8):
            p.t("matmul", r="scT awb%d" % sl, w="ps0", out=PB(0)[0:1, :], lhsT=scT[:, kc, 0:1], rhs=awb[sl][:, kc, :],
                start=(kc == 0), stop=(kc == 7))
        if blk < 4:
            for kc in range(8):
                p.t("matmul", r="scT awb%d" % sl, w="ps1", out=PB(1)[0:1, :], lhsT=scT[:, kc, 1:2], rhs=awb[sl][:, kc, :],
                    start=(kc == 0), stop=(kc == 7))
            p.v("tensor_tensor", r="ps1 adab", w="modrow_c", out=modrow_c[:, blk * 512:(blk + 1) * 512], in0=PB(1)[0:1, :],
                in1=adab[:, blk * 512:(blk + 1) * 512], op=ALU.add)
        p.v("tensor_tensor", r="ps0 adab", w="modrow", out=modrow[:, blk * 512:(blk + 1) * 512], in0=PB(0)[0:1, :],
            in1=adab[:, blk * 512:(blk + 1) * 512], op=ALU.add)
    plan = [(0, modrow, 1, 0), (1, modrow, 0, None), (2, modrow_c, 1, 0), (3, modrow_c, 0, None), (4, modrow, 2, None),
            (5, modrow, 4, 1), (6, modrow, 3, None), (7, modrow, 5, None)]
    for i, (ti, src, mi, ni) in enumerate(plan):
        bt = bct[i % 2]
        bk = "bct%d" % (i % 2)
        for hf in range(2):
            p.t("matmul", r="ones_f modrow modrow_c", w="ps%d" % (2 + hf), out=PB(2 + hf), lhsT=ones_f[0:1, :],
                rhs=src[:, mi * 1024 + hf * 512: mi * 1024 + (hf + 1) * 512], start=True, stop=True)
            if ni is None:
                p.v("tensor_copy", r="ps%d" % (2 + hf), w=bk, out=bt[:, hf * 512:(hf + 1) * 512], in_=PB(2 + hf))
            else:
                p.v("scalar_tensor_tensor", r="ps%d nrm%d" % (2 + hf, ni), w=bk, out=bt[:, hf * 512:(hf + 1) * 512], in0=PB(2 + hf),
                    scalar=1.0, in1=nrm[ni][:, hf * 512:(hf + 1) * 512], op0=ALU.add, op1=ALU.mult)
        p.dma(modb_s[ti], bt, r=bk, w="modb_s", grp="modb_w")
    if dbg:
        p.dma(dout("modrow", [1, 6144]), modrow, r="modrow", w="dbg_modrow")
    ar.release(m0)
    p.barrier()
    if stop == "0":
        return fin(nc, p, ar, dbg_d)

    m_big = ar.mark()
    xbcT = ar.a((8, L), BF16)
    xbcTc = ar.a((8, LC), BF16)
    m_uT = ar.mark()
    uT = ar.a((4, L), BF16)

    mA = ar.mark()
    Wb = ar.a((8, 2576), BF16)
    wst = [ar.a((8, 128)) for _ in range(2)]
    W1 = ar.a(1024); S1 = ar.a(1024)

    def load_mod(i0):
        p.dma(W1, modb_s[i0], r="modb_s", w="modA0", grp="modA")
        p.dma(S1, modb_s[i0 + 1], r="modb_s", w="modA1", grp="modA")
    ncol = 2576
    pieces = [(c0, min(128, ncol - c0)) for c0 in range(0, ncol, 128)]
    for i, (c0, cw) in enumerate(pieces):
        sl = i % 2
        p.dma(wst[sl][:, :, 0:cw], win_d[:, :, c0:c0 + cw], w="wst%d" % sl)
        if i % 2 == 0:
            p.v("tensor_copy", r="wst%d" % sl, w="Wb", out=Wb[:, :, c0:c0 + cw], in_=wst[sl][:, :, 0:cw])
        else:
            p.s("copy", r="wst%d" % sl, w="Wb", out=Wb[:, :, c0:c0 + cw], in_=wst[sl][:, :, 0:cw])
    xt = [ar.a(1024) for _ in range(2)]
    junk = ar.a(1024, BF16)
    hb = [ar.a(1024, BF16) for _ in range(2)]
    hT = ar.a((8, 512), BF16)
    ss = ar.a(2); rs = ar.a(2)
    sig = ar.a(512)
    zsb = [ar.a(512, BF16) for _ in range(2)]
    dtt = ar.a(16)

    def inproj_block(src_d, t0, ntile, Wm, Sm, is_ctx, tile0):
        ntok = ntile * 128
        for ts in range(ntile):
            sl = ts % 2
            p.dma(xt[sl], src_d[t0 + ts * 128:t0 + (ts + 1) * 128, :], w="xt%d" % sl)
            p.s("activation", r="xt%d" % sl, w="junk ss", out=junk, in_=xt[sl], func=AF.Square, accum_out=ss[:, 0:1])
            p.v("tensor_scalar", r="ss", w="rs", out=rs[:, 0:1], in0=ss[:, 0:1], scalar1=1.0 / 1024, scalar2=EPS, op0=ALU.mult, op1=ALU.add)
            p.s("activation", r="rs", w="rs", out=rs[:, 0:1], in_=rs[:, 0:1], func=AF.Ln)
            p.s("activation", r="rs", w="rs", out=rs[:, 0:1], in_=rs[:, 0:1], func=AF.Exp, scale=-0.5)
            p.v("scalar_tensor_tensor", r="xt%d rs modA0" % sl, w="xt%d" % sl, out=xt[sl], in0=xt[sl], scalar=rs[:, 0:1], in1=Wm,
                op0=ALU.mult, op1=ALU.mult)
            p.g("tensor_tensor", r="xt%d modA1" % sl, w="hb%d" % sl, out=hb[sl], in0=xt[sl], in1=Sm, op=ALU.add)
            for kc in range(8):
                p.t("transpose", r="hb%d ident" % sl, w="ps%d" % (4 + sl), out=PBb(4 + sl)[:, kc * 128:(kc + 1) * 128],
                    in_=hb[sl][:, kc * 128:(kc + 1) * 128], identity=ident)
            p.s("copy", r="ps%d" % (4 + sl), w="hT", out=hT[:, :, ts * 128:(ts + 1) * 128],
                in_=PBb(4 + sl).rearrange("p (a b) -> p a b", a=8))
        for j in range(8):
            pk = j % 2
            c0 = 512 + j * 128
            for kc in range(8):
                p.t("matmul", r="Wb hT", w="ps%d" % pk, out=PB(pk)[:, 0:ntok], lhsT=Wb[:, kc, c0:c0 + 128], rhs=hT[:, kc, 0:ntok],
                    start=(kc == 0), stop=(kc == 7))
            dst = xbcTc[:, j, 0:ntok] if is_ctx else xbcT[:, j, t0:t0 + ntok]
            if j % 2 == 0:
                p.s("copy", r="ps%d" % pk, w="xbcT", out=dst, in_=PB(pk)[:, 0:ntok])
            else:
                p.v("tensor_copy", r="ps%d" % pk, w="xbcT", out=dst, in_=PB(pk)[:, 0:ntok])
        if not is_ctx:
            for j in range(4):
                ca = 1552 + j * 128
                cb = 1552 + 512 + j * 128
                for kc in range(8):
                    p.t("matmul", r="Wb hT", w="ps2", out=PB(2)[:, 0:ntok], lhsT=Wb[:, kc, ca:ca + 128], rhs=hT[:, kc, 0:ntok],
                        start=(kc == 0), stop=(kc == 7))
                for kc in range(8):
                    p.t("matmul", r="Wb hT", w="ps3", out=PB(3)[:, 0:ntok], lhsT=Wb[:, kc, cb:cb + 128], rhs=hT[:, kc, 0:ntok],
                        start=(kc == 0), stop=(kc == 7))
                p.s("activation", r="ps3", w="sig", out=sig[:, 0:ntok], in_=PB(3)[:, 0:ntok], func=AF.Sigmoid)
                p.v("tensor_tensor", r="ps2 sig", w="uT", out=uT[:, j, t0:t0 + ntok], in0=PB(2)[:, 0:ntok], in1=sig[:, 0:ntok], op=ALU.mult)
        for ts in range(ntile):
            sl = ts % 2
            if not is_ctx:
                for kc in range(8):
                    p.t("matmul", r="Wb hT", w="ps%d" % (6 + sl), out=PB(6 + sl), lhsT=hT[:, kc, ts * 128:(ts + 1) * 128], rhs=Wb[:, kc, 0:512],
                        start=(kc == 0), stop=(kc == 7))
                p.s("activation", r="ps%d" % (6 + sl), w="zsb%d" % sl, out=zsb[sl], in_=PB(6 + sl), func=AF.Silu)
                p.dma(zs_s[t0 + ts * 128:t0 + (ts + 1) * 128, :], zsb[sl], r="zsb%d" % sl, w="zs_s", e="sync", grp="zsw%d" % sl)
            for kc in range(8):
                p.t("matmul", r="Wb hT", w="ps0", out=PB(0)[:, 0:16], lhsT=hT[:, kc, ts * 128:(ts + 1) * 128], rhs=Wb[:, kc, 1536:1552],
                    start=(kc == 0), stop=(kc == 7))
            p.v("tensor_tensor", r="ps0 dtbias", w="dtt", out=dtt, in0=PB(0)[:, 0:16], in1=dtbias, op=ALU.add)
            p.s("activation", r="dtt", w="dtt", out=dtt, in_=dtt, func=AF.Exp)
            p.s("activation", r="dtt", w="dt_all", out=dt_all[:, tile0 + ts, :], in_=dtt, func=AF.Ln, bias=1.0)

    load_mod(2)
    inproj_block(ctx_d, 0, 2, W1, S1, True, NT)
    if stop == "A1":
        p.dma(dout("dt_all", [128, NT + 2, 16]), dt_all, r="dt_all", w="dbg_dt")
        return fin(nc, p, ar, dbg_d)
    load_mod(0)
    for b in range(L // 512):
        inproj_block(x_d, b * 512, 4, W1, S1, False, b * 4)
    if dbg:
        p.dma(dout("dt_all", [128, NT + 2, 16]), dt_all, r="dt_all", w="dbg_dt")
        dxb = dout("xbc_pre", [128, 8, 128])
        tdb = ar.a((8, 128))
        p.v("tensor_copy", r="xbcT", w="tdb", out=tdb, in_=xbcT[:, :, 512:640])
        p.dma(dxb, tdb, r="tdb", w="dbg_xb")
        dub = dout("u_pre", [128, 4, 128])
        tdb2 = ar.a((4, 128))
        p.v("tensor_copy", r="uT", w="tdb2", out=tdb2, in_=uT[:, :, 512:640])
        p.dma(dub, tdb2, r="tdb2", w="dbg_ub")
    ar.release(mA)
    p.barrier()
    if stop == "A":
        return fin(nc, p, ar, dbg_d)

    mB = ar.mark()
    acc = [ar.a(L) for _ in range(2)]
    EN = [p.v, p.v]

    def ssm_conv(src, n, ei, j, key):
        E = EN[ei]
        ak = "acc%d" % ei
        a_ = acc[ei][:, 0:n]
        x_ = src[:, j, 0:n]
        E("tensor_scalar", r=key + " scw", w=ak, out=a_, in0=x_, scalar1=scw[:, j, 2:3], scalar2=None, op0=ALU.mult)
        for k in (0, 1, 3, 4):
            o = k - 2
            lo, hi = max(0, -o), min(n, n - o)
            E("scalar_tensor_tensor", r=key + " scw " + ak, w=ak, out=a_[:, lo:hi], in0=x_[:, lo + o:hi + o], scalar=scw[:, j, k:k + 1],
              in1=a_[:, lo:hi], op0=ALU.mult, op1=ALU.add)
        p.s("activation", r=ak + " scb", w=key, out=x_, in_=a_, func=AF.Silu, bias=scb[:, j:j + 1])

    tmpc2 = [ar.a(L) for _ in range(2)]

    def cfm_conv(j, ei):
        ak = "acc%d" % ei
        key = "uT%d" % j
        a_ = acc[ei]
        u_ = uT[:, j, :]
        a3 = a_.rearrange("p (r c) -> p r c", c=64)
        u3 = u_.rearrange("p (r c) -> p r c", c=64)
        if ei == 0:
            p.v("tensor_scalar", r=key + " ccw", w=ak, out=a_, in0=u_, scalar1=ccw[:, j, 15:16], scalar2=None, op0=ALU.mult)
        else:
            p.s("activation", r=key + " ccw", w=ak, out=a_, in_=u_, func=AF.Copy, scale=ccw[:, j, 15:16])
        n = 0
        for k in range(31):
            o = k - 15
            if o == 0:
                continue
            lo, hi = max(0, -o), min(64, 64 - o)
            if j < 2:
                oo, i0 = a3[:, :, lo:hi], u3[:, :, lo + o:hi + o]
            else:
                oo, i0 = a_[:, lo * 64:hi * 64], u_[:, (lo + o) * 64:(hi + o) * 64]
            if ei == 0:
                p.v("scalar_tensor_tensor", r=key + " ccw " + ak, w=ak, out=oo, in0=i0, scalar=ccw[:, j, k:k + 1], in1=oo, op0=ALU.mult, op1=ALU.add)
            else:
                tk = "tmpc2%d" % (n % 2)
                tt = tmpc2[n % 2]
                if j < 2:
                    t_ = tt.rearrange("p (r c) -> p r c", c=64)[:, :, lo:hi]
                else:
                    t_ = tt[:, lo * 64:hi * 64]
                p.s("activation", r=key + " ccw", w=tk, out=t_, in_=i0, func=AF.Copy, scale=ccw[:, j, k:k + 1])
                p.g("tensor_tensor", r=tk + " " + ak, w=ak, out=oo, in0=oo, in1=t_, op=ALU.add)
                n += 1
        p.s("activation", r=ak + " cvec", w=key, out=u_, in_=a_, func=AF.Identity, bias=cvec[:, 0, j:j + 1])

    import os
    NCD = int(os.environ.get("CFMDVE", "4"))
    for j in range(8):
        ssm_conv(xbcTc, LC, 0, j, "xbcTc%d" % j)
    pool_chunks = [2, 3, 0, 1][:4 - NCD]
    dve_chunks = [2, 3, 0, 1][4 - NCD:]
    if pool_chunks:
        cfm_conv(pool_chunks[0], 1)
    for j in range(8):
        ssm_conv(xbcT, L, 0, j, "xbcT%d" % j)
    for j in pool_chunks[1:]:
        cfm_conv(j, 1)
    for j in dve_chunks:
        cfm_conv(j, 0)
    if dbg:
        tdb = ar.a((8, 128))
        p.v("tensor_copy", r=" ".join("xbcT%d" % j for j in range(8)), w="tdb", out=tdb, in_=xbcT[:, :, 512:640])
        p.dma(dout("xbc_post", [128, 8, 128]), tdb, r="tdb", w="dbg_xb2")
        tdb2 = ar.a((4, 128))
        p.v("tensor_copy", r=" ".join("uT%d" % j for j in range(4)), w="tdb2", out=tdb2, in_=uT[:, :, 512:640])
        p.dma(dout("u_post", [128, 4, 128]), tdb2, r="tdb2", w="dbg_ub2")
    for j in range(4):
        p.dma(uts_s[:, j, :], uT[:, j, :], r="uT%d" % j, w="uts_s", grp="utsw")
    ar.release(m_uT)
    p.barrier()
    if stop == "B":
        return fin(nc, p, ar, dbg_d)

    mE = ar.mark()
    SB_all = ar.a((NT, 512), BF16)
    Wo = ar.a((8, 1024), BF16)
    G1 = ar.a(1024); NW = ar.a(512); Drow = ar.a(512)
    p.dma(G1, modb_s[4], r="modb_s", w="G1")
    p.dma(NW, snw_d, w="NW")
    p.dma(Drow, drow_d, w="Drow")
    a16 = ar.a(16); cs = ar.a(32); dec = ar.a(16); tmp16 = ar.a(16); w16 = ar.a(16)
    rowsrc = ar.a(16); bias16 = ar.a(16); e16 = ar.a(16); RT = ar.a(128); RT3 = ar.a((3, 128), BF16); RTr = ar.a(128)
    xs_tok = ar.a(512, BF16); Btok = ar.a(256, BF16)
    xw = [ar.a(512, BF16) for _ in range(2)]
    xdt = [ar.a(512, BF16) for _ in range(2)]
    ST_sb = ar.a((2, 128))
    LT = [ar.a(128) for _ in range(2)]
    SmT = ar.a((16, 128), BF16)
    Sst = [ar.a(512) for _ in range(2)]
    Sbf = [ar.a(512, BF16) for _ in range(2)]
    yc = ar.a(512); yc2 = ar.a(512); zst = ar.a(512, BF16); gg = ar.a(512)
    ss2 = ar.a(2); rs2 = ar.a(2)
    ssm_tok = ar.a(512, BF16); catT = ar.a((8, 128), BF16)
    vsq = ar.a((4, 128)); vsqh = ar.a((4, 128), BF16); vsql = ar.a((4, 128), BF16); mean = ar.a(128); msq = ar.a(128); var = ar.a(128); ntmp = ar.a((4, 128))
    xe = [ar.a(1024) for _ in range(2)]
    for i in range(8):
        sl = i % 2
        wv_ = xe[sl].rearrange("p (a b) -> p a b", a=8)
        p.dma(wv_, wout_d[:, :, i * 128:(i + 1) * 128], w="xe%d" % sl)
        if i % 2 == 0:
            p.v("tensor_copy", r="xe%d" % sl, w="Wo", out=Wo[:, :, i * 128:(i + 1) * 128], in_=wv_)
        else:
            p.s("copy", r="xe%d" % sl, w="Wo", out=Wo[:, :, i * 128:(i + 1) * 128], in_=wv_)
    junk2 = ar.a(256, BF16)
    uTt = [ar.a((4, 128), BF16) for _ in range(2)]
    PT = PBb(0)
    PC = PB(1)
    PK = PB(2)
    PY = PB(3)
    PLS = PB(6)
    PM = PBb(7)
    PM2 = PB(7)

    def bc8(ap8):
        return ap8.unsqueeze(2).to_broadcast([128, 8, 64])

    def v3(ap512):
        return ap512.rearrange("p (h q) -> p h q", h=8)

    def prep(is_ctx, c, dirs):
        src = xbcTc if is_ctx else xbcT
        kx = "xbcTc%d" if is_ctx else "xbcT%d"
        tl = (NT + c) if is_ctx else c
        tok = slice(c * 128, (c + 1) * 128)
        p.v("tensor_tensor", r="dt_all Aneg", w="a16", out=a16, in0=dt_all[:, tl, :], in1=Aneg, op=ALU.mult)
        p.t("matmul", r="triU a16", w="b1", out=PC[:, 0:8], lhsT=triU, rhs=a16[:, 0:8], start=True, stop=True)
        p.t("matmul", r="triS a16", w="b1", out=PC[:, 8:16], lhsT=triS, rhs=a16[:, 8:16], start=True, stop=True)
        p.t("matmul", r="ones_f a16", w="b1", out=PC[:, 16:32], lhsT=ones_f, rhs=a16, start=True, stop=True)
        p.s("copy", r="b1", w="cs", out=cs, in_=PC[:, 0:32])
        p.s("activation", r="cs", w="dec", out=dec, in_=cs[:, 16:32], func=AF.Exp)
        p.v("tensor_tensor", r="cs", w="tmp16", out=tmp16[:, 0:8], in0=cs[:, 16:24], in1=cs[:, 0:8], op=ALU.subtract)
        p.v("tensor_copy", r="cs tmp16", w="tmp16", out=tmp16[:, 8:16], in_=cs[:, 8:16])
        p.s("activation", r="tmp16", w="tmp16", out=tmp16, in_=tmp16, func=AF.Exp)
        p.v("tensor_tensor", r="tmp16 dt_all", w="w16", out=w16, in0=tmp16, in1=dt_all[:, tl, :], op=ALU.mult)
        for j in range(4):
            p.t("transpose", r=(kx % j) + " ident", w="b0", out=PT[:, j * 128:(j + 1) * 128], in_=src[:, j, tok], identity=ident)
        for g in range(2):
            p.t("transpose", r=(kx % (4 + g)) + " ident", w="b0", out=PT[:, 512 + g * 128:512 + (g + 1) * 128], in_=src[:, 4 + g, tok], identity=ident)
        p.v("tensor_copy", r="b0", w="Btok", out=Btok, in_=PT[:, 512:768])
        for d in dirs:
            p.v("tensor_tensor", r="b0 w16", w="xw%d" % d, out=v3(xw[d]), in0=v3(PT[:, 0:512]), in1=bc8(w16[:, d * 8:(d + 1) * 8]), op=ALU.mult)

    def local_states(d):
        for h in range(8):
            g = h // 4
            p.t("matmul", r="Btok xw%d" % d, w="b6", out=PLS[:, h * 64:(h + 1) * 64], lhsT=Btok[:, g * 128:(g + 1) * 128],
                rhs=xw[d][:, h * 64:(h + 1) * 64], start=True, stop=True)

    def upd(d):
        p.v("tensor_tensor", r="S%d dec" % d, w="S%d" % d, out=v3(Sst[d]), in0=v3(Sst[d]), in1=bc8(dec[:, d * 8:(d + 1) * 8]), op=ALU.mult)
        p.v("tensor_tensor", r="S%d b6" % d, w="S%d" % d, out=Sst[d], in0=Sst[d], in1=PLS, op=ALU.add)
        p.s("copy", r="S%d" % d, w="Sbf%d" % d, out=Sbf[d], in_=Sst[d])

    p.v("memset", w="RT3", ap=RT3, constant=0.0)
    for d in range(2):
        p.v("memset", w="S%d" % d, ap=Sst[d], constant=0.0)
        p.v("memset", w="Sbf%d" % d, ap=Sbf[d], constant=0.0)
    prep(True, 0, (0,)); local_states(0); upd(0)
    prep(True, 1, (0, 1)); local_states(0); upd(0); local_states(1); upd(1)
    prep(True, 0, (1,)); local_states(1); upd(1)
    if stop == "C":
        p.dma(dout("S_f", [128, 512]), Sst[0], r="S0", w="dbg_sf")
        p.dma(dout("S_b", [128, 512]), Sst[1], r="S1", w="dbg_sb")
        return fin(nc, p, ar, dbg_d)
    for c in range(NT - 1, -1, -1):
        p.s("copy", r="Sbf1", w="SB%d" % c, out=SB_all[:, c, :], in_=Sbf[1])
        prep(False, c, (1,)); local_states(1); upd(1)
        if npeer == 128:
            if c >= 16:
                hp_ = c - 16
                p.dma(wqb_s[hp_].rearrange("p (k c) -> p k c", k=8), wq_d[:, :, hp_ * 128:(hp_ + 1) * 128], w="wqb_s", e="gpsimd", grp="castq")
            for i2 in range(c * 4, c * 4 + 4):
                p.dma(utb_s[i2], ut_d[i2].rearrange("p k i -> p (k i)"), w="utb_s", e="gpsimd", grp="castu")
                p.dma(vtb_s[i2], vt_d[i2], w="vtb_s", e="gpsimd", grp="castv")
    if dbg:
        p.dma(dout("S_b0", [128, 512]), Sst[1], r="S1", w="dbg_sb0")
    if stop == "D":
        return fin(nc, p, ar, dbg_d)
    for c in range(NT):
        tok = slice(c * 128, (c + 1) * 128)
        pe = c % 2
        p.dma(xe[pe], x_d[tok, :], w="xe%d" % pe)
        p.dma(zst, zs_s[tok, :], r="zs_s", w="zst")
        p.dma(uTt[pe], uts_s[:, :, tok], r="uts_s", w="uTt%d" % pe)
        uTc = uTt[pe]
        if stop == "E0":
            return fin(nc, p, ar, dbg_d)
        prep(False, c, (0,))
        if stop == "E1":
            return fin(nc, p, ar, dbg_d)
        p.v("tensor_copy", r="b0", w="xs_tok", out=xs_tok, in_=PT[:, 0:512])
        for d in range(2):
            p.v("tensor_tensor", r="b0 dt_all", w="xdt%d" % d, out=v3(xdt[d]), in0=v3(PT[:, 0:512]),
                in1=bc8(dt_all[:, c, d * 8:(d + 1) * 8]), op=ALU.mult)
        if stop == "E1a":
            return fin(nc, p, ar, dbg_d)
        for g in range(2):
            p.t("matmul", r="xbcT%d xbcT%d" % (4 + g, 6 + g), w="b1", out=PC[:, 256 + g * 128:256 + (g + 1) * 128],
                lhsT=xbcT[:, 4 + g, tok], rhs=xbcT[:, 6 + g, tok], start=True, stop=True)
        if stop == "E1b":
            return fin(nc, p, ar, dbg_d)
        p.s("copy", r="b1", w="ST_sb", out=ST_sb, in_=PC[:, 256:512].rearrange("p (g l) -> p g l", g=2))
        if stop == "E2":
            return fin(nc, p, ar, dbg_d)

        p.v("tensor_copy", r="cs", w="rowsrc", out=rowsrc[:, 0:8], in_=cs[:, 0:8])
        p.v("tensor_scalar", r="cs rowsrc", w="rowsrc", out=rowsrc[:, 8:16], in0=cs[:, 8:16], scalar1=-1.0, scalar2=None, op0=ALU.mult)
        p.v("tensor_scalar", r="rowsrc", w="bias16", out=bias16, in0=rowsrc, scalar1=-1.0, scalar2=None, op0=ALU.mult)
        p.t("transpose", r="rowsrc identf", w="b1", out=PC[0:16, 32:160], in_=rowsrc, identity=ident_f)
        p.s("copy", r="b1", w="RT", out=RT[0:16, :], in_=PC[0:16, 32:160])
        p.v("tensor_copy", r="RT", w="RT3", out=RT3[0:16, 0, :], in_=RT[0:16, :])
        p.v("tensor_tensor", r="RT RT3", w="RTr", out=RTr[0:16, :], in0=RT[0:16, :], in1=RT3[0:16, 0, :], op=ALU.subtract)
        p.v("tensor_copy", r="RTr RT3", w="RT3", out=RT3[0:16, 1, :], in_=RTr[0:16, :])
        p.v("tensor_tensor", r="RTr RT3", w="RTr", out=RTr[0:16, :], in0=RTr[0:16, :], in1=RT3[0:16, 1, :], op=ALU.subtract)
        p.v("tensor_copy", r="RTr RT3", w="RT3", out=RT3[0:16, 2, :], in_=RTr[0:16, :])
        if stop == "E3":
            return fin(nc, p, ar, dbg_d)

        p.v("tensor_copy", r="cs", w="e16", out=e16[:, 0:8], in_=cs[:, 0:8])
        p.v("tensor_tensor", r="cs e16", w="e16", out=e16[:, 8:16], in0=cs[:, 24:32], in1=cs[:, 8:16], op=ALU.subtract)
        p.s("activation", r="e16", w="e16", out=e16, in_=e16, func=AF.Exp)
        if stop == "E4":
            return fin(nc, p, ar, dbg_d)

        for idx in range(16):
            d, h = idx // 8, idx % 8
            g = h // 4
            pk = "b2" if idx % 2 == 0 else "b6"
            PKs = (PB(2) if idx % 2 == 0 else PB(6))[:, 0:128]
            for q in range(3):
                p.t("matmul", r="sel RT3", w=pk, out=PKs, lhsT=sel[:, idx, :], rhs=RT3[:, q, :], start=(q == 0), stop=False)
            p.t("matmul", r="ident maskF maskB", w=pk, out=PKs, lhsT=ident, rhs=(maskF if d == 0 else maskB), start=False, stop=True)
            lk = "LT%d" % (idx % 2)
            p.s("activation", r=pk + " bias16", w=lk, out=LT[idx % 2], in_=PKs, func=AF.Exp, bias=bias16[:, idx:idx + 1])
            p.v("tensor_tensor", r=lk + " ST_sb", w="SmT%d" % idx, out=SmT[:, idx, :], in0=LT[idx % 2], in1=ST_sb[:, g, :], op=ALU.mult)
        if stop == "E5a":
            return fin(nc, p, ar, dbg_d)
        for h in range(8):
            hs = slice(h * 64, (h + 1) * 64)
            p.t("matmul", r="SmT%d xdt0" % h, w="b3", out=PY[:, hs], lhsT=SmT[:, h, :], rhs=xdt[0][:, hs], start=True, stop=False)
            p.t("matmul", r="SmT%d xdt1" % (8 + h), w="b3", out=PY[:, hs], lhsT=SmT[:, 8 + h, :], rhs=xdt[1][:, hs], start=False, stop=True)
        for h in range(8):
            g = h // 4
            hs = slice(h * 64, (h + 1) * 64)
            p.t("matmul", r="xbcT%d Sbf0" % (6 + g), w="b4", out=PB(4)[:, hs], lhsT=xbcT[:, 6 + g, tok], rhs=Sbf[0][:, hs], start=True, stop=True)
            p.t("matmul", r="xbcT%d SB%d" % (6 + g, c), w="b5", out=PB(5)[:, hs], lhsT=xbcT[:, 6 + g, tok], rhs=SB_all[:, c, hs], start=True, stop=True)
        if stop == "E5b":
            return fin(nc, p, ar, dbg_d)
        p.v("tensor_tensor", r="b4 e16", w="yc", out=v3(yc), in0=v3(PB(4)), in1=bc8(e16[:, 0:8]), op=ALU.mult)
        p.v("tensor_tensor", r="b5 e16", w="yc2", out=v3(yc2), in0=v3(PB(5)), in1=bc8(e16[:, 8:16]), op=ALU.mult)
        p.g("tensor_tensor", r="yc yc2", w="yc", out=yc, in0=yc, in1=yc2, op=ALU.add)
        if stop == "E6":
            return fin(nc, p, ar, dbg_d)

        p.v("tensor_tensor", r="yc b3", w="yc", out=yc, in0=yc, in1=PY, op=ALU.add)
        p.g("tensor_tensor", r="xs_tok Drow", w="yc2", out=yc2, in0=xs_tok, in1=Drow, op=ALU.mult)
        p.v("tensor_tensor", r="yc yc2", w="yc", out=yc, in0=yc, in1=yc2, op=ALU.add)
        p.v("tensor_tensor", r="yc zst", w="gg", out=gg, in0=yc, in1=zst, op=ALU.mult)
        for g in range(2):
            p.s("activation", r="gg", w="junk2 ss2", out=junk2, in_=gg[:, g * 256:(g + 1) * 256], func=AF.Square, accum_out=ss2[:, g:g + 1])
        p.v("tensor_scalar", r="ss2", w="rs2", out=rs2, in0=ss2, scalar1=1.0 / 256, scalar2=EPS, op0=ALU.mult, op1=ALU.add)
        p.s("activation", r="rs2", w="rs2", out=rs2, in_=rs2, func=AF.Ln)
        p.s("activation", r="rs2", w="rs2", out=rs2, in_=rs2, func=AF.Exp, scale=-0.5)
        for g in range(2):
            gs = slice(g * 256, (g + 1) * 256)
            p.v("scalar_tensor_tensor", r="gg rs2 NW", w="ssm_tok", out=ssm_tok[:, gs], in0=gg[:, gs], scalar=rs2[:, g:g + 1], in1=NW[:, gs],
                op0=ALU.mult, op1=ALU.mult)
        for j in range(4):
            p.t("transpose", r="ssm_tok ident", w="b7", out=PM[:, j * 128:(j + 1) * 128], in_=ssm_tok[:, j * 128:(j + 1) * 128], identity=ident)
        p.s("copy", r="b7", w="catT", out=catT[:, 0:4, :], in_=PM[:, 0:512].rearrange("p (a b) -> p a b", a=4))
        if stop == "E7":
            return fin(nc, p, ar, dbg_d)

        ukeys = "uTt%d" % pe
        p.s("activation", r=ukeys, w="vsq", out=vsq, in_=uTc, func=AF.Square)
        for j in range(4):
            p.t("matmul", r="ones_b " + ukeys, w="b7", out=PM2[:, 256:384], lhsT=ones_b, rhs=uTc[:, j, :], start=(j == 0), stop=(j == 3))
        p.v("tensor_copy", r="vsq", w="vsqh", out=vsqh, in_=vsq)
        p.v("tensor_tensor", r="vsq vsqh", w="vsql", out=vsql, in0=vsq, in1=vsqh, op=ALU.subtract)
        for j in range(4):
            p.t("matmul", r="ones_b vsqh", w="b7", out=PM2[:, 384:512], lhsT=ones_b, rhs=vsqh[:, j, :], start=(j == 0), stop=False)
            p.t("matmul", r="ones_b vsql", w="b7", out=PM2[:, 384:512], lhsT=ones_b, rhs=vsql[:, j, :], start=False, stop=(j == 3))
        p.s("activation", r="b7", w="mean", out=mean, in_=PM2[:, 256:384], func=AF.Copy, scale=1.0 / 512)
        p.s("activation", r="b7", w="var", out=var, in_=PM2[:, 384:512], func=AF.Copy, scale=1.0 / 512)
        p.g("tensor_tensor", r="mean", w="msq", out=msq, in0=mean, in1=mean, op=ALU.mult)
        p.v("tensor_tensor", r="var msq", w="var", out=var, in0=var, in1=msq, op=ALU.subtract)
        p.v("tensor_scalar", r="var", w="var", out=var, in0=var, scalar1=EPS, scalar2=None, op0=ALU.add)
        p.s("activation", r="var", w="var", out=var, in_=var, func=AF.Ln)
        p.s("activation", r="var", w="var", out=var, in_=var, func=AF.Exp, scale=-0.5)
        p.v("tensor_tensor", r=ukeys + " mean", w="ntmp", out=ntmp, in0=uTc, in1=mean.unsqueeze(1).to_broadcast([128, 4, 128]), op=ALU.subtract)
        p.v("tensor_tensor", r="ntmp var", w="ntmp", out=ntmp, in0=ntmp, in1=var.unsqueeze(1).to_broadcast([128, 4, 128]), op=ALU.mult)
        for j in range(4):
            p.s("activation", r="ntmp cvec", w="catT", out=catT[:, 4 + j, :], in_=ntmp[:, j, :], func=AF.Silu, scale=cvec[:, 1, j:j + 1],
                bias=cvec[:, 2, j:j + 1])
        for nb in range(2):
            for kc in range(8):
                p.t("matmul", r="catT Wo", w="b%d" % (4 + nb), out=PB(4 + nb), lhsT=catT[:, kc, :], rhs=Wo[:, kc, nb * 512:(nb + 1) * 512],
                    start=(kc == 0), stop=(kc == 7))
            ns = slice(nb * 512, (nb + 1) * 512)
            tq, tk_ = (yc2, "yc2") if nb == 0 else (gg, "gg")
            p.v("tensor_tensor", r="b%d G1" % (4 + nb), w=tk_, out=tq, in0=PB(4 + nb), in1=G1[:, ns], op=ALU.mult)
            p.g("tensor_tensor", r=tk_ + " xe%d" % pe, w="xe%d" % pe, out=xe[pe][:, ns], in0=xe[pe][:, ns], in1=tq, op=ALU.add)
        p.dma(x1_s[tok, :], xe[pe], r="xe%d" % pe, w="x1_s", grp="x1w%d" % pe)
        local_states(0)
        upd(0)
    if dbg:
        dx1 = dout("x1", [L, 1024])
        p.dma(None, None, r="x1_s", w="dbg_x1", parts=[(dx1[i * 128:(i + 1) * 128, :], x1_s[i * 128:(i + 1) * 128, :]) for i in range(NT)])
    ar.release(mE)
    p.barrier()
    if stop == "E":
        return fin(nc, p, ar, dbg_d)

    ar.release(m_big)
    TB = 256
    KT = ar.a((16, 128), BF16)
    W2 = ar.a(1024); S2 = ar.a(1024); G2 = ar.a(1024); FW = ar.a(1024)
    p.dma(W2, modb_s[5], r="modb_s", w="W2")
    p.dma(S2, modb_s[6], r="modb_s", w="S2")
    p.dma(G2, modb_s[7], r="modb_s", w="G2")
    p.dma(FW, nrm_d[2], w="FW")
    xs1 = [ar.a(1024) for _ in range(2)]
    for i in range(4):
        sl = i % 2
        wv_ = xs1[sl][:, 0:512].rearrange("p (a b) -> p a b", a=4)
        p.dma(wv_, kt_d[:, i * 4:(i + 1) * 4, :], w="xs1%d" % sl)
        p.v("tensor_copy", r="xs1%d" % sl, w="KT", out=KT[:, i * 4:(i + 1) * 4, :], in_=wv_)
    iota_b = ar.a(128, BF16)
    p.v("tensor_copy", r="iota", w="iota_b", out=iota_b, in_=iota_f)
    Wqs = [ar.a((8, 128), BF16) for _ in range(3)]
    tmpF = ar.a(512)
    junkF = tmpF.bitcast(BF16)
    ssF = ar.a(2); rsF = ar.a(2)
    h2 = ar.a(1024, BF16)
    h2T = [ar.a((8, TB), BF16) for _ in range(3)]
    qT = ar.a((16, TB), BF16)
    sc = ar.a((16, 128))
    sv = ar.a((16, 16)); si = ar.a((16, 16), U32); sif = ar.a((16, 16)); wk = ar.a(128); wk2 = ar.a(256)
    best = ar.a((8, 16)); fl = ar.a((8, 16), U32); ai = ar.a((8, 16), U32); bi = ar.a((8, 16), U32)
    af = ar.a((8, 16)); bf_ = ar.a((8, 16)); gt = ar.a((8, 16)); gs = ar.a(8)
    IG = ar.a((3, 128))
    IGTb = [ar.a((2, 2, 128), BF16) for _ in range(3)]
    IGTg = [ar.a((2, 128)) for _ in range(3)]
    NG = 16
    ohA2 = [ar.a((NG, 128), BF16) for _ in range(2)]
    ohB2 = [ar.a((NG, 64), BF16) for _ in range(2)]
    ohc = [0]
    oh = ar.a((8, 16, 16))
    GmH = [ar.a((TB, 64), BF16) for _ in range(2)]
    wu = [ar.a((2, 1024), BF16) for _ in range(3)]
    wv = [ar.a((2, 1024), BF16) for _ in range(3)]
    ga = [ar.a(TB) for _ in range(2)]
    gab = [ar.a(TB, BF16) for _ in range(2)]
    cand = sc.rearrange("p a b -> p (a b)").rearrange("p (h a b) -> p h a b", h=8, a=16)
    svv = sv.rearrange("p (h t) a -> p h t a", t=2)
    sifv = sif.rearrange("p (h t) a -> p h t a", t=2)
    io16 = iota_f[:, 0:16].unsqueeze(1).unsqueeze(1).to_broadcast([128, 8, 16, 16])
    NBLK = L // TB
    wqi = [0]

    def part1(blk):
        par = blk % 3
        hT = h2T[par]
        hk = "h2T%d" % par
        for ts in range(2):
            t0 = blk * TB + ts * 128
            xk = "xs1%d" % ts
            p.dma(xs1[ts], x1_s[t0:t0 + 128, :], r="x1_s", w=xk, e="gpsimd", grp="gx%d" % ts)
            yield "sync"
            p.s("activation", r=xk, w="tmpF ssF", out=junkF, in_=xs1[ts], func=AF.Square, accum_out=ssF[:, 0:1])
            p.v("tensor_scalar", r="ssF", w="rsF", out=rsF[:, 0:1], in0=ssF[:, 0:1], scalar1=1.0 / 1024, scalar2=EPS, op0=ALU.mult, op1=ALU.add)
            p.s("activation", r="rsF", w="rsF", out=rsF[:, 0:1], in_=rsF[:, 0:1], func=AF.Ln)
            p.s("activation", r="rsF", w="rsF", out=rsF[:, 0:1], in_=rsF[:, 0:1], func=AF.Exp, scale=-0.5)
            yield
            p.v("scalar_tensor_tensor", r=xk + " rsF W2", w=xk, out=xs1[ts], in0=xs1[ts], scalar=rsF[:, 0:1], in1=W2, op0=ALU.mult, op1=ALU.mult)
            p.g("tensor_tensor", r=xk + " S2", w="h2", out=h2, in0=xs1[ts], in1=S2, op=ALU.add)
            yield "sync"
            for kc in range(8):
                p.t("transpose", r="h2 ident", w="b2", out=PBb(2)[:, kc * 128:(kc + 1) * 128], in_=h2[:, kc * 128:(kc + 1) * 128], identity=ident)
            yield "sync"
            p.s("copy", r="b2", w=hk, out=hT[:, :, ts * 128:(ts + 1) * 128], in_=PBb(2).rearrange("p (a b) -> p a b", a=8))
            yield
        def wq_load(hp_):
            ws_ = (wqi[0] + hp_) % 3
            p.dma(Wqs[ws_], wqb_s[hp_].rearrange("p (k c) -> p k c", k=8), r="wqb_s", w="Wqs%d" % ws_, e="gpsimd", grp="gwq%d" % ws_)
        wq_load(0)
        wq_load(1)
        yield "sync"
        for hp in range(17):
            if hp >= 1:
                if hp % 2 == 1:
                    p.s("copy", r="b2", w="qT", out=qT[:, hp - 1, :], in_=PB(2)[:, 0:TB])
                else:
                    p.v("tensor_copy", r="b2", w="qT", out=qT[:, hp - 1, :], in_=PB(2)[:, 0:TB])
            if hp < 16:
                ws = (wqi[0] + hp) % 3
                if hp + 2 < 16:
                    wq_load(hp + 2)
                for kc in range(8):
                    p.t("matmul", r=("Wqs%d " % ws) + hk, w="b2", out=PB(2)[:, 0:TB], lhsT=Wqs[ws][:, kc, :], rhs=hT[:, kc, :],
                        start=(kc == 0), stop=(kc == 7))
            yield "sync"
        wqi[0] += 16
        yield "sync"
        for ts in range(2):
            tsl = slice(ts * 128, (ts + 1) * 128)
            for g4 in range(5):
                if g4 >= 1:
                    p.s("copy", r="b2", w="sc", out=sc[:, (g4 - 1) * 4:g4 * 4, :], in_=PB(2).rearrange("p (a b) -> p a b", a=4))
                if g4 < 4:
                    for q4 in range(4):
                        hp = g4 * 4 + q4
                        p.t("matmul", r="qT KT", w="b2", out=PB(2)[:, q4 * 128:(q4 + 1) * 128], lhsT=qT[:, hp, tsl], rhs=KT[:, hp, :],
                            start=True, stop=True)
                yield "sync"
            yield "sync"
            for hp in range(16):
                p.v("max", r="sc", w="sv", out=sv[:, hp, 0:8], in_=sc[:, hp, :])
                p.v("max_index", r="sc sv", w="si", out=si[:, hp, 0:8], in_max=sv[:, hp, 0:8], in_values=sc[:, hp, :])
                p.v("match_replace", r="sc sv", w="wk", out=wk, in_to_replace=sv[:, hp, 0:8], in_values=sc[:, hp, :], imm_value=-1e30)
                yield
                p.v("max", r="wk", w="sv", out=sv[:, hp, 8:16], in_=wk)
                p.v("max_index", r="wk sv", w="si", out=si[:, hp, 8:16], in_max=sv[:, hp, 8:16], in_values=wk)
                yield
            p.v("tensor_copy", r="si", w="sif", out=sif, in_=si)
            p.v("tensor_tensor", r="sv sc", w="sc", out=cand, in0=svv[:, :, 0, :].unsqueeze(3).to_broadcast([128, 8, 16, 16]),
                in1=svv[:, :, 1, :].unsqueeze(2).to_broadcast([128, 8, 16, 16]), op=ALU.add)
            yield
            for h in range(8):
                ch = cand[:, h].rearrange("p a b -> p (a b)")
                p.v("max", r="sc", w="best", out=best[:, h, 0:8], in_=ch)
                p.v("max_index", r="sc best", w="fl", out=fl[:, h, 0:8], in_max=best[:, h, 0:8], in_values=ch)
                p.v("match_replace", r="sc best", w="wk2", out=wk2, in_to_replace=best[:, h, 0:8], in_values=ch, imm_value=-1e30)
                yield
                p.v("max", r="wk2", w="best", out=best[:, h, 8:16], in_=wk2)
                p.v("max_index", r="wk2 best", w="fl", out=fl[:, h, 8:16], in_max=best[:, h, 8:16], in_values=wk2)
                yield
            p.v("tensor_tensor", r="best", w="gt", out=gt, in0=best, in1=best[:, :, 0:1].to_broadcast([128, 8, 16]), op=ALU.subtract)
            yield "sync"
            p.s("activation", r="gt", w="gt", out=gt, in_=gt, func=AF.Exp)
            yield "sync"
            p.v("tensor_reduce", r="gt", w="gs", out=gs, in_=gt, axis=AX.X, op=ALU.add)
            p.v("reciprocal", r="gs", w="gs", out=gs, in_=gs)
            yield
            p.v("tensor_tensor", r="gt gs IG", w="IG", out=IG[:, 2, :].rearrange("p (h j) -> p h j", h=8), in0=gt,
                in1=gs.unsqueeze(2).to_broadcast([128, 8, 16]), op=ALU.mult)
            p.v("tensor_single_scalar", r="fl", w="ai", out=ai, in_=fl, scalar=4, op=ALU.logical_shift_right)
            p.v("tensor_single_scalar", r="fl", w="bi", out=bi, in_=fl, scalar=15, op=ALU.bitwise_and)
            p.v("tensor_copy", r="ai", w="af", out=af, in_=ai)
            p.v("tensor_copy", r="bi", w="bf", out=bf_, in_=bi)
            yield
            for t_, (xf, key) in enumerate(((af, "af"), (bf_, "bf"))):
                p.v("tensor_tensor", r=key + " iota", w="oh", out=oh, in0=xf.unsqueeze(3).to_broadcast([128, 8, 16, 16]), in1=io16, op=ALU.is_equal)
                yield
                p.v("tensor_tensor", r="oh sif", w="oh", out=oh, in0=oh, in1=sifv[:, :, t_, :].unsqueeze(2).to_broadcast([128, 8, 16, 16]), op=ALU.mult)
                yield
                p.v("tensor_reduce", r="oh IG", w="IG", out=IG[:, t_, :].rearrange("p (h j) -> p h j", h=8), in_=oh, axis=AX.X, op=ALU.add)
                yield
            yield "sync"
            for q in range(3):
                p.t("transpose", r="IG identf", w="b2", out=PB(2)[:, q * 128:(q + 1) * 128], in_=IG[:, q, :], identity=ident_f)
            yield "sync"
            p.s("copy", r="b2", w="IGTb%d" % par, out=IGTb[par][:, ts, :, :], in_=PB(2)[:, 0:256].rearrange("p (a b) -> p a b", a=2))
            p.s("copy", r="b2", w="IGTg%d" % par, out=IGTg[par][:, ts, :], in_=PB(2)[:, 256:384])
            yield

    def part2(blk, half):
        par = blk % 3
        kb, kg = "IGTb%d" % par, "IGTg%d" % par
        Gh = GmH[half]
        gk = "Gm%d" % half
        groups = [(ts, tg) for ts in range(2) for tg in range(128 // NG)]
        bufs = {}

        def gen_(n):
            ts, tg = groups[n]
            tq = slice(tg * NG, (tg + 1) * NG)
            ob = ohc[0] % 2
            ohc[0] += 1
            bufs[n] = ob
            ohA, ohB = ohA2[ob], ohB2[ob]
            ka, kbb = "ohA%d" % ob, "ohB%d" % ob
            p.v("tensor_tensor", r=kb + " iota_b", w=ka, out=ohA, in0=iota_b.unsqueeze(1).to_broadcast([128, NG, 128]),
                in1=IGTb[par][:, ts, 0, tq].unsqueeze(2).to_broadcast([128, NG, 128]), op=ALU.is_equal)
            p.v("tensor_tensor", r=kb + " iota_b", w=kbb, out=ohB, in0=iota_b[:, half * 64:(half + 1) * 64].unsqueeze(1).to_broadcast([128, NG, 64]),
                in1=IGTb[par][:, ts, 1, tq].unsqueeze(2).to_broadcast([128, NG, 64]), op=ALU.is_equal)
            import os
            if os.environ.get("NOPOOLMULT") != "1":
                p.g("tensor_tensor", r=kg + " " + kbb, w=kbb, out=ohB, in0=ohB, in1=IGTg[par][:, ts, tq].unsqueeze(2).to_broadcast([128, NG, 64]), op=ALU.mult)

        def mm_(m):
            n, t8 = m // 2, m % 2
            ob = bufs[n]
            ohA, ohB = ohA2[ob], ohB2[ob]
            ka, kbb = "ohA%d" % ob, "ohB%d" % ob
            for q8 in range(8):
                tt_ = t8 * 8 + q8
                p.t("matmul", r=ka + " " + kbb, w="b3", out=PB(3)[:, q8 * 64:(q8 + 1) * 64], lhsT=ohA[:, tt_, :], rhs=ohB[:, tt_, :],
                    start=True, stop=True)

        def ev_(m):
            n, t8 = m // 2, m % 2
            ts, tg = groups[n]
            g0 = ts * 128 + tg * NG + t8 * 8
            if m % 2 == 0:
                p.s("copy", r="b3", w=gk, out=Gh[:, g0:g0 + 8, :], in_=PB(3).rearrange("p (a b) -> p a b", a=8))
            else:
                p.v("tensor_copy", r="b3", w=gk, out=Gh[:, g0:g0 + 8, :], in_=PB(3).rearrange("p (a b) -> p a b", a=8))

        ng = len(groups)
        gen_(0)
        yield "sync"
        for m in range(2 * ng + 1):
            if m % 2 == 0 and m // 2 + 1 < ng:
                gen_(m // 2 + 1)
            if m >= 1:
                ev_(m - 1)
            if m < 2 * ng:
                mm_(m)
            yield "sync"

    widx = [0]

    wslot = {}

    def main_step(blk, s_):
        par = blk % 3
        hT = h2T[par]
        if s_ < 128:
            pr, e2 = s_ // 2, s_ % 2
            if e2 == 0:
                sl = widx[0] % 3
                widx[0] += 1
                wslot[pr % 4] = sl
                p.dma(wu[sl], utb_s[2 * pr:2 * pr + 2].rearrange("a p c -> p a c"), r="utb_s", w="wu%d" % sl)
                p.dma(wv[sl], vtb_s[2 * pr:2 * pr + 2].rearrange("a p c -> p a c"), r="vtb_s", w="wv%d" % sl)
            sl = wslot[pr % 4]
            half = s_ // 64
            ab = s_ % 2
            for kc in range(8):
                p.t("matmul", r="wu%d h2T%d" % (sl, par), w="b%d" % ab, out=PB(ab)[:, 0:TB], lhsT=wu[sl][:, e2, kc * 128:(kc + 1) * 128], rhs=hT[:, kc, :],
                    start=(kc == 0), stop=(kc == 7))
            p.s("activation", r="b%d" % ab, w="ga%d" % ab, out=ga[ab], in_=PB(ab)[:, 0:TB], func=AF.Gelu)
            p.v("tensor_tensor", r="ga%d Gm%d" % (ab, half), w="gab%d" % ab, out=gab[ab], in0=ga[ab], in1=GmH[half][:, :, s_ % 64], op=ALU.mult)
        if s_ >= 1:
            o_ = s_ - 1
            pr, e2 = o_ // 2, o_ % 2
            sl = wslot[pr % 4]
            ab = o_ % 2
            for ts in range(2):
                for nb in range(2):
                    bk = 4 + 2 * ts + nb
                    p.t("matmul", r="gab%d wv%d" % (ab, sl), w="b%d" % bk, out=PB(bk), lhsT=gab[ab][:, ts * 128:(ts + 1) * 128],
                        rhs=wv[sl][:, e2, nb * 512:(nb + 1) * 512], start=(o_ == 0), stop=(o_ == 127))

    def finalize(blk):
        for ts in range(2):
            t0 = blk * TB + ts * 128
            xk = "xs1%d" % ts
            p.dma(xs1[ts], x1_s[t0:t0 + 128, :], r="x1_s", w=xk, e="gpsimd", grp="gx%d" % ts)
            for nb in range(2):
                bk = 4 + 2 * ts + nb
                ns = slice(nb * 512, (nb + 1) * 512)
                p.v("tensor_tensor", r="b%d G2" % bk, w="tmpF", out=tmpF, in0=PB(bk), in1=G2[:, ns], op=ALU.mult)
                p.g("tensor_tensor", r="tmpF " + xk, w=xk, out=xs1[ts][:, ns], in0=xs1[ts][:, ns], in1=tmpF, op=ALU.add)
            p.s("activation", r=xk, w="tmpF ssF", out=junkF, in_=xs1[ts], func=AF.Square, accum_out=ssF[:, 1:2])
            p.v("tensor_scalar", r="ssF", w="rsF", out=rsF[:, 1:2], in0=ssF[:, 1:2], scalar1=1.0 / 1024, scalar2=EPS, op0=ALU.mult, op1=ALU.add)
            p.s("activation", r="rsF", w="rsF", out=rsF[:, 1:2], in_=rsF[:, 1:2], func=AF.Ln)
            p.s("activation", r="rsF", w="rsF", out=rsF[:, 1:2], in_=rsF[:, 1:2], func=AF.Exp, scale=-0.5)
            p.v("scalar_tensor_tensor", r=xk + " rsF FW", w=xk, out=xs1[ts], in0=xs1[ts], scalar=rsF[:, 1:2], in1=FW, op0=ALU.mult, op1=ALU.mult)
            p.dma(out_d[t0:t0 + 128, :], xs1[ts], r=xk, w="out", grp="gout%d" % ts, e="gpsimd")

    nblk = 1 if stop == "F1" else (3 if stop == "F2" else (7 if stop == "F7" else NBLK))

    def drain(g):
        if g is not None:
            for _ in g:
                pass

    def drip(g, n):
        if g is None:
            return None
        try:
            for _ in range(n):
                if next(g) == "sync":
                    break
        except StopIteration:
            return None
        return g

    drain(part1(0))
    if nblk > 1:
        drain(part1(1))
    drain(part2(0, 0))
    for blk in range(nblk):
        st = {"g1": part1(blk + 2) if blk + 2 < nblk else None, "gB": part2(blk, 1), "gA": None}

        import os
        DM = os.environ.get("DRIPMODE", "3")
        if DM in ("0", "2"):
            drain(st["g1"]); st["g1"] = None
        if DM in ("0", "1"):
            drain(st["gB"]); st["gB"] = None

        N1 = int(os.environ.get("N1", "2"))
        SPREAD = int(os.environ.get("SPREAD", "58"))

        def dr(s_, st=st, blk=blk):
            st["g1"] = drip(st["g1"], N1)
            k = s_ % 64
            if (k + 1) * 33 // SPREAD == k * 33 // SPREAD and k < SPREAD:
                return
            if s_ < 64:
                st["gB"] = drip(st["gB"], 1)
            elif DM in ("2", "3"):
                st["gA"] = drip(st["gA"], 1)

        for s_ in range(129 if os.environ.get("SKIPMAIN") != "1" else 0):
            main_step(blk, s_)
            if s_ == 63:
                drain(st["gB"])
                st["gB"] = None
                st["gA"] = part2(blk + 1, 0) if blk + 1 < nblk else None
            dr(s_)
        g1, gA = st["g1"], st["gA"]
        drain(g1)
        drain(gA)
        finalize(blk)
    return fin(nc, p, ar, dbg_d)


def fin(nc, p, ar, dbg_d):
    p.finish()
    p.emit()
    return nc, dbg_d


def host_layout(inp, b):
    f = np.float32
    A = np.ascontiguousarray
    d = {}
    d["x"] = A(inp["x"][b])
    d["ctx"] = A(inp["ctx"][b])
    cc = np.stack([inp["c"][b].reshape(8, 128).T, inp["c_ctx"].reshape(8, 128).T], axis=-1)
    d["cc"] = A(cc.astype(f))
    return d


def shared_layout(inp):
    A = np.ascontiguousarray
    d = {}
    d["ada_w"] = A(inp["ada_w"][0].reshape(8, 128, 6144).transpose(1, 0, 2))
    d["ada_b"] = A(inp["ada_b"][0].reshape(1, 6144))
    d["nrm"] = A(np.stack([np.broadcast_to(inp[k].reshape(1, 1024), (128, 1024)) for k in ("norm1_w", "norm2_w", "final_norm_w")]))
    d["w_in"] = A(inp["w_in"][0].reshape(8, 128, 2576).transpose(1, 0, 2))
    d["scw"] = A(inp["ssm_conv_w"][0].reshape(5, 8, 128).transpose(2, 1, 0))
    d["scb"] = A(inp["ssm_conv_b"][0].reshape(8, 128).T)
    d["dtb"] = A(np.broadcast_to(inp["ssm_dt_bias"][0].reshape(1, 16), (128, 16)))
    d["alog"] = A(np.broadcast_to(inp["ssm_a_log"][0].reshape(1, 16), (128, 16)))
    d["drow"] = A(np.broadcast_to(np.repeat(inp["ssm_d"][0], 64).reshape(1, 512), (128, 512)))
    d["snw"] = A(np.broadcast_to(inp["ssm_norm_w"][0].reshape(1, 512), (128, 512)))
    d["ccw"] = A(inp["cfm_conv_w"][0].reshape(31, 4, 128).transpose(2, 1, 0))
    d["cvec"] = A(np.stack([inp[k][0].reshape(4, 128).T for k in ("cfm_conv_b", "cfm_ln_w", "cfm_ln_b")], axis=1))
    d["w_out"] = A(inp["w_out"][0].reshape(8, 128, 1024).transpose(1, 0, 2))
    d["wq"] = A(inp["peer_wq"][0].reshape(8, 128, 2048).transpose(1, 0, 2))
    d["kt"] = A(inp["peer_subkeys"][0].reshape(16, 128, 128).transpose(2, 0, 1))
    d["ut"] = A(inp["peer_u"][0].reshape(128, 128, 8, 128).transpose(1, 3, 2, 0))
    d["vt"] = A(inp["peer_v"][0].reshape(128, 128, 1024).transpose(1, 0, 2))
    return d


_CACHE = {}


def kernel(**inputs):
    inp = {k: np.asarray(v) for k, v in inputs.items()}
    if "nc" not in _CACHE:
        _CACHE["nc"] = build()[0]
    nc = _CACHE["nc"]
    sh = shared_layout(inp)
    in_maps = []
    for b in range(8):
        d = dict(sh)
        d.update(host_layout(inp, b))
        in_maps.append(d)
    res = run_bass_kernel_spmd(nc, in_maps, core_ids=list(range(8)))
    return np.stack([np.asarray(r["out"], dtype=np.float32) for r in res.results], axis=0)
```

```python
import re
import numpy as np
import concourse.bass as bass
import concourse.mybir as mybir
from concourse.bass_utils import run_bass_kernel_spmd

F32 = mybir.dt.float32
BF16 = mybir.dt.bfloat16
I32 = mybir.dt.int32
U32 = mybir.dt.uint32
AF = mybir.ActivationFunctionType
ALU = mybir.AluOpType
AX = mybir.AxisListType
ENGS = ("sync", "scalar", "vector", "gpsimd", "tensor")
CH = 4096
EPS = 1e-6
DSZ = {F32: 4, BF16: 2, I32: 4, U32: 4}


class Prog:
    def __init__(self, nc):
        self.nc = nc
        self.q = {e: [] for e in ENGS}
        self.cnt = {e: 0 for e in ENGS}
        self.known = {e: {} for e in ENGS}
        self.lastw = {}
        self.readers = {}
        self.sems = {}
        self.dmacum = {}
        self.pend = {e: {} for e in ENGS}
        self.lasttok = {}
        self.grp = None

    def sem(self, key):
        s = self.sems.get(key)
        if s is None:
            s = self.nc.alloc_semaphore(name="s%d" % len(self.sems))
            self.sems[key] = s
        return s

    def _waits(self, e, reads, writes):
        toks = dict(self.pend[e])
        self.pend[e] = {}

        def add(t):
            if t is None:
                return
            k, v = t
            if k[0] == "eng" and k[1] == e and e == "tensor":
                return
            if toks.get(k, 0) < v:
                toks[k] = v
        for r in reads:
            add(self.lastw.get(r))
        for w in writes:
            add(self.lastw.get(w))
            for t in self.readers.get(w, ()):
                add(t)
        out = []
        for k, v in toks.items():
            if self.known[e].get(k, 0) >= v:
                continue
            self.known[e][k] = v
            out.append((self.sem(k), v))
        return out

    def _commit(self, tok, reads, writes):
        self.lasttok[tok[0]] = tok[1]
        for w in writes:
            self.lastw[w] = tok
            self.readers[w] = []
        for r in reads:
            self.readers.setdefault(r, []).append(tok)

    def begin(self):
        self.grp = {"ops": [], "r": [], "w": []}

    def end(self):
        g, self.grp = self.grp, None
        if not g["ops"]:
            return
        reads = list(dict.fromkeys(g["r"]))
        writes = list(dict.fromkeys(g["w"]))
        waits = self._waits("tensor", reads, writes)
        idx = self.cnt["tensor"]
        self.cnt["tensor"] += 1
        k = ("eng", "tensor", idx // CH)
        n = len(g["ops"])
        for i, (name, kw) in enumerate(g["ops"]):
            self.q["tensor"].append((waits if i == 0 else [], name, kw, self.sem(k) if i == n - 1 else None, 1 if i == n - 1 else 0))
        self._commit((k, idx % CH + 1), reads, writes)

    def op(self, e, name, r="", w="", **kw):
        reads, writes = r.split(), w.split()
        if e == "tensor" and self.grp is not None:
            ex = [k for k in reads if re.fullmatch(r"(ps|b)\d", k)]
            self.grp["ops"].append((name, kw))
            self.grp["r"] += [k for k in reads if k not in ex]
            self.grp["w"] += writes + ex
            return
        ex = [k for k in reads if re.fullmatch(r"(ps|b)\d", k)]
        reads = [k for k in reads if k not in ex]
        writes = writes + [k for k in ex if k not in writes]
        waits = self._waits(e, reads, writes)
        idx = self.cnt[e]
        self.cnt[e] += 1
        k = ("eng", e, idx // CH)
        self.q[e].append((waits, name, kw, self.sem(k), 1))
        self._commit((k, idx % CH + 1), reads, writes)

    def v(self, name, r="", w="", **kw):
        self.op("vector", name, r, w, **kw)

    def s(self, name, r="", w="", **kw):
        self.op("scalar", name, r, w, **kw)

    def g(self, name, r="", w="", **kw):
        self.op("gpsimd", name, r, w, **kw)

    def t(self, name, r="", w="", **kw):
        self.op("tensor", name, r, w, **kw)

    def dma(self, out, in_, r="", w="", e="sync", grp=None, parts=None, **kw):
        reads, writes = r.split(), w.split()
        sk = ("dma", grp if grp is not None else writes[0])
        waits = self._waits(e, reads, writes)
        pieces = parts if parts is not None else [(out, in_)]
        for i, (o, a) in enumerate(pieces):
            self.dmacum[sk] = self.dmacum.get(sk, 0) + 16
            kk = dict(kw)
            kk.update(out=o, in_=a)
            self.q[e].append((waits if i == 0 else [], "dma_start", kk, self.sem(sk), 16))
        self._commit((sk, self.dmacum[sk]), reads, writes)

    def barrier(self):
        for e in ENGS:
            for k, v in self.lasttok.items():
                if self.pend[e].get(k, 0) < v:
                    self.pend[e][k] = v

    def finish(self):
        self.barrier()
        for e in ("sync", "gpsimd"):
            waits = self._waits(e, [], [])
            self.q[e].append((waits, None, None, None, 0))

    def emit(self):
        nc = self.nc
        with nc.Block() as block:
            def mk(e):
                def body(eng):
                    for waits, name, kw, s, inc in self.q[e]:
                        for ws, wv in waits:
                            eng.wait_ge(ws, wv)
                        if name is not None:
                            ins = getattr(eng, name)(**kw)
                            if s is not None:
                                ins.then_inc(s, inc)
                return body
            block.sync(mk("sync"))
            block.scalar(mk("scalar"))
            block.vector(mk("vector"))
            block.gpsimd(mk("gpsimd"))
            block.tensor(mk("tensor"))


class Arena:
    def __init__(self, nc, nbytes):
        self.t = nc.alloc_sbuf_tensor("arena", [128, nbytes // 4], F32)
        self.cap = nbytes
        self.off = 0
        self.peak = 0

    def a(self, free, dt=F32, parts=128):
        if isinstance(free, int):
            free = (free,)
        n = int(np.prod(free)) * DSZ[dt]
        n = (n + 63) // 64 * 64
        assert self.off + n <= self.cap, ("SBUF arena overflow", self.off, n, self.cap)
        ap = self.t[0:parts, self.off // 4:(self.off + n) // 4]
        self.off += n
        self.peak = max(self.peak, self.off)
        if dt != F32:
            ap = ap.bitcast(dt)
        ap = ap[:, 0:int(np.prod(free))]
        if len(free) == 2:
            ap = ap.rearrange("p (a b) -> p a b", a=free[0])
        elif len(free) == 3:
            ap = ap.rearrange("p (a b c) -> p a b c", a=free[0], b=free[1])
        return ap

    def mark(self):
        return self.off

    def release(self, m):
        self.off = m


L = 4096
LC = 256
NT = L // 128


def build(stop=None, dbg=False, npeer=128):
    nc = bass.Bass("TRN2", target_bir_lowering=False)
    D = nc.dram_tensor

    def din(name, shape, dt=F32):
        return D(name, list(shape), dt, kind="ExternalInput").ap()
    x_d = din("x", [L, 1024])
    ctx_d = din("ctx", [LC, 1024])
    cc_d = din("cc", [128, 8, 2])
    adaw_d = din("ada_w", [128, 8, 6144])
    adab_d = din("ada_b", [1, 6144])
    nrm_d = din("nrm", [3, 128, 1024])
    win_d = din("w_in", [128, 8, 2576])
    scw_d = din("scw", [128, 8, 5])
    scb_d = din("scb", [128, 8])
    dtb_d = din("dtb", [128, 16])
    alog_d = din("alog", [128, 16])
    drow_d = din("drow", [128, 512])
    snw_d = din("snw", [128, 512])
    ccw_d = din("ccw", [128, 4, 31])
    cvec_d = din("cvec", [128, 3, 4])
    wout_d = din("w_out", [128, 8, 1024])
    wq_d = din("wq", [128, 8, 2048])
    kt_d = din("kt", [128, 16, 128])
    ut_d = din("ut", [npeer, 128, 8, 128])
    vt_d = din("vt", [npeer, 128, 1024])
    out_d = D("out", [L, 1024], F32, kind="ExternalOutput").ap()
    dbg_d = {}

    def dout(name, shape):
        dbg_d[name] = D("dbg_" + name, list(shape), F32, kind="ExternalOutput").ap()
        return dbg_d[name]
    modb_s = D("modb_s", [8, 128, 1024], F32).ap()
    zs_s = D("zs_s", [L, 512], BF16).ap()
    x1_s = D("x1_s", [L, 1024], F32).ap()
    utb_s = D("utb_s", [128, 128, 1024], BF16).ap()
    vtb_s = D("vtb_s", [128, 128, 1024], BF16).ap()
    uts_s = D("uts_s", [128, 4, L], BF16).ap()
    wqb_s = D("wqb_s", [16, 128, 1024], BF16).ap()

    p = Prog(nc)
    ar = Arena(nc, 212000)
    banks = [nc.alloc_psum_tensor("pb%d" % i, [128, 512], F32) for i in range(8)]

    def PB(i):
        return banks[i][:]

    def PBb(i):
        return banks[i][:].bitcast(BF16)

    ident_f = ar.a(128)
    ident = ar.a(128, BF16)
    iota_f = ar.a(128)
    pidx = ar.a(128)
    ones_f = ar.a(128)
    ones_b = ar.a(128, BF16)
    triU = ar.a(128)
    triS = ar.a(128)
    maskF = ar.a(128, BF16)
    maskB = ar.a(128, BF16)
    p.g("iota", w="iota", out=iota_f, pattern=[[1, 128]], base=0, channel_multiplier=0, allow_small_or_imprecise_dtypes=True)
    p.g("iota", w="pidx", out=pidx, pattern=[[0, 128]], base=0, channel_multiplier=1, allow_small_or_imprecise_dtypes=True)
    p.v("tensor_tensor", r="iota pidx", w="identf", out=ident_f, in0=iota_f, in1=pidx, op=ALU.is_equal)
    p.v("tensor_copy", r="identf", w="ident", out=ident, in_=ident_f)
    p.v("memset", w="ones_f", ap=ones_f, constant=1.0)
    p.v("memset", w="ones_b", ap=ones_b, constant=1.0)
    p.v("tensor_tensor", r="iota pidx", w="triU", out=triU, in0=iota_f, in1=pidx, op=ALU.is_ge)
    p.v("tensor_tensor", r="iota pidx", w="triS", out=triS, in0=iota_f, in1=pidx, op=ALU.is_gt)
    p.v("tensor_scalar", r="triU", w="maskF", out=maskF, in0=triU, scalar1=-1.0, scalar2=30000.0, op0=ALU.add, op1=ALU.mult)
    tmpc = ar.a(128)
    p.v("tensor_tensor", r="iota pidx", w="tmpc", out=tmpc, in0=iota_f, in1=pidx, op=ALU.is_le)
    p.v("tensor_scalar", r="tmpc", w="maskB", out=maskB, in0=tmpc, scalar1=-1.0, scalar2=30000.0, op0=ALU.add, op1=ALU.mult)
    sel = ar.a((16, 128), BF16)
    p.v("memset", w="sel", ap=sel, constant=0.0)
    p.v("tensor_copy", r="identf sel", w="sel", out=sel[0:16], in_=ident_f[0:16, 0:16].unsqueeze(2).to_broadcast([16, 16, 128]))
    scw = ar.a((8, 5)); scb = ar.a(8); dtbias = ar.a(16); alog = ar.a(16); ccw = ar.a((4, 31)); cvec = ar.a((3, 4))
    Aneg = ar.a(16)
    p.dma(scw, scw_d, w="scw", grp="small")
    p.dma(scb, scb_d, w="scb", grp="small")
    p.dma(dtbias, dtb_d, w="dtbias", grp="small")
    p.dma(alog, alog_d, w="alog", grp="small")
    p.dma(ccw, ccw_d, w="ccw", grp="small")
    p.dma(cvec, cvec_d, w="cvec", grp="small")
    p.s("activation", r="alog", w="Aneg", out=Aneg, in_=alog, func=AF.Exp)
    p.v("tensor_scalar", r="Aneg", w="Aneg", out=Aneg, in0=Aneg, scalar1=-1.0, scalar2=None, op0=ALU.mult)
    dt_all = ar.a((NT + 2, 16))
    m_persist = ar.mark()

    m0 = ar.mark()
    cc = ar.a((8, 2)); scT = ar.a((8, 2))
    adab = ar.a(6144, parts=1)
    modrow = ar.a(6144, parts=1)
    modrow_c = ar.a(2048, parts=1)
    awb = [ar.a((8, 512)) for _ in range(2)]
    nrm = [ar.a(1024) for _ in range(2)]
    bct = [ar.a(1024) for _ in range(2)]
    p.dma(cc, cc_d, w="cc")
    p.dma(adab, adab_d, w="adab")
    p.dma(nrm[0], nrm_d[0], w="nrm0")
    p.dma(nrm[1], nrm_d[1], w="nrm1")
    p.s("activation", r="cc", w="scT", out=scT, in_=cc, func=AF.Silu)
    for blk in range(12):
        sl = blk % 2
        p.dma(awb[sl], adaw_d[:, :, blk * 512:(blk + 1) * 512], w="awb%d" % sl)
        for kc in range(8):
            p.t("matmul", r="scT awb%d" % sl, w="ps0", out=PB(0)[0:1, :], lhsT=scT[:, kc, 0:1], rhs=awb[sl][:, kc, :],
                start=(kc == 0), stop=(kc == 7))
        if blk < 4:
            for kc in range(8):
                p.t("matmul", r="scT awb%d" % sl, w="ps1", out=PB(1)[0:1, :], lhsT=scT[:, kc, 1:2], rhs=awb[sl][:, kc, :],
                    start=(kc == 0), stop=(kc == 7))
            p.v("tensor_tensor", r="ps1 adab", w="modrow_c", out=modrow_c[:, blk * 512:(blk + 1) * 512], in0=PB(1)[0:1, :],
                in1=adab[:, blk * 512:(blk + 1) * 512], op=ALU.add)
        p.v("tensor_tensor", r="ps0 adab", w="modrow", out=modrow[:, blk * 512:(blk + 1) * 512], in0=PB(0)[0:1, :],
            in1=adab[:, blk * 512:(blk + 1) * 512], op=ALU.add)
    plan = [(0, modrow, 1, 0), (1, modrow, 0, None), (2, modrow_c, 1, 0), (3, modrow_c, 0, None), (4, modrow, 2, None),
            (5, modrow, 4, 1), (6, modrow, 3, None), (7, modrow, 5, None)]
    for i, (ti, src, mi, ni) in enumerate(plan):
        bt = bct[i % 2]
        bk = "bct%d" % (i % 2)
        for hf in range(2):
            p.t("matmul", r="ones_f modrow modrow_c", w="ps%d" % (2 + hf), out=PB(2 + hf), lhsT=ones_f[0:1, :],
                rhs=src[:, mi * 1024 + hf * 512: mi * 1024 + (hf + 1) * 512], start=True, stop=True)
            if ni is None:
                p.v("tensor_copy", r="ps%d" % (2 + hf), w=bk, out=bt[:, hf * 512:(hf + 1) * 512], in_=PB(2 + hf))
            else:
                p.v("scalar_tensor_tensor", r="ps%d nrm%d" % (2 + hf, ni), w=bk, out=bt[:, hf * 512:(hf + 1) * 512], in0=PB(2 + hf),
                    scalar=1.0, in1=nrm[ni][:, hf * 512:(hf + 1) * 512], op0=ALU.add, op1=ALU.mult)
        p.dma(modb_s[ti], bt, r=bk, w="modb_s", grp="modb_w")
    if dbg:
        p.dma(dout("modrow", [1, 6144]), modrow, r="modrow", w="dbg_modrow")
    ar.release(m0)
    p.barrier()
    if stop == "0":
        return fin(nc, p, ar, dbg_d)

    m_big = ar.mark()
    xbcT = ar.a((8, L), BF16)
    xbcTc = ar.a((8, LC), BF16)
    m_uT = ar.mark()
    uT = ar.a((4, L), BF16)

    mA = ar.mark()
    Wb = ar.a((8, 2576), BF16)
    wst = [ar.a((8, 128)) for _ in range(2)]
    W1 = ar.a(1024); S1 = ar.a(1024)

    def load_mod(i0):
        p.dma(W1, modb_s[i0], r="modb_s", w="modA0", grp="modA")
        p.dma(S1, modb_s[i0 + 1], r="modb_s", w="modA1", grp="modA")
    ncol = 2576
    pieces = [(c0, min(128, ncol - c0)) for c0 in range(0, ncol, 128)]
    for i, (c0, cw) in enumerate(pieces):
        sl = i % 2
        p.dma(wst[sl][:, :, 0:cw], win_d[:, :, c0:c0 + cw], w="wst%d" % sl)
        if i % 2 == 0:
            p.v("tensor_copy", r="wst%d" % sl, w="Wb", out=Wb[:, :, c0:c0 + cw], in_=wst[sl][:, :, 0:cw])
        else:
            p.s("copy", r="wst%d" % sl, w="Wb", out=Wb[:, :, c0:c0 + cw], in_=wst[sl][:, :, 0:cw])
    xt = [ar.a(1024) for _ in range(2)]
    junk = ar.a(1024, BF16)
    hb = [ar.a(1024, BF16) for _ in range(2)]
    hT2 = [ar.a((8, 512), BF16) for _ in range(2)]
    ss = ar.a(2); rs = ar.a(2)
    sig = ar.a(512)
    zsb = [ar.a(512, BF16) for _ in range(2)]
    dtt = ar.a(16)

    def inproj_pro(src_d, t0, ntile, Wm, Sm, par):
        hT = hT2[par]
        hk = "hT%d" % par
        for ts in range(ntile):
            sl = ts % 2
            p.dma(xt[sl], src_d[t0 + ts * 128:t0 + (ts + 1) * 128, :], w="xt%d" % sl)
            p.s("activation", r="xt%d" % sl, w="junk ss", out=junk, in_=xt[sl], func=AF.Square, accum_out=ss[:, 0:1])
            p.v("tensor_scalar", r="ss", w="rs", out=rs[:, 0:1], in0=ss[:, 0:1], scalar1=1.0 / 1024, scalar2=EPS, op0=ALU.mult, op1=ALU.add)
            p.s("activation", r="rs", w="rs", out=rs[:, 0:1], in_=rs[:, 0:1], func=AF.Ln)
            p.s("activation", r="rs", w="rs", out=rs[:, 0:1], in_=rs[:, 0:1], func=AF.Exp, scale=-0.5)
            p.v("scalar_tensor_tensor", r="xt%d rs modA0" % sl, w="xt%d" % sl, out=xt[sl], in0=xt[sl], scalar=rs[:, 0:1], in1=Wm,
                op0=ALU.mult, op1=ALU.mult)
            p.g("tensor_tensor", r="xt%d modA1" % sl, w="hb%d" % sl, out=hb[sl], in0=xt[sl], in1=Sm, op=ALU.add)
            yield
            for kc in range(8):
                p.t("transpose", r="hb%d ident" % sl, w="ps%d" % (4 + sl), out=PBb(4 + sl)[:, kc * 128:(kc + 1) * 128],
                    in_=hb[sl][:, kc * 128:(kc + 1) * 128], identity=ident)
            yield
            p.s("copy", r="ps%d" % (4 + sl), w=hk, out=hT[:, :, ts * 128:(ts + 1) * 128],
                in_=PBb(4 + sl).rearrange("p (a b) -> p a b", a=8))
            yield

    def inproj_mm(t0, ntile, is_ctx, tile0, par, nxt):
        ntok = ntile * 128
        hT = hT2[par]
        hk = "hT%d" % par

        def tick():
            if nxt[0] is not None:
                try:
                    next(nxt[0])
                except StopIteration:
                    nxt[0] = None
        for j in range(8):
            pk = j % 2
            c0 = 512 + j * 128
            for kc in range(8):
                p.t("matmul", r="Wb " + hk, w="ps%d" % pk, out=PB(pk)[:, 0:ntok], lhsT=Wb[:, kc, c0:c0 + 128], rhs=hT[:, kc, 0:ntok],
                    start=(kc == 0), stop=(kc == 7))
            dst = xbcTc[:, j, 0:ntok] if is_ctx else xbcT[:, j, t0:t0 + ntok]
            if j % 2 == 0:
                p.s("copy", r="ps%d" % pk, w="xbcT", out=dst, in_=PB(pk)[:, 0:ntok])
            else:
                p.v("tensor_copy", r="ps%d" % pk, w="xbcT", out=dst, in_=PB(pk)[:, 0:ntok])
            tick()
        if not is_ctx:
            for j in range(4):
                ca = 1552 + j * 128
                cb = 1552 + 512 + j * 128
                for kc in range(8):
                    p.t("matmul", r="Wb " + hk, w="ps2", out=PB(2)[:, 0:ntok], lhsT=Wb[:, kc, ca:ca + 128], rhs=hT[:, kc, 0:ntok],
                        start=(kc == 0), stop=(kc == 7))
                for kc in range(8):
                    p.t("matmul", r="Wb " + hk, w="ps3", out=PB(3)[:, 0:ntok], lhsT=Wb[:, kc, cb:cb + 128], rhs=hT[:, kc, 0:ntok],
                        start=(kc == 0), stop=(kc == 7))
                p.s("activation", r="ps3", w="sig", out=sig[:, 0:ntok], in_=PB(3)[:, 0:ntok], func=AF.Sigmoid)
                p.v("tensor_tensor", r="ps2 sig", w="uT", out=uT[:, j, t0:t0 + ntok], in0=PB(2)[:, 0:ntok], in1=sig[:, 0:ntok], op=ALU.mult)
                tick()
        for ts in range(ntile):
            sl = ts % 2
            if not is_ctx:
                for kc in range(8):
                    p.t("matmul", r="Wb " + hk, w="ps%d" % (6 + sl), out=PB(6 + sl), lhsT=hT[:, kc, ts * 128:(ts + 1) * 128], rhs=Wb[:, kc, 0:512],
                        start=(kc == 0), stop=(kc == 7))
                p.s("activation", r="ps%d" % (6 + sl), w="zsb%d" % sl, out=zsb[sl], in_=PB(6 + sl), func=AF.Silu)
                p.dma(zs_s[t0 + ts * 128:t0 + (ts + 1) * 128, :], zsb[sl], r="zsb%d" % sl, w="zs_s", e="sync", grp="zsw%d" % sl)
            for kc in range(8):
                p.t("matmul", r="Wb " + hk, w="ps0", out=PB(0)[:, 0:16], lhsT=hT[:, kc, ts * 128:(ts + 1) * 128], rhs=Wb[:, kc, 1536:1552],
                    start=(kc == 0), stop=(kc == 7))
            p.v("tensor_tensor", r="ps0 dtbias", w="dtt", out=dtt, in0=PB(0)[:, 0:16], in1=dtbias, op=ALU.add)
            p.s("activation", r="dtt", w="dtt", out=dtt, in_=dtt, func=AF.Exp)
            p.s("activation", r="dtt", w="dt_all", out=dt_all[:, tile0 + ts, :], in_=dtt, func=AF.Ln, bias=1.0)
            tick()
        while nxt[0] is not None:
            tick()

    load_mod(2)
    for _ in inproj_pro(ctx_d, 0, 2, W1, S1, 0):
        pass
    inproj_mm(0, 2, True, NT, 0, [None])
    load_mod(0)
    nbk = L // 512
    for _ in inproj_pro(x_d, 0, 4, W1, S1, 1):
        pass
    for b in range(nbk):
        par = (b + 1) % 2
        nxt = [inproj_pro(x_d, (b + 1) * 512, 4, W1, S1, 1 - par) if b + 1 < nbk else None]
        inproj_mm(b * 512, 4, False, b * 4, par, nxt)
    if dbg:
        p.dma(dout("dt_all", [128, NT + 2, 16]), dt_all, r="dt_all", w="dbg_dt")
    ar.release(mA)
    p.barrier()
    if stop == "A":
        return fin(nc, p, ar, dbg_d)

    mB = ar.mark()
    acc = [ar.a(L) for _ in range(2)]
    EN = [p.v, p.v]

    def ssm_conv(src, n, ei, j, key):
        E = EN[ei]
        ak = "acc%d" % ei
        a_ = acc[ei][:, 0:n]
        x_ = src[:, j, 0:n]
        E("tensor_scalar", r=key + " scw", w=ak, out=a_, in0=x_, scalar1=scw[:, j, 2:3], scalar2=None, op0=ALU.mult)
        for k in (0, 1, 3, 4):
            o = k - 2
            lo, hi = max(0, -o), min(n, n - o)
            E("scalar_tensor_tensor", r=key + " scw " + ak, w=ak, out=a_[:, lo:hi], in0=x_[:, lo + o:hi + o], scalar=scw[:, j, k:k + 1],
              in1=a_[:, lo:hi], op0=ALU.mult, op1=ALU.add)
        p.s("activation", r=ak + " scb", w=key, out=x_, in_=a_, func=AF.Silu, bias=scb[:, j:j + 1])

    tmpc2 = [ar.a(L) for _ in range(2)]

    def cfm_conv(j, ei):
        ak = "acc%d" % ei
        key = "uT%d" % j
        a_ = acc[ei]
        u_ = uT[:, j, :]
        a3 = a_.rearrange("p (r c) -> p r c", c=64)
        u3 = u_.rearrange("p (r c) -> p r c", c=64)
        if ei == 0:
            p.v("tensor_scalar", r=key + " ccw", w=ak, out=a_, in0=u_, scalar1=ccw[:, j, 15:16], scalar2=None, op0=ALU.mult)
        else:
            p.s("activation", r=key + " ccw", w=ak, out=a_, in_=u_, func=AF.Copy, scale=ccw[:, j, 15:16])
        n = 0
        for k in range(31):
            o = k - 15
            if o == 0:
                continue
            lo, hi = max(0, -o), min(64, 64 - o)
            if j < 2:
                oo, i0 = a3[:, :, lo:hi], u3[:, :, lo + o:hi + o]
            else:
                oo, i0 = a_[:, lo * 64:hi * 64], u_[:, (lo + o) * 64:(hi + o) * 64]
            if ei == 0:
                p.v("scalar_tensor_tensor", r=key + " ccw " + ak, w=ak, out=oo, in0=i0, scalar=ccw[:, j, k:k + 1], in1=oo, op0=ALU.mult, op1=ALU.add)
            else:
                tk = "tmpc2%d" % (n % 2)
                tt = tmpc2[n % 2]
                if j < 2:
                    t_ = tt.rearrange("p (r c) -> p r c", c=64)[:, :, lo:hi]
                else:
                    t_ = tt[:, lo * 64:hi * 64]
                p.s("activation", r=key + " ccw", w=tk, out=t_, in_=i0, func=AF.Copy, scale=ccw[:, j, k:k + 1])
                p.g("tensor_tensor", r=tk + " " + ak, w=ak, out=oo, in0=oo, in1=t_, op=ALU.add)
                n += 1
        p.s("activation", r=ak + " cvec", w=key, out=u_, in_=a_, func=AF.Identity, bias=cvec[:, 0, j:j + 1])

    import os
    NCD = int(os.environ.get("CFMDVE", "4"))
    for j in range(8):
        ssm_conv(xbcTc, LC, 0, j, "xbcTc%d" % j)
    pool_chunks = [2, 3, 0, 1][:4 - NCD]
    dve_chunks = [2, 3, 0, 1][4 - NCD:]
    if pool_chunks:
        cfm_conv(pool_chunks[0], 1)
    for j in range(8):
        ssm_conv(xbcT, L, 0, j, "xbcT%d" % j)
    for j in pool_chunks[1:]:
        cfm_conv(j, 1)
    for j in dve_chunks:
        cfm_conv(j, 0)
    if dbg:
        tdb = ar.a((8, 128))
        p.v("tensor_copy", r=" ".join("xbcT%d" % j for j in range(8)), w="tdb", out=tdb, in_=xbcT[:, :, 512:640])
        p.dma(dout("xbc_post", [128, 8, 128]), tdb, r="tdb", w="dbg_xb2")
        tdb2 = ar.a((4, 128))
        p.v("tensor_copy", r=" ".join("uT%d" % j for j in range(4)), w="tdb2", out=tdb2, in_=uT[:, :, 512:640])
        p.dma(dout("u_post", [128, 4, 128]), tdb2, r="tdb2", w="dbg_ub2")
    for j in range(4):
        p.dma(uts_s[:, j, :], uT[:, j, :], r="uT%d" % j, w="uts_s", grp="utsw")
    ar.release(m_uT)
    p.barrier()
    if stop == "B":
        return fin(nc, p, ar, dbg_d)

    mE = ar.mark()
    SB_all = ar.a((NT, 512), BF16)
    Wo = ar.a((8, 1024), BF16)
    G1 = ar.a(1024); NW = ar.a(512); Drow = ar.a(512)
    p.dma(G1, modb_s[4], r="modb_s", w="G1")
    p.dma(NW, snw_d, w="NW")
    p.dma(Drow, drow_d, w="Drow")
    a16 = ar.a(16); cs = ar.a(32); dec = ar.a(16); tmp16 = ar.a(16); w16 = ar.a(16)
    rowsrc = ar.a(16); bias16 = ar.a(16); e16 = ar.a(16); RT = ar.a(128); RT3 = ar.a((3, 128), BF16); RTr = ar.a(128)
    xs_tok = ar.a(512, BF16); Btok = ar.a(256, BF16)
    xw = [ar.a(512, BF16) for _ in range(2)]
    xdt = [ar.a(512, BF16) for _ in range(2)]
    ST_sb = ar.a((2, 128))
    LT = [ar.a(128) for _ in range(2)]
    SmT = ar.a((16, 128), BF16)
    Sst = [ar.a(512) for _ in range(2)]
    Sbf = [ar.a(512, BF16) for _ in range(2)]
    yc = ar.a(512); yc2 = ar.a(512); zst = ar.a(512, BF16); gg = ar.a(512)
    ss2 = ar.a(2); rs2 = ar.a(2)
    ssm_tok = ar.a(512, BF16); catT = ar.a((8, 128), BF16)
    vsq = ar.a((4, 128)); vsqh = ar.a((4, 128), BF16); vsql = ar.a((4, 128), BF16); mean = ar.a(128); msq = ar.a(128); var = ar.a(128); ntmp = ar.a((4, 128))
    xe = [ar.a(1024) for _ in range(2)]
    for i in range(8):
        sl = i % 2
        wv_ = xe[sl].rearrange("p (a b) -> p a b", a=8)
        p.dma(wv_, wout_d[:, :, i * 128:(i + 1) * 128], w="xe%d" % sl)
        if i % 2 == 0:
            p.v("tensor_copy", r="xe%d" % sl, w="Wo", out=Wo[:, :, i * 128:(i + 1) * 128], in_=wv_)
        else:
            p.s("copy", r="xe%d" % sl, w="Wo", out=Wo[:, :, i * 128:(i + 1) * 128], in_=wv_)
    junk2 = ar.a(256, BF16)
    uTt = [ar.a((4, 128), BF16) for _ in range(2)]
    PT = PBb(0)
    PC = PB(1)
    PK = PB(2)
    PY = PB(3)
    PLS = PB(6)
    PM = PBb(7)
    PM2 = PB(7)

    def bc8(ap8):
        return ap8.unsqueeze(2).to_broadcast([128, 8, 64])

    def v3(ap512):
        return ap512.rearrange("p (h q) -> p h q", h=8)

    def prep(is_ctx, c, dirs):
        src = xbcTc if is_ctx else xbcT
        kx = "xbcTc%d" if is_ctx else "xbcT%d"
        tl = (NT + c) if is_ctx else c
        tok = slice(c * 128, (c + 1) * 128)
        p.v("tensor_tensor", r="dt_all Aneg", w="a16", out=a16, in0=dt_all[:, tl, :], in1=Aneg, op=ALU.mult)
        p.t("matmul", r="triU a16", w="b1", out=PC[:, 0:8], lhsT=triU, rhs=a16[:, 0:8], start=True, stop=True)
        p.t("matmul", r="triS a16", w="b1", out=PC[:, 8:16], lhsT=triS, rhs=a16[:, 8:16], start=True, stop=True)
        p.t("matmul", r="ones_f a16", w="b1", out=PC[:, 16:32], lhsT=ones_f, rhs=a16, start=True, stop=True)
        p.s("copy", r="b1", w="cs", out=cs, in_=PC[:, 0:32])
        p.s("activation", r="cs", w="dec", out=dec, in_=cs[:, 16:32], func=AF.Exp)
        p.v("tensor_tensor", r="cs", w="tmp16", out=tmp16[:, 0:8], in0=cs[:, 16:24], in1=cs[:, 0:8], op=ALU.subtract)
        p.v("tensor_copy", r="cs tmp16", w="tmp16", out=tmp16[:, 8:16], in_=cs[:, 8:16])
        p.s("activation", r="tmp16", w="tmp16", out=tmp16, in_=tmp16, func=AF.Exp)
        p.v("tensor_tensor", r="tmp16 dt_all", w="w16", out=w16, in0=tmp16, in1=dt_all[:, tl, :], op=ALU.mult)
        for j in range(4):
            p.t("transpose", r=(kx % j) + " ident", w="b0", out=PT[:, j * 128:(j + 1) * 128], in_=src[:, j, tok], identity=ident)
        for g in range(2):
            p.t("transpose", r=(kx % (4 + g)) + " ident", w="b0", out=PT[:, 512 + g * 128:512 + (g + 1) * 128], in_=src[:, 4 + g, tok], identity=ident)
        p.v("tensor_copy", r="b0", w="Btok", out=Btok, in_=PT[:, 512:768])
        for d in dirs:
            p.v("tensor_tensor", r="b0 w16", w="xw%d" % d, out=v3(xw[d]), in0=v3(PT[:, 0:512]), in1=bc8(w16[:, d * 8:(d + 1) * 8]), op=ALU.mult)

    def local_states(d):
        for h in range(8):
            g = h // 4
            p.t("matmul", r="Btok xw%d" % d, w="b6", out=PLS[:, h * 64:(h + 1) * 64], lhsT=Btok[:, g * 128:(g + 1) * 128],
                rhs=xw[d][:, h * 64:(h + 1) * 64], start=True, stop=True)

    def upd(d):
        p.v("tensor_tensor", r="S%d dec" % d, w="S%d" % d, out=v3(Sst[d]), in0=v3(Sst[d]), in1=bc8(dec[:, d * 8:(d + 1) * 8]), op=ALU.mult)
        p.v("tensor_tensor", r="S%d b6" % d, w="S%d" % d, out=Sst[d], in0=Sst[d], in1=PLS, op=ALU.add)
        p.s("copy", r="S%d" % d, w="Sbf%d" % d, out=Sbf[d], in_=Sst[d])

    p.v("memset", w="RT3", ap=RT3, constant=0.0)
    for d in range(2):
        p.v("memset", w="S%d" % d, ap=Sst[d], constant=0.0)
        p.v("memset", w="Sbf%d" % d, ap=Sbf[d], constant=0.0)
    prep(True, 0, (0,)); local_states(0); upd(0)
    prep(True, 1, (0, 1)); local_states(0); upd(0); local_states(1); upd(1)
    prep(True, 0, (1,)); local_states(1); upd(1)
    if stop == "C":
        p.dma(dout("S_f", [128, 512]), Sst[0], r="S0", w="dbg_sf")
        p.dma(dout("S_b", [128, 512]), Sst[1], r="S1", w="dbg_sb")
        return fin(nc, p, ar, dbg_d)
    for c in range(NT - 1, -1, -1):
        p.s("copy", r="Sbf1", w="SB%d" % c, out=SB_all[:, c, :], in_=Sbf[1])
        prep(False, c, (1,)); local_states(1); upd(1)
        if npeer == 128:
            if c >= 16:
                hp_ = c - 16
                p.dma(wqb_s[hp_].rearrange("p (k c) -> p k c", k=8), wq_d[:, :, hp_ * 128:(hp_ + 1) * 128], w="wqb_s", e="gpsimd", grp="castq")
            for i2 in range(c * 4, c * 4 + 4):
                p.dma(utb_s[i2], ut_d[i2].rearrange("p k i -> p (k i)"), w="utb_s", e="gpsimd", grp="castu")
                p.dma(vtb_s[i2], vt_d[i2], w="vtb_s", e="gpsimd", grp="castv")
    if dbg:
        p.dma(dout("S_b0", [128, 512]), Sst[1], r="S1", w="dbg_sb0")
    if stop == "D":
        return fin(nc, p, ar, dbg_d)
    for c in range(NT):
        tok = slice(c * 128, (c + 1) * 128)
        pe = c % 2
        p.dma(xe[pe], x_d[tok, :], w="xe%d" % pe)
        p.dma(zst, zs_s[tok, :], r="zs_s", w="zst")
        p.dma(uTt[pe], uts_s[:, :, tok], r="uts_s", w="uTt%d" % pe)
        uTc = uTt[pe]
        if stop == "E0":
            return fin(nc, p, ar, dbg_d)
        prep(False, c, (0,))
        if stop == "E1":
            return fin(nc, p, ar, dbg_d)
        p.v("tensor_copy", r="b0", w="xs_tok", out=xs_tok, in_=PT[:, 0:512])
        for d in range(2):
            p.v("tensor_tensor", r="b0 dt_all", w="xdt%d" % d, out=v3(xdt[d]), in0=v3(PT[:, 0:512]),
                in1=bc8(dt_all[:, c, d * 8:(d + 1) * 8]), op=ALU.mult)
        if stop == "E1a":
            return fin(nc, p, ar, dbg_d)
        for g in range(2):
            p.t("matmul", r="xbcT%d xbcT%d" % (4 + g, 6 + g), w="b1", out=PC[:, 256 + g * 128:256 + (g + 1) * 128],
                lhsT=xbcT[:, 4 + g, tok], rhs=xbcT[:, 6 + g, tok], start=True, stop=True)
        if stop == "E1b":
            return fin(nc, p, ar, dbg_d)
        p.s("copy", r="b1", w="ST_sb", out=ST_sb, in_=PC[:, 256:512].rearrange("p (g l) -> p g l", g=2))
        if stop == "E2":
            return fin(nc, p, ar, dbg_d)

        p.v("tensor_copy", r="cs", w="rowsrc", out=rowsrc[:, 0:8], in_=cs[:, 0:8])
        p.v("tensor_scalar", r="cs rowsrc", w="rowsrc", out=rowsrc[:, 8:16], in0=cs[:, 8:16], scalar1=-1.0, scalar2=None, op0=ALU.mult)
        p.v("tensor_scalar", r="rowsrc", w="bias16", out=bias16, in0=rowsrc, scalar1=-1.0, scalar2=None, op0=ALU.mult)
        p.t("transpose", r="rowsrc identf", w="b1", out=PC[0:16, 32:160], in_=rowsrc, identity=ident_f)
        p.s("copy", r="b1", w="RT", out=RT[0:16, :], in_=PC[0:16, 32:160])
        p.v("tensor_copy", r="RT", w="RT3", out=RT3[0:16, 0, :], in_=RT[0:16, :])
        p.v("tensor_tensor", r="RT RT3", w="RTr", out=RTr[0:16, :], in0=RT[0:16, :], in1=RT3[0:16, 0, :], op=ALU.subtract)
        p.v("tensor_copy", r="RTr RT3", w="RT3", out=RT3[0:16, 1, :], in_=RTr[0:16, :])
        p.v("tensor_tensor", r="RTr RT3", w="RTr", out=RTr[0:16, :], in0=RTr[0:16, :], in1=RT3[0:16, 1, :], op=ALU.subtract)
        p.v("tensor_copy", r="RTr RT3", w="RT3", out=RT3[0:16, 2, :], in_=RTr[0:16, :])
        if stop == "E3":
            return fin(nc, p, ar, dbg_d)

        p.v("tensor_copy", r="cs", w="e16", out=e16[:, 0:8], in_=cs[:, 0:8])
        p.v("tensor_tensor", r="cs e16", w="e16", out=e16[:, 8:16], in0=cs[:, 24:32], in1=cs[:, 8:16], op=ALU.subtract)
        p.s("activation", r="e16", w="e16", out=e16, in_=e16, func=AF.Exp)
        if stop == "E4":
            return fin(nc, p, ar, dbg_d)

        for idx in range(16):
            d, h = idx // 8, idx % 8
            g = h // 4
            pk = "b2" if idx % 2 == 0 else "b6"
            PKs = (PB(2) if idx % 2 == 0 else PB(6))[:, 0:128]
            for q in range(3):
                p.t("matmul", r="sel RT3", w=pk, out=PKs, lhsT=sel[:, idx, :], rhs=RT3[:, q, :], start=(q == 0), stop=False)
            p.t("matmul", r="ident maskF maskB", w=pk, out=PKs, lhsT=ident, rhs=(maskF if d == 0 else maskB), start=False, stop=True)
            lk = "LT%d" % (idx % 2)
            p.s("activation", r=pk + " bias16", w=lk, out=LT[idx % 2], in_=PKs, func=AF.Exp, bias=bias16[:, idx:idx + 1])
            p.v("tensor_tensor", r=lk + " ST_sb", w="SmT%d" % idx, out=SmT[:, idx, :], in0=LT[idx % 2], in1=ST_sb[:, g, :], op=ALU.mult)
        if stop == "E5a":
            return fin(nc, p, ar, dbg_d)
        for h in range(8):
            hs = slice(h * 64, (h + 1) * 64)
            p.t("matmul", r="SmT%d xdt0" % h, w="b3", out=PY[:, hs], lhsT=SmT[:, h, :], rhs=xdt[0][:, hs], start=True, stop=False)
            p.t("matmul", r="SmT%d xdt1" % (8 + h), w="b3", out=PY[:, hs], lhsT=SmT[:, 8 + h, :], rhs=xdt[1][:, hs], start=False, stop=True)
        for h in range(8):
            g = h // 4
            hs = slice(h * 64, (h + 1) * 64)
            p.t("matmul", r="xbcT%d Sbf0" % (6 + g), w="b4", out=PB(4)[:, hs], lhsT=xbcT[:, 6 + g, tok], rhs=Sbf[0][:, hs], start=True, stop=True)
            p.t("matmul", r="xbcT%d SB%d" % (6 + g, c), w="b5", out=PB(5)[:, hs], lhsT=xbcT[:, 6 + g, tok], rhs=SB_all[:, c, hs], start=True, stop=True)
        if stop == "E5b":
            return fin(nc, p, ar, dbg_d)
        p.v("tensor_tensor", r="b4 e16", w="yc", out=v3(yc), in0=v3(PB(4)), in1=bc8(e16[:, 0:8]), op=ALU.mult)
        p.v("tensor_tensor", r="b5 e16", w="yc2", out=v3(yc2), in0=v3(PB(5)), in1=bc8(e16[:, 8:16]), op=ALU.mult)
        p.g("tensor_tensor", r="yc yc2", w="yc", out=yc, in0=yc, in1=yc2, op=ALU.add)
        if stop == "E6":
            return fin(nc, p, ar, dbg_d)

        p.v("tensor_tensor", r="yc b3", w="yc", out=yc, in0=yc, in1=PY, op=ALU.add)
        p.g("tensor_tensor", r="xs_tok Drow", w="yc2", out=yc2, in0=xs_tok, in1=Drow, op=ALU.mult)
        p.v("tensor_tensor", r="yc yc2", w="yc", out=yc, in0=yc, in1=yc2, op=ALU.add)
        p.v("tensor_tensor", r="yc zst", w="gg", out=gg, in0=yc, in1=zst, op=ALU.mult)
        for g in range(2):
            p.s("activation", r="gg", w="junk2 ss2", out=junk2, in_=gg[:, g * 256:(g + 1) * 256], func=AF.Square, accum_out=ss2[:, g:g + 1])
        p.v("tensor_scalar", r="ss2", w="rs2", out=rs2, in0=ss2, scalar1=1.0 / 256, scalar2=EPS, op0=ALU.mult, op1=ALU.add)
        p.s("activation", r="rs2", w="rs2", out=rs2, in_=rs2, func=AF.Ln)
        p.s("activation", r="rs2", w="rs2", out=rs2, in_=rs2, func=AF.Exp, scale=-0.5)
        for g in range(2):
            gs = slice(g * 256, (g + 1) * 256)
            p.v("scalar_tensor_tensor", r="gg rs2 NW", w="ssm_tok", out=ssm_tok[:, gs], in0=gg[:, gs], scalar=rs2[:, g:g + 1], in1=NW[:, gs],
                op0=ALU.mult, op1=ALU.mult)
        for j in range(4):
            p.t("transpose", r="ssm_tok ident", w="b7", out=PM[:, j * 128:(j + 1) * 128], in_=ssm_tok[:, j * 128:(j + 1) * 128], identity=ident)
        p.s("copy", r="b7", w="catT", out=catT[:, 0:4, :], in_=PM[:, 0:512].rearrange("p (a b) -> p a b", a=4))
        if stop == "E7":
            return fin(nc, p, ar, dbg_d)

        ukeys = "uTt%d" % pe
        p.s("activation", r=ukeys, w="vsq", out=vsq, in_=uTc, func=AF.Square)
        for j in range(4):
            p.t("matmul", r="ones_b " + ukeys, w="b7", out=PM2[:, 256:384], lhsT=ones_b, rhs=uTc[:, j, :], start=(j == 0), stop=(j == 3))
        p.v("tensor_copy", r="vsq", w="vsqh", out=vsqh, in_=vsq)
        p.v("tensor_tensor", r="vsq vsqh", w="vsql", out=vsql, in0=vsq, in1=vsqh, op=ALU.subtract)
        for j in range(4):
            p.t("matmul", r="ones_b vsqh", w="b7", out=PM2[:, 384:512], lhsT=ones_b, rhs=vsqh[:, j, :], start=(j == 0), stop=False)
            p.t("matmul", r="ones_b vsql", w="b7", out=PM2[:, 384:512], lhsT=ones_b, rhs=vsql[:, j, :], start=False, stop=(j == 3))
        p.s("activation", r="b7", w="mean", out=mean, in_=PM2[:, 256:384], func=AF.Copy, scale=1.0 / 512)
        p.s("activation", r="b7", w="var", out=var, in_=PM2[:, 384:512], func=AF.Copy, scale=1.0 / 512)
        p.g("tensor_tensor", r="mean", w="msq", out=msq, in0=mean, in1=mean, op=ALU.mult)
        p.v("tensor_tensor", r="var msq", w="var", out=var, in0=var, in1=msq, op=ALU.subtract)
        p.v("tensor_scalar", r="var", w="var", out=var, in0=var, scalar1=EPS, scalar2=None, op0=ALU.add)
        p.s("activation", r="var", w="var", out=var, in_=var, func=AF.Ln)
        p.s("activation", r="var", w="var", out=var, in_=var, func=AF.Exp, scale=-0.5)
        p.v("tensor_tensor", r=ukeys + " mean", w="ntmp", out=ntmp, in0=uTc, in1=mean.unsqueeze(1).to_broadcast([128, 4, 128]), op=ALU.subtract)
        p.v("tensor_tensor", r="ntmp var", w="ntmp", out=ntmp, in0=ntmp, in1=var.unsqueeze(1).to_broadcast([128, 4, 128]), op=ALU.mult)
        for j in range(4):
            p.s("activation", r="ntmp cvec", w="catT", out=catT[:, 4 + j, :], in_=ntmp[:, j, :], func=AF.Silu, scale=cvec[:, 1, j:j + 1],
                bias=cvec[:, 2, j:j + 1])
        for nb in range(2):
            for kc in range(8):
                p.t("matmul", r="catT Wo", w="b%d" % (4 + nb), out=PB(4 + nb), lhsT=catT[:, kc, :], rhs=Wo[:, kc, nb * 512:(nb + 1) * 512],
                    start=(kc == 0), stop=(kc == 7))
            ns = slice(nb * 512, (nb + 1) * 512)
            tq, tk_ = (yc2, "yc2") if nb == 0 else (gg, "gg")
            p.v("tensor_tensor", r="b%d G1" % (4 + nb), w=tk_, out=tq, in0=PB(4 + nb), in1=G1[:, ns], op=ALU.mult)
            p.g("tensor_tensor", r=tk_ + " xe%d" % pe, w="xe%d" % pe, out=xe[pe][:, ns], in0=xe[pe][:, ns], in1=tq, op=ALU.add)
        p.dma(x1_s[tok, :], xe[pe], r="xe%d" % pe, w="x1_s", grp="x1w%d" % pe)
        local_states(0)
        upd(0)
    if dbg:
        dx1 = dout("x1", [L, 1024])
        p.dma(None, None, r="x1_s", w="dbg_x1", parts=[(dx1[i * 128:(i + 1) * 128, :], x1_s[i * 128:(i + 1) * 128, :]) for i in range(NT)])
    ar.release(mE)
    p.barrier()
    if stop == "E":
        return fin(nc, p, ar, dbg_d)

    ar.release(m_big)
    TB = 256
    KT = ar.a((16, 128), BF16)
    W2 = ar.a(1024); S2 = ar.a(1024); G2 = ar.a(1024); FW = ar.a(1024)
    p.dma(W2, modb_s[5], r="modb_s", w="W2")
    p.dma(S2, modb_s[6], r="modb_s", w="S2")
    p.dma(G2, modb_s[7], r="modb_s", w="G2")
    p.dma(FW, nrm_d[2], w="FW")
    xs1 = [ar.a(1024) for _ in range(2)]
    for i in range(4):
        sl = i % 2
        wv_ = xs1[sl][:, 0:512].rearrange("p (a b) -> p a b", a=4)
        p.dma(wv_, kt_d[:, i * 4:(i + 1) * 4, :], w="xs1%d" % sl)
        p.v("tensor_copy", r="xs1%d" % sl, w="KT", out=KT[:, i * 4:(i + 1) * 4, :], in_=wv_)
    iota_b = ar.a(128, BF16)
    p.v("tensor_copy", r="iota", w="iota_b", out=iota_b, in_=iota_f)
    Wqs = [ar.a((8, 128), BF16) for _ in range(3)]
    tmpF = ar.a(512)
    junkF = tmpF.bitcast(BF16)
    ssF = ar.a(2); rsF = ar.a(2)
    h2 = ar.a(1024, BF16)
    h2T = [ar.a((8, TB), BF16) for _ in range(3)]
    qT = ar.a((16, TB), BF16)
    sc = ar.a((16, 128))
    sv = ar.a((16, 16)); si = ar.a((16, 16), U32); sif = ar.a((16, 16)); wk = ar.a(128); wk2 = ar.a(256)
    best = ar.a((8, 16)); fl = ar.a((8, 16), U32); ai = ar.a((8, 16), U32); bi = ar.a((8, 16), U32)
    af = ar.a((8, 16)); bf_ = ar.a((8, 16)); gt = ar.a((8, 16)); gs = ar.a(8)
    IG = ar.a((3, 128))
    IGTb = [ar.a((2, 2, 128), BF16) for _ in range(3)]
    IGTg = [ar.a((2, 128)) for _ in range(3)]
    NG = 16
    ohA2 = [ar.a((NG, 128), BF16) for _ in range(2)]
    ohB2 = [ar.a((NG, 64), BF16) for _ in range(2)]
    ohc = [0]
    oh = ar.a((8, 16, 16))
    GmH = [ar.a((TB, 64), BF16) for _ in range(2)]
    wu = [ar.a((2, 1024), BF16) for _ in range(3)]
    wv = [ar.a((2, 1024), BF16) for _ in range(3)]
    ga = [ar.a(TB) for _ in range(2)]
    gab = [ar.a(TB, BF16) for _ in range(2)]
    cand = sc.rearrange("p a b -> p (a b)").rearrange("p (h a b) -> p h a b", h=8, a=16)
    svv = sv.rearrange("p (h t) a -> p h t a", t=2)
    sifv = sif.rearrange("p (h t) a -> p h t a", t=2)
    io16 = iota_f[:, 0:16].unsqueeze(1).unsqueeze(1).to_broadcast([128, 8, 16, 16])
    NBLK = L // TB
    wqi = [0]

    def part1(blk):
        par = blk % 3
        hT = h2T[par]
        hk = "h2T%d" % par
        for ts in range(2):
            t0 = blk * TB + ts * 128
            xk = "xs1%d" % ts
            p.dma(xs1[ts], x1_s[t0:t0 + 128, :], r="x1_s", w=xk, e="gpsimd", grp="gx%d" % ts)
            yield "sync"
            p.s("activation", r=xk, w="tmpF ssF", out=junkF, in_=xs1[ts], func=AF.Square, accum_out=ssF[:, 0:1])
            p.v("tensor_scalar", r="ssF", w="rsF", out=rsF[:, 0:1], in0=ssF[:, 0:1], scalar1=1.0 / 1024, scalar2=EPS, op0=ALU.mult, op1=ALU.add)
            p.s("activation", r="rsF", w="rsF", out=rsF[:, 0:1], in_=rsF[:, 0:1], func=AF.Ln)
            p.s("activation", r="rsF", w="rsF", out=rsF[:, 0:1], in_=rsF[:, 0:1], func=AF.Exp, scale=-0.5)
            yield
            p.v("scalar_tensor_tensor", r=xk + " rsF W2", w=xk, out=xs1[ts], in0=xs1[ts], scalar=rsF[:, 0:1], in1=W2, op0=ALU.mult, op1=ALU.mult)
            p.g("tensor_tensor", r=xk + " S2", w="h2", out=h2, in0=xs1[ts], in1=S2, op=ALU.add)
            yield "sync"
            for kc in range(8):
                p.t("transpose", r="h2 ident", w="b2", out=PBb(2)[:, kc * 128:(kc + 1) * 128], in_=h2[:, kc * 128:(kc + 1) * 128], identity=ident)
            yield "sync"
            p.s("copy", r="b2", w=hk, out=hT[:, :, ts * 128:(ts + 1) * 128], in_=PBb(2).rearrange("p (a b) -> p a b", a=8))
            yield
        def wq_load(hp_):
            ws_ = (wqi[0] + hp_) % 3
            p.dma(Wqs[ws_], wqb_s[hp_].rearrange("p (k c) -> p k c", k=8), r="wqb_s", w="Wqs%d" % ws_, e="gpsimd", grp="gwq%d" % ws_)
        wq_load(0)
        wq_load(1)
        yield "sync"
        for hp in range(17):
            if hp >= 1:
                if hp % 2 == 1:
                    p.s("copy", r="b2", w="qT", out=qT[:, hp - 1, :], in_=PB(2)[:, 0:TB])
                else:
                    p.v("tensor_copy", r="b2", w="qT", out=qT[:, hp - 1, :], in_=PB(2)[:, 0:TB])
            if hp < 16:
                ws = (wqi[0] + hp) % 3
                if hp + 2 < 16:
                    wq_load(hp + 2)
                p.begin()
                for kc in range(8):
                    p.t("matmul", r=("Wqs%d " % ws) + hk, w="b2", out=PB(2)[:, 0:TB], lhsT=Wqs[ws][:, kc, :], rhs=hT[:, kc, :],
                        start=(kc == 0), stop=(kc == 7))
                p.end()
            yield "sync"
        wqi[0] += 16
        yield "sync"
        for ts in range(2):
            tsl = slice(ts * 128, (ts + 1) * 128)
            for g4 in range(5):
                if g4 >= 1:
                    p.s("copy", r="b2", w="sc", out=sc[:, (g4 - 1) * 4:g4 * 4, :], in_=PB(2).rearrange("p (a b) -> p a b", a=4))
                if g4 < 4:
                    for q4 in range(4):
                        hp = g4 * 4 + q4
                        p.t("matmul", r="qT KT", w="b2", out=PB(2)[:, q4 * 128:(q4 + 1) * 128], lhsT=qT[:, hp, tsl], rhs=KT[:, hp, :],
                            start=True, stop=True)
                yield "sync"
            yield "sync"
            for hp in range(16):
                p.v("max", r="sc", w="sv", out=sv[:, hp, 0:8], in_=sc[:, hp, :])
                p.v("max_index", r="sc sv", w="si", out=si[:, hp, 0:8], in_max=sv[:, hp, 0:8], in_values=sc[:, hp, :])
                p.v("match_replace", r="sc sv", w="wk", out=wk, in_to_replace=sv[:, hp, 0:8], in_values=sc[:, hp, :], imm_value=-1e30)
                yield
                p.v("max", r="wk", w="sv", out=sv[:, hp, 8:16], in_=wk)
                p.v("max_index", r="wk sv", w="si", out=si[:, hp, 8:16], in_max=sv[:, hp, 8:16], in_values=wk)
                yield
            p.v("tensor_copy", r="si", w="sif", out=sif, in_=si)
            p.v("tensor_tensor", r="sv sc", w="sc", out=cand, in0=svv[:, :, 0, :].unsqueeze(3).to_broadcast([128, 8, 16, 16]),
                in1=svv[:, :, 1, :].unsqueeze(2).to_broadcast([128, 8, 16, 16]), op=ALU.add)
            yield
            for h in range(8):
                ch = cand[:, h].rearrange("p a b -> p (a b)")
                p.v("max", r="sc", w="best", out=best[:, h, 0:8], in_=ch)
                p.v("max_index", r="sc best", w="fl", out=fl[:, h, 0:8], in_max=best[:, h, 0:8], in_values=ch)
                p.v("match_replace", r="sc best", w="wk2", out=wk2, in_to_replace=best[:, h, 0:8], in_values=ch, imm_value=-1e30)
                yield
                p.v("max", r="wk2", w="best", out=best[:, h, 8:16], in_=wk2)
                p.v("max_index", r="wk2 best", w="fl", out=fl[:, h, 8:16], in_max=best[:, h, 8:16], in_values=wk2)
                yield
            p.v("tensor_tensor", r="best", w="gt", out=gt, in0=best, in1=best[:, :, 0:1].to_broadcast([128, 8, 16]), op=ALU.subtract)
            yield "sync"
            p.s("activation", r="gt", w="gt", out=gt, in_=gt, func=AF.Exp)
            yield "sync"
            p.v("tensor_reduce", r="gt", w="gs", out=gs, in_=gt, axis=AX.X, op=ALU.add)
            p.v("reciprocal", r="gs", w="gs", out=gs, in_=gs)
            yield
            p.v("tensor_tensor", r="gt gs IG", w="IG", out=IG[:, 2, :].rearrange("p (h j) -> p h j", h=8), in0=gt,
                in1=gs.unsqueeze(2).to_broadcast([128, 8, 16]), op=ALU.mult)
            p.v("tensor_single_scalar", r="fl", w="ai", out=ai, in_=fl, scalar=4, op=ALU.logical_shift_right)
            p.v("tensor_single_scalar", r="fl", w="bi", out=bi, in_=fl, scalar=15, op=ALU.bitwise_and)
            p.v("tensor_copy", r="ai", w="af", out=af, in_=ai)
            p.v("tensor_copy", r="bi", w="bf", out=bf_, in_=bi)
            yield
            for t_, (xf, key) in enumerate(((af, "af"), (bf_, "bf"))):
                p.v("tensor_tensor", r=key + " iota", w="oh", out=oh, in0=xf.unsqueeze(3).to_broadcast([128, 8, 16, 16]), in1=io16, op=ALU.is_equal)
                yield
                p.v("tensor_tensor", r="oh sif", w="oh", out=oh, in0=oh, in1=sifv[:, :, t_, :].unsqueeze(2).to_broadcast([128, 8, 16, 16]), op=ALU.mult)
                yield
                p.v("tensor_reduce", r="oh IG", w="IG", out=IG[:, t_, :].rearrange("p (h j) -> p h j", h=8), in_=oh, axis=AX.X, op=ALU.add)
                yield
            yield "sync"
            for q in range(3):
                p.t("transpose", r="IG identf", w="b2", out=PB(2)[:, q * 128:(q + 1) * 128], in_=IG[:, q, :], identity=ident_f)
            yield "sync"
            p.s("copy", r="b2", w="IGTb%d" % par, out=IGTb[par][:, ts, :, :], in_=PB(2)[:, 0:256].rearrange("p (a b) -> p a b", a=2))
            p.s("copy", r="b2", w="IGTg%d" % par, out=IGTg[par][:, ts, :], in_=PB(2)[:, 256:384])
            yield

    import os

    def part2(blk, half):
        par = blk % 3
        kb, kg = "IGTb%d" % par, "IGTg%d" % par
        Gh = GmH[half]
        gk = "Gm%d" % half
        groups = [(ts, tg) for ts in range(2) for tg in range(128 // NG)]
        bufs = {}

        def gen_(n):
            ts, tg = groups[n]
            tq = slice(tg * NG, (tg + 1) * NG)
            ob = ohc[0] % 2
            ohc[0] += 1
            bufs[n] = ob
            ohA, ohB = ohA2[ob], ohB2[ob]
            ka, kbb = "ohA%d" % ob, "ohB%d" % ob
            p.v("tensor_tensor", r=kb + " iota_b", w=ka, out=ohA, in0=iota_b.unsqueeze(1).to_broadcast([128, NG, 128]),
                in1=IGTb[par][:, ts, 0, tq].unsqueeze(2).to_broadcast([128, NG, 128]), op=ALU.is_equal)
            p.v("tensor_tensor", r=kb + " iota_b", w=kbb, out=ohB, in0=iota_b[:, half * 64:(half + 1) * 64].unsqueeze(1).to_broadcast([128, NG, 64]),
                in1=IGTb[par][:, ts, 1, tq].unsqueeze(2).to_broadcast([128, NG, 64]), op=ALU.is_equal)
            import os
            if os.environ.get("NOPOOLMULT") != "1":
                p.g("tensor_tensor", r=kg + " " + kbb, w=kbb, out=ohB, in0=ohB, in1=IGTg[par][:, ts, tq].unsqueeze(2).to_broadcast([128, NG, 64]), op=ALU.mult)

        def mm_(m):
            n, t8 = m // 2, m % 2
            ob = bufs[n]
            ohA, ohB = ohA2[ob], ohB2[ob]
            ka, kbb = "ohA%d" % ob, "ohB%d" % ob
            p.begin()
            for q8 in range(8):
                tt_ = t8 * 8 + q8
                p.t("matmul", r=ka + " " + kbb, w="b3", out=PB(3)[:, q8 * 64:(q8 + 1) * 64], lhsT=ohA[:, tt_, :], rhs=ohB[:, tt_, :],
                    start=True, stop=True)
            p.end()

        def ev_(m):
            n, t8 = m // 2, m % 2
            ts, tg = groups[n]
            g0 = ts * 128 + tg * NG + t8 * 8
            if m % 2 == 0 or os.environ.get("EVACT") == "1":
                p.s("copy", r="b3", w=gk, out=Gh[:, g0:g0 + 8, :], in_=PB(3).rearrange("p (a b) -> p a b", a=8))
            else:
                p.v("tensor_copy", r="b3", w=gk, out=Gh[:, g0:g0 + 8, :], in_=PB(3).rearrange("p (a b) -> p a b", a=8))

        ng = len(groups)
        gen_(0)
        yield "sync"
        for m in range(2 * ng + 1):
            if m % 2 == 0 and m // 2 + 1 < ng:
                gen_(m // 2 + 1)
            if m >= 1:
                ev_(m - 1)
            if m < 2 * ng:
                mm_(m)
            yield "sync"

    widx = [0]

    wslot = {}

    def main_step(blk, s_):
        par = blk % 3
        hT = h2T[par]
        if s_ < 128:
            pr, e2 = s_ // 2, s_ % 2
            if e2 == 0:
                sl = widx[0] % 3
                widx[0] += 1
                wslot[pr % 4] = sl
                p.dma(wu[sl], utb_s[2 * pr:2 * pr + 2].rearrange("a p c -> p a c"), r="utb_s", w="wu%d" % sl)
                p.dma(wv[sl], vtb_s[2 * pr:2 * pr + 2].rearrange("a p c -> p a c"), r="vtb_s", w="wv%d" % sl)
            sl = wslot[pr % 4]
            half = s_ // 64
            ab = s_ % 2
            p.begin()
            for kc in range(8):
                p.t("matmul", r="wu%d h2T%d" % (sl, par), w="b%d" % ab, out=PB(ab)[:, 0:TB], lhsT=wu[sl][:, e2, kc * 128:(kc + 1) * 128], rhs=hT[:, kc, :],
                    start=(kc == 0), stop=(kc == 7))
            p.end()
            p.s("activation", r="b%d" % ab, w="ga%d" % ab, out=ga[ab], in_=PB(ab)[:, 0:TB], func=AF.Gelu)
            p.v("tensor_tensor", r="ga%d Gm%d" % (ab, half), w="gab%d" % ab, out=gab[ab], in0=ga[ab], in1=GmH[half][:, :, s_ % 64], op=ALU.mult)
        if s_ >= 1:
            o_ = s_ - 1
            pr, e2 = o_ // 2, o_ % 2
            sl = wslot[pr % 4]
            ab = o_ % 2
            p.begin()
            for ts in range(2):
                for nb in range(2):
                    bk = 4 + 2 * ts + nb
                    p.t("matmul", r="gab%d wv%d" % (ab, sl), w="b%d" % bk, out=PB(bk), lhsT=gab[ab][:, ts * 128:(ts + 1) * 128],
                        rhs=wv[sl][:, e2, nb * 512:(nb + 1) * 512], start=(o_ == 0), stop=(o_ == 127))
            p.end()

    def finalize(blk):
        for ts in range(2):
            t0 = blk * TB + ts * 128
            xk = "xs1%d" % ts
            p.dma(xs1[ts], x1_s[t0:t0 + 128, :], r="x1_s", w=xk, e="gpsimd", grp="gx%d" % ts)
            for nb in range(2):
                bk = 4 + 2 * ts + nb
                ns = slice(nb * 512, (nb + 1) * 512)
                p.v("tensor_tensor", r="b%d G2" % bk, w="tmpF", out=tmpF, in0=PB(bk), in1=G2[:, ns], op=ALU.mult)
                p.g("tensor_tensor", r="tmpF " + xk, w=xk, out=xs1[ts][:, ns], in0=xs1[ts][:, ns], in1=tmpF, op=ALU.add)
            p.s("activation", r=xk, w="tmpF ssF", out=junkF, in_=xs1[ts], func=AF.Square, accum_out=ssF[:, 1:2])
            p.v("tensor_scalar", r="ssF", w="rsF", out=rsF[:, 1:2], in0=ssF[:, 1:2], scalar1=1.0 / 1024, scalar2=EPS, op0=ALU.mult, op1=ALU.add)
            p.s("activation", r="rsF", w="rsF", out=rsF[:, 1:2], in_=rsF[:, 1:2], func=AF.Ln)
            p.s("activation", r="rsF", w="rsF", out=rsF[:, 1:2], in_=rsF[:, 1:2], func=AF.Exp, scale=-0.5)
            p.v("scalar_tensor_tensor", r=xk + " rsF FW", w=xk, out=xs1[ts], in0=xs1[ts], scalar=rsF[:, 1:2], in1=FW, op0=ALU.mult, op1=ALU.mult)
            p.dma(out_d[t0:t0 + 128, :], xs1[ts], r=xk, w="out", grp="gout%d" % ts, e="gpsimd")

    nblk = 1 if stop == "F1" else (3 if stop == "F2" else (7 if stop == "F7" else NBLK))

    def drain(g):
        if g is not None:
            for _ in g:
                pass

    def drip(g, n):
        if g is None:
            return None
        try:
            for _ in range(n):
                if next(g) == "sync":
                    break
        except StopIteration:
            return None
        return g

    drain(part1(0))
    if nblk > 1:
        drain(part1(1))
    drain(part2(0, 0))
    for blk in range(nblk):
        st = {"g1": part1(blk + 2) if blk + 2 < nblk else None, "gB": part2(blk, 1), "gA": None}

        import os
        DM = os.environ.get("DRIPMODE", "3")
        if DM in ("0", "2"):
            drain(st["g1"]); st["g1"] = None
        if DM in ("0", "1"):
            drain(st["gB"]); st["gB"] = None

        N1 = int(os.environ.get("N1", "2"))
        SPREAD = int(os.environ.get("SPREAD", "58"))

        def dr(s_, st=st, blk=blk):
            st["g1"] = drip(st["g1"], N1)
            k = s_ % 64
            if (k + 1) * 33 // SPREAD == k * 33 // SPREAD and k < SPREAD:
                return
            if s_ < 64:
                st["gB"] = drip(st["gB"], 1)
            elif DM in ("2", "3"):
                st["gA"] = drip(st["gA"], 1)

        for s_ in range(129 if os.environ.get("SKIPMAIN") != "1" else 0):
            main_step(blk, s_)
            if s_ == 63:
                drain(st["gB"])
                st["gB"] = None
                st["gA"] = part2(blk + 1, 0) if blk + 1 < nblk else None
            dr(s_)
        g1, gA = st["g1"], st["gA"]
        drain(g1)
        drain(gA)
        finalize(blk)
    return fin(nc, p, ar, dbg_d)


def fin(nc, p, ar, dbg_d):
    p.finish()
    p.emit()
    return nc, dbg_d


def host_layout(inp, b):
    f = np.float32
    A = np.ascontiguousarray
    d = {}
    d["x"] = A(inp["x"][b])
    d["ctx"] = A(inp["ctx"][b])
    cc = np.stack([inp["c"][b].reshape(8, 128).T, inp["c_ctx"].reshape(8, 128).T], axis=-1)
    d["cc"] = A(cc.astype(f))
    return d


def shared_layout(inp):
    A = np.ascontiguousarray
    d = {}
    d["ada_w"] = A(inp["ada_w"][0].reshape(8, 128, 6144).transpose(1, 0, 2))
    d["ada_b"] = A(inp["ada_b"][0].reshape(1, 6144))
    d["nrm"] = A(np.stack([np.broadcast_to(inp[k].reshape(1, 1024), (128, 1024)) for k in ("norm1_w", "norm2_w", "final_norm_w")]))
    d["w_in"] = A(inp["w_in"][0].reshape(8, 128, 2576).transpose(1, 0, 2))
    d["scw"] = A(inp["ssm_conv_w"][0].reshape(5, 8, 128).transpose(2, 1, 0))
    d["scb"] = A(inp["ssm_conv_b"][0].reshape(8, 128).T)
    d["dtb"] = A(np.broadcast_to(inp["ssm_dt_bias"][0].reshape(1, 16), (128, 16)))
    d["alog"] = A(np.broadcast_to(inp["ssm_a_log"][0].reshape(1, 16), (128, 16)))
    d["drow"] = A(np.broadcast_to(np.repeat(inp["ssm_d"][0], 64).reshape(1, 512), (128, 512)))
    d["snw"] = A(np.broadcast_to(inp["ssm_norm_w"][0].reshape(1, 512), (128, 512)))
    d["ccw"] = A(inp["cfm_conv_w"][0].reshape(31, 4, 128).transpose(2, 1, 0))
    d["cvec"] = A(np.stack([inp[k][0].reshape(4, 128).T for k in ("cfm_conv_b", "cfm_ln_w", "cfm_ln_b")], axis=1))
    d["w_out"] = A(inp["w_out"][0].reshape(8, 128, 1024).transpose(1, 0, 2))
    d["wq"] = A(inp["peer_wq"][0].reshape(8, 128, 2048).transpose(1, 0, 2))
    d["kt"] = A(inp["peer_subkeys"][0].reshape(16, 128, 128).transpose(2, 0, 1))
    d["ut"] = A(inp["peer_u"][0].reshape(128, 128, 8, 128).transpose(1, 3, 2, 0))
    d["vt"] = A(inp["peer_v"][0].reshape(128, 128, 1024).transpose(1, 0, 2))
    return d


_CACHE = {}


def kernel(**inputs):
    inp = {k: np.asarray(v) for k, v in inputs.items()}
    if "nc" not in _CACHE:
        _CACHE["nc"] = build()[0]
    nc = _CACHE["nc"]
    sh = shared_layout(inp)
    in_maps = []
    for b in range(8):
        d = dict(sh)
        d.update(host_layout(inp, b))
        in_maps.append(d)
    res = run_bass_kernel_spmd(nc, in_maps, core_ids=list(range(8)))
    return np.stack([np.asarray(r["out"], dtype=np.float32) for r in res.results], axis=0)
```

```python
import re
import numpy as np
import concourse.bass as bass
import concourse.mybir as mybir
from concourse.bass_utils import run_bass_kernel_spmd

F32 = mybir.dt.float32
BF16 = mybir.dt.bfloat16
I32 = mybir.dt.int32
U32 = mybir.dt.uint32
AF = mybir.ActivationFunctionType
ALU = mybir.AluOpType
AX = mybir.AxisListType
ENGS = ("sync", "scalar", "vector", "gpsimd", "tensor")
CH = 4096
EPS = 1e-6
DSZ = {F32: 4, BF16: 2, I32: 4, U32: 4}


class Prog:
    def __init__(self, nc):
        self.nc = nc
        self.q = {e: [] for e in ENGS}
        self.cnt = {e: 0 for e in ENGS}
        self.known = {e: {} for e in ENGS}
        self.lastw = {}
        self.readers = {}
        self.sems = {}
        self.dmacum = {}
        self.pend = {e: {} for e in ENGS}
        self.lasttok = {}
        self.grp = None

    def sem(self, key):
        s = self.sems.get(key)
        if s is None:
            s = self.nc.alloc_semaphore(name="s%d" % len(self.sems))
            self.sems[key] = s
        return s

    def _waits(self, e, reads, writes):
        toks = dict(self.pend[e])
        self.pend[e] = {}

        def add(t):
            if t is None:
                return
            k, v = t
            if k[0] == "eng" and k[1] == e and e == "tensor":
                return
            if toks.get(k, 0) < v:
                toks[k] = v
        for r in reads:
            add(self.lastw.get(r))
        for w in writes:
            add(self.lastw.get(w))
            for t in self.readers.get(w, ()):
                add(t)
        out = []
        for k, v in toks.items():
            if self.known[e].get(k, 0) >= v:
                continue
            self.known[e][k] = v
            out.append((self.sem(k), v))
        return out

    def _commit(self, tok, reads, writes):
        self.lasttok[tok[0]] = tok[1]
        for w in writes:
            self.lastw[w] = tok
            self.readers[w] = []
        for r in reads:
            self.readers.setdefault(r, []).append(tok)

    def begin(self):
        self.grp = {"ops": [], "r": [], "w": []}

    def end(self):
        g, self.grp = self.grp, None
        if not g["ops"]:
            return
        reads = list(dict.fromkeys(g["r"]))
        writes = list(dict.fromkeys(g["w"]))
        waits = self._waits("tensor", reads, writes)
        idx = self.cnt["tensor"]
        self.cnt["tensor"] += 1
        k = ("eng", "tensor", idx // CH)
        n = len(g["ops"])
        for i, (name, kw) in enumerate(g["ops"]):
            self.q["tensor"].append((waits if i == 0 else [], name, kw, self.sem(k) if i == n - 1 else None, 1 if i == n - 1 else 0))
        self._commit((k, idx % CH + 1), reads, writes)

    def op(self, e, name, r="", w="", **kw):
        reads, writes = r.split(), w.split()
        if e == "tensor" and self.grp is not None:
            ex = [k for k in reads if re.fullmatch(r"(ps|b)\d", k)]
            self.grp["ops"].append((name, kw))
            self.grp["r"] += [k for k in reads if k not in ex]
            self.grp["w"] += writes + ex
            return
        ex = [k for k in reads if re.fullmatch(r"(ps|b)\d", k)]
        reads = [k for k in reads if k not in ex]
        writes = writes + [k for k in ex if k not in writes]
        waits = self._waits(e, reads, writes)
        idx = self.cnt[e]
        self.cnt[e] += 1
        k = ("eng", e, idx // CH)
        self.q[e].append((waits, name, kw, self.sem(k), 1))
        self._commit((k, idx % CH + 1), reads, writes)

    def v(self, name, r="", w="", **kw):
        self.op("vector", name, r, w, **kw)

    def s(self, name, r="", w="", **kw):
        self.op("scalar", name, r, w, **kw)

    def g(self, name, r="", w="", **kw):
        self.op("gpsimd", name, r, w, **kw)

    def t(self, name, r="", w="", **kw):
        self.op("tensor", name, r, w, **kw)

    def dma(self, out, in_, r="", w="", e="sync", grp=None, parts=None, **kw):
        reads, writes = r.split(), w.split()
        sk = ("dma", grp if grp is not None else writes[0])
        waits = self._waits(e, reads, writes)
        pieces = parts if parts is not None else [(out, in_)]
        for i, (o, a) in enumerate(pieces):
            self.dmacum[sk] = self.dmacum.get(sk, 0) + 16
            kk = dict(kw)
            kk.update(out=o, in_=a)
            self.q[e].append((waits if i == 0 else [], "dma_start", kk, self.sem(sk), 16))
        self._commit((sk, self.dmacum[sk]), reads, writes)

    def barrier(self):
        for e in ENGS:
            for k, v in self.lasttok.items():
                if self.pend[e].get(k, 0) < v:
                    self.pend[e][k] = v

    def finish(self):
        self.barrier()
        for e in ("sync", "gpsimd"):
            waits = self._waits(e, [], [])
            self.q[e].append((waits, None, None, None, 0))

    def emit(self):
        nc = self.nc
        with nc.Block() as block:
            def mk(e):
                def body(eng):
                    for waits, name, kw, s, inc in self.q[e]:
                        for ws, wv in waits:
                            eng.wait_ge(ws, wv)
                        if name is not None:
                            ins = getattr(eng, name)(**kw)
                            if s is not None:
                                ins.then_inc(s, inc)
                return body
            block.sync(mk("sync"))
            block.scalar(mk("scalar"))
            block.vector(mk("vector"))
            block.gpsimd(mk("gpsimd"))
            block.tensor(mk("tensor"))


class Arena:
    def __init__(self, nc, nbytes):
        self.t = nc.alloc_sbuf_tensor("arena", [128, nbytes // 4], F32)
        self.cap = nbytes
        self.off = 0
        self.peak = 0

    def a(self, free, dt=F32, parts=128):
        if isinstance(free, int):
            free = (free,)
        n = int(np.prod(free)) * DSZ[dt]
        n = (n + 63) // 64 * 64
        assert self.off + n <= self.cap, ("SBUF arena overflow", self.off, n, self.cap)
        ap = self.t[0:parts, self.off // 4:(self.off + n) // 4]
        self.off += n
        self.peak = max(self.peak, self.off)
        if dt != F32:
            ap = ap.bitcast(dt)
        ap = ap[:, 0:int(np.prod(free))]
        if len(free) == 2:
            ap = ap.rearrange("p (a b) -> p a b", a=free[0])
        elif len(free) == 3:
            ap = ap.rearrange("p (a b c) -> p a b c", a=free[0], b=free[1])
        return ap

    def mark(self):
        return self.off

    def release(self, m):
        self.off = m


L = 4096
LC = 256
NT = L // 128


def build(stop=None, dbg=False, npeer=128):
    nc = bass.Bass("TRN2", target_bir_lowering=False)
    D = nc.dram_tensor

    def din(name, shape, dt=F32):
        return D(name, list(shape), dt, kind="ExternalInput").ap()
    x_d = din("x", [L, 1024])
    ctx_d = din("ctx", [LC, 1024])
    cc_d = din("cc", [128, 8, 2])
    adaw_d = din("ada_w", [128, 8, 6144])
    adab_d = din("ada_b", [1, 6144])
    nrm_d = din("nrm", [3, 128, 1024])
    win_d = din("w_in", [128, 8, 2576])
    scw_d = din("scw", [128, 8, 5])
    scb_d = din("scb", [128, 8])
    dtb_d = din("dtb", [128, 16])
    alog_d = din("alog", [128, 16])
    drow_d = din("drow", [128, 512])
    snw_d = din("snw", [128, 512])
    ccw_d = din("ccw", [128, 4, 31])
    cvec_d = din("cvec", [128, 3, 4])
    wout_d = din("w_out", [128, 8, 1024])
    wq_d = din("wq", [128, 8, 2048])
    kt_d = din("kt", [128, 16, 128])
    ut_d = din("ut", [npeer, 128, 8, 128])
    vt_d = din("vt", [npeer, 128, 1024])
    out_d = D("out", [L, 1024], F32, kind="ExternalOutput").ap()
    dbg_d = {}

    def dout(name, shape):
        dbg_d[name] = D("dbg_" + name, list(shape), F32, kind="ExternalOutput").ap()
        return dbg_d[name]
    modb_s = D("modb_s", [8, 128, 1024], F32).ap()
    zs_s = D("zs_s", [L, 512], BF16).ap()
    x1_s = D("x1_s", [L, 1024], F32).ap()
    utb_s = D("utb_s", [128, 128, 1024], BF16).ap()
    vtb_s = D("vtb_s", [128, 128, 1024], BF16).ap()
    uts_s = D("uts_s", [128, 4, L], BF16).ap()
    wqb_s = D("wqb_s", [16, 128, 1024], BF16).ap()

    p = Prog(nc)
    ar = Arena(nc, 212000)
    banks = [nc.alloc_psum_tensor("pb%d" % i, [128, 512], F32) for i in range(8)]

    def PB(i):
        return banks[i][:]

    def PBb(i):
        return banks[i][:].bitcast(BF16)

    ident_f = ar.a(128)
    ident = ar.a(128, BF16)
    iota_f = ar.a(128)
    pidx = ar.a(128)
    ones_f = ar.a(128)
    ones_b = ar.a(128, BF16)
    triU = ar.a(128)
    triS = ar.a(128)
    maskF = ar.a(128, BF16)
    maskB = ar.a(128, BF16)
    p.g("iota", w="iota", out=iota_f, pattern=[[1, 128]], base=0, channel_multiplier=0, allow_small_or_imprecise_dtypes=True)
    p.g("iota", w="pidx", out=pidx, pattern=[[0, 128]], base=0, channel_multiplier=1, allow_small_or_imprecise_dtypes=True)
    p.v("tensor_tensor", r="iota pidx", w="identf", out=ident_f, in0=iota_f, in1=pidx, op=ALU.is_equal)
    p.v("tensor_copy", r="identf", w="ident", out=ident, in_=ident_f)
    p.v("memset", w="ones_f", ap=ones_f, constant=1.0)
    p.v("memset", w="ones_b", ap=ones_b, constant=1.0)
    p.v("tensor_tensor", r="iota pidx", w="triU", out=triU, in0=iota_f, in1=pidx, op=ALU.is_ge)
    p.v("tensor_tensor", r="iota pidx", w="triS", out=triS, in0=iota_f, in1=pidx, op=ALU.is_gt)
    p.v("tensor_scalar", r="triU", w="maskF", out=maskF, in0=triU, scalar1=-1.0, scalar2=30000.0, op0=ALU.add, op1=ALU.mult)
    tmpc = ar.a(128)
    p.v("tensor_tensor", r="iota pidx", w="tmpc", out=tmpc, in0=iota_f, in1=pidx, op=ALU.is_le)
    p.v("tensor_scalar", r="tmpc", w="maskB", out=maskB, in0=tmpc, scalar1=-1.0, scalar2=30000.0, op0=ALU.add, op1=ALU.mult)
    sel = ar.a((16, 128), BF16)
    p.v("memset", w="sel", ap=sel, constant=0.0)
    p.v("tensor_copy", r="identf sel", w="sel", out=sel[0:16], in_=ident_f[0:16, 0:16].unsqueeze(2).to_broadcast([16, 16, 128]))
    scw = ar.a((8, 5)); scb = ar.a(8); dtbias = ar.a(16); alog = ar.a(16); ccw = ar.a((4, 31)); cvec = ar.a((3, 4))
    Aneg = ar.a(16)
    p.dma(scw, scw_d, w="scw", grp="small")
    p.dma(scb, scb_d, w="scb", grp="small")
    p.dma(dtbias, dtb_d, w="dtbias", grp="small")
    p.dma(alog, alog_d, w="alog", grp="small")
    p.dma(ccw, ccw_d, w="ccw", grp="small")
    p.dma(cvec, cvec_d, w="cvec", grp="small")
    p.s("activation", r="alog", w="Aneg", out=Aneg, in_=alog, func=AF.Exp)
    p.v("tensor_scalar", r="Aneg", w="Aneg", out=Aneg, in0=Aneg, scalar1=-1.0, scalar2=None, op0=ALU.mult)
    dt_all = ar.a((NT + 2, 16))
    m_persist = ar.mark()

    m0 = ar.mark()
    cc = ar.a((8, 2)); scT = ar.a((8, 2))
    adab = ar.a(6144, parts=1)
    modrow = ar.a(6144, parts=1)
    modrow_c = ar.a(2048, parts=1)
    awb = [ar.a((8, 512)) for _ in range(2)]
    nrm = [ar.a(1024) for _ in range(2)]
    bct = [ar.a(1024) for _ in range(2)]
    p.dma(cc, cc_d, w="cc")
    p.dma(adab, adab_d, w="adab")
    p.dma(nrm[0], nrm_d[0], w="nrm0")
    p.dma(nrm[1], nrm_d[1], w="nrm1")
    p.s("activation", r="cc", w="scT", out=scT, in_=cc, func=AF.Silu)
    for blk in range(12):
        sl = blk % 2
        p.dma(awb[sl], adaw_d[:, :, blk * 512:(blk + 1) * 512], w="awb%d" % sl)
        for kc in range(8):
            p.t("matmul", r="scT awb%d" % sl, w="ps0", out=PB(0)[0:1, :], lhsT=scT[:, kc, 0:1], rhs=awb[sl][:, kc, :],
                start=(kc == 0), stop=(kc == 7))
        if blk < 4:
            for kc in range(8):
                p.t("matmul", r="scT awb%d" % sl, w="ps1", out=PB(1)[0:1, :], lhsT=scT[:, kc, 1:2], rhs=awb[sl][:, kc, :],
                    start=(kc == 0), stop=(kc == 7))
            p.v("tensor_tensor", r="ps1 adab", w="modrow_c", out=modrow_c[:, blk * 512:(blk + 1) * 512], in0=PB(1)[0:1, :],
                in1=adab[:, blk * 512:(blk + 1) * 512], op=ALU.add)
        p.v("tensor_tensor", r="ps0 adab", w="modrow", out=modrow[:, blk * 512:(blk + 1) * 512], in0=PB(0)[0:1, :],
            in1=adab[:, blk * 512:(blk + 1) * 512], op=ALU.add)
    plan = [(0, modrow, 1, 0), (1, modrow, 0, None), (2, modrow_c, 1, 0), (3, modrow_c, 0, None), (4, modrow, 2, None),
            (5, modrow, 4, 1), (6, modrow, 3, None), (7, modrow, 5, None)]
    for i, (ti, src, mi, ni) in enumerate(plan):
        bt = bct[i % 2]
        bk = "bct%d" % (i % 2)
        for hf in range(2):
            p.t("matmul", r="ones_f modrow modrow_c", w="ps%d" % (2 + hf), out=PB(2 + hf), lhsT=ones_f[0:1, :],
                rhs=src[:, mi * 1024 + hf * 512: mi * 1024 + (hf + 1) * 512], start=True, stop=True)
            if ni is None:
                p.v("tensor_copy", r="ps%d" % (2 + hf), w=bk, out=bt[:, hf * 512:(hf + 1) * 512], in_=PB(2 + hf))
            else:
                p.v("scalar_tensor_tensor", r="ps%d nrm%d" % (2 + hf, ni), w=bk, out=bt[:, hf * 512:(hf + 1) * 512], in0=PB(2 + hf),
                    scalar=1.0, in1=nrm[ni][:, hf * 512:(hf + 1) * 512], op0=ALU.add, op1=ALU.mult)
        p.dma(modb_s[ti], bt, r=bk, w="modb_s", grp="modb_w")
    if dbg:
        p.dma(dout("modrow", [1, 6144]), modrow, r="modrow", w="dbg_modrow")
    ar.release(m0)
    p.barrier()
    if stop == "0":
        return fin(nc, p, ar, dbg_d)

    m_big = ar.mark()
    xbcT = ar.a((8, L), BF16)
    xbcTc = ar.a((8, LC), BF16)
    m_uT = ar.mark()
    uT = ar.a((4, L), BF16)

    mA = ar.mark()
    Wb = ar.a((8, 2576), BF16)
    wst = [ar.a((8, 128)) for _ in range(2)]
    W1 = ar.a(1024); S1 = ar.a(1024)

    def load_mod(i0):
        p.dma(W1, modb_s[i0], r="modb_s", w="modA0", grp="modA")
        p.dma(S1, modb_s[i0 + 1], r="modb_s", w="modA1", grp="modA")
    ncol = 2576
    pieces = [(c0, min(128, ncol - c0)) for c0 in range(0, ncol, 128)]
    for i, (c0, cw) in enumerate(pieces):
        sl = i % 2
        p.dma(wst[sl][:, :, 0:cw], win_d[:, :, c0:c0 + cw], w="wst%d" % sl)
        if i % 2 == 0:
            p.v("tensor_copy", r="wst%d" % sl, w="Wb", out=Wb[:, :, c0:c0 + cw], in_=wst[sl][:, :, 0:cw])
        else:
            p.s("copy", r="wst%d" % sl, w="Wb", out=Wb[:, :, c0:c0 + cw], in_=wst[sl][:, :, 0:cw])
    xt = [ar.a(1024) for _ in range(2)]
    junk = ar.a(1024, BF16)
    hb = [ar.a(1024, BF16) for _ in range(2)]
    hT2 = [ar.a((8, 512), BF16) for _ in range(2)]
    ss = ar.a(2); rs = ar.a(2)
    sig = ar.a(512)
    zsb = [ar.a(512, BF16) for _ in range(2)]
    dtt = ar.a(16)

    def inproj_pro(src_d, t0, ntile, Wm, Sm, par):
        hT = hT2[par]
        hk = "hT%d" % par
        for ts in range(ntile):
            sl = ts % 2
            p.dma(xt[sl], src_d[t0 + ts * 128:t0 + (ts + 1) * 128, :], w="xt%d" % sl)
            p.s("activation", r="xt%d" % sl, w="junk ss", out=junk, in_=xt[sl], func=AF.Square, accum_out=ss[:, 0:1])
            p.v("tensor_scalar", r="ss", w="rs", out=rs[:, 0:1], in0=ss[:, 0:1], scalar1=1.0 / 1024, scalar2=EPS, op0=ALU.mult, op1=ALU.add)
            p.s("activation", r="rs", w="rs", out=rs[:, 0:1], in_=rs[:, 0:1], func=AF.Ln)
            p.s("activation", r="rs", w="rs", out=rs[:, 0:1], in_=rs[:, 0:1], func=AF.Exp, scale=-0.5)
            p.v("scalar_tensor_tensor", r="xt%d rs modA0" % sl, w="xt%d" % sl, out=xt[sl], in0=xt[sl], scalar=rs[:, 0:1], in1=Wm,
                op0=ALU.mult, op1=ALU.mult)
            p.g("tensor_tensor", r="xt%d modA1" % sl, w="hb%d" % sl, out=hb[sl], in0=xt[sl], in1=Sm, op=ALU.add)
            yield
            for kc in range(8):
                p.t("transpose", r="hb%d ident" % sl, w="ps%d" % (4 + sl), out=PBb(4 + sl)[:, kc * 128:(kc + 1) * 128],
                    in_=hb[sl][:, kc * 128:(kc + 1) * 128], identity=ident)
            yield
            p.s("copy", r="ps%d" % (4 + sl), w=hk, out=hT[:, :, ts * 128:(ts + 1) * 128],
                in_=PBb(4 + sl).rearrange("p (a b) -> p a b", a=8))
            yield

    def inproj_mm(t0, ntile, is_ctx, tile0, par, nxt):
        ntok = ntile * 128
        hT = hT2[par]
        hk = "hT%d" % par

        def tick():
            if nxt[0] is not None:
                try:
                    next(nxt[0])
                except StopIteration:
                    nxt[0] = None
        for j in range(8):
            pk = j % 2
            c0 = 512 + j * 128
            for kc in range(8):
                p.t("matmul", r="Wb " + hk, w="ps%d" % pk, out=PB(pk)[:, 0:ntok], lhsT=Wb[:, kc, c0:c0 + 128], rhs=hT[:, kc, 0:ntok],
                    start=(kc == 0), stop=(kc == 7))
            dst = xbcTc[:, j, 0:ntok] if is_ctx else xbcT[:, j, t0:t0 + ntok]
            if j % 2 == 0:
                p.s("copy", r="ps%d" % pk, w="xbcT", out=dst, in_=PB(pk)[:, 0:ntok])
            else:
                p.v("tensor_copy", r="ps%d" % pk, w="xbcT", out=dst, in_=PB(pk)[:, 0:ntok])
            tick()
        if not is_ctx:
            for j in range(4):
                ca = 1552 + j * 128
                cb = 1552 + 512 + j * 128
                for kc in range(8):
                    p.t("matmul", r="Wb " + hk, w="ps2", out=PB(2)[:, 0:ntok], lhsT=Wb[:, kc, ca:ca + 128], rhs=hT[:, kc, 0:ntok],
                        start=(kc == 0), stop=(kc == 7))
                for kc in range(8):
                    p.t("matmul", r="Wb " + hk, w="ps3", out=PB(3)[:, 0:ntok], lhsT=Wb[:, kc, cb:cb + 128], rhs=hT[:, kc, 0:ntok],
                        start=(kc == 0), stop=(kc == 7))
                p.s("activation", r="ps3", w="sig", out=sig[:, 0:ntok], in_=PB(3)[:, 0:ntok], func=AF.Sigmoid)
                p.v("tensor_tensor", r="ps2 sig", w="uT", out=uT[:, j, t0:t0 + ntok], in0=PB(2)[:, 0:ntok], in1=sig[:, 0:ntok], op=ALU.mult)
                tick()
        for ts in range(ntile):
            sl = ts % 2
            if not is_ctx:
                for kc in range(8):
                    p.t("matmul", r="Wb " + hk, w="ps%d" % (6 + sl), out=PB(6 + sl), lhsT=hT[:, kc, ts * 128:(ts + 1) * 128], rhs=Wb[:, kc, 0:512],
                        start=(kc == 0), stop=(kc == 7))
                p.s("activation", r="ps%d" % (6 + sl), w="zsb%d" % sl, out=zsb[sl], in_=PB(6 + sl), func=AF.Silu)
                p.dma(zs_s[t0 + ts * 128:t0 + (ts + 1) * 128, :], zsb[sl], r="zsb%d" % sl, w="zs_s", e="sync", grp="zsw%d" % sl)
            for kc in range(8):
                p.t("matmul", r="Wb " + hk, w="ps0", out=PB(0)[:, 0:16], lhsT=hT[:, kc, ts * 128:(ts + 1) * 128], rhs=Wb[:, kc, 1536:1552],
                    start=(kc == 0), stop=(kc == 7))
            p.v("tensor_tensor", r="ps0 dtbias", w="dtt", out=dtt, in0=PB(0)[:, 0:16], in1=dtbias, op=ALU.add)
            p.s("activation", r="dtt", w="dtt", out=dtt, in_=dtt, func=AF.Exp)
            p.s("activation", r="dtt", w="dt_all", out=dt_all[:, tile0 + ts, :], in_=dtt, func=AF.Ln, bias=1.0)
            tick()
        while nxt[0] is not None:
            tick()

    load_mod(2)
    for _ in inproj_pro(ctx_d, 0, 2, W1, S1, 0):
        pass
    inproj_mm(0, 2, True, NT, 0, [None])
    load_mod(0)
    nbk = L // 512
    for _ in inproj_pro(x_d, 0, 4, W1, S1, 1):
        pass
    for b in range(nbk):
        par = (b + 1) % 2
        nxt = [inproj_pro(x_d, (b + 1) * 512, 4, W1, S1, 1 - par) if b + 1 < nbk else None]
        inproj_mm(b * 512, 4, False, b * 4, par, nxt)
    if dbg:
        p.dma(dout("dt_all", [128, NT + 2, 16]), dt_all, r="dt_all", w="dbg_dt")
    ar.release(mA)
    p.barrier()
    if stop == "A":
        return fin(nc, p, ar, dbg_d)

    mB = ar.mark()
    acc = [ar.a(L) for _ in range(2)]
    EN = [p.v, p.v]

    def ssm_conv(src, n, ei, j, key):
        E = EN[ei]
        ak = "acc%d" % ei
        a_ = acc[ei][:, 0:n]
        x_ = src[:, j, 0:n]
        E("tensor_scalar", r=key + " scw", w=ak, out=a_, in0=x_, scalar1=scw[:, j, 2:3], scalar2=None, op0=ALU.mult)
        for k in (0, 1, 3, 4):
            o = k - 2
            lo, hi = max(0, -o), min(n, n - o)
            E("scalar_tensor_tensor", r=key + " scw " + ak, w=ak, out=a_[:, lo:hi], in0=x_[:, lo + o:hi + o], scalar=scw[:, j, k:k + 1],
              in1=a_[:, lo:hi], op0=ALU.mult, op1=ALU.add)
        p.s("activation", r=ak + " scb", w=key, out=x_, in_=a_, func=AF.Silu, bias=scb[:, j:j + 1])

    tmpc2 = [ar.a(L) for _ in range(2)]

    def cfm_conv(j, ei):
        ak = "acc%d" % ei
        key = "uT%d" % j
        a_ = acc[ei]
        u_ = uT[:, j, :]
        a3 = a_.rearrange("p (r c) -> p r c", c=64)
        u3 = u_.rearrange("p (r c) -> p r c", c=64)
        if ei == 0:
            p.v("tensor_scalar", r=key + " ccw", w=ak, out=a_, in0=u_, scalar1=ccw[:, j, 15:16], scalar2=None, op0=ALU.mult)
        else:
            p.s("activation", r=key + " ccw", w=ak, out=a_, in_=u_, func=AF.Copy, scale=ccw[:, j, 15:16])
        n = 0
        for k in range(31):
            o = k - 15
            if o == 0:
                continue
            lo, hi = max(0, -o), min(64, 64 - o)
            if j < 2:
                oo, i0 = a3[:, :, lo:hi], u3[:, :, lo + o:hi + o]
            else:
                oo, i0 = a_[:, lo * 64:hi * 64], u_[:, (lo + o) * 64:(hi + o) * 64]
            if ei == 0:
                p.v("scalar_tensor_tensor", r=key + " ccw " + ak, w=ak, out=oo, in0=i0, scalar=ccw[:, j, k:k + 1], in1=oo, op0=ALU.mult, op1=ALU.add)
            else:
                tk = "tmpc2%d" % (n % 2)
                tt = tmpc2[n % 2]
                if j < 2:
                    t_ = tt.rearrange("p (r c) -> p r c", c=64)[:, :, lo:hi]
                else:
                    t_ = tt[:, lo * 64:hi * 64]
                p.s("activation", r=key + " ccw", w=tk, out=t_, in_=i0, func=AF.Copy, scale=ccw[:, j, k:k + 1])
                p.g("tensor_tensor", r=tk + " " + ak, w=ak, out=oo, in0=oo, in1=t_, op=ALU.add)
                n += 1
        p.s("activation", r=ak + " cvec", w=key, out=u_, in_=a_, func=AF.Identity, bias=cvec[:, 0, j:j + 1])

    NCD = 4
    for j in range(8):
        ssm_conv(xbcTc, LC, 0, j, "xbcTc%d" % j)
    pool_chunks = [2, 3, 0, 1][:4 - NCD]
    dve_chunks = [2, 3, 0, 1][4 - NCD:]
    if pool_chunks:
        cfm_conv(pool_chunks[0], 1)
    for j in range(8):
        ssm_conv(xbcT, L, 0, j, "xbcT%d" % j)
    for j in pool_chunks[1:]:
        cfm_conv(j, 1)
    for j in dve_chunks:
        cfm_conv(j, 0)
    if dbg:
        tdb = ar.a((8, 128))
        p.v("tensor_copy", r=" ".join("xbcT%d" % j for j in range(8)), w="tdb", out=tdb, in_=xbcT[:, :, 512:640])
        p.dma(dout("xbc_post", [128, 8, 128]), tdb, r="tdb", w="dbg_xb2")
        tdb2 = ar.a((4, 128))
        p.v("tensor_copy", r=" ".join("uT%d" % j for j in range(4)), w="tdb2", out=tdb2, in_=uT[:, :, 512:640])
        p.dma(dout("u_post", [128, 4, 128]), tdb2, r="tdb2", w="dbg_ub2")
    for j in range(4):
        p.dma(uts_s[:, j, :], uT[:, j, :], r="uT%d" % j, w="uts_s", grp="utsw")
    ar.release(m_uT)
    p.barrier()
    if stop == "B":
        return fin(nc, p, ar, dbg_d)

    mE = ar.mark()
    SB_all = ar.a((NT, 512), BF16)
    Wo = ar.a((8, 1024), BF16)
    G1 = ar.a(1024); NW = ar.a(512); Drow = ar.a(512)
    p.dma(G1, modb_s[4], r="modb_s", w="G1")
    p.dma(NW, snw_d, w="NW")
    p.dma(Drow, drow_d, w="Drow")
    a16 = ar.a(16); cs = ar.a(32); dec = ar.a(16); tmp16 = ar.a(16); w16 = ar.a(16)
    rowsrc = ar.a(16); bias16 = ar.a(16); e16 = ar.a(16); RT = ar.a(128); RT3 = ar.a((3, 128), BF16); RTr = ar.a(128)
    xs_tok = ar.a(512, BF16); Btok = ar.a(256, BF16)
    xw = [ar.a(512, BF16) for _ in range(2)]
    xdt = [ar.a(512, BF16) for _ in range(2)]
    ST_sb = ar.a((2, 128))
    LT = [ar.a(128) for _ in range(2)]
    SmT = ar.a((16, 128), BF16)
    Sst = [ar.a(512) for _ in range(2)]
    Sbf = [ar.a(512, BF16) for _ in range(2)]
    yc = ar.a(512); yc2 = ar.a(512); zst = ar.a(512, BF16); gg = ar.a(512)
    ss2 = ar.a(2); rs2 = ar.a(2)
    ssm_tok = ar.a(512, BF16); catT = ar.a((8, 128), BF16)
    vsq = ar.a((4, 128)); vsqh = ar.a((4, 128), BF16); vsql = ar.a((4, 128), BF16); mean = ar.a(128); msq = ar.a(128); var = ar.a(128); ntmp = ar.a((4, 128))
    xe = [ar.a(1024) for _ in range(2)]
    for i in range(8):
        sl = i % 2
        wv_ = xe[sl].rearrange("p (a b) -> p a b", a=8)
        p.dma(wv_, wout_d[:, :, i * 128:(i + 1) * 128], w="xe%d" % sl)
        if i % 2 == 0:
            p.v("tensor_copy", r="xe%d" % sl, w="Wo", out=Wo[:, :, i * 128:(i + 1) * 128], in_=wv_)
        else:
            p.s("copy", r="xe%d" % sl, w="Wo", out=Wo[:, :, i * 128:(i + 1) * 128], in_=wv_)
    junk2 = ar.a(256, BF16)
    uTt = [ar.a((4, 128), BF16) for _ in range(2)]
    PT = PBb(0)
    PC = PB(1)
    PK = PB(2)
    PY = PB(3)
    PLS = PB(6)
    PM = PBb(7)
    PM2 = PB(7)

    def bc8(ap8):
        return ap8.unsqueeze(2).to_broadcast([128, 8, 64])

    def v3(ap512):
        return ap512.rearrange("p (h q) -> p h q", h=8)

    def prep(is_ctx, c, dirs):
        src = xbcTc if is_ctx else xbcT
        kx = "xbcTc%d" if is_ctx else "xbcT%d"
        tl = (NT + c) if is_ctx else c
        tok = slice(c * 128, (c + 1) * 128)
        p.v("tensor_tensor", r="dt_all Aneg", w="a16", out=a16, in0=dt_all[:, tl, :], in1=Aneg, op=ALU.mult)
        p.t("matmul", r="triU a16", w="b1", out=PC[:, 0:8], lhsT=triU, rhs=a16[:, 0:8], start=True, stop=True)
        p.t("matmul", r="triS a16", w="b1", out=PC[:, 8:16], lhsT=triS, rhs=a16[:, 8:16], start=True, stop=True)
        p.t("matmul", r="ones_f a16", w="b1", out=PC[:, 16:32], lhsT=ones_f, rhs=a16, start=True, stop=True)
        p.s("copy", r="b1", w="cs", out=cs, in_=PC[:, 0:32])
        p.s("activation", r="cs", w="dec", out=dec, in_=cs[:, 16:32], func=AF.Exp)
        p.v("tensor_tensor", r="cs", w="tmp16", out=tmp16[:, 0:8], in0=cs[:, 16:24], in1=cs[:, 0:8], op=ALU.subtract)
        p.v("tensor_copy", r="cs tmp16", w="tmp16", out=tmp16[:, 8:16], in_=cs[:, 8:16])
        p.s("activation", r="tmp16", w="tmp16", out=tmp16, in_=tmp16, func=AF.Exp)
        p.v("tensor_tensor", r="tmp16 dt_all", w="w16", out=w16, in0=tmp16, in1=dt_all[:, tl, :], op=ALU.mult)
        for j in range(4):
            p.t("transpose", r=(kx % j) + " ident", w="b0", out=PT[:, j * 128:(j + 1) * 128], in_=src[:, j, tok], identity=ident)
        for g in range(2):
            p.t("transpose", r=(kx % (4 + g)) + " ident", w="b0", out=PT[:, 512 + g * 128:512 + (g + 1) * 128], in_=src[:, 4 + g, tok], identity=ident)
        p.v("tensor_copy", r="b0", w="Btok", out=Btok, in_=PT[:, 512:768])
        for d in dirs:
            p.v("tensor_tensor", r="b0 w16", w="xw%d" % d, out=v3(xw[d]), in0=v3(PT[:, 0:512]), in1=bc8(w16[:, d * 8:(d + 1) * 8]), op=ALU.mult)

    def local_states(d):
        for h in range(8):
            g = h // 4
            p.t("matmul", r="Btok xw%d" % d, w="b6", out=PLS[:, h * 64:(h + 1) * 64], lhsT=Btok[:, g * 128:(g + 1) * 128],
                rhs=xw[d][:, h * 64:(h + 1) * 64], start=True, stop=True)

    def upd(d):
        p.v("tensor_tensor", r="S%d dec" % d, w="S%d" % d, out=v3(Sst[d]), in0=v3(Sst[d]), in1=bc8(dec[:, d * 8:(d + 1) * 8]), op=ALU.mult)
        p.v("tensor_tensor", r="S%d b6" % d, w="S%d" % d, out=Sst[d], in0=Sst[d], in1=PLS, op=ALU.add)
        p.s("copy", r="S%d" % d, w="Sbf%d" % d, out=Sbf[d], in_=Sst[d])

    p.v("memset", w="RT3", ap=RT3, constant=0.0)
    for d in range(2):
        p.v("memset", w="S%d" % d, ap=Sst[d], constant=0.0)
        p.v("memset", w="Sbf%d" % d, ap=Sbf[d], constant=0.0)
    prep(True, 0, (0,)); local_states(0); upd(0)
    prep(True, 1, (0, 1)); local_states(0); upd(0); local_states(1); upd(1)
    prep(True, 0, (1,)); local_states(1); upd(1)
    if stop == "C":
        p.dma(dout("S_f", [128, 512]), Sst[0], r="S0", w="dbg_sf")
        p.dma(dout("S_b", [128, 512]), Sst[1], r="S1", w="dbg_sb")
        return fin(nc, p, ar, dbg_d)
    for c in range(NT - 1, -1, -1):
        p.s("copy", r="Sbf1", w="SB%d" % c, out=SB_all[:, c, :], in_=Sbf[1])
        prep(False, c, (1,)); local_states(1); upd(1)
        if npeer == 128:
            if c >= 16:
                hp_ = c - 16
                p.dma(wqb_s[hp_].rearrange("p (k c) -> p k c", k=8), wq_d[:, :, hp_ * 128:(hp_ + 1) * 128], w="wqb_s", e="gpsimd", grp="castq")
            for i2 in range(c * 4, c * 4 + 4):
                p.dma(utb_s[i2], ut_d[i2].rearrange("p k i -> p (k i)"), w="utb_s", e="gpsimd", grp="castu")
                p.dma(vtb_s[i2], vt_d[i2], w="vtb_s", e="gpsimd", grp="castv")
    if dbg:
        p.dma(dout("S_b0", [128, 512]), Sst[1], r="S1", w="dbg_sb0")
    if stop == "D":
        return fin(nc, p, ar, dbg_d)
    for c in range(NT):
        tok = slice(c * 128, (c + 1) * 128)
        pe = c % 2
        p.dma(xe[pe], x_d[tok, :], w="xe%d" % pe)
        p.dma(zst, zs_s[tok, :], r="zs_s", w="zst")
        p.dma(uTt[pe], uts_s[:, :, tok], r="uts_s", w="uTt%d" % pe)
        uTc = uTt[pe]
        if stop == "E0":
            return fin(nc, p, ar, dbg_d)
        prep(False, c, (0,))
        if stop == "E1":
            return fin(nc, p, ar, dbg_d)
        p.v("tensor_copy", r="b0", w="xs_tok", out=xs_tok, in_=PT[:, 0:512])
        for d in range(2):
            p.v("tensor_tensor", r="b0 dt_all", w="xdt%d" % d, out=v3(xdt[d]), in0=v3(PT[:, 0:512]),
                in1=bc8(dt_all[:, c, d * 8:(d + 1) * 8]), op=ALU.mult)
        if stop == "E1a":
            return fin(nc, p, ar, dbg_d)
        for g in range(2):
            p.t("matmul", r="xbcT%d xbcT%d" % (4 + g, 6 + g), w="b1", out=PC[:, 256 + g * 128:256 + (g + 1) * 128],
                lhsT=xbcT[:, 4 + g, tok], rhs=xbcT[:, 6 + g, tok], start=True, stop=True)
        if stop == "E1b":
            return fin(nc, p, ar, dbg_d)
        p.s("copy", r="b1", w="ST_sb", out=ST_sb, in_=PC[:, 256:512].rearrange("p (g l) -> p g l", g=2))
        if stop == "E2":
            return fin(nc, p, ar, dbg_d)

        p.v("tensor_copy", r="cs", w="rowsrc", out=rowsrc[:, 0:8], in_=cs[:, 0:8])
        p.v("tensor_scalar", r="cs rowsrc", w="rowsrc", out=rowsrc[:, 8:16], in0=cs[:, 8:16], scalar1=-1.0, scalar2=None, op0=ALU.mult)
        p.v("tensor_scalar", r="rowsrc", w="bias16", out=bias16, in0=rowsrc, scalar1=-1.0, scalar2=None, op0=ALU.mult)
        p.t("transpose", r="rowsrc identf", w="b1", out=PC[0:16, 32:160], in_=rowsrc, identity=ident_f)
        p.s("copy", r="b1", w="RT", out=RT[0:16, :], in_=PC[0:16, 32:160])
        p.v("tensor_copy", r="RT", w="RT3", out=RT3[0:16, 0, :], in_=RT[0:16, :])
        p.v("tensor_tensor", r="RT RT3", w="RTr", out=RTr[0:16, :], in0=RT[0:16, :], in1=RT3[0:16, 0, :], op=ALU.subtract)
        p.v("tensor_copy", r="RTr RT3", w="RT3", out=RT3[0:16, 1, :], in_=RTr[0:16, :])
        p.v("tensor_tensor", r="RTr RT3", w="RTr", out=RTr[0:16, :], in0=RTr[0:16, :], in1=RT3[0:16, 1, :], op=ALU.subtract)
        p.v("tensor_copy", r="RTr RT3", w="RT3", out=RT3[0:16, 2, :], in_=RTr[0:16, :])
        if stop == "E3":
            return fin(nc, p, ar, dbg_d)

        p.v("tensor_copy", r="cs", w="e16", out=e16[:, 0:8], in_=cs[:, 0:8])
        p.v("tensor_tensor", r="cs e16", w="e16", out=e16[:, 8:16], in0=cs[:, 24:32], in1=cs[:, 8:16], op=ALU.subtract)
        p.s("activation", r="e16", w="e16", out=e16, in_=e16, func=AF.Exp)
        if stop == "E4":
            return fin(nc, p, ar, dbg_d)

        for idx in range(16):
            d, h = idx // 8, idx % 8
            g = h // 4
            pk = "b2" if idx % 2 == 0 else "b6"
            PKs = (PB(2) if idx % 2 == 0 else PB(6))[:, 0:128]
            for q in range(3):
                p.t("matmul", r="sel RT3", w=pk, out=PKs, lhsT=sel[:, idx, :], rhs=RT3[:, q, :], start=(q == 0), stop=False)
            p.t("matmul", r="ident maskF maskB", w=pk, out=PKs, lhsT=ident, rhs=(maskF if d == 0 else maskB), start=False, stop=True)
            lk = "LT%d" % (idx % 2)
            p.s("activation", r=pk + " bias16", w=lk, out=LT[idx % 2], in_=PKs, func=AF.Exp, bias=bias16[:, idx:idx + 1])
            p.v("tensor_tensor", r=lk + " ST_sb", w="SmT%d" % idx, out=SmT[:, idx, :], in0=LT[idx % 2], in1=ST_sb[:, g, :], op=ALU.mult)
        if stop == "E5a":
            return fin(nc, p, ar, dbg_d)
        for h in range(8):
            hs = slice(h * 64, (h + 1) * 64)
            p.t("matmul", r="SmT%d xdt0" % h, w="b3", out=PY[:, hs], lhsT=SmT[:, h, :], rhs=xdt[0][:, hs], start=True, stop=False)
            p.t("matmul", r="SmT%d xdt1" % (8 + h), w="b3", out=PY[:, hs], lhsT=SmT[:, 8 + h, :], rhs=xdt[1][:, hs], start=False, stop=True)
        for h in range(8):
            g = h // 4
            hs = slice(h * 64, (h + 1) * 64)
            p.t("matmul", r="xbcT%d Sbf0" % (6 + g), w="b4", out=PB(4)[:, hs], lhsT=xbcT[:, 6 + g, tok], rhs=Sbf[0][:, hs], start=True, stop=True)
            p.t("matmul", r="xbcT%d SB%d" % (6 + g, c), w="b5", out=PB(5)[:, hs], lhsT=xbcT[:, 6 + g, tok], rhs=SB_all[:, c, hs], start=True, stop=True)
        if stop == "E5b":
            return fin(nc, p, ar, dbg_d)
        p.v("tensor_tensor", r="b4 e16", w="yc", out=v3(yc), in0=v3(PB(4)), in1=bc8(e16[:, 0:8]), op=ALU.mult)
        p.v("tensor_tensor", r="b5 e16", w="yc2", out=v3(yc2), in0=v3(PB(5)), in1=bc8(e16[:, 8:16]), op=ALU.mult)
        p.g("tensor_tensor", r="yc yc2", w="yc", out=yc, in0=yc, in1=yc2, op=ALU.add)
        if stop == "E6":
            return fin(nc, p, ar, dbg_d)

        p.v("tensor_tensor", r="yc b3", w="yc", out=yc, in0=yc, in1=PY, op=ALU.add)
        p.g("tensor_tensor", r="xs_tok Drow", w="yc2", out=yc2, in0=xs_tok, in1=Drow, op=ALU.mult)
        p.v("tensor_tensor", r="yc yc2", w="yc", out=yc, in0=yc, in1=yc2, op=ALU.add)
        p.v("tensor_tensor", r="yc zst", w="gg", out=gg, in0=yc, in1=zst, op=ALU.mult)
        for g in range(2):
            p.s("activation", r="gg", w="junk2 ss2", out=junk2, in_=gg[:, g * 256:(g + 1) * 256], func=AF.Square, accum_out=ss2[:, g:g + 1])
        p.v("tensor_scalar", r="ss2", w="rs2", out=rs2, in0=ss2, scalar1=1.0 / 256, scalar2=EPS, op0=ALU.mult, op1=ALU.add)
        p.s("activation", r="rs2", w="rs2", out=rs2, in_=rs2, func=AF.Ln)
        p.s("activation", r="rs2", w="rs2", out=rs2, in_=rs2, func=AF.Exp, scale=-0.5)
        for g in range(2):
            gs = slice(g * 256, (g + 1) * 256)
            p.v("scalar_tensor_tensor", r="gg rs2 NW", w="ssm_tok", out=ssm_tok[:, gs], in0=gg[:, gs], scalar=rs2[:, g:g + 1], in1=NW[:, gs],
                op0=ALU.mult, op1=ALU.mult)
        for j in range(4):
            p.t("transpose", r="ssm_tok ident", w="b7", out=PM[:, j * 128:(j + 1) * 128], in_=ssm_tok[:, j * 128:(j + 1) * 128], identity=ident)
        p.s("copy", r="b7", w="catT", out=catT[:, 0:4, :], in_=PM[:, 0:512].rearrange("p (a b) -> p a b", a=4))
        if stop == "E7":
            return fin(nc, p, ar, dbg_d)

        ukeys = "uTt%d" % pe
        p.s("activation", r=ukeys, w="vsq", out=vsq, in_=uTc, func=AF.Square)
        for j in range(4):
            p.t("matmul", r="ones_b " + ukeys, w="b7", out=PM2[:, 256:384], lhsT=ones_b, rhs=uTc[:, j, :], start=(j == 0), stop=(j == 3))
        p.v("tensor_copy", r="vsq", w="vsqh", out=vsqh, in_=vsq)
        p.v("tensor_tensor", r="vsq vsqh", w="vsql", out=vsql, in0=vsq, in1=vsqh, op=ALU.subtract)
        for j in range(4):
            p.t("matmul", r="ones_b vsqh", w="b7", out=PM2[:, 384:512], lhsT=ones_b, rhs=vsqh[:, j, :], start=(j == 0), stop=False)
            p.t("matmul", r="ones_b vsql", w="b7", out=PM2[:, 384:512], lhsT=ones_b, rhs=vsql[:, j, :], start=False, stop=(j == 3))
        p.s("activation", r="b7", w="mean", out=mean, in_=PM2[:, 256:384], func=AF.Copy, scale=1.0 / 512)
        p.s("activation", r="b7", w="var", out=var, in_=PM2[:, 384:512], func=AF.Copy, scale=1.0 / 512)
        p.g("tensor_tensor", r="mean", w="msq", out=msq, in0=mean, in1=mean, op=ALU.mult)
        p.v("tensor_tensor", r="var msq", w="var", out=var, in0=var, in1=msq, op=ALU.subtract)
        p.v("tensor_scalar", r="var", w="var", out=var, in0=var, scalar1=EPS, scalar2=None, op0=ALU.add)
        p.s("activation", r="var", w="var", out=var, in_=var, func=AF.Ln)
        p.s("activation", r="var", w="var", out=var, in_=var, func=AF.Exp, scale=-0.5)
        p.v("tensor_tensor", r=ukeys + " mean", w="ntmp", out=ntmp, in0=uTc, in1=mean.unsqueeze(1).to_broadcast([128, 4, 128]), op=ALU.subtract)
        p.v("tensor_tensor", r="ntmp var", w="ntmp", out=ntmp, in0=ntmp, in1=var.unsqueeze(1).to_broadcast([128, 4, 128]), op=ALU.mult)
        for j in range(4):
            p.s("activation", r="ntmp cvec", w="catT", out=catT[:, 4 + j, :], in_=ntmp[:, j, :], func=AF.Silu, scale=cvec[:, 1, j:j + 1],
                bias=cvec[:, 2, j:j + 1])
        for nb in range(2):
            for kc in range(8):
                p.t("matmul", r="catT Wo", w="b%d" % (4 + nb), out=PB(4 + nb), lhsT=catT[:, kc, :], rhs=Wo[:, kc, nb * 512:(nb + 1) * 512],
                    start=(kc == 0), stop=(kc == 7))
            ns = slice(nb * 512, (nb + 1) * 512)
            tq, tk_ = (yc2, "yc2") if nb == 0 else (gg, "gg")
            p.v("tensor_tensor", r="b%d G1" % (4 + nb), w=tk_, out=tq, in0=PB(4 + nb), in1=G1[:, ns], op=ALU.mult)
            p.g("tensor_tensor", r=tk_ + " xe%d" % pe, w="xe%d" % pe, out=xe[pe][:, ns], in0=xe[pe][:, ns], in1=tq, op=ALU.add)
        p.dma(x1_s[tok, :], xe[pe], r="xe%d" % pe, w="x1_s", grp="x1w%d" % pe)
        local_states(0)
        upd(0)
    if dbg:
        dx1 = dout("x1", [L, 1024])
        p.dma(None, None, r="x1_s", w="dbg_x1", parts=[(dx1[i * 128:(i + 1) * 128, :], x1_s[i * 128:(i + 1) * 128, :]) for i in range(NT)])
    ar.release(mE)
    p.barrier()
    if stop == "E":
        return fin(nc, p, ar, dbg_d)

    ar.release(m_big)
    TB = 256
    KT = ar.a((16, 128), BF16)
    W2 = ar.a(1024); S2 = ar.a(1024); G2 = ar.a(1024); FW = ar.a(1024)
    p.dma(W2, modb_s[5], r="modb_s", w="W2")
    p.dma(S2, modb_s[6], r="modb_s", w="S2")
    p.dma(G2, modb_s[7], r="modb_s", w="G2")
    p.dma(FW, nrm_d[2], w="FW")
    xs1 = [ar.a(1024) for _ in range(2)]
    for i in range(4):
        sl = i % 2
        wv_ = xs1[sl][:, 0:512].rearrange("p (a b) -> p a b", a=4)
        p.dma(wv_, kt_d[:, i * 4:(i + 1) * 4, :], w="xs1%d" % sl)
        p.v("tensor_copy", r="xs1%d" % sl, w="KT", out=KT[:, i * 4:(i + 1) * 4, :], in_=wv_)
    iota_b = ar.a(128, BF16)
    p.v("tensor_copy", r="iota", w="iota_b", out=iota_b, in_=iota_f)
    Wqs = [ar.a((8, 128), BF16) for _ in range(3)]
    tmpF = ar.a(512)
    junkF = tmpF.bitcast(BF16)
    ssF = ar.a(2); rsF = ar.a(2)
    h2 = ar.a(1024, BF16)
    h2T = [ar.a((8, TB), BF16) for _ in range(3)]
    qT = ar.a((16, TB), BF16)
    sc = ar.a((16, 128))
    sv = ar.a((16, 16)); si = ar.a((16, 16), U32); sif = ar.a((16, 16)); wk = ar.a(128); wk2 = ar.a(256)
    best = ar.a((8, 16)); fl = ar.a((8, 16), U32); ai = ar.a((8, 16), U32); bi = ar.a((8, 16), U32)
    af = ar.a((8, 16)); bf_ = ar.a((8, 16)); gt = ar.a((8, 16)); gs = ar.a(8)
    IG = ar.a((3, 128))
    IGTb = [ar.a((2, 2, 128), BF16) for _ in range(3)]
    IGTg = [ar.a((2, 128)) for _ in range(3)]
    NG = 16
    ohA2 = [ar.a((NG, 128), BF16) for _ in range(2)]
    ohB2 = [ar.a((NG, 64), BF16) for _ in range(2)]
    ohc = [0]
    oh = ar.a((8, 16, 16))
    GmH = [ar.a((TB, 64), BF16) for _ in range(2)]
    wu = [ar.a((2, 1024), BF16) for _ in range(3)]
    wv = [ar.a((2, 1024), BF16) for _ in range(3)]
    ga = [ar.a(TB) for _ in range(2)]
    gab = [ar.a(TB, BF16) for _ in range(2)]
    cand = sc.rearrange("p a b -> p (a b)").rearrange("p (h a b) -> p h a b", h=8, a=16)
    svv = sv.rearrange("p (h t) a -> p h t a", t=2)
    sifv = sif.rearrange("p (h t) a -> p h t a", t=2)
    io16 = iota_f[:, 0:16].unsqueeze(1).unsqueeze(1).to_broadcast([128, 8, 16, 16])
    NBLK = L // TB
    wqi = [0]

    def part1(blk):
        par = blk % 3
        hT = h2T[par]
        hk = "h2T%d" % par
        for ts in range(2):
            t0 = blk * TB + ts * 128
            xk = "xs1%d" % ts
            p.dma(xs1[ts], x1_s[t0:t0 + 128, :], r="x1_s", w=xk, e="gpsimd", grp="gx%d" % ts)
            yield "sync"
            p.s("activation", r=xk, w="tmpF ssF", out=junkF, in_=xs1[ts], func=AF.Square, accum_out=ssF[:, 0:1])
            p.v("tensor_scalar", r="ssF", w="rsF", out=rsF[:, 0:1], in0=ssF[:, 0:1], scalar1=1.0 / 1024, scalar2=EPS, op0=ALU.mult, op1=ALU.add)
            p.s("activation", r="rsF", w="rsF", out=rsF[:, 0:1], in_=rsF[:, 0:1], func=AF.Ln)
            p.s("activation", r="rsF", w="rsF", out=rsF[:, 0:1], in_=rsF[:, 0:1], func=AF.Exp, scale=-0.5)
            yield
            p.v("scalar_tensor_tensor", r=xk + " rsF W2", w=xk, out=xs1[ts], in0=xs1[ts], scalar=rsF[:, 0:1], in1=W2, op0=ALU.mult, op1=ALU.mult)
            p.g("tensor_tensor", r=xk + " S2", w="h2", out=h2, in0=xs1[ts], in1=S2, op=ALU.add)
            yield "sync"
            for kc in range(8):
                p.t("transpose", r="h2 ident", w="b2", out=PBb(2)[:, kc * 128:(kc + 1) * 128], in_=h2[:, kc * 128:(kc + 1) * 128], identity=ident)
            yield "sync"
            p.s("copy", r="b2", w=hk, out=hT[:, :, ts * 128:(ts + 1) * 128], in_=PBb(2).rearrange("p (a b) -> p a b", a=8))
            yield
        def wq_load(hp_):
            ws_ = (wqi[0] + hp_) % 3
            p.dma(Wqs[ws_], wqb_s[hp_].rearrange("p (k c) -> p k c", k=8), r="wqb_s", w="Wqs%d" % ws_, e="gpsimd", grp="gwq%d" % ws_)
        wq_load(0)
        wq_load(1)
        yield "sync"
        for hp in range(17):
            if hp >= 1:
                if hp % 2 == 1:
                    p.s("copy", r="b2", w="qT", out=qT[:, hp - 1, :], in_=PB(2)[:, 0:TB])
                else:
                    p.v("tensor_copy", r="b2", w="qT", out=qT[:, hp - 1, :], in_=PB(2)[:, 0:TB])
            if hp < 16:
                ws = (wqi[0] + hp) % 3
                if hp + 2 < 16:
                    wq_load(hp + 2)
                p.begin()
                for kc in range(8):
                    p.t("matmul", r=("Wqs%d " % ws) + hk, w="b2", out=PB(2)[:, 0:TB], lhsT=Wqs[ws][:, kc, :], rhs=hT[:, kc, :],
                        start=(kc == 0), stop=(kc == 7))
                p.end()
            yield "sync"
        wqi[0] += 16
        yield "sync"
        for ts in range(2):
            tsl = slice(ts * 128, (ts + 1) * 128)
            for g4 in range(5):
                if g4 >= 1:
                    p.s("copy", r="b2", w="sc", out=sc[:, (g4 - 1) * 4:g4 * 4, :], in_=PB(2).rearrange("p (a b) -> p a b", a=4))
                if g4 < 4:
                    for q4 in range(4):
                        hp = g4 * 4 + q4
                        p.t("matmul", r="qT KT", w="b2", out=PB(2)[:, q4 * 128:(q4 + 1) * 128], lhsT=qT[:, hp, tsl], rhs=KT[:, hp, :],
                            start=True, stop=True)
                yield "sync"
            yield "sync"
            for hp in range(16):
                p.v("max", r="sc", w="sv", out=sv[:, hp, 0:8], in_=sc[:, hp, :])
                p.v("max_index", r="sc sv", w="si", out=si[:, hp, 0:8], in_max=sv[:, hp, 0:8], in_values=sc[:, hp, :])
                p.v("match_replace", r="sc sv", w="wk", out=wk, in_to_replace=sv[:, hp, 0:8], in_values=sc[:, hp, :], imm_value=-1e30)
                yield
                p.v("max", r="wk", w="sv", out=sv[:, hp, 8:16], in_=wk)
                p.v("max_index", r="wk sv", w="si", out=si[:, hp, 8:16], in_max=sv[:, hp, 8:16], in_values=wk)
                yield
            p.v("tensor_copy", r="si", w="sif", out=sif, in_=si)
            p.v("tensor_tensor", r="sv sc", w="sc", out=cand, in0=svv[:, :, 0, :].unsqueeze(3).to_broadcast([128, 8, 16, 16]),
                in1=svv[:, :, 1, :].unsqueeze(2).to_broadcast([128, 8, 16, 16]), op=ALU.add)
            yield
            for h in range(8):
                ch = cand[:, h].rearrange("p a b -> p (a b)")
                p.v("max", r="sc", w="best", out=best[:, h, 0:8], in_=ch)
                p.v("max_index", r="sc best", w="fl", out=fl[:, h, 0:8], in_max=best[:, h, 0:8], in_values=ch)
                p.v("match_replace", r="sc best", w="wk2", out=wk2, in_to_replace=best[:, h, 0:8], in_values=ch, imm_value=-1e30)
                yield
                p.v("max", r="wk2", w="best", out=best[:, h, 8:16], in_=wk2)
                p.v("max_index", r="wk2 best", w="fl", out=fl[:, h, 8:16], in_max=best[:, h, 8:16], in_values=wk2)
                yield
            p.v("tensor_tensor", r="best", w="gt", out=gt, in0=best, in1=best[:, :, 0:1].to_broadcast([128, 8, 16]), op=ALU.subtract)
            yield "sync"
            p.s("activation", r="gt", w="gt", out=gt, in_=gt, func=AF.Exp)
            yield "sync"
            p.v("tensor_reduce", r="gt", w="gs", out=gs, in_=gt, axis=AX.X, op=ALU.add)
            p.v("reciprocal", r="gs", w="gs", out=gs, in_=gs)
            yield
            p.v("tensor_tensor", r="gt gs IG", w="IG", out=IG[:, 2, :].rearrange("p (h j) -> p h j", h=8), in0=gt,
                in1=gs.unsqueeze(2).to_broadcast([128, 8, 16]), op=ALU.mult)
            p.v("tensor_single_scalar", r="fl", w="ai", out=ai, in_=fl, scalar=4, op=ALU.logical_shift_right)
            p.v("tensor_single_scalar", r="fl", w="bi", out=bi, in_=fl, scalar=15, op=ALU.bitwise_and)
            p.v("tensor_copy", r="ai", w="af", out=af, in_=ai)
            p.v("tensor_copy", r="bi", w="bf", out=bf_, in_=bi)
            yield
            for t_, (xf, key) in enumerate(((af, "af"), (bf_, "bf"))):
                p.v("tensor_tensor", r=key + " iota", w="oh", out=oh, in0=xf.unsqueeze(3).to_broadcast([128, 8, 16, 16]), in1=io16, op=ALU.is_equal)
                yield
                p.v("tensor_tensor", r="oh sif", w="oh", out=oh, in0=oh, in1=sifv[:, :, t_, :].unsqueeze(2).to_broadcast([128, 8, 16, 16]), op=ALU.mult)
                yield
                p.v("tensor_reduce", r="oh IG", w="IG", out=IG[:, t_, :].rearrange("p (h j) -> p h j", h=8), in_=oh, axis=AX.X, op=ALU.add)
                yield
            yield "sync"
            for q in range(3):
                p.t("transpose", r="IG identf", w="b2", out=PB(2)[:, q * 128:(q + 1) * 128], in_=IG[:, q, :], identity=ident_f)
            yield "sync"
            p.s("copy", r="b2", w="IGTb%d" % par, out=IGTb[par][:, ts, :, :], in_=PB(2)[:, 0:256].rearrange("p (a b) -> p a b", a=2))
            p.s("copy", r="b2", w="IGTg%d" % par, out=IGTg[par][:, ts, :], in_=PB(2)[:, 256:384])
            yield

    def part2(blk, half):
        par = blk % 3
        kb, kg = "IGTb%d" % par, "IGTg%d" % par
        Gh = GmH[half]
        gk = "Gm%d" % half
        groups = [(ts, tg) for ts in range(2) for tg in range(128 // NG)]
        bufs = {}

        def gen_(n):
            ts, tg = groups[n]
            tq = slice(tg * NG, (tg + 1) * NG)
            ob = ohc[0] % 2
            ohc[0] += 1
            bufs[n] = ob
            ohA, ohB = ohA2[ob], ohB2[ob]
            ka, kbb = "ohA%d" % ob, "ohB%d" % ob
            p.v("tensor_tensor", r=kb + " iota_b", w=ka, out=ohA, in0=iota_b.unsqueeze(1).to_broadcast([128, NG, 128]),
                in1=IGTb[par][:, ts, 0, tq].unsqueeze(2).to_broadcast([128, NG, 128]), op=ALU.is_equal)
            p.v("tensor_tensor", r=kb + " iota_b", w=kbb, out=ohB, in0=iota_b[:, half * 64:(half + 1) * 64].unsqueeze(1).to_broadcast([128, NG, 64]),
                in1=IGTb[par][:, ts, 1, tq].unsqueeze(2).to_broadcast([128, NG, 64]), op=ALU.is_equal)
            p.g("tensor_tensor", r=kg + " " + kbb, w=kbb, out=ohB, in0=ohB, in1=IGTg[par][:, ts, tq].unsqueeze(2).to_broadcast([128, NG, 64]), op=ALU.mult)

        def mm_(m):
            n, t8 = m // 2, m % 2
            ob = bufs[n]
            ohA, ohB = ohA2[ob], ohB2[ob]
            ka, kbb = "ohA%d" % ob, "ohB%d" % ob
            p.begin()
            for q8 in range(8):
                tt_ = t8 * 8 + q8
                p.t("matmul", r=ka + " " + kbb, w="b3", out=PB(3)[:, q8 * 64:(q8 + 1) * 64], lhsT=ohA[:, tt_, :], rhs=ohB[:, tt_, :],
                    start=True, stop=True)
            p.end()

        def ev_(m):
            n, t8 = m // 2, m % 2
            ts, tg = groups[n]
            g0 = ts * 128 + tg * NG + t8 * 8
            if m % 2 == 0:
                p.s("copy", r="b3", w=gk, out=Gh[:, g0:g0 + 8, :], in_=PB(3).rearrange("p (a b) -> p a b", a=8))
            else:
                p.v("tensor_copy", r="b3", w=gk, out=Gh[:, g0:g0 + 8, :], in_=PB(3).rearrange("p (a b) -> p a b", a=8))

        ng = len(groups)
        gen_(0)
        yield "sync"
        for m in range(2 * ng + 1):
            if m % 2 == 0 and m // 2 + 1 < ng:
                gen_(m // 2 + 1)
            if m >= 1:
                ev_(m - 1)
            if m < 2 * ng:
                mm_(m)
            yield "sync"

    widx = [0]

    wslot = {}

    def main_step(blk, s_):
        par = blk % 3
        hT = h2T[par]
        if s_ < 128:
            pr, e2 = s_ // 2, s_ % 2
            if e2 == 0:
                sl = widx[0] % 3
                widx[0] += 1
                wslot[pr % 4] = sl
                p.dma(wu[sl], utb_s[2 * pr:2 * pr + 2].rearrange("a p c -> p a c"), r="utb_s", w="wu%d" % sl)
                p.dma(wv[sl], vtb_s[2 * pr:2 * pr + 2].rearrange("a p c -> p a c"), r="vtb_s", w="wv%d" % sl)
            sl = wslot[pr % 4]
            half = s_ // 64
            ab = s_ % 2
            p.begin()
            for kc in range(8):
                p.t("matmul", r="wu%d h2T%d" % (sl, par), w="b%d" % ab, out=PB(ab)[:, 0:TB], lhsT=wu[sl][:, e2, kc * 128:(kc + 1) * 128], rhs=hT[:, kc, :],
                    start=(kc == 0), stop=(kc == 7))
            p.end()
            p.s("activation", r="b%d" % ab, w="ga%d" % ab, out=ga[ab], in_=PB(ab)[:, 0:TB], func=AF.Gelu)
            p.v("tensor_tensor", r="ga%d Gm%d" % (ab, half), w="gab%d" % ab, out=gab[ab], in0=ga[ab], in1=GmH[half][:, :, s_ % 64], op=ALU.mult)
        if s_ >= 1:
            o_ = s_ - 1
            pr, e2 = o_ // 2, o_ % 2
            sl = wslot[pr % 4]
            ab = o_ % 2
            p.begin()
            for ts in range(2):
                for nb in range(2):
                    bk = 4 + 2 * ts + nb
                    p.t("matmul", r="gab%d wv%d" % (ab, sl), w="b%d" % bk, out=PB(bk), lhsT=gab[ab][:, ts * 128:(ts + 1) * 128],
                        rhs=wv[sl][:, e2, nb * 512:(nb + 1) * 512], start=(o_ == 0), stop=(o_ == 127))
            p.end()

    def finalize(blk):
        for ts in range(2):
            t0 = blk * TB + ts * 128
            xk = "xs1%d" % ts
            p.dma(xs1[ts], x1_s[t0:t0 + 128, :], r="x1_s", w=xk, e="gpsimd", grp="gx%d" % ts)
            for nb in range(2):
                bk = 4 + 2 * ts + nb
                ns = slice(nb * 512, (nb + 1) * 512)
                p.v("tensor_tensor", r="b%d G2" % bk, w="tmpF", out=tmpF, in0=PB(bk), in1=G2[:, ns], op=ALU.mult)
                p.g("tensor_tensor", r="tmpF " + xk, w=xk, out=xs1[ts][:, ns], in0=xs1[ts][:, ns], in1=tmpF, op=ALU.add)
            p.s("activation", r=xk, w="tmpF ssF", out=junkF, in_=xs1[ts], func=AF.Square, accum_out=ssF[:, 1:2])
            p.v("tensor_scalar", r="ssF", w="rsF", out=rsF[:, 1:2], in0=ssF[:, 1:2], scalar1=1.0 / 1024, scalar2=EPS, op0=ALU.mult, op1=ALU.add)
            p.s("activation", r="rsF", w="rsF", out=rsF[:, 1:2], in_=rsF[:, 1:2], func=AF.Ln)
            p.s("activation", r="rsF", w="rsF", out=rsF[:, 1:2], in_=rsF[:, 1:2], func=AF.Exp, scale=-0.5)
            p.v("scalar_tensor_tensor", r=xk + " rsF FW", w=xk, out=xs1[ts], in0=xs1[ts], scalar=rsF[:, 1:2], in1=FW, op0=ALU.mult, op1=ALU.mult)
            p.dma(out_d[t0:t0 + 128, :], xs1[ts], r=xk, w="out", grp="gout%d" % ts, e="gpsimd")

    nblk = 1 if stop == "F1" else (3 if stop == "F2" else (7 if stop == "F7" else NBLK))

    def drain(g):
        if g is not None:
            for _ in g:
                pass

    def drip(g, n):
        if g is None:
            return None
        try:
            for _ in range(n):
                if next(g) == "sync":
                    break
        except StopIteration:
            return None
        return g

    drain(part1(0))
    if nblk > 1:
        drain(part1(1))
    drain(part2(0, 0))
    for blk in range(nblk):
        st = {"g1": part1(blk + 2) if blk + 2 < nblk else None, "gB": part2(blk, 1), "gA": None}

        DM = "3"
        if DM in ("0", "2"):
            drain(st["g1"]); st["g1"] = None
        if DM in ("0", "1"):
            drain(st["gB"]); st["gB"] = None

        N1 = 2
        SPREAD = 58

        def dr(s_, st=st, blk=blk):
            st["g1"] = drip(st["g1"], N1)
            k = s_ % 64
            if (k + 1) * 33 // SPREAD == k * 33 // SPREAD and k < SPREAD:
                return
            if s_ < 64:
                st["gB"] = drip(st["gB"], 1)
            elif DM in ("2", "3"):
                st["gA"] = drip(st["gA"], 1)

        for s_ in range(129):
            main_step(blk, s_)
            if s_ == 63:
                drain(st["gB"])
                st["gB"] = None
                st["gA"] = part2(blk + 1, 0) if blk + 1 < nblk else None
            dr(s_)
        g1, gA = st["g1"], st["gA"]
        drain(g1)
        drain(gA)
        finalize(blk)
    return fin(nc, p, ar, dbg_d)


def fin(nc, p, ar, dbg_d):
    p.finish()
    p.emit()
    return nc, dbg_d


def host_layout(inp, b):
    f = np.float32
    A = np.ascontiguousarray
    d = {}
    d["x"] = A(inp["x"][b])
    d["ctx"] = A(inp["ctx"][b])
    cc = np.stack([inp["c"][b].reshape(8, 128).T, inp["c_ctx"].reshape(8, 128).T], axis=-1)
    d["cc"] = A(cc.astype(f))
    return d


def shared_layout(inp):
    A = np.ascontiguousarray
    d = {}
    d["ada_w"] = A(inp["ada_w"][0].reshape(8, 128, 6144).transpose(1, 0, 2))
    d["ada_b"] = A(inp["ada_b"][0].reshape(1, 6144))
    d["nrm"] = A(np.stack([np.broadcast_to(inp[k].reshape(1, 1024), (128, 1024)) for k in ("norm1_w", "norm2_w", "final_norm_w")]))
    d["w_in"] = A(inp["w_in"][0].reshape(8, 128, 2576).transpose(1, 0, 2))
    d["scw"] = A(inp["ssm_conv_w"][0].reshape(5, 8, 128).transpose(2, 1, 0))
    d["scb"] = A(inp["ssm_conv_b"][0].reshape(8, 128).T)
    d["dtb"] = A(np.broadcast_to(inp["ssm_dt_bias"][0].reshape(1, 16), (128, 16)))
    d["alog"] = A(np.broadcast_to(inp["ssm_a_log"][0].reshape(1, 16), (128, 16)))
    d["drow"] = A(np.broadcast_to(np.repeat(inp["ssm_d"][0], 64).reshape(1, 512), (128, 512)))
    d["snw"] = A(np.broadcast_to(inp["ssm_norm_w"][0].reshape(1, 512), (128, 512)))
    d["ccw"] = A(inp["cfm_conv_w"][0].reshape(31, 4, 128).transpose(2, 1, 0))
    d["cvec"] = A(np.stack([inp[k][0].reshape(4, 128).T for k in ("cfm_conv_b", "cfm_ln_w", "cfm_ln_b")], axis=1))
    d["w_out"] = A(inp["w_out"][0].reshape(8, 128, 1024).transpose(1, 0, 2))
    d["wq"] = A(inp["peer_wq"][0].reshape(8, 128, 2048).transpose(1, 0, 2))
    d["kt"] = A(inp["peer_subkeys"][0].reshape(16, 128, 128).transpose(2, 0, 1))
    d["ut"] = A(inp["peer_u"][0].reshape(128, 128, 8, 128).transpose(1, 3, 2, 0))
    d["vt"] = A(inp["peer_v"][0].reshape(128, 128, 1024).transpose(1, 0, 2))
    return d


_CACHE = {}


def kernel(**inputs):
    inp = {k: np.asarray(v) for k, v in inputs.items()}
    if "nc" not in _CACHE:
        _CACHE["nc"] = build()[0]
    nc = _CACHE["nc"]
    sh = shared_layout(inp)
    in_maps = []
    for b in range(8):
        d = dict(sh)
        d.update(host_layout(inp, b))
        in_maps.append(d)
    res = run_bass_kernel_spmd(nc, in_maps, core_ids=list(range(8)))
    return np.stack([np.asarray(r["out"], dtype=np.float32) for r in res.results], axis=0)
```

```python
import re
import numpy as np
import concourse.bass as bass
import concourse.mybir as mybir
from concourse.bass_utils import run_bass_kernel_spmd

F32 = mybir.dt.float32
BF16 = mybir.dt.bfloat16
I32 = mybir.dt.int32
U32 = mybir.dt.uint32
AF = mybir.ActivationFunctionType
ALU = mybir.AluOpType
AX = mybir.AxisListType
ENGS = ("sync", "scalar", "vector", "gpsimd", "tensor")
CH = 4096
EPS = 1e-6
DSZ = {F32: 4, BF16: 2, I32: 4, U32: 4}


class Prog:
    def __init__(self, nc):
        self.nc = nc
        self.q = {e: [] for e in ENGS}
        self.cnt = {e: 0 for e in ENGS}
        self.known = {e: {} for e in ENGS}
        self.lastw = {}
        self.readers = {}
        self.sems = {}
        self.dmacum = {}
        self.pend = {e: {} for e in ENGS}
        self.lasttok = {}
        self.grp = None

    def sem(self, key):
        s = self.sems.get(key)
        if s is None:
            s = self.nc.alloc_semaphore(name="s%d" % len(self.sems))
            self.sems[key] = s
        return s

    def _waits(self, e, reads, writes):
        toks = dict(self.pend[e])
        self.pend[e] = {}

        def add(t):
            if t is None:
                return
            k, v = t
            if k[0] == "eng" and k[1] == e and e == "tensor":
                return
            if toks.get(k, 0) < v:
                toks[k] = v
        for r in reads:
            add(self.lastw.get(r))
        for w in writes:
            add(self.lastw.get(w))
            for t in self.readers.get(w, ()):
                add(t)
        out = []
        for k, v in toks.items():
            if self.known[e].get(k, 0) >= v:
                continue
            self.known[e][k] = v
            out.append((self.sem(k), v))
        return out

    def _commit(self, tok, reads, writes):
        self.lasttok[tok[0]] = tok[1]
        for w in writes:
            self.lastw[w] = tok
            self.readers[w] = []
        for r in reads:
            self.readers.setdefault(r, []).append(tok)

    def begin(self):
        self.grp = {"ops": [], "r": [], "w": []}

    def end(self):
        g, self.grp = self.grp, None
        if not g["ops"]:
            return
        reads = list(dict.fromkeys(g["r"]))
        writes = list(dict.fromkeys(g["w"]))
        waits = self._waits("tensor", reads, writes)
        idx = self.cnt["tensor"]
        self.cnt["tensor"] += 1
        k = ("eng", "tensor", idx // CH)
        n = len(g["ops"])
        for i, (name, kw) in enumerate(g["ops"]):
            self.q["tensor"].append((waits if i == 0 else [], name, kw, self.sem(k) if i == n - 1 else None, 1 if i == n - 1 else 0))
        self._commit((k, idx % CH + 1), reads, writes)

    def op(self, e, name, r="", w="", **kw):
        reads, writes = r.split(), w.split()
        if e == "tensor" and self.grp is not None:
            ex = [k for k in reads if re.fullmatch(r"(ps|b)\d", k)]
            self.grp["ops"].append((name, kw))
            self.grp["r"] += [k for k in reads if k not in ex]
            self.grp["w"] += writes + ex
            return
        ex = [k for k in reads if re.fullmatch(r"(ps|b)\d", k)]
        reads = [k for k in reads if k not in ex]
        writes = writes + [k for k in ex if k not in writes]
        waits = self._waits(e, reads, writes)
        idx = self.cnt[e]
        self.cnt[e] += 1
        k = ("eng", e, idx // CH)
        self.q[e].append((waits, name, kw, self.sem(k), 1))
        self._commit((k, idx % CH + 1), reads, writes)

    def v(self, name, r="", w="", **kw):
        self.op("vector", name, r, w, **kw)

    def s(self, name, r="", w="", **kw):
        self.op("scalar", name, r, w, **kw)

    def g(self, name, r="", w="", **kw):
        self.op("gpsimd", name, r, w, **kw)

    def t(self, name, r="", w="", **kw):
        self.op("tensor", name, r, w, **kw)

    def dma(self, out, in_, r="", w="", e="sync", grp=None, parts=None, **kw):
        reads, writes = r.split(), w.split()
        sk = ("dma", grp if grp is not None else writes[0])
        waits = self._waits(e, reads, writes)
        pieces = parts if parts is not None else [(out, in_)]
        for i, (o, a) in enumerate(pieces):
            self.dmacum[sk] = self.dmacum.get(sk, 0) + 16
            kk = dict(kw)
            kk.update(out=o, in_=a)
            self.q[e].append((waits if i == 0 else [], "dma_start", kk, self.sem(sk), 16))
        self._commit((sk, self.dmacum[sk]), reads, writes)

    def barrier(self):
        for e in ENGS:
            for k, v in self.lasttok.items():
                if self.pend[e].get(k, 0) < v:
                    self.pend[e][k] = v

    def finish(self):
        self.barrier()
        for e in ("sync", "gpsimd"):
            waits = self._waits(e, [], [])
            self.q[e].append((waits, None, None, None, 0))

    def emit(self):
        nc = self.nc
        with nc.Block() as block:
            def mk(e):
                def body(eng):
                    for waits, name, kw, s, inc in self.q[e]:
                        for ws, wv in waits:
                            eng.wait_ge(ws, wv)
                        if name is not None:
                            ins = getattr(eng, name)(**kw)
                            if s is not None:
                                ins.then_inc(s, inc)
                return body
            block.sync(mk("sync"))
            block.scalar(mk("scalar"))
            block.vector(mk("vector"))
            block.gpsimd(mk("gpsimd"))
            block.tensor(mk("tensor"))


class Arena:
    def __init__(self, nc, nbytes):
        self.t = nc.alloc_sbuf_tensor("arena", [128, nbytes // 4], F32)
        self.cap = nbytes
        self.off = 0
        self.peak = 0

    def a(self, free, dt=F32, parts=128):
        if isinstance(free, int):
            free = (free,)
        n = int(np.prod(free)) * DSZ[dt]
        n = (n + 63) // 64 * 64
        assert self.off + n <= self.cap, ("SBUF arena overflow", self.off, n, self.cap)
        ap = self.t[0:parts, self.off // 4:(self.off + n) // 4]
        self.off += n
        self.peak = max(self.peak, self.off)
        if dt != F32:
            ap = ap.bitcast(dt)
        ap = ap[:, 0:int(np.prod(free))]
        if len(free) == 2:
            ap = ap.rearrange("p (a b) -> p a b", a=free[0])
        elif len(free) == 3:
            ap = ap.rearrange("p (a b c) -> p a b c", a=free[0], b=free[1])
        return ap

    def mark(self):
        return self.off

    def release(self, m):
        self.off = m


L = 4096
LC = 256
NT = L // 128


def build(stop=None, dbg=False, npeer=128):
    nc = bass.Bass("TRN2", target_bir_lowering=False)
    D = nc.dram_tensor

    def din(name, shape, dt=F32):
        return D(name, list(shape), dt, kind="ExternalInput").ap()
    x_d = din("x", [L, 1024])
    ctx_d = din("ctx", [LC, 1024])
    cc_d = din("cc", [128, 8, 2])
    adaw_d = din("ada_w", [128, 8, 6144])
    adab_d = din("ada_b", [1, 6144])
    nrm_d = din("nrm", [3, 128, 1024])
    win_d = din("w_in", [128, 8, 2576])
    scw_d = din("scw", [128, 8, 5])
    scb_d = din("scb", [128, 8])
    dtb_d = din("dtb", [128, 16])
    alog_d = din("alog", [128, 16])
    drow_d = din("drow", [128, 512])
    snw_d = din("snw", [128, 512])
    ccw_d = din("ccw", [128, 4, 31])
    cvec_d = din("cvec", [128, 3, 4])
    wout_d = din("w_out", [128, 8, 1024])
    wq_d = din("wq", [128, 8, 2048])
    kt_d = din("kt", [128, 16, 128])
    ut_d = din("ut", [npeer, 128, 8, 128])
    vt_d = din("vt", [npeer, 128, 1024])
    out_d = D("out", [L, 1024], F32, kind="ExternalOutput").ap()
    dbg_d = {}

    def dout(name, shape):
        dbg_d[name] = D("dbg_" + name, list(shape), F32, kind="ExternalOutput").ap()
        return dbg_d[name]
    modb_s = D("modb_s", [8, 128, 1024], F32).ap()
    zs_s = D("zs_s", [L, 512], BF16).ap()
    x1_s = D("x1_s", [L, 1024], F32).ap()
    utb_s = D("utb_s", [128, 128, 1024], BF16).ap()
    vtb_s = D("vtb_s", [128, 128, 1024], BF16).ap()
    uts_s = D("uts_s", [128, 4, L], BF16).ap()
    wqb_s = D("wqb_s", [16, 128, 1024], BF16).ap()

    p = Prog(nc)
    ar = Arena(nc, 212000)
    banks = [nc.alloc_psum_tensor("pb%d" % i, [128, 512], F32) for i in range(8)]

    def PB(i):
        return banks[i][:]

    def PBb(i):
        return banks[i][:].bitcast(BF16)

    ident_f = ar.a(128)
    ident = ar.a(128, BF16)
    iota_f = ar.a(128)
    pidx = ar.a(128)
    ones_f = ar.a(128)
    ones_b = ar.a(128, BF16)
    triU = ar.a(128)
    triS = ar.a(128)
    maskF = ar.a(128, BF16)
    maskB = ar.a(128, BF16)
    p.g("iota", w="iota", out=iota_f, pattern=[[1, 128]], base=0, channel_multiplier=0, allow_small_or_imprecise_dtypes=True)
    p.g("iota", w="pidx", out=pidx, pattern=[[0, 128]], base=0, channel_multiplier=1, allow_small_or_imprecise_dtypes=True)
    p.v("tensor_tensor", r="iota pidx", w="identf", out=ident_f, in0=iota_f, in1=pidx, op=ALU.is_equal)
    p.v("tensor_copy", r="identf", w="ident", out=ident, in_=ident_f)
    p.v("memset", w="ones_f", ap=ones_f, constant=1.0)
    p.v("memset", w="ones_b", ap=ones_b, constant=1.0)
    p.v("tensor_tensor", r="iota pidx", w="triU", out=triU, in0=iota_f, in1=pidx, op=ALU.is_ge)
    p.v("tensor_tensor", r="iota pidx", w="triS", out=triS, in0=iota_f, in1=pidx, op=ALU.is_gt)
    p.v("tensor_scalar", r="triU", w="maskF", out=maskF, in0=triU, scalar1=-1.0, scalar2=30000.0, op0=ALU.add, op1=ALU.mult)
    tmpc = ar.a(128)
    p.v("tensor_tensor", r="iota pidx", w="tmpc", out=tmpc, in0=iota_f, in1=pidx, op=ALU.is_le)
    p.v("tensor_scalar", r="tmpc", w="maskB", out=maskB, in0=tmpc, scalar1=-1.0, scalar2=30000.0, op0=ALU.add, op1=ALU.mult)
    sel = ar.a((16, 128), BF16)
    p.v("memset", w="sel", ap=sel, constant=0.0)
    p.v("tensor_copy", r="identf sel", w="sel", out=sel[0:16], in_=ident_f[0:16, 0:16].unsqueeze(2).to_broadcast([16, 16, 128]))
    scw = ar.a((8, 5)); scb = ar.a(8); dtbias = ar.a(16); alog = ar.a(16); ccw = ar.a((4, 31)); cvec = ar.a((3, 4))
    Aneg = ar.a(16)
    p.dma(scw, scw_d, w="scw", grp="small")
    p.dma(scb, scb_d, w="scb", grp="small")
    p.dma(dtbias, dtb_d, w="dtbias", grp="small")
    p.dma(alog, alog_d, w="alog", grp="small")
    p.dma(ccw, ccw_d, w="ccw", grp="small")
    p.dma(cvec, cvec_d, w="cvec", grp="small")
    p.s("activation", r="alog", w="Aneg", out=Aneg, in_=alog, func=AF.Exp)
    p.v("tensor_scalar", r="Aneg", w="Aneg", out=Aneg, in0=Aneg, scalar1=-1.0, scalar2=None, op0=ALU.mult)
    dt_all = ar.a((NT + 2, 16))
    m_persist = ar.mark()

    m0 = ar.mark()
    cc = ar.a((8, 2)); scT = ar.a((8, 2))
    adab = ar.a(6144, parts=1)
    modrow = ar.a(6144, parts=1)
    modrow_c = ar.a(2048, parts=1)
    awb = [ar.a((8, 512)) for _ in range(2)]
    nrm = [ar.a(1024) for _ in range(2)]
    bct = [ar.a(1024) for _ in range(2)]
    p.dma(cc, cc_d, w="cc")
    p.dma(adab, adab_d, w="adab")
    p.dma(nrm[0], nrm_d[0], w="nrm0")
    p.dma(nrm[1], nrm_d[1], w="nrm1")
    p.s("activation", r="cc", w="scT", out=scT, in_=cc, func=AF.Silu)
    for blk in range(12):
        sl = blk % 2
        p.dma(awb[sl], adaw_d[:, :, blk * 512:(blk + 1) * 512], w="awb%d" % sl)
        for kc in range(8):
            p.t("matmul", r="scT awb%d" % sl, w="ps0", out=PB(0)[0:1, :], lhsT=scT[:, kc, 0:1], rhs=awb[sl][:, kc, :],
                start=(kc == 0), stop=(kc == 7))
        if blk < 4:
            for kc in range(8):
                p.t("matmul", r="scT awb%d" % sl, w="ps1", out=PB(1)[0:1, :], lhsT=scT[:, kc, 1:2], rhs=awb[sl][:, kc, :],
                    start=(kc == 0), stop=(kc == 7))
            p.v("tensor_tensor", r="ps1 adab", w="modrow_c", out=modrow_c[:, blk * 512:(blk + 1) * 512], in0=PB(1)[0:1, :],
                in1=adab[:, blk * 512:(blk + 1) * 512], op=ALU.add)
        p.v("tensor_tensor", r="ps0 adab", w="modrow", out=modrow[:, blk * 512:(blk + 1) * 512], in0=PB(0)[0:1, :],
            in1=adab[:, blk * 512:(blk + 1) * 512], op=ALU.add)
    plan = [(0, modrow, 1, 0), (1, modrow, 0, None), (2, modrow_c, 1, 0), (3, modrow_c, 0, None), (4, modrow, 2, None),
            (5, modrow, 4, 1), (6, modrow, 3, None), (7, modrow, 5, None)]
    for i, (ti, src, mi, ni) in enumerate(plan):
        bt = bct[i % 2]
        bk = "bct%d" % (i % 2)
        for hf in range(2):
            p.t("matmul", r="ones_f modrow modrow_c", w="ps%d" % (2 + hf), out=PB(2 + hf), lhsT=ones_f[0:1, :],
                rhs=src[:, mi * 1024 + hf * 512: mi * 1024 + (hf + 1) * 512], start=True, stop=True)
            if ni is None:
                p.v("tensor_copy", r="ps%d" % (2 + hf), w=bk, out=bt[:, hf * 512:(hf + 1) * 512], in_=PB(2 + hf))
            else:
                p.v("scalar_tensor_tensor", r="ps%d nrm%d" % (2 + hf, ni), w=bk, out=bt[:, hf * 512:(hf + 1) * 512], in0=PB(2 + hf),
                    scalar=1.0, in1=nrm[ni][:, hf * 512:(hf + 1) * 512], op0=ALU.add, op1=ALU.mult)
        p.dma(modb_s[ti], bt, r=bk, w="modb_s", grp="modb_w")
    if dbg:
        p.dma(dout("modrow", [1, 6144]), modrow, r="modrow", w="dbg_modrow")
    ar.release(m0)
    p.barrier()
    if stop == "0":
        return fin(nc, p, ar, dbg_d)

    m_big = ar.mark()
    xbcT = ar.a((8, L), BF16)
    xbcTc = ar.a((8, LC), BF16)
    m_uT = ar.mark()
    uT = ar.a((4, L), BF16)

    mA = ar.mark()
    Wb = ar.a((8, 2576), BF16)
    wst = [ar.a((8, 128)) for _ in range(2)]
    W1 = ar.a(1024); S1 = ar.a(1024)

    def load_mod(i0):
        p.dma(W1, modb_s[i0], r="modb_s", w="modA0", grp="modA")
        p.dma(S1, modb_s[i0 + 1], r="modb_s", w="modA1", grp="modA")
    ncol = 2576
    pieces = [(c0, min(128, ncol - c0)) for c0 in range(0, ncol, 128)]
    for i, (c0, cw) in enumerate(pieces):
        sl = i % 2
        p.dma(wst[sl][:, :, 0:cw], win_d[:, :, c0:c0 + cw], w="wst%d" % sl)
        if i % 2 == 0:
            p.v("tensor_copy", r="wst%d" % sl, w="Wb", out=Wb[:, :, c0:c0 + cw], in_=wst[sl][:, :, 0:cw])
        else:
            p.s("copy", r="wst%d" % sl, w="Wb", out=Wb[:, :, c0:c0 + cw], in_=wst[sl][:, :, 0:cw])
    xt = [ar.a(1024) for _ in range(2)]
    junk = ar.a(1024, BF16)
    hb = [ar.a(1024, BF16) for _ in range(2)]
    hT2 = [ar.a((8, 512), BF16) for _ in range(2)]
    ss = ar.a(2); rs = ar.a(2)
    sig = ar.a(512)
    zsb = [ar.a(512, BF16) for _ in range(2)]
    dtt = ar.a(16)

    def inproj_pro(src_d, t0, ntile, Wm, Sm, par):
        hT = hT2[par]
        hk = "hT%d" % par
        for ts in range(ntile):
            sl = ts % 2
            p.dma(xt[sl], src_d[t0 + ts * 128:t0 + (ts + 1) * 128, :], w="xt%d" % sl)
            p.s("activation", r="xt%d" % sl, w="junk ss", out=junk, in_=xt[sl], func=AF.Square, accum_out=ss[:, 0:1])
            p.v("tensor_scalar", r="ss", w="rs", out=rs[:, 0:1], in0=ss[:, 0:1], scalar1=1.0 / 1024, scalar2=EPS, op0=ALU.mult, op1=ALU.add)
            p.s("activation", r="rs", w="rs", out=rs[:, 0:1], in_=rs[:, 0:1], func=AF.Ln)
            p.s("activation", r="rs", w="rs", out=rs[:, 0:1], in_=rs[:, 0:1], func=AF.Exp, scale=-0.5)
            p.v("scalar_tensor_tensor", r="xt%d rs modA0" % sl, w="xt%d" % sl, out=xt[sl], in0=xt[sl], scalar=rs[:, 0:1], in1=Wm,
                op0=ALU.mult, op1=ALU.mult)
            p.g("tensor_tensor", r="xt%d modA1" % sl, w="hb%d" % sl, out=hb[sl], in0=xt[sl], in1=Sm, op=ALU.add)
            yield
            for kc in range(8):
                p.t("transpose", r="hb%d ident" % sl, w="ps%d" % (4 + sl), out=PBb(4 + sl)[:, kc * 128:(kc + 1) * 128],
                    in_=hb[sl][:, kc * 128:(kc + 1) * 128], identity=ident)
            yield
            p.s("copy", r="ps%d" % (4 + sl), w=hk, out=hT[:, :, ts * 128:(ts + 1) * 128],
                in_=PBb(4 + sl).rearrange("p (a b) -> p a b", a=8))
            yield

    def inproj_mm(t0, ntile, is_ctx, tile0, par, nxt):
        ntok = ntile * 128
        hT = hT2[par]
        hk = "hT%d" % par

        def tick():
            if nxt[0] is not None:
                try:
                    next(nxt[0])
                except StopIteration:
                    nxt[0] = None
        for j in range(8):
            pk = j % 2
            c0 = 512 + j * 128
            for kc in range(8):
                p.t("matmul", r="Wb " + hk, w="ps%d" % pk, out=PB(pk)[:, 0:ntok], lhsT=Wb[:, kc, c0:c0 + 128], rhs=hT[:, kc, 0:ntok],
                    start=(kc == 0), stop=(kc == 7))
            dst = xbcTc[:, j, 0:ntok] if is_ctx else xbcT[:, j, t0:t0 + ntok]
            if j % 2 == 0:
                p.s("copy", r="ps%d" % pk, w="xbcT", out=dst, in_=PB(pk)[:, 0:ntok])
            else:
                p.v("tensor_copy", r="ps%d" % pk, w="xbcT", out=dst, in_=PB(pk)[:, 0:ntok])
            tick()
        if not is_ctx:
            for j in range(4):
                ca = 1552 + j * 128
                cb = 1552 + 512 + j * 128
                for kc in range(8):
                    p.t("matmul", r="Wb " + hk, w="ps2", out=PB(2)[:, 0:ntok], lhsT=Wb[:, kc, ca:ca + 128], rhs=hT[:, kc, 0:ntok],
                        start=(kc == 0), stop=(kc == 7))
                for kc in range(8):
                    p.t("matmul", r="Wb " + hk, w="ps3", out=PB(3)[:, 0:ntok], lhsT=Wb[:, kc, cb:cb + 128], rhs=hT[:, kc, 0:ntok],
                        start=(kc == 0), stop=(kc == 7))
                p.s("activation", r="ps3", w="sig", out=sig[:, 0:ntok], in_=PB(3)[:, 0:ntok], func=AF.Sigmoid)
                p.v("tensor_tensor", r="ps2 sig", w="uT", out=uT[:, j, t0:t0 + ntok], in0=PB(2)[:, 0:ntok], in1=sig[:, 0:ntok], op=ALU.mult)
                tick()
        for ts in range(ntile):
            sl = ts % 2
            if not is_ctx:
                for kc in range(8):
                    p.t("matmul", r="Wb " + hk, w="ps%d" % (6 + sl), out=PB(6 + sl), lhsT=hT[:, kc, ts * 128:(ts + 1) * 128], rhs=Wb[:, kc, 0:512],
                        start=(kc == 0), stop=(kc == 7))
                p.s("activation", r="ps%d" % (6 + sl), w="zsb%d" % sl, out=zsb[sl], in_=PB(6 + sl), func=AF.Silu)
                p.dma(zs_s[t0 + ts * 128:t0 + (ts + 1) * 128, :], zsb[sl], r="zsb%d" % sl, w="zs_s", e="sync", grp="zsw%d" % sl)
            for kc in range(8):
                p.t("matmul", r="Wb " + hk, w="ps0", out=PB(0)[:, 0:16], lhsT=hT[:, kc, ts * 128:(ts + 1) * 128], rhs=Wb[:, kc, 1536:1552],
                    start=(kc == 0), stop=(kc == 7))
            p.v("tensor_tensor", r="ps0 dtbias", w="dtt", out=dtt, in0=PB(0)[:, 0:16], in1=dtbias, op=ALU.add)
            p.s("activation", r="dtt", w="dtt", out=dtt, in_=dtt, func=AF.Exp)
            p.s("activation", r="dtt", w="dt_all", out=dt_all[:, tile0 + ts, :], in_=dtt, func=AF.Ln, bias=1.0)
            tick()
        while nxt[0] is not None:
            tick()

    load_mod(2)
    for _ in inproj_pro(ctx_d, 0, 2, W1, S1, 0):
        pass
    inproj_mm(0, 2, True, NT, 0, [None])
    load_mod(0)
    nbk = L // 512
    for _ in inproj_pro(x_d, 0, 4, W1, S1, 1):
        pass
    for b in range(nbk):
        par = (b + 1) % 2
        nxt = [inproj_pro(x_d, (b + 1) * 512, 4, W1, S1, 1 - par) if b + 1 < nbk else None]
        inproj_mm(b * 512, 4, False, b * 4, par, nxt)
    if dbg:
        p.dma(dout("dt_all", [128, NT + 2, 16]), dt_all, r="dt_all", w="dbg_dt")
    ar.release(mA)
    p.barrier()
    if stop == "A":
        return fin(nc, p, ar, dbg_d)

    mB = ar.mark()
    acc = [ar.a(L) for _ in range(2)]
    EN = [p.v, p.v]

    def ssm_conv(src, n, ei, j, key):
        E = EN[ei]
        ak = "acc%d" % ei
        a_ = acc[ei][:, 0:n]
        x_ = src[:, j, 0:n]
        E("tensor_scalar", r=key + " scw", w=ak, out=a_, in0=x_, scalar1=scw[:, j, 2:3], scalar2=None, op0=ALU.mult)
        for k in (0, 1, 3, 4):
            o = k - 2
            lo, hi = max(0, -o), min(n, n - o)
            E("scalar_tensor_tensor", r=key + " scw " + ak, w=ak, out=a_[:, lo:hi], in0=x_[:, lo + o:hi + o], scalar=scw[:, j, k:k + 1],
              in1=a_[:, lo:hi], op0=ALU.mult, op1=ALU.add)
        p.s("activation", r=ak + " scb", w=key, out=x_, in_=a_, func=AF.Silu, bias=scb[:, j:j + 1])

    tmpc2 = [ar.a(L) for _ in range(2)]

    def cfm_conv(j, ei):
        ak = "acc%d" % ei
        key = "uT%d" % j
        a_ = acc[ei]
        u_ = uT[:, j, :]
        a3 = a_.rearrange("p (r c) -> p r c", c=64)
        u3 = u_.rearrange("p (r c) -> p r c", c=64)
        if ei == 0:
            p.v("tensor_scalar", r=key + " ccw", w=ak, out=a_, in0=u_, scalar1=ccw[:, j, 15:16], scalar2=None, op0=ALU.mult)
        else:
            p.s("activation", r=key + " ccw", w=ak, out=a_, in_=u_, func=AF.Copy, scale=ccw[:, j, 15:16])
        n = 0
        for k in range(31):
            o = k - 15
            if o == 0:
                continue
            lo, hi = max(0, -o), min(64, 64 - o)
            if j < 2:
                oo, i0 = a3[:, :, lo:hi], u3[:, :, lo + o:hi + o]
            else:
                oo, i0 = a_[:, lo * 64:hi * 64], u_[:, (lo + o) * 64:(hi + o) * 64]
            if ei == 0:
                p.v("scalar_tensor_tensor", r=key + " ccw " + ak, w=ak, out=oo, in0=i0, scalar=ccw[:, j, k:k + 1], in1=oo, op0=ALU.mult, op1=ALU.add)
            else:
                tk = "tmpc2%d" % (n % 2)
                tt = tmpc2[n % 2]
                if j < 2:
                    t_ = tt.rearrange("p (r c) -> p r c", c=64)[:, :, lo:hi]
                else:
                    t_ = tt[:, lo * 64:hi * 64]
                p.s("activation", r=key + " ccw", w=tk, out=t_, in_=i0, func=AF.Copy, scale=ccw[:, j, k:k + 1])
                p.g("tensor_tensor", r=tk + " " + ak, w=ak, out=oo, in0=oo, in1=t_, op=ALU.add)
                n += 1
        p.s("activation", r=ak + " cvec", w=key, out=u_, in_=a_, func=AF.Identity, bias=cvec[:, 0, j:j + 1])

    NCD = 4
    for j in range(8):
        ssm_conv(xbcTc, LC, 0, j, "xbcTc%d" % j)
    pool_chunks = [2, 3, 0, 1][:4 - NCD]
    dve_chunks = [2, 3, 0, 1][4 - NCD:]
    if pool_chunks:
        cfm_conv(pool_chunks[0], 1)
    for j in range(8):
        ssm_conv(xbcT, L, 0, j, "xbcT%d" % j)
    for j in pool_chunks[1:]:
        cfm_conv(j, 1)
    for j in dve_chunks:
        cfm_conv(j, 0)
    if dbg:
        tdb = ar.a((8, 128))
        p.v("tensor_copy", r=" ".join("xbcT%d" % j for j in range(8)), w="tdb", out=tdb, in_=xbcT[:, :, 512:640])
        p.dma(dout("xbc_post", [128, 8, 128]), tdb, r="tdb", w="dbg_xb2")
        tdb2 = ar.a((4, 128))
        p.v("tensor_copy", r=" ".join("uT%d" % j for j in range(4)), w="tdb2", out=tdb2, in_=uT[:, :, 512:640])
        p.dma(dout("u_post", [128, 4, 128]), tdb2, r="tdb2", w="dbg_ub2")
    for j in range(4):
        p.dma(uts_s[:, j, :], uT[:, j, :], r="uT%d" % j, w="uts_s", grp="utsw")
    ar.release(m_uT)
    p.barrier()
    if stop == "B":
        return fin(nc, p, ar, dbg_d)

    mE = ar.mark()
    SB_all = ar.a((NT, 512), BF16)
    Wo = ar.a((8, 1024), BF16)
    G1 = ar.a(1024); NW = ar.a(512); Drow = ar.a(512)
    p.dma(G1, modb_s[4], r="modb_s", w="G1")
    p.dma(NW, snw_d, w="NW")
    p.dma(Drow, drow_d, w="Drow")
    a16 = ar.a(16); cs = ar.a(32); dec = ar.a(16); tmp16 = ar.a(16); w16 = ar.a(16)
    rowsrc = ar.a(16); bias16 = ar.a(16); e16 = ar.a(16); RT = ar.a(128); RT3 = ar.a((3, 128), BF16); RTr = ar.a(128)
    xs_tok = ar.a(512, BF16); Btok = ar.a(256, BF16)
    xw = [ar.a(512, BF16) for _ in range(2)]
    xdt = [ar.a(512, BF16) for _ in range(2)]
    ST_sb = ar.a((2, 128))
    LT = [ar.a(128) for _ in range(2)]
    SmT = ar.a((16, 128), BF16)
    Sst = [ar.a(512) for _ in range(2)]
    Sbf = [ar.a(512, BF16) for _ in range(2)]
    yc = ar.a(512); yc2 = ar.a(512); zst = ar.a(512, BF16); gg = ar.a(512)
    ss2 = ar.a(2); rs2 = ar.a(2)
    ssm_tok = ar.a(512, BF16); catT = ar.a((8, 128), BF16)
    vsq = ar.a((4, 128)); vsqh = ar.a((4, 128), BF16); vsql = ar.a((4, 128), BF16); mean = ar.a(128); msq = ar.a(128); var = ar.a(128); ntmp = ar.a((4, 128))
    xe = [ar.a(1024) for _ in range(2)]
    for i in range(8):
        sl = i % 2
        wv_ = xe[sl].rearrange("p (a b) -> p a b", a=8)
        p.dma(wv_, wout_d[:, :, i * 128:(i + 1) * 128], w="xe%d" % sl)
        if i % 2 == 0:
            p.v("tensor_copy", r="xe%d" % sl, w="Wo", out=Wo[:, :, i * 128:(i + 1) * 128], in_=wv_)
        else:
            p.s("copy", r="xe%d" % sl, w="Wo", out=Wo[:, :, i * 128:(i + 1) * 128], in_=wv_)
    junk2 = ar.a(256, BF16)
    uTt = [ar.a((4, 128), BF16) for _ in range(2)]
    PT = PBb(0)
    PC = PB(1)
    PK = PB(2)
    PY = PB(3)
    PLS = PB(6)
    PM = PBb(7)
    PM2 = PB(7)

    def bc8(ap8):
        return ap8.unsqueeze(2).to_broadcast([128, 8, 64])

    def v3(ap512):
        return ap512.rearrange("p (h q) -> p h q", h=8)

    def prep(is_ctx, c, dirs):
        src = xbcTc if is_ctx else xbcT
        kx = "xbcTc%d" if is_ctx else "xbcT%d"
        tl = (NT + c) if is_ctx else c
        tok = slice(c * 128, (c + 1) * 128)
        p.v("tensor_tensor", r="dt_all Aneg", w="a16", out=a16, in0=dt_all[:, tl, :], in1=Aneg, op=ALU.mult)
        p.t("matmul", r="triU a16", w="b1", out=PC[:, 0:8], lhsT=triU, rhs=a16[:, 0:8], start=True, stop=True)
        p.t("matmul", r="triS a16", w="b1", out=PC[:, 8:16], lhsT=triS, rhs=a16[:, 8:16], start=True, stop=True)
        p.t("matmul", r="ones_f a16", w="b1", out=PC[:, 16:32], lhsT=ones_f, rhs=a16, start=True, stop=True)
        p.s("copy", r="b1", w="cs", out=cs, in_=PC[:, 0:32])
        p.s("activation", r="cs", w="dec", out=dec, in_=cs[:, 16:32], func=AF.Exp)
        p.v("tensor_tensor", r="cs", w="tmp16", out=tmp16[:, 0:8], in0=cs[:, 16:24], in1=cs[:, 0:8], op=ALU.subtract)
        p.v("tensor_copy", r="cs tmp16", w="tmp16", out=tmp16[:, 8:16], in_=cs[:, 8:16])
        p.s("activation", r="tmp16", w="tmp16", out=tmp16, in_=tmp16, func=AF.Exp)
        p.v("tensor_tensor", r="tmp16 dt_all", w="w16", out=w16, in0=tmp16, in1=dt_all[:, tl, :], op=ALU.mult)
        for j in range(4):
            p.t("transpose", r=(kx % j) + " ident", w="b0", out=PT[:, j * 128:(j + 1) * 128], in_=src[:, j, tok], identity=ident)
        for g in range(2):
            p.t("transpose", r=(kx % (4 + g)) + " ident", w="b0", out=PT[:, 512 + g * 128:512 + (g + 1) * 128], in_=src[:, 4 + g, tok], identity=ident)
        p.v("tensor_copy", r="b0", w="Btok", out=Btok, in_=PT[:, 512:768])
        for d in dirs:
            p.v("tensor_tensor", r="b0 w16", w="xw%d" % d, out=v3(xw[d]), in0=v3(PT[:, 0:512]), in1=bc8(w16[:, d * 8:(d + 1) * 8]), op=ALU.mult)

    def local_states(d):
        for h in range(8):
            g = h // 4
            p.t("matmul", r="Btok xw%d" % d, w="b6", out=PLS[:, h * 64:(h + 1) * 64], lhsT=Btok[:, g * 128:(g + 1) * 128],
                rhs=xw[d][:, h * 64:(h + 1) * 64], start=True, stop=True)

    def upd(d):
        p.v("tensor_tensor", r="S%d dec" % d, w="S%d" % d, out=v3(Sst[d]), in0=v3(Sst[d]), in1=bc8(dec[:, d * 8:(d + 1) * 8]), op=ALU.mult)
        p.v("tensor_tensor", r="S%d b6" % d, w="S%d" % d, out=Sst[d], in0=Sst[d], in1=PLS, op=ALU.add)
        p.s("copy", r="S%d" % d, w="Sbf%d" % d, out=Sbf[d], in_=Sst[d])

    p.v("memset", w="RT3", ap=RT3, constant=0.0)
    for d in range(2):
        p.v("memset", w="S%d" % d, ap=Sst[d], constant=0.0)
        p.v("memset", w="Sbf%d" % d, ap=Sbf[d], constant=0.0)
    prep(True, 0, (0,)); local_states(0); upd(0)
    prep(True, 1, (0, 1)); local_states(0); upd(0); local_states(1); upd(1)
    prep(True, 0, (1,)); local_states(1); upd(1)
    if stop == "C":
        p.dma(dout("S_f", [128, 512]), Sst[0], r="S0", w="dbg_sf")
        p.dma(dout("S_b", [128, 512]), Sst[1], r="S1", w="dbg_sb")
        return fin(nc, p, ar, dbg_d)
    for c in range(NT - 1, -1, -1):
        p.s("copy", r="Sbf1", w="SB%d" % c, out=SB_all[:, c, :], in_=Sbf[1])
        prep(False, c, (1,)); local_states(1); upd(1)
        if npeer == 128:
            if c >= 16:
                hp_ = c - 16
                p.dma(wqb_s[hp_].rearrange("p (k c) -> p k c", k=8), wq_d[:, :, hp_ * 128:(hp_ + 1) * 128], w="wqb_s", e="gpsimd", grp="castq")
            for i2 in range(c * 4, c * 4 + 4):
                p.dma(utb_s[i2], ut_d[i2].rearrange("p k i -> p (k i)"), w="utb_s", e="gpsimd", grp="castu")
                p.dma(vtb_s[i2], vt_d[i2], w="vtb_s", e="gpsimd", grp="castv")
    if dbg:
        p.dma(dout("S_b0", [128, 512]), Sst[1], r="S1", w="dbg_sb0")
    if stop == "D":
        return fin(nc, p, ar, dbg_d)
    for c in range(NT):
        tok = slice(c * 128, (c + 1) * 128)
        pe = c % 2
        p.dma(xe[pe], x_d[tok, :], w="xe%d" % pe)
        p.dma(zst, zs_s[tok, :], r="zs_s", w="zst")
        p.dma(uTt[pe], uts_s[:, :, tok], r="uts_s", w="uTt%d" % pe)
        uTc = uTt[pe]
        if stop == "E0":
            return fin(nc, p, ar, dbg_d)
        prep(False, c, (0,))
        if stop == "E1":
            return fin(nc, p, ar, dbg_d)
        p.v("tensor_copy", r="b0", w="xs_tok", out=xs_tok, in_=PT[:, 0:512])
        for d in range(2):
            p.v("tensor_tensor", r="b0 dt_all", w="xdt%d" % d, out=v3(xdt[d]), in0=v3(PT[:, 0:512]),
                in1=bc8(dt_all[:, c, d * 8:(d + 1) * 8]), op=ALU.mult)
        if stop == "E1a":
            return fin(nc, p, ar, dbg_d)
        for g in range(2):
            p.t("matmul", r="xbcT%d xbcT%d" % (4 + g, 6 + g), w="b1", out=PC[:, 256 + g * 128:256 + (g + 1) * 128],
                lhsT=xbcT[:, 4 + g, tok], rhs=xbcT[:, 6 + g, tok], start=True, stop=True)
        if stop == "E1b":
            return fin(nc, p, ar, dbg_d)
        p.s("copy", r="b1", w="ST_sb", out=ST_sb, in_=PC[:, 256:512].rearrange("p (g l) -> p g l", g=2))
        if stop == "E2":
            return fin(nc, p, ar, dbg_d)

        p.v("tensor_copy", r="cs", w="rowsrc", out=rowsrc[:, 0:8], in_=cs[:, 0:8])
        p.v("tensor_scalar", r="cs rowsrc", w="rowsrc", out=rowsrc[:, 8:16], in0=cs[:, 8:16], scalar1=-1.0, scalar2=None, op0=ALU.mult)
        p.v("tensor_scalar", r="rowsrc", w="bias16", out=bias16, in0=rowsrc, scalar1=-1.0, scalar2=None, op0=ALU.mult)
        p.t("transpose", r="rowsrc identf", w="b1", out=PC[0:16, 32:160], in_=rowsrc, identity=ident_f)
        p.s("copy", r="b1", w="RT", out=RT[0:16, :], in_=PC[0:16, 32:160])
        p.v("tensor_copy", r="RT", w="RT3", out=RT3[0:16, 0, :], in_=RT[0:16, :])
        p.v("tensor_tensor", r="RT RT3", w="RTr", out=RTr[0:16, :], in0=RT[0:16, :], in1=RT3[0:16, 0, :], op=ALU.subtract)
        p.v("tensor_copy", r="RTr RT3", w="RT3", out=RT3[0:16, 1, :], in_=RTr[0:16, :])
        p.v("tensor_tensor", r="RTr RT3", w="RTr", out=RTr[0:16, :], in0=RTr[0:16, :], in1=RT3[0:16, 1, :], op=ALU.subtract)
        p.v("tensor_copy", r="RTr RT3", w="RT3", out=RT3[0:16, 2, :], in_=RTr[0:16, :])
        if stop == "E3":
            return fin(nc, p, ar, dbg_d)

        p.v("tensor_copy", r="cs", w="e16", out=e16[:, 0:8], in_=cs[:, 0:8])
        p.v("tensor_tensor", r="cs e16", w="e16", out=e16[:, 8:16], in0=cs[:, 24:32], in1=cs[:, 8:16], op=ALU.subtract)
        p.s("activation", r="e16", w="e16", out=e16, in_=e16, func=AF.Exp)
        if stop == "E4":
            return fin(nc, p, ar, dbg_d)

        for idx in range(16):
            d, h = idx // 8, idx % 8
            g = h // 4
            pk = "b2" if idx % 2 == 0 else "b6"
            PKs = (PB(2) if idx % 2 == 0 else PB(6))[:, 0:128]
            for q in range(3):
                p.t("matmul", r="sel RT3", w=pk, out=PKs, lhsT=sel[:, idx, :], rhs=RT3[:, q, :], start=(q == 0), stop=False)
            p.t("matmul", r="ident maskF maskB", w=pk, out=PKs, lhsT=ident, rhs=(maskF if d == 0 else maskB), start=False, stop=True)
            lk = "LT%d" % (idx % 2)
            p.s("activation", r=pk + " bias16", w=lk, out=LT[idx % 2], in_=PKs, func=AF.Exp, bias=bias16[:, idx:idx + 1])
            p.v("tensor_tensor", r=lk + " ST_sb", w="SmT%d" % idx, out=SmT[:, idx, :], in0=LT[idx % 2], in1=ST_sb[:, g, :], op=ALU.mult)
        if stop == "E5a":
            return fin(nc, p, ar, dbg_d)
        for h in range(8):
            hs = slice(h * 64, (h + 1) * 64)
            p.t("matmul", r="SmT%d xdt0" % h, w="b3", out=PY[:, hs], lhsT=SmT[:, h, :], rhs=xdt[0][:, hs], start=True, stop=False)
            p.t("matmul", r="SmT%d xdt1" % (8 + h), w="b3", out=PY[:, hs], lhsT=SmT[:, 8 + h, :], rhs=xdt[1][:, hs], start=False, stop=True)
        for h in range(8):
            g = h // 4
            hs = slice(h * 64, (h + 1) * 64)
            p.t("matmul", r="xbcT%d Sbf0" % (6 + g), w="b4", out=PB(4)[:, hs], lhsT=xbcT[:, 6 + g, tok], rhs=Sbf[0][:, hs], start=True, stop=True)
            p.t("matmul", r="xbcT%d SB%d" % (6 + g, c), w="b5", out=PB(5)[:, hs], lhsT=xbcT[:, 6 + g, tok], rhs=SB_all[:, c, hs], start=True, stop=True)
        if stop == "E5b":
            return fin(nc, p, ar, dbg_d)
        p.v("tensor_tensor", r="b4 e16", w="yc", out=v3(yc), in0=v3(PB(4)), in1=bc8(e16[:, 0:8]), op=ALU.mult)
        p.v("tensor_tensor", r="b5 e16", w="yc2", out=v3(yc2), in0=v3(PB(5)), in1=bc8(e16[:, 8:16]), op=ALU.mult)
        p.g("tensor_tensor", r="yc yc2", w="yc", out=yc, in0=yc, in1=yc2, op=ALU.add)
        if stop == "E6":
            return fin(nc, p, ar, dbg_d)

        p.v("tensor_tensor", r="yc b3", w="yc", out=yc, in0=yc, in1=PY, op=ALU.add)
        p.g("tensor_tensor", r="xs_tok Drow", w="yc2", out=yc2, in0=xs_tok, in1=Drow, op=ALU.mult)
        p.v("tensor_tensor", r="yc yc2", w="yc", out=yc, in0=yc, in1=yc2, op=ALU.add)
        p.v("tensor_tensor", r="yc zst", w="gg", out=gg, in0=yc, in1=zst, op=ALU.mult)
        for g in range(2):
            p.s("activation", r="gg", w="junk2 ss2", out=junk2, in_=gg[:, g * 256:(g + 1) * 256], func=AF.Square, accum_out=ss2[:, g:g + 1])
        p.v("tensor_scalar", r="ss2", w="rs2", out=rs2, in0=ss2, scalar1=1.0 / 256, scalar2=EPS, op0=ALU.mult, op1=ALU.add)
        p.s("activation", r="rs2", w="rs2", out=rs2, in_=rs2, func=AF.Ln)
        p.s("activation", r="rs2", w="rs2", out=rs2, in_=rs2, func=AF.Exp, scale=-0.5)
        for g in range(2):
            gs = slice(g * 256, (g + 1) * 256)
            p.v("scalar_tensor_tensor", r="gg rs2 NW", w="ssm_tok", out=ssm_tok[:, gs], in0=gg[:, gs], scalar=rs2[:, g:g + 1], in1=NW[:, gs],
                op0=ALU.mult, op1=ALU.mult)
        for j in range(4):
            p.t("transpose", r="ssm_tok ident", w="b7", out=PM[:, j * 128:(j + 1) * 128], in_=ssm_tok[:, j * 128:(j + 1) * 128], identity=ident)
        p.s("copy", r="b7", w="catT", out=catT[:, 0:4, :], in_=PM[:, 0:512].rearrange("p (a b) -> p a b", a=4))
        if stop == "E7":
            return fin(nc, p, ar, dbg_d)

        ukeys = "uTt%d" % pe
        p.s("activation", r=ukeys, w="vsq", out=vsq, in_=uTc, func=AF.Square)
        for j in range(4):
            p.t("matmul", r="ones_b " + ukeys, w="b7", out=PM2[:, 256:384], lhsT=ones_b, rhs=uTc[:, j, :], start=(j == 0), stop=(j == 3))
        p.v("tensor_copy", r="vsq", w="vsqh", out=vsqh, in_=vsq)
        p.v("tensor_tensor", r="vsq vsqh", w="vsql", out=vsql, in0=vsq, in1=vsqh, op=ALU.subtract)
        for j in range(4):
            p.t("matmul", r="ones_b vsqh", w="b7", out=PM2[:, 384:512], lhsT=ones_b, rhs=vsqh[:, j, :], start=(j == 0), stop=False)
            p.t("matmul", r="ones_b vsql", w="b7", out=PM2[:, 384:512], lhsT=ones_b, rhs=vsql[:, j, :], start=False, stop=(j == 3))
        p.s("activation", r="b7", w="mean", out=mean, in_=PM2[:, 256:384], func=AF.Copy, scale=1.0 / 512)
        p.s("activation", r="b7", w="var", out=var, in_=PM2[:, 384:512], func=AF.Copy, scale=1.0 / 512)
        p.g("tensor_tensor", r="mean", w="msq", out=msq, in0=mean, in1=mean, op=ALU.mult)
        p.v("tensor_tensor", r="var msq", w="var", out=var, in0=var, in1=msq, op=ALU.subtract)
        p.v("tensor_scalar", r="var", w="var", out=var, in0=var, scalar1=EPS, scalar2=None, op0=ALU.add)
        p.s("activation", r="var", w="var", out=var, in_=var, func=AF.Ln)
        p.s("activation", r="var", w="var", out=var, in_=var, func=AF.Exp, scale=-0.5)
        p.v("tensor_tensor", r=ukeys + " mean", w="ntmp", out=ntmp, in0=uTc, in1=mean.unsqueeze(1).to_broadcast([128, 4, 128]), op=ALU.subtract)
        p.v("tensor_tensor", r="ntmp var", w="ntmp", out=ntmp, in0=ntmp, in1=var.unsqueeze(1).to_broadcast([128, 4, 128]), op=ALU.mult)
        for j in range(4):
            p.s("activation", r="ntmp cvec", w="catT", out=catT[:, 4 + j, :], in_=ntmp[:, j, :], func=AF.Silu, scale=cvec[:, 1, j:j + 1],
                bias=cvec[:, 2, j:j + 1])
        for nb in range(2):
            for kc in range(8):
                p.t("matmul", r="catT Wo", w="b%d" % (4 + nb), out=PB(4 + nb), lhsT=catT[:, kc, :], rhs=Wo[:, kc, nb * 512:(nb + 1) * 512],
                    start=(kc == 0), stop=(kc == 7))
            ns = slice(nb * 512, (nb + 1) * 512)
            tq, tk_ = (yc2, "yc2") if nb == 0 else (gg, "gg")
            p.v("tensor_tensor", r="b%d G1" % (4 + nb), w=tk_, out=tq, in0=PB(4 + nb), in1=G1[:, ns], op=ALU.mult)
            p.g("tensor_tensor", r=tk_ + " xe%d" % pe, w="xe%d" % pe, out=xe[pe][:, ns], in0=xe[pe][:, ns], in1=tq, op=ALU.add)
        p.dma(x1_s[tok, :], xe[pe], r="xe%d" % pe, w="x1_s", grp="x1w%d" % pe)
        local_states(0)
        upd(0)
    if dbg:
        dx1 = dout("x1", [L, 1024])
        p.dma(None, None, r="x1_s", w="dbg_x1", parts=[(dx1[i * 128:(i + 1) * 128, :], x1_s[i * 128:(i + 1) * 128, :]) for i in range(NT)])
    ar.release(mE)
    p.barrier()
    if stop == "E":
        return fin(nc, p, ar, dbg_d)

    ar.release(m_big)
    TB = 256
    KT = ar.a((16, 128), BF16)
    W2 = ar.a(1024); S2 = ar.a(1024); G2 = ar.a(1024); FW = ar.a(1024)
    p.dma(W2, modb_s[5], r="modb_s", w="W2")
    p.dma(S2, modb_s[6], r="modb_s", w="S2")
    p.dma(G2, modb_s[7], r="modb_s", w="G2")
    p.dma(FW, nrm_d[2], w="FW")
    xs1 = [ar.a(1024) for _ in range(2)]
    for i in range(4):
        sl = i % 2
        wv_ = xs1[sl][:, 0:512].rearrange("p (a b) -> p a b", a=4)
        p.dma(wv_, kt_d[:, i * 4:(i + 1) * 4, :], w="xs1%d" % sl)
        p.v("tensor_copy", r="xs1%d" % sl, w="KT", out=KT[:, i * 4:(i + 1) * 4, :], in_=wv_)
    iota_b = ar.a(128, BF16)
    p.v("tensor_copy", r="iota", w="iota_b", out=iota_b, in_=iota_f)
    Wqs = [ar.a((8, 128), BF16) for _ in range(3)]
    tmpF = ar.a(512)
    junkF = tmpF.bitcast(BF16)
    ssF = ar.a(2); rsF = ar.a(2)
    h2 = ar.a(1024, BF16)
    h2T = [ar.a((8, TB), BF16) for _ in range(3)]
    qT = ar.a((16, TB), BF16)
    sc = ar.a((16, 128))
    sv = ar.a((16, 16)); si = ar.a((16, 16), U32); sif = ar.a((16, 16)); wk = ar.a(128); wk2 = ar.a(256)
    best = ar.a((8, 16)); fl = ar.a((8, 16), U32); ai = ar.a((8, 16), U32); bi = ar.a((8, 16), U32)
    af = ar.a((8, 16)); bf_ = ar.a((8, 16)); gt = ar.a((8, 16)); gs = ar.a(8)
    IG = ar.a((3, 128))
    IGTb = [ar.a((2, 2, 128), BF16) for _ in range(3)]
    IGTg = [ar.a((2, 128)) for _ in range(3)]
    NG = 16
    ohA2 = [ar.a((NG, 128), BF16) for _ in range(2)]
    ohB2 = [ar.a((NG, 64), BF16) for _ in range(2)]
    ohc = [0]
    oh = ar.a((8, 16, 16))
    GmH = [ar.a((TB, 64), BF16) for _ in range(2)]
    wu = [ar.a((2, 1024), BF16) for _ in range(3)]
    wv = [ar.a((2, 1024), BF16) for _ in range(3)]
    ga = [ar.a(TB) for _ in range(2)]
    gab = [ar.a(TB, BF16) for _ in range(2)]
    cand = sc.rearrange("p a b -> p (a b)").rearrange("p (h a b) -> p h a b", h=8, a=16)
    svv = sv.rearrange("p (h t) a -> p h t a", t=2)
    sifv = sif.rearrange("p (h t) a -> p h t a", t=2)
    io16 = iota_f[:, 0:16].unsqueeze(1).unsqueeze(1).to_broadcast([128, 8, 16, 16])
    NBLK = L // TB
    wqi = [0]

    def part1(blk):
        par = blk % 3
        hT = h2T[par]
        hk = "h2T%d" % par
        for ts in range(2):
            t0 = blk * TB + ts * 128
            xk = "xs1%d" % ts
            p.dma(xs1[ts], x1_s[t0:t0 + 128, :], r="x1_s", w=xk, e="gpsimd", grp="gx%d" % ts)
            yield "sync"
            p.s("activation", r=xk, w="tmpF ssF", out=junkF, in_=xs1[ts], func=AF.Square, accum_out=ssF[:, 0:1])
            p.v("tensor_scalar", r="ssF", w="rsF", out=rsF[:, 0:1], in0=ssF[:, 0:1], scalar1=1.0 / 1024, scalar2=EPS, op0=ALU.mult, op1=ALU.add)
            p.s("activation", r="rsF", w="rsF", out=rsF[:, 0:1], in_=rsF[:, 0:1], func=AF.Ln)
            p.s("activation", r="rsF", w="rsF", out=rsF[:, 0:1], in_=rsF[:, 0:1], func=AF.Exp, scale=-0.5)
            yield
            p.v("scalar_tensor_tensor", r=xk + " rsF W2", w=xk, out=xs1[ts], in0=xs1[ts], scalar=rsF[:, 0:1], in1=W2, op0=ALU.mult, op1=ALU.mult)
            p.g("tensor_tensor", r=xk + " S2", w="h2", out=h2, in0=xs1[ts], in1=S2, op=ALU.add)
            yield "sync"
            for kc in range(8):
                p.t("transpose", r="h2 ident", w="b2", out=PBb(2)[:, kc * 128:(kc + 1) * 128], in_=h2[:, kc * 128:(kc + 1) * 128], identity=ident)
            yield "sync"
            p.s("copy", r="b2", w=hk, out=hT[:, :, ts * 128:(ts + 1) * 128], in_=PBb(2).rearrange("p (a b) -> p a b", a=8))
            yield
        def wq_load(hp_):
            ws_ = (wqi[0] + hp_) % 3
            p.dma(Wqs[ws_], wqb_s[hp_].rearrange("p (k c) -> p k c", k=8), r="wqb_s", w="Wqs%d" % ws_, e="gpsimd", grp="gwq%d" % ws_)
        wq_load(0)
        wq_load(1)
        yield "sync"
        for hp in range(17):
            if hp >= 1:
                if hp % 2 == 1:
                    p.s("copy", r="b2", w="qT", out=qT[:, hp - 1, :], in_=PB(2)[:, 0:TB])
                else:
                    p.v("tensor_copy", r="b2", w="qT", out=qT[:, hp - 1, :], in_=PB(2)[:, 0:TB])
            if hp < 16:
                ws = (wqi[0] + hp) % 3
                if hp + 2 < 16:
                    wq_load(hp + 2)
                p.begin()
                for kc in range(8):
                    p.t("matmul", r=("Wqs%d " % ws) + hk, w="b2", out=PB(2)[:, 0:TB], lhsT=Wqs[ws][:, kc, :], rhs=hT[:, kc, :],
                        start=(kc == 0), stop=(kc == 7))
                p.end()
            yield "sync"
        wqi[0] += 16
        yield "sync"
        for ts in range(2):
            tsl = slice(ts * 128, (ts + 1) * 128)
            for g4 in range(5):
                if g4 >= 1:
                    p.s("copy", r="b2", w="sc", out=sc[:, (g4 - 1) * 4:g4 * 4, :], in_=PB(2).rearrange("p (a b) -> p a b", a=4))
                if g4 < 4:
                    for q4 in range(4):
                        hp = g4 * 4 + q4
                        p.t("matmul", r="qT KT", w="b2", out=PB(2)[:, q4 * 128:(q4 + 1) * 128], lhsT=qT[:, hp, tsl], rhs=KT[:, hp, :],
                            start=True, stop=True)
                yield "sync"
            yield "sync"
            for hp in range(16):
                p.v("max", r="sc", w="sv", out=sv[:, hp, 0:8], in_=sc[:, hp, :])
                p.v("max_index", r="sc sv", w="si", out=si[:, hp, 0:8], in_max=sv[:, hp, 0:8], in_values=sc[:, hp, :])
                p.v("match_replace", r="sc sv", w="wk", out=wk, in_to_replace=sv[:, hp, 0:8], in_values=sc[:, hp, :], imm_value=-1e30)
                yield
                p.v("max", r="wk", w="sv", out=sv[:, hp, 8:16], in_=wk)
                p.v("max_index", r="wk sv", w="si", out=si[:, hp, 8:16], in_max=sv[:, hp, 8:16], in_values=wk)
                yield
            p.v("tensor_copy", r="si", w="sif", out=sif, in_=si)
            p.v("tensor_tensor", r="sv sc", w="sc", out=cand, in0=svv[:, :, 0, :].unsqueeze(3).to_broadcast([128, 8, 16, 16]),
                in1=svv[:, :, 1, :].unsqueeze(2).to_broadcast([128, 8, 16, 16]), op=ALU.add)
            yield
            for h in range(8):
                ch = cand[:, h].rearrange("p a b -> p (a b)")
                p.v("max", r="sc", w="best", out=best[:, h, 0:8], in_=ch)
                p.v("max_index", r="sc best", w="fl", out=fl[:, h, 0:8], in_max=best[:, h, 0:8], in_values=ch)
                p.v("match_replace", r="sc best", w="wk2", out=wk2, in_to_replace=best[:, h, 0:8], in_values=ch, imm_value=-1e30)
                yield
                p.v("max", r="wk2", w="best", out=best[:, h, 8:16], in_=wk2)
                p.v("max_index", r="wk2 best", w="fl", out=fl[:, h, 8:16], in_max=best[:, h, 8:16], in_values=wk2)
                yield
            p.v("tensor_tensor", r="best", w="gt", out=gt, in0=best, in1=best[:, :, 0:1].to_broadcast([128, 8, 16]), op=ALU.subtract)
            yield "sync"
            p.s("activation", r="gt", w="gt", out=gt, in_=gt, func=AF.Exp)
            yield "sync"
            p.v("tensor_reduce", r="gt", w="gs", out=gs, in_=gt, axis=AX.X, op=ALU.add)
            p.v("reciprocal", r="gs", w="gs", out=gs, in_=gs)
            yield
            p.v("tensor_tensor", r="gt gs IG", w="IG", out=IG[:, 2, :].rearrange("p (h j) -> p h j", h=8), in0=gt,
                in1=gs.unsqueeze(2).to_broadcast([128, 8, 16]), op=ALU.mult)
            p.v("tensor_single_scalar", r="fl", w="ai", out=ai, in_=fl, scalar=4, op=ALU.logical_shift_right)
            p.v("tensor_single_scalar", r="fl", w="bi", out=bi, in_=fl, scalar=15, op=ALU.bitwise_and)
            p.v("tensor_copy", r="ai", w="af", out=af, in_=ai)
            p.v("tensor_copy", r="bi", w="bf", out=bf_, in_=bi)
            yield
            for t_, (xf, key) in enumerate(((af, "af"), (bf_, "bf"))):
                p.v("tensor_tensor", r=key + " iota", w="oh", out=oh, in0=xf.unsqueeze(3).to_broadcast([128, 8, 16, 16]), in1=io16, op=ALU.is_equal)
                yield
                p.v("tensor_tensor", r="oh sif", w="oh", out=oh, in0=oh, in1=sifv[:, :, t_, :].unsqueeze(2).to_broadcast([128, 8, 16, 16]), op=ALU.mult)
                yield
                p.v("tensor_reduce", r="oh IG", w="IG", out=IG[:, t_, :].rearrange("p (h j) -> p h j", h=8), in_=oh, axis=AX.X, op=ALU.add)
                yield
            yield "sync"
            for q in range(3):
                p.t("transpose", r="IG identf", w="b2", out=PB(2)[:, q * 128:(q + 1) * 128], in_=IG[:, q, :], identity=ident_f)
            yield "sync"
            p.s("copy", r="b2", w="IGTb%d" % par, out=IGTb[par][:, ts, :, :], in_=PB(2)[:, 0:256].rearrange("p (a b) -> p a b", a=2))
            p.s("copy", r="b2", w="IGTg%d" % par, out=IGTg[par][:, ts, :], in_=PB(2)[:, 256:384])
            yield

    def part2(blk, half):
        par = blk % 3
        kb, kg = "IGTb%d" % par, "IGTg%d" % par
        Gh = GmH[half]
        gk = "Gm%d" % half
        groups = [(ts, tg) for ts in range(2) for tg in range(128 // NG)]
        bufs = {}

        def gen_(n):
            ts, tg = groups[n]
            tq = slice(tg * NG, (tg + 1) * NG)
            ob = ohc[0] % 2
            ohc[0] += 1
            bufs[n] = ob
            ohA, ohB = ohA2[ob], ohB2[ob]
            ka, kbb = "ohA%d" % ob, "ohB%d" % ob
            p.v("tensor_tensor", r=kb + " iota_b", w=ka, out=ohA, in0=iota_b.unsqueeze(1).to_broadcast([128, NG, 128]),
                in1=IGTb[par][:, ts, 0, tq].unsqueeze(2).to_broadcast([128, NG, 128]), op=ALU.is_equal)
            p.v("tensor_tensor", r=kb + " iota_b", w=kbb, out=ohB, in0=iota_b[:, half * 64:(half + 1) * 64].unsqueeze(1).to_broadcast([128, NG, 64]),
                in1=IGTb[par][:, ts, 1, tq].unsqueeze(2).to_broadcast([128, NG, 64]), op=ALU.is_equal)
            p.g("tensor_tensor", r=kg + " " + kbb, w=kbb, out=ohB, in0=ohB, in1=IGTg[par][:, ts, tq].unsqueeze(2).to_broadcast([128, NG, 64]), op=ALU.mult)

        def mm_(m):
            n, t8 = m // 2, m % 2
            ob = bufs[n]
            ohA, ohB = ohA2[ob], ohB2[ob]
            ka, kbb = "ohA%d" % ob, "ohB%d" % ob
            p.begin()
            for q8 in range(8):
                tt_ = t8 * 8 + q8
                p.t("matmul", r=ka + " " + kbb, w="b3", out=PB(3)[:, q8 * 64:(q8 + 1) * 64], lhsT=ohA[:, tt_, :], rhs=ohB[:, tt_, :],
                    start=True, stop=True)
            p.end()

        def ev_(m):
            n, t8 = m // 2, m % 2
            ts, tg = groups[n]
            g0 = ts * 128 + tg * NG + t8 * 8
            if m % 2 == 0:
                p.s("copy", r="b3", w=gk, out=Gh[:, g0:g0 + 8, :], in_=PB(3).rearrange("p (a b) -> p a b", a=8))
            else:
                p.v("tensor_copy", r="b3", w=gk, out=Gh[:, g0:g0 + 8, :], in_=PB(3).rearrange("p (a b) -> p a b", a=8))

        ng = len(groups)
        gen_(0)
        yield "sync"
        for m in range(2 * ng + 1):
            if m % 2 == 0 and m // 2 + 1 < ng:
                gen_(m // 2 + 1)
            if m >= 1:
                ev_(m - 1)
            if m < 2 * ng:
                mm_(m)
            yield "sync"

    widx = [0]

    wslot = {}

    def main_step(blk, s_):
        par = blk % 3
        hT = h2T[par]
        if s_ < 128:
            pr, e2 = s_ // 2, s_ % 2
            if e2 == 0:
                sl = widx[0] % 3
                widx[0] += 1
                wslot[pr % 4] = sl
                p.dma(wu[sl], utb_s[2 * pr:2 * pr + 2].rearrange("a p c -> p a c"), r="utb_s", w="wu%d" % sl)
                p.dma(wv[sl], vtb_s[2 * pr:2 * pr + 2].rearrange("a p c -> p a c"), r="vtb_s", w="wv%d" % sl)
            sl = wslot[pr % 4]
            half = s_ // 64
            ab = s_ % 2
            p.begin()
            for kc in range(8):
                p.t("matmul", r="wu%d h2T%d" % (sl, par), w="b%d" % ab, out=PB(ab)[:, 0:TB], lhsT=wu[sl][:, e2, kc * 128:(kc + 1) * 128], rhs=hT[:, kc, :],
                    start=(kc == 0), stop=(kc == 7))
            p.end()
            p.s("activation", r="b%d" % ab, w="ga%d" % ab, out=ga[ab], in_=PB(ab)[:, 0:TB], func=AF.Gelu)
            p.v("tensor_tensor", r="ga%d Gm%d" % (ab, half), w="gab%d" % ab, out=gab[ab], in0=ga[ab], in1=GmH[half][:, :, s_ % 64], op=ALU.mult)
        if s_ >= 1:
            o_ = s_ - 1
            pr, e2 = o_ // 2, o_ % 2
            sl = wslot[pr % 4]
            ab = o_ % 2
            p.begin()
            for ts in range(2):
                for nb in range(2):
                    bk = 4 + 2 * ts + nb
                    p.t("matmul", r="gab%d wv%d" % (ab, sl), w="b%d" % bk, out=PB(bk), lhsT=gab[ab][:, ts * 128:(ts + 1) * 128],
                        rhs=wv[sl][:, e2, nb * 512:(nb + 1) * 512], start=(o_ == 0), stop=(o_ == 127))
            p.end()

    def finalize(blk):
        for ts in range(2):
            t0 = blk * TB + ts * 128
            xk = "xs1%d" % ts
            p.dma(xs1[ts], x1_s[t0:t0 + 128, :], r="x1_s", w=xk, e="gpsimd", grp="gx%d" % ts)
            for nb in range(2):
                bk = 4 + 2 * ts + nb
                ns = slice(nb * 512, (nb + 1) * 512)
                p.v("tensor_tensor", r="b%d G2" % bk, w="tmpF", out=tmpF, in0=PB(bk), in1=G2[:, ns], op=ALU.mult)
                p.g("tensor_tensor", r="tmpF " + xk, w=xk, out=xs1[ts][:, ns], in0=xs1[ts][:, ns], in1=tmpF, op=ALU.add)
            p.s("activation", r=xk, w="tmpF ssF", out=junkF, in_=xs1[ts], func=AF.Square, accum_out=ssF[:, 1:2])
            p.v("tensor_scalar", r="ssF", w="rsF", out=rsF[:, 1:2], in0=ssF[:, 1:2], scalar1=1.0 / 1024, scalar2=EPS, op0=ALU.mult, op1=ALU.add)
            p.s("activation", r="rsF", w="rsF", out=rsF[:, 1:2], in_=rsF[:, 1:2], func=AF.Ln)
            p.s("activation", r="rsF", w="rsF", out=rsF[:, 1:2], in_=rsF[:, 1:2], func=AF.Exp, scale=-0.5)
            p.v("scalar_tensor_tensor", r=xk + " rsF FW", w=xk, out=xs1[ts], in0=xs1[ts], scalar=rsF[:, 1:2], in1=FW, op0=ALU.mult, op1=ALU.mult)
            p.dma(out_d[t0:t0 + 128, :], xs1[ts], r=xk, w="out", grp="gout%d" % ts, e="gpsimd")

    nblk = 1 if stop == "F1" else (3 if stop == "F2" else (7 if stop == "F7" else NBLK))

    def drain(g):
        if g is not None:
            for _ in g:
                pass

    def drip(g, n):
        if g is None:
            return None
        try:
            for _ in range(n):
                if next(g) == "sync":
                    break
        except StopIteration:
            return None
        return g

    drain(part1(0))
    if nblk > 1:
        drain(part1(1))
    drain(part2(0, 0))
    for blk in range(nblk):
        st = {"g1": part1(blk + 2) if blk + 2 < nblk else None, "gB": part2(blk, 1), "gA": None}

        DM = "3"
        if DM in ("0", "2"):
            drain(st["g1"]); st["g1"] = None
        if DM in ("0", "1"):
            drain(st["gB"]); st["gB"] = None

        N1 = 2
        SPREAD = 58

        def dr(s_, st=st, blk=blk):
            k = s_ % 64
            if not ((k + 1) * 33 // SPREAD == k * 33 // SPREAD and k < SPREAD):
                if s_ < 64:
                    st["gB"] = drip(st["gB"], 1)
                else:
                    st["gA"] = drip(st["gA"], 1)
            st["g1"] = drip(st["g1"], N1)

        for s_ in range(129):
            main_step(blk, s_)
            if s_ == 63:
                drain(st["gB"])
                st["gB"] = None
                st["gA"] = part2(blk + 1, 0) if blk + 1 < nblk else None
            dr(s_)
        g1, gA = st["g1"], st["gA"]
        drain(g1)
        drain(gA)
        finalize(blk)
    return fin(nc, p, ar, dbg_d)


def fin(nc, p, ar, dbg_d):
    p.finish()
    p.emit()
    return nc, dbg_d


def host_layout(inp, b):
    f = np.float32
    A = np.ascontiguousarray
    d = {}
    d["x"] = A(inp["x"][b])
    d["ctx"] = A(inp["ctx"][b])
    cc = np.stack([inp["c"][b].reshape(8, 128).T, inp["c_ctx"].reshape(8, 128).T], axis=-1)
    d["cc"] = A(cc.astype(f))
    return d


def shared_layout(inp):
    A = np.ascontiguousarray
    d = {}
    d["ada_w"] = A(inp["ada_w"][0].reshape(8, 128, 6144).transpose(1, 0, 2))
    d["ada_b"] = A(inp["ada_b"][0].reshape(1, 6144))
    d["nrm"] = A(np.stack([np.broadcast_to(inp[k].reshape(1, 1024), (128, 1024)) for k in ("norm1_w", "norm2_w", "final_norm_w")]))
    d["w_in"] = A(inp["w_in"][0].reshape(8, 128, 2576).transpose(1, 0, 2))
    d["scw"] = A(inp["ssm_conv_w"][0].reshape(5, 8, 128).transpose(2, 1, 0))
    d["scb"] = A(inp["ssm_conv_b"][0].reshape(8, 128).T)
    d["dtb"] = A(np.broadcast_to(inp["ssm_dt_bias"][0].reshape(1, 16), (128, 16)))
    d["alog"] = A(np.broadcast_to(inp["ssm_a_log"][0].reshape(1, 16), (128, 16)))
    d["drow"] = A(np.broadcast_to(np.repeat(inp["ssm_d"][0], 64).reshape(1, 512), (128, 512)))
    d["snw"] = A(np.broadcast_to(inp["ssm_norm_w"][0].reshape(1, 512), (128, 512)))
    d["ccw"] = A(inp["cfm_conv_w"][0].reshape(31, 4, 128).transpose(2, 1, 0))
    d["cvec"] = A(np.stack([inp[k][0].reshape(4, 128).T for k in ("cfm_conv_b", "cfm_ln_w", "cfm_ln_b")], axis=1))
    d["w_out"] = A(inp["w_out"][0].reshape(8, 128, 1024).transpose(1, 0, 2))
    d["wq"] = A(inp["peer_wq"][0].reshape(8, 128, 2048).transpose(1, 0, 2))
    d["kt"] = A(inp["peer_subkeys"][0].reshape(16, 128, 128).transpose(2, 0, 1))
    d["ut"] = A(inp["peer_u"][0].reshape(128, 128, 8, 128).transpose(1, 3, 2, 0))
    d["vt"] = A(inp["peer_v"][0].reshape(128, 128, 1024).transpose(1, 0, 2))
    return d


_CACHE = {}


def kernel(**inputs):
    inp = {k: np.asarray(v) for k, v in inputs.items()}
    if "nc" not in _CACHE:
        _CACHE["nc"] = build()[0]
    nc = _CACHE["nc"]
    sh = shared_layout(inp)
    in_maps = []
    for b in range(8):
        d = dict(sh)
        d.update(host_layout(inp, b))
        in_maps.append(d)
    res = run_bass_kernel_spmd(nc, in_maps, core_ids=list(range(8)))
    return np.stack([np.asarray(r["out"], dtype=np.float32) for r in res.results], axis=0)
```
